# Optimizing a Trainium2 kernel written in Bass

```python
import jax
import jax.numpy as jnp
from jax import lax
import numpy as np

D_MODEL = 1024
BATCH = 8
SEQ = 4096
DEPTH = 2

GRID_W = 64
CTX_LEN = 256

HEAD_DIM = 64
CONV_WIDTH = D_MODEL // 4
CONV_K = 3
RET_WIDTH = D_MODEL // 4
RET_HEADS = RET_WIDTH // HEAD_DIM
RET_CHUNK = 128
ATT_WIDTH = D_MODEL // 2
ATT_HEADS = ATT_WIDTH // HEAD_DIM
ATT_KV_HEADS = ATT_HEADS // 4
ATT_KV_WIDTH = ATT_KV_HEADS * HEAD_DIM
ATT_BLOCK = 128
MIX_WIDTH = CONV_WIDTH + RET_WIDTH + ATT_WIDTH
ROPE_BASE = 10000.0
N_EXPERTS = 32
TOP_K = 4
EXPERT_FF = D_MODEL
SWIGLU_ALPHA = 1.702
SWIGLU_LIMIT = 7.0
MOE_BLOCK = 512
N_MOD = 6
EPS = 1e-6
IN_SPLITS = (CONV_WIDTH, CONV_WIDTH, CONV_WIDTH, RET_WIDTH, RET_WIDTH, RET_WIDTH, RET_WIDTH, ATT_WIDTH, ATT_KV_WIDTH, ATT_KV_WIDTH)
IN_WIDTH = sum(IN_SPLITS)
SPLIT_POINTS = tuple(sum(IN_SPLITS[: i + 1]) for i in range(len(IN_SPLITS) - 1))

kernel_name = 'hybrid_conv_retention_gqa_moe_dit'


def _layer_norm(x, g, b):
    xf = x.astype(jnp.float32)
    mu = xf.mean(-1, keepdims=True)
    var = jnp.square(xf - mu).mean(-1, keepdims=True)
    return ((xf - mu) * lax.rsqrt(var + EPS) * g + b).astype(x.dtype)


def _rms_norm(x, g):
    xf = x.astype(jnp.float32)
    return (xf * lax.rsqrt(jnp.mean(xf * xf, -1, keepdims=True) + EPS) * g).astype(x.dtype)


def _heads(z, n_heads):
    b, l, w = z.shape
    return z.reshape(b, l, n_heads, w // n_heads).transpose(0, 2, 1, 3)


def _merge_heads(y):
    b, n, l, dh = y.shape
    return y.transpose(0, 2, 1, 3).reshape(b, l, n * dh)


def _flip(a):
    return jnp.flip(a, axis=2)


def _axial_rope(seq_len, head_dim):
    rows = seq_len // GRID_W
    axis_dim = head_dim // 2
    inv_freq = ROPE_BASE ** (-jnp.arange(0, axis_dim, 2, dtype=jnp.float32) / axis_dim)
    row = jnp.repeat(jnp.arange(rows, dtype=jnp.float32), GRID_W)
    col = jnp.tile(jnp.arange(GRID_W, dtype=jnp.float32), rows)
    ang = jnp.stack([row[:, None] * inv_freq, col[:, None] * inv_freq], axis=1)
    return jnp.cos(ang), jnp.sin(ang)


def _apply_rope(x, cos, sin):
    b, h, l, dh = x.shape
    xa = x.astype(jnp.float32).reshape(b, h, l, 2, 2, dh // 4)
    x1, x2 = xa[..., 0, :], xa[..., 1, :]
    out = jnp.stack([x1 * cos - x2 * sin, x1 * sin + x2 * cos], axis=-2)
    return out.reshape(b, h, l, dh).astype(x.dtype)


def _short_conv(u, w):
    l = u.shape[1]
    up = jnp.pad(u, ((0, 0), (1, 1), (0, 0)))
    return up[:, :l] * w[:, 0] + up[:, 1:l + 1] * w[:, 1] + up[:, 2:] * w[:, 2]


def _retention_scan(q, k, v, log_gamma, s0, include_diag):
    b, h, l, dk = q.shape
    dv = v.shape[-1]
    nc = l // RET_CHUNK
    qc = q.reshape(b, h, nc, RET_CHUNK, dk)
    kc = k.reshape(b, h, nc, RET_CHUNK, dk)
    vc = v.reshape(b, h, nc, RET_CHUNK, dv)
    pos = jnp.arange(RET_CHUNK, dtype=jnp.float32)
    diff = pos[:, None] - pos[None, :]
    keep = (diff >= 0) if include_diag else (diff > 0)
    decay = jnp.where(keep, jnp.exp(log_gamma[:, None, None] * jnp.maximum(diff, 0.0)), 0.0)
    scores = jnp.einsum('bhnid,bhnjd->bhnij', qc, kc) * decay[:, None]
    y_intra = jnp.einsum('bhnij,bhnjv->bhniv', scores, vc)
    zeta = jnp.exp(log_gamma[:, None] * (RET_CHUNK - 1.0 - pos))
    chunk_kv = jnp.einsum('bhnjd,hj,bhnjv->nbhdv', kc, zeta, vc)
    chunk_decay = jnp.exp(log_gamma * RET_CHUNK)[None, :, None, None]

    def step(state, kv_n):
        return chunk_decay * state + kv_n, state

    s_final, s_prev = lax.scan(step, s0, chunk_kv)
    xi = jnp.exp(log_gamma[:, None] * (pos + 1.0))
    y_cross = jnp.einsum('bhnid,hi,nbhdv->bhniv', qc, xi, s_prev)
    return (y_intra + y_cross).reshape(b, h, l, dv), s_final


def _retention_state(k, v, log_gamma):
    l = k.shape[2]
    w = jnp.exp(log_gamma[:, None] * (l - 1.0 - jnp.arange(l, dtype=jnp.float32)))
    return jnp.einsum('bhld,hl,bhlv->bhdv', k, w, v)


def _ret_output(y, gate_z, gn_g):
    mu = y.mean(-1, keepdims=True)
    var = jnp.square(y - mu).mean(-1, keepdims=True)
    yn = _merge_heads((y - mu) * lax.rsqrt(var + EPS)) * gn_g
    return yn.astype(gate_z.dtype) * jax.nn.silu(gate_z)


def _block_attention(q, k, v):
    b, hq, lq, dh = q.shape
    hkv = k.shape[1]
    g = hq // hkv
    nb = lq // ATT_BLOCK
    qb = q.reshape(b, hkv, g, nb, ATT_BLOCK, dh).transpose(3, 0, 1, 2, 4, 5)
    scale = dh ** -0.5

    def one_block(q_blk):
        s = jnp.einsum('bkgqd,bksd->bkgqs', q_blk, k, preferred_element_type=jnp.float32) * scale
        p = jax.nn.softmax(s, axis=-1)
        return jnp.einsum('bkgqs,bksd->bkgqd', p.astype(v.dtype), v)

    out = lax.map(one_block, qb)
    return out.transpose(1, 2, 3, 0, 4, 5).reshape(b, hq, lq, dh)


def _token_mixers(h_lat, h_ctx, w_in, conv_w, ret_decay_exp, ret_gn_g, q_norm_g, k_norm_g, w_out, with_ctx_out):
    f32 = jnp.float32
    z_lat = jnp.split(h_lat @ w_in, SPLIT_POINTS, axis=-1)
    z_ctx = jnp.split(h_ctx @ w_in, SPLIT_POINTS, axis=-1)
    cos, sin = _axial_rope(h_lat.shape[1], HEAD_DIM)
    log_gamma = jnp.log1p(-jnp.exp2(-ret_decay_exp.astype(f32)))

    def conv_group(z):
        return z[1] * _short_conv(z[2] * z[0], conv_w)

    def ret_q(z):
        return _heads(z[3], RET_HEADS)

    def ret_kv(z):
        return _heads(z[4], RET_HEADS) * HEAD_DIM ** -0.5, _heads(z[5], RET_HEADS)

    def att_q(z):
        return _rms_norm(_heads(z[7], ATT_HEADS), q_norm_g)

    def att_kv(z):
        return _rms_norm(_heads(z[8], ATT_KV_HEADS), k_norm_g), _heads(z[9], ATT_KV_HEADS)

    kc, vc = ret_kv(z_ctx)
    kc, vc = kc.astype(f32), vc.astype(f32)
    ql = _apply_rope(ret_q(z_lat), cos, sin).astype(f32)
    kl, vl = ret_kv(z_lat)
    kl, vl = _apply_rope(kl, cos, sin).astype(f32), vl.astype(f32)
    if with_ctx_out:
        qc = ret_q(z_ctx).astype(f32)
        zeros = jnp.zeros(kc.shape[:2] + (HEAD_DIM, HEAD_DIM), f32)
        yf_c, s_fwd = _retention_scan(qc, kc, vc, log_gamma[0], zeros, True)
        yb_c, s_bwd = _retention_scan(_flip(qc), _flip(kc), _flip(vc), log_gamma[1], zeros, False)
        ret_ctx = yf_c + _flip(yb_c)
    else:
        s_fwd = _retention_state(kc, vc, log_gamma[0])
        s_bwd = _retention_state(_flip(kc), _flip(vc), log_gamma[1])
    yf_l, _ = _retention_scan(ql, kl, vl, log_gamma[0], s_fwd, True)
    yb_l, _ = _retention_scan(_flip(ql), _flip(kl), _flip(vl), log_gamma[1], s_bwd, False)
    ret_lat = yf_l + _flip(yb_l)

    ka_c, va_c = att_kv(z_ctx)
    qa_l = _apply_rope(att_q(z_lat), cos, sin)
    ka_l, va_l = att_kv(z_lat)
    ka_l = _apply_rope(ka_l, cos, sin)
    att_lat = _block_attention(qa_l, jnp.concatenate([ka_c, ka_l], axis=2), jnp.concatenate([va_c, va_l], axis=2))

    y_lat = jnp.concatenate([conv_group(z_lat), _ret_output(ret_lat, z_lat[6], ret_gn_g), _merge_heads(att_lat)], axis=-1) @ w_out
    if with_ctx_out:
        att_ctx = _block_attention(att_q(z_ctx), ka_c, va_c)
        y_ctx = jnp.concatenate([conv_group(z_ctx), _ret_output(ret_ctx, z_ctx[6], ret_gn_g), _merge_heads(att_ctx)], axis=-1) @ w_out
    else:
        y_ctx = None
    return y_lat, y_ctx


def _moe(h, w_router, b_router, w_gate_up, b_gate_up, w_down, b_down):
    t, d = h.shape
    logits = (h @ w_router + b_router).astype(jnp.float32)
    top_v, top_e = lax.top_k(logits, TOP_K)
    top_w = jax.nn.softmax(top_v, axis=-1)
    n_assign = t * TOP_K
    flat_e = top_e.reshape(n_assign).astype(jnp.int32)
    flat_w = top_w.reshape(n_assign)
    flat_tok = jnp.arange(n_assign, dtype=jnp.int32) // TOP_K
    order = jnp.argsort(flat_e)
    e_sorted = flat_e[order]
    counts = jnp.bincount(flat_e, length=N_EXPERTS).astype(jnp.int32)
    padded = (counts + MOE_BLOCK - 1) // MOE_BLOCK * MOE_BLOCK
    grp_start = jnp.cumsum(counts) - counts
    pad_end = jnp.cumsum(padded)
    pad_start = pad_end - padded
    dest = pad_start[e_sorted] + jnp.arange(n_assign, dtype=jnp.int32) - grp_start[e_sorted]
    n_blocks = (n_assign + N_EXPERTS * (MOE_BLOCK - 1) + MOE_BLOCK - 1) // MOE_BLOCK
    n_rows = n_blocks * MOE_BLOCK
    row_tok = jnp.full((n_rows,), t, jnp.int32).at[dest].set(flat_tok[order])
    row_w = jnp.zeros((n_rows,), jnp.float32).at[dest].set(flat_w[order])
    block_e = jnp.minimum(jnp.searchsorted(pad_end, jnp.arange(n_blocks, dtype=jnp.int32) * MOE_BLOCK, side='right'), N_EXPERTS - 1)
    h_pad = jnp.concatenate([h, jnp.zeros((1, d), h.dtype)], axis=0)

    def step(acc, blk):
        tok, wt, e = blk
        xb = h_pad[tok]
        gu = xb @ w_gate_up[e] + b_gate_up[e]
        gate, up = jnp.split(gu, 2, axis=-1)
        gate = jnp.minimum(gate, SWIGLU_LIMIT)
        up = jnp.clip(up, -SWIGLU_LIMIT, SWIGLU_LIMIT)
        act = (up + 1.0) * (gate * jax.nn.sigmoid(SWIGLU_ALPHA * gate))
        yb = act @ w_down[e] + b_down[e]
        return acc.at[tok].add(yb.astype(jnp.float32) * wt[:, None]), None

    acc, _ = lax.scan(step, jnp.zeros((t + 1, d), jnp.float32),
                      (row_tok.reshape(n_blocks, MOE_BLOCK), row_w.reshape(n_blocks, MOE_BLOCK), block_e))
    return acc[:t].astype(h.dtype)


def setup_inputs(seed: int = 0) -> dict:
    key = jax.random.key(seed)
    ks = jax.random.split(key, 24)
    f32 = jnp.float32

    def nrm(k, shape, scale):
        return jax.random.normal(k, shape, f32) * scale

    beta = (8.0 * DEPTH) ** -0.25
    return {
        'x': nrm(ks[0], (BATCH, SEQ, D_MODEL), 1.0),
        'c': nrm(ks[1], (BATCH, D_MODEL), 1.0),
        'ctx': nrm(ks[2], (BATCH, CTX_LEN, D_MODEL), 1.0),
        'c_ctx': nrm(ks[3], (D_MODEL,), 1.0),
        'w_mod': nrm(ks[4], (DEPTH, D_MODEL, N_MOD * D_MODEL), 0.5 * D_MODEL ** -0.5),
        'b_mod': nrm(ks[5], (DEPTH, N_MOD * D_MODEL), 0.02),
        'w_in': nrm(ks[6], (DEPTH, D_MODEL, IN_WIDTH), D_MODEL ** -0.5),
        'conv_w': nrm(ks[7], (DEPTH, CONV_WIDTH, CONV_K), CONV_K ** -0.5),
        'ret_decay_exp': 5.0 + jnp.arange(RET_HEADS, dtype=f32) + 0.25 * jax.random.uniform(ks[8], (DEPTH, 2, RET_HEADS), f32),
        'ret_gn_g': 1.0 + nrm(ks[9], (DEPTH, RET_WIDTH), 0.02),
        'q_norm_g': 1.0 + nrm(ks[10], (DEPTH, HEAD_DIM), 0.02),
        'k_norm_g': 1.0 + nrm(ks[11], (DEPTH, HEAD_DIM), 0.02),
        'w_out': nrm(ks[12], (DEPTH, MIX_WIDTH, D_MODEL), beta * MIX_WIDTH ** -0.5),
        'ln_g': 1.0 + nrm(ks[13], (DEPTH, 2, D_MODEL), 0.02),
        'ln_b': nrm(ks[14], (DEPTH, 2, D_MODEL), 0.02),
        'w_router': nrm(ks[15], (DEPTH, D_MODEL, N_EXPERTS), D_MODEL ** -0.5),
        'b_router': nrm(ks[16], (DEPTH, N_EXPERTS), 0.01),
        'w_gate_up': nrm(ks[17], (DEPTH, N_EXPERTS, D_MODEL, 2 * EXPERT_FF), D_MODEL ** -0.5),
        'b_gate_up': nrm(ks[18], (DEPTH, N_EXPERTS, 2 * EXPERT_FF), 0.01),
        'w_down': nrm(ks[19], (DEPTH, N_EXPERTS, EXPERT_FF, D_MODEL), beta * EXPERT_FF ** -0.5),
        'b_down': nrm(ks[20], (DEPTH, N_EXPERTS, D_MODEL), 0.01),
    }


def reference(x, c, ctx, c_ctx, w_mod, b_mod, w_in, conv_w, ret_decay_exp, ret_gn_g, q_norm_g, k_norm_g, w_out, ln_g, ln_b, w_router, b_router, w_gate_up, b_gate_up, w_down, b_down):
    alpha = (2.0 * DEPTH) ** 0.25
    b, l, d = x.shape
    lc = ctx.shape[1]
    cond = jax.nn.silu(c)
    cond_ctx = jax.nn.silu(c_ctx)
    for layer in range(DEPTH):
        last = layer == DEPTH - 1
        mod = (cond @ w_mod[layer] + b_mod[layer])[:, None, :]
        mod_c = cond_ctx @ w_mod[layer] + b_mod[layer]
        sh1, sc1, g1, sh2, sc2, g2 = jnp.split(mod, N_MOD, axis=-1)
        sh1c, sc1c, g1c, sh2c, sc2c, g2c = jnp.split(mod_c, N_MOD, axis=-1)

        y_lat, y_ctx = _token_mixers(x * (1.0 + sc1) + sh1, ctx * (1.0 + sc1c) + sh1c,
                                     w_in[layer], conv_w[layer], ret_decay_exp[layer], ret_gn_g[layer],
                                     q_norm_g[layer], k_norm_g[layer], w_out[layer], not last)
        x = _layer_norm(alpha * x + g1 * y_lat, ln_g[layer, 0], ln_b[layer, 0])

        h_lat = (x * (1.0 + sc2) + sh2).reshape(b * l, d)
        if not last:
            ctx = _layer_norm(alpha * ctx + g1c * y_ctx, ln_g[layer, 0], ln_b[layer, 0])
            h_ctx = (ctx * (1.0 + sc2c) + sh2c).reshape(b * lc, d)
            f = _moe(jnp.concatenate([h_ctx, h_lat], axis=0), w_router[layer], b_router[layer],
                     w_gate_up[layer], b_gate_up[layer], w_down[layer], b_down[layer])
            f_ctx = f[: b * lc].reshape(b, lc, d)
            f_lat = f[b * lc:].reshape(b, l, d)
            ctx = _layer_norm(alpha * ctx + g2c * f_ctx, ln_g[layer, 1], ln_b[layer, 1])
        else:
            f_lat = _moe(h_lat, w_router[layer], b_router[layer], w_gate_up[layer], b_gate_up[layer],
                         w_down[layer], b_down[layer]).reshape(b, l, d)
        x = _layer_norm(alpha * x + g2 * f_lat, ln_g[layer, 1], ln_b[layer, 1])
    return x
```

```python
import contextlib
import math
import numpy as np
import concourse.bass as bass
import concourse.mybir as mybir
from concourse.bass_utils import run_bass_kernel_spmd

F32 = mybir.dt.float32
BF16 = mybir.dt.bfloat16
ALU = mybir.AluOpType
AF = mybir.ActivationFunctionType
AX = mybir.AxisListType

D = 1024
L = 4096
LC = 256
NT_C = 2
NT = 34
DEPTH = 2
NE = 32
BS = 512
NBMAX = 66
I32 = mybir.dt.int32
U32 = mybir.dt.uint32
ALPHA = (2.0 * DEPTH) ** 0.25
EPS = 1e-6


class Prog:
    ENGS = ('pe', 'act', 'dve', 'pool', 'sp')

    def __init__(self, nc, stack):
        self.nc = nc
        self.ops = []
        self.sems = {}
        self.stack = stack
        for eng in ('pe', 'act', 'dve', 'pool'):
            self.sems[('e', eng)] = stack.enter_context(nc.semaphore('sem_' + eng))
        self.cnt = {}
        self.streams = {}
        self.pool_cnt = []
        self.waited = {e: {} for e in self.ENGS}
        self.total_ops = 0

    def add(self, eng, fn, reads=(), writes=(), stream=None):
        self.ops.append((eng, fn, tuple(reads), tuple(writes), stream))

    def pe(self, fn, reads=(), writes=()):
        self.add('pe', fn, reads, writes)

    def act(self, fn, reads=(), writes=()):
        self.add('act', fn, reads, writes)

    def dve(self, fn, reads=(), writes=()):
        self.add('dve', fn, reads, writes)

    def pool(self, fn, reads=(), writes=()):
        self.add('pool', fn, reads, writes)

    def dma(self, q, out, in_, reads, writes, stream, **kw):
        self.add(q, lambda e: e.dma_start(out=out, in_=in_, **kw), reads, writes, stream)

    def flush(self):
        nc = self.nc
        ops = self.ops
        self.ops = []
        n = len(ops)
        if n == 0:
            return
        self.total_ops += n
        last_writer = {}
        readers = {}
        deps = [None] * n
        for i, (eng, fn, rd, wr, st) in enumerate(ops):
            d = set()
            for r in rd:
                j = last_writer.get(r)
                if j is not None:
                    d.add((j, 0))
            for w in wr:
                j = last_writer.get(w)
                if j is not None:
                    d.add((j, 1))
                for k in readers.get(w, ()):
                    if k != i:
                        d.add((k, 2))
            deps[i] = d
            for r in rd:
                readers.setdefault(r, []).append(i)
            for w in wr:
                last_writer[w] = i
                readers[w] = []
        sig = [False] * n
        need = [None] * n
        last_compute = {}
        for i in range(n):
            eng = ops[i][0]
            lst = set()
            for (j, kind) in deps[i]:
                jeng, _, _, _, jst = ops[j]
                if jst is None and jeng == eng:
                    if eng == 'pe' or eng == 'sp':
                        continue
                    if kind == 2:
                        continue
                if jst is None:
                    sig[j] = True
                lst.add(j)
            need[i] = lst
            if ops[i][4] is None and ops[i][1] is not None:
                last_compute[eng] = i
        for eng, i in last_compute.items():
            if eng != 'sp':
                sig[i] = True
        sval = [None] * n
        phase_map = {}
        for i, (eng, fn, rd, wr, st) in enumerate(ops):
            if st is not None:
                if st not in phase_map:
                    k = len(phase_map)
                    phase_map[st] = k
                    if k >= len(self.pool_cnt):
                        self.pool_cnt.append(0)
                        self.sems[('s', k)] = self.stack.enter_context(nc.semaphore('sd_%d' % k))
                k = phase_map[st]
                self.pool_cnt[k] += 1
                sval[i] = (('s', k), 16 * self.pool_cnt[k])
            elif sig[i]:
                self.cnt[eng] = self.cnt.get(eng, 0) + 1
                sval[i] = (('e', eng), self.cnt[eng])
        per_eng = {e: [] for e in self.ENGS}
        for i, op in enumerate(ops):
            per_eng[op[0]].append(i)
        sems = self.sems
        final = {}
        for eng in ('pe', 'act', 'dve', 'pool'):
            if self.cnt.get(eng, 0) > 0:
                final[('e', eng)] = self.cnt[eng]
        for k, c in enumerate(self.pool_cnt):
            final[('s', k)] = 16 * c

        def run(engname, e):
            waited = self.waited[engname]
            for i in per_eng[engname]:
                _, fn, rd, wr, st = ops[i]
                w = {}
                for j in need[i]:
                    key, val = sval[j]
                    if w.get(key, 0) < val:
                        w[key] = val
                for key, val in w.items():
                    if waited.get(key, 0) >= val:
                        continue
                    waited[key] = val
                    e.wait_ge(sems[key], val)
                if fn is None:
                    continue
                ins = fn(e)
                if sval[i] is not None:
                    key, val = sval[i]
                    ins.then_inc(sems[key], 16 if key[0] == 's' else 1)
            for key, val in final.items():
                if key == ('e', engname):
                    continue
                if waited.get(key, 0) >= val:
                    continue
                waited[key] = val
                e.wait_ge(sems[key], val)

        with nc.Block() as block:
            @block.tensor
            def _(e):
                run('pe', e)

            @block.scalar
            def _(e):
                run('act', e)

            @block.vector
            def _(e):
                run('dve', e)

            @block.gpsimd
            def _(e):
                run('pool', e)

            @block.sync
            def _(e):
                run('sp', e)


class Ring:
    def __init__(self, name, tiles):
        self.name = name
        self.tiles = tiles
        self.i = 0

    def next(self):
        k = self.i % len(self.tiles)
        self.i += 1
        return self.tiles[k], '%s%d' % (self.name, k)


SEC = dict(u=(0, 256), B=(256, 256), C=(512, 256), rq=(768, 256), rk=(1024, 256), rv=(1280, 256),
           rg=(1536, 256), aq=(1792, 512), ak=(2304, 128), av=(2432, 128))
MYCOL = dict(u=0, C=256, B=512, rv=768, rq=1024, rk=1280, aq=1536, rg=2048, ak=2304, av=2432)


def build(phases=('p0', 'p1', 'att', 'ret', 'p3', 'moe'), layers=(0, 1), debug=False, moe_experts=NE, cut=99):
    nc = bass.Bass("TRN2", target_bir_lowering=False)

    _uc = [0]

    def uname(name):
        _uc[0] += 1
        return '%s_u%d' % (name, _uc[0])

    def din(name, shape, dt=F32):
        return nc.dram_tensor(name, list(shape), dt, kind="ExternalInput").ap()

    def dscr(name, shape, dt=F32):
        return nc.dram_tensor(name, list(shape), dt, kind=("ExternalOutput" if debug else "Internal")).ap()

    x_in = din("x", [L, D])
    c_in = din("c", [D])
    ctx_in = din("ctx", [LC, D])
    cctx_in = din("c_ctx", [D])
    w_mod = din("w_mod", [DEPTH, D, 6 * D])
    b_mod = din("b_mod", [DEPTH, 6 * D])
    w_in = din("w_in", [DEPTH, D, 2560])
    conv_w = din("conv_w", [DEPTH, 256, 3])
    rde = din("ret_decay_exp", [DEPTH, 2, 4])
    gn_g = din("ret_gn_g", [DEPTH, 256])
    qn_g = din("q_norm_g", [DEPTH, 64])
    kn_g = din("k_norm_g", [DEPTH, 64])
    w_out = din("w_out", [DEPTH, D, D])
    ln_g = din("ln_g", [DEPTH, 2, D])
    ln_b = din("ln_b", [DEPTH, 2, D])
    w_router = din("w_router", [DEPTH, D, NE])
    b_router = din("b_router", [DEPTH, NE])
    if 'moe' in phases:
        w_gu = din("w_gate_up", [DEPTH, NE, D, 2 * D])
        b_gu = din("b_gate_up", [DEPTH, NE, 2 * D])
        w_dn = din("w_down", [DEPTH, NE, D, D])
        b_dn = din("b_down", [DEPTH, NE, D])
    ident_in = din("c_ident", [128, 128])
    cos_in = din("c_cos", [L, 512])
    sin_in = din("c_sin", [L, 512])
    pos_in = din("c_pos", [128, 4])
    dpos_in = din("c_dpos", [128, 128])
    dneg_in = din("c_dneg", [128, 128])
    mge_in = din("c_mge", [128, 128])
    zeros_in = din("c_zeros", [2, 256])
    iota32_in = din("c_iota32", [128, NE])
    lts_in = din("c_lts", [128, 128])
    bstart_in = din("c_bstart", [128, NBMAX])
    kp_in = din("c_kp", [128, 8])
    iotap_in = din("c_iotap", [128, 1])

    out = nc.dram_tensor("out", [L, D], F32, kind="ExternalOutput").ap()

    N = NT * 128
    modv = dscr("modv", [DEPTH, 2, 6 * D])
    PB = dscr("PB", [N + 4, 512])
    RQT = dscr("RQT", [3, 4, 64, N], BF16)
    RKT = dscr("RKT", [4, 64, N], BF16)
    RTOK = dscr("RTOK", [N, 768], BF16)
    RG = dscr("RG", [N, 256])
    AQT = dscr("AQT", [8, 64, N], BF16)
    AKT = dscr("AKT", [2, 64, N], BF16)
    AV = dscr("AV", [N, 128], BF16)
    MIX = dscr("MIX", [N, 1024])
    XS0 = dscr("XS0", [N, D])
    XM = dscr("XM", [N, D])
    H2R = dscr("H2R", [N, D], BF16)
    RW = dscr("RW", [N, 4])
    RE = dscr("RE", [N, 4])
    XS = dscr("XS", [NBMAX * BS, D], BF16)
    SW = dscr("SW", [NBMAX * BS, 1])
    YS = dscr("YS", [NBMAX * BS, D])

    def prow(i):
        return 1 + i * 128 if i < NT_C else 259 + (i - NT_C) * 128

    with contextlib.ExitStack() as gst:
        P = Prog(nc, gst)
        psum = gst.enter_context(nc.psum_tensor("psum", [128, 8, 512], F32))
        ident = gst.enter_context(nc.sbuf_tensor("ident", [128, 128], F32))
        P.dma('sp', ident[:], ident_in, [], ['ident'], 'ident')

        def bank(b):
            return psum[:, b, :]

        if 'p0' in phases:
            with contextlib.ExitStack() as st:
                def sb(name, shape, dt=F32):
                    return st.enter_context(nc.sbuf_tensor(uname(name), list(shape), dt))
                cnd = sb("cnd", [128, 2, 8])
                P.dma('sp', cnd[:, 0, :], c_in.rearrange("(k p) -> p k", p=128), [], ['cnd'], 'cnd0',
                      allow_slow_non_contiguous=True)
                P.dma('sp', cnd[:, 1, :], cctx_in.rearrange("(k p) -> p k", p=128), [], ['cnd'], 'cnd1',
                      allow_slow_non_contiguous=True)
                P.act(lambda e: e.activation(out=cnd[:], in_=cnd[:], func=AF.Silu), ['cnd'], ['cnd'])
                wring = Ring('wm', [sb("wm%d" % i, [128, 8, 512]) for i in range(2)])
                bring = Ring('bm', [sb("bm%d" % i, [1, 512]) for i in range(2)])
                oring = Ring('om', [sb("om%d" % i, [1, 2, 512]) for i in range(2)])
                for l in layers:
                    for n in range(12):
                        wt, wk = wring.next()
                        bt, bk = bring.next()
                        ot, ok = oring.next()
                        P.dma('sp', wt[:], w_mod[l, :, n * 512:(n + 1) * 512].rearrange("(k p) n -> p k n", p=128),
                              [], [wk], wk)
                        P.dma('act', bt[:], b_mod[l, n * 512:(n + 1) * 512].rearrange("(o n) -> o n", o=1), [], [bk], bk)
                        for s in range(2):
                            pb_ = 'ps0_%d' % s
                            for k in range(8):
                                P.pe(lambda e, s=s, k=k, wt=wt: e.matmul(psum[0:1, s, :], cnd[:, s, k:k + 1], wt[:, k, :],
                                                                     start=(k == 0), stop=(k == 7)),
                                     ['cnd', wk], [pb_])
                            P.dve(lambda e, s=s, ot=ot, bt=bt: e.tensor_tensor(out=ot[:, s, :], in0=psum[0:1, s, :],
                                                                               in1=bt[:], op=ALU.add),
                                  [pb_, bk], [ok])
                        if n in (2, 3, 8, 9):
                            P.dve(lambda e, ot=ot: e.tensor_scalar_add(out=ot[:], in0=ot[:], scalar1=1.0), [ok], [ok])
                        P.dma('sp', modv[l, :, n * 512:(n + 1) * 512].rearrange("(o s) n -> o s n", o=1), ot[:],
                              [ok], [('modv', l)], ok)
                P.flush()

        def bload(q, tile_ap, vec_ap, key, reads=()):
            P.dma(q, tile_ap, vec_ap.partition_broadcast(128), list(reads), [key], key)

        for l in layers:
            last = (l == DEPTH - 1)
            xsrc = (lambda i: (ctx_in[i * 128:(i + 1) * 128, :] if i < NT_C else x_in[(i - NT_C) * 128:(i - NT_C + 1) * 128, :])) \
                if l == 0 else (lambda i: XS0[i * 128:(i + 1) * 128, :])
            xs_key = (lambda i: ('xin', i)) if l == 0 else (lambda i: ('XS0', i))
            out_tiles = list(range(NT)) if not last else list(range(NT_C, NT))

            if 'p1' in phases:
                with contextlib.ExitStack() as st:
                    def sb(name, shape, dt=F32):
                        return st.enter_context(nc.sbuf_tensor(uname(name), list(shape), dt))
                    win = sb("win", [128, 8, 2560], BF16)
                    for name, (c0, w) in SEC.items():
                        m0 = MYCOL[name]
                        P.dma('pool', win[:, :, m0:m0 + w], w_in[l, :, c0:c0 + w].rearrange("(k p) n -> p k n", p=128),
                              [], ['win_' + name], 'win_' + name)
                    winkeys = ['win_' + k for k in SEC]
                    sc1 = [sb("sc1_%d" % s, [128, D]) for s in range(2)]
                    sh1 = [sb("sh1_%d" % s, [128, D]) for s in range(2)]
                    for s in range(2):
                        bload('sp', sc1[s][:], modv[l, s, D:2 * D], 'sc1_%d' % s, [('modv', l)])
                        bload('sp', sh1[s][:], modv[l, s, 0:D], 'sh1_%d' % s, [('modv', l)])
                    gq = sb("gq", [128, 64])
                    gk = sb("gk", [128, 64])
                    bload('act', gq[:], qn_g[l], 'gq')
                    bload('act', gk[:], kn_g[l], 'gk')
                    zpad = sb("zpad", [2, 256])
                    P.dma('act', zpad[:], zeros_in, [], ['zpad'], 'zpad')
                    for r0 in (0, 257):
                        P.dma('act', PB[r0:r0 + 2, 0:256] if r0 else PB[0:1, 0:256], zpad[0:2, :] if r0 else zpad[0:1, :],
                              ['zpad'], [('PBpad', r0)], 'zp%d' % r0)
                    P.dma('act', PB[N + 3:N + 4, 0:256], zpad[0:1, :], ['zpad'], [('PBpad', 3)], 'zp3')
                    posc = sb("posc", [128, 4])
                    P.dma('act', posc[:], pos_in, [], ['posc'], 'posc')
                    lg = sb("lg", [128, 8])
                    bload('act', lg[:], rde[l].rearrange("a h -> (a h)"), 'lg')
                    P.act(lambda e: e.activation(out=lg[:], in_=lg[:], func=AF.Exp, scale=-math.log(2.0)), ['lg'], ['lg'])
                    P.act(lambda e: e.activation(out=lg[:], in_=lg[:], func=AF.Ln, scale=-1.0, bias=1.0), ['lg'], ['lg'])
                    tab4 = sb("tab4", [128, 4, 4])
                    for ti, (di, pc) in enumerate(((0, 0), (1, 1), (0, 2), (1, 3))):
                        P.act(lambda e, ti=ti, di=di, pc=pc: e.activation(out=tab4[:, ti, :], in_=lg[:, di * 4:(di + 1) * 4],
                                                                          func=AF.Exp, scale=posc[:, pc:pc + 1]),
                              ['lg', 'posc'], ['tab4'])
                    P.dve(lambda e: e.tensor_scalar_mul(out=tab4[:, 0:2, :], in0=tab4[:, 0:2, :], scalar1=0.125), ['tab4'], ['tab4'])
                    TAB = sb("TAB", [128, 4, 4, 64])
                    P.dve(lambda e: e.tensor_copy(out=TAB[:].rearrange("p t h d -> p (t h) d"),
                                                  in_=tab4[:].rearrange("p t h -> p (t h)").unsqueeze(2).to_broadcast([128, 16, 64])),
                          ['tab4'], ['TAB'])
                    gq8 = sb("gq8", [128, 8, 64])
                    P.dve(lambda e: e.tensor_copy(out=gq8[:], in_=gq[:].unsqueeze(1).to_broadcast([128, 8, 64])), ['gq'], ['gq8'])
                    gk2 = sb("gk2", [128, 2, 64])
                    P.dve(lambda e: e.tensor_copy(out=gk2[:], in_=gk[:].unsqueeze(1).to_broadcast([128, 2, 64])), ['gk'], ['gk2'])

                    xring = Ring('xt', [sb("xt%d" % i, [128, D]) for i in range(3)])
                    csring = Ring('cs', [sb("cs%d" % i, [128, 512]) for i in range(3)])
                    snring = Ring('sn', [sb("sn%d" % i, [128, 512]) for i in range(3)])
                    hring = Ring('h', [sb("h%d" % i, [128, D]) for i in range(2)])
                    hTring = Ring('hT', [sb("hT%d" % i, [128, 8, 128], BF16) for i in range(2)])
                    usb = sb("usb", [128, 256])
                    pcb = Ring('pcb', [sb("pcb%d" % i, [128, 512]) for i in range(2)])
                    t1 = sb("t1", [128, 512])
                    t2 = sb("t2", [128, 512])
                    rq = sb("rq", [128, 4, 256])
                    rtok = Ring('rtok', [sb("rtok%d" % i, [128, 768], BF16) for i in range(2)])
                    rgt = Ring('rgt', [sb("rgt%d" % i, [128, 256]) for i in range(2)])
                    rT = Ring('rT', [sb("rT%d" % i, [128, 8, 128], BF16) for i in range(2)])
                    sq = sb("sq", [128, 512])
                    ss = sb("ss", [128, 8])
                    aq = sb("aq", [128, 512])
                    aqn = sb("aqn", [128, 512])
                    akn = sb("akn", [128, 128])
                    ak = sb("ak", [128, 128])
                    aT = Ring('aT', [sb("aT%d" % i, [128, 5, 128], BF16) for i in range(2)])
                    avt = Ring('avt', [sb("avt%d" % i, [128, 128], BF16) for i in range(2)])

                    def rope(src_ap, W, dst_ap, src_keys, dst_key, cs, ck, sn, sk):
                        g = W // 32
                        P.dve(lambda e: e.tensor_tensor(out=t1[:, :W], in0=src_ap, in1=cs[:, :W], op=ALU.mult),
                              src_keys + [ck], ['t1'])
                        s4 = src_ap.rearrange("p (g a f) -> p g a f", a=2, f=16)
                        t4 = t2[:, :W].rearrange("p (g a f) -> p g a f", a=2, f=16)
                        n4 = sn[:, :W].rearrange("p (g a f) -> p g a f", a=2, f=16)
                        P.dve(lambda e: e.tensor_tensor(out=t4[:, :, 0, :], in0=s4[:, :, 1, :], in1=n4[:, :, 0, :], op=ALU.mult),
                              src_keys + [sk], ['t2a'])
                        P.dve(lambda e: e.tensor_tensor(out=t4[:, :, 1, :], in0=s4[:, :, 0, :], in1=n4[:, :, 1, :], op=ALU.mult),
                              src_keys + [sk], ['t2b'])
                        P.pool(lambda e: e.tensor_tensor(out=dst_ap, in0=t1[:, :W], in1=t2[:, :W], op=ALU.add),
                               ['t1', 't2a', 't2b'], [dst_key])

                    ld = {}

                    def issue_loads(i):
                        xt, xk = xring.next()
                        P.dma('sp', xt[:], xsrc(i), [xs_key(i)], [xk], xk)
                        if i >= NT_C:
                            cs, ck = csring.next()
                            sn, sk = snring.next()
                            t0 = (i - NT_C) * 128
                            P.dma('sp', cs[:], cos_in[t0:t0 + 128, :], [], [ck], ck)
                            P.dma('sp', sn[:], sin_in[t0:t0 + 128, :], [], [sk], sk)
                            ld[i] = (xt, xk, cs, ck, sn, sk)
                        else:
                            ld[i] = (xt, xk, None, None, None, None)
                    issue_loads(0)
                    for i in range(NT):
                        isctx = i < NT_C
                        s = 1 if isctx else 0
                        if i + 1 < NT:
                            issue_loads(i + 1)
                        xt, xk, cs, ck, sn, sk = ld.pop(i)
                        h, hk = hring.next()
                        P.dve(lambda e, h=h, xt=xt, s=s: e.tensor_tensor(out=h[:], in0=xt[:], in1=sc1[s][:], op=ALU.mult),
                              [xk, 'sc1_%d' % s], [hk])
                        P.dve(lambda e, h=h, s=s: e.tensor_tensor(out=h[:], in0=h[:], in1=sh1[s][:], op=ALU.add),
                              [hk, 'sh1_%d' % s], [hk])
                        for k in range(8):
                            P.pe(lambda e, h=h, k=k: e.transpose(psum[:, 5 + k // 4, (k % 4) * 128:(k % 4 + 1) * 128],
                                                                 h[:, k * 128:(k + 1) * 128], ident[:]),
                                 [hk, 'ident'], ['pT%d' % k])
                        hT, hTk = hTring.next()
                        for hh in range(2):
                            P.act(lambda e, hT=hT, hh=hh: e.activation(out=hT[:, hh * 4:(hh + 1) * 4, :].rearrange("p k n -> p (k n)"),
                                                                       in_=psum[:, 5 + hh, :], func=AF.Copy),
                                  ['pT%d' % k for k in range(hh * 4, hh * 4 + 4)], [hTk + '_%d' % hh])
                        for b in range(5):
                            for k in range(8):
                                P.pe(lambda e, hT=hT, b=b, k=k: e.matmul(bank(b), hT[:, k, :], win[:, k, b * 512:(b + 1) * 512],
                                                                         start=(k == 0), stop=(k == 7)),
                                     [hTk + '_%d' % (k // 4)] + winkeys, ['z%d' % b])
                        pc, pck = pcb.next()
                        P.act(lambda e: e.activation(out=usb[:], in_=psum[:, 0, 0:256], func=AF.Copy), ['z0'], ['usb'])
                        P.dve(lambda e, pc=pc: e.tensor_tensor(out=pc[:, 0:256], in0=psum[:, 0, 256:512], in1=usb[:], op=ALU.mult),
                              ['z0', 'usb'], [pck + 'a'])
                        P.act(lambda e, pc=pc: e.activation(out=pc[:, 256:512], in_=psum[:, 1, 0:256], func=AF.Copy), ['z1'], [pck + 'b'])
                        r0 = prow(i)
                        P.dma('sp', PB[r0:r0 + 128, :], pc[:], [pck + 'a', pck + 'b'], [('PB', i)], pck)
                        rt, rtk = rtok.next()
                        P.act(lambda e, rt=rt: e.activation(out=rt[:, 512:768], in_=psum[:, 1, 256:512], func=AF.Copy), ['z1'], [rtk + 'v'])
                        if isctx:
                            P.act(lambda e: e.activation(out=rq[:, 0, :], in_=psum[:, 2, 0:256], func=AF.Copy), ['z2'], ['rq0'])
                            P.act(lambda e: e.activation(out=rq[:, 3, :], in_=psum[:, 2, 256:512], func=AF.Copy), ['z2'], ['rq3'])
                        else:
                            rope(psum[:, 2, 0:256], 256, rq[:, 0, :], ['z2'], 'rq0', cs, ck, sn, sk)
                            rope(psum[:, 2, 256:512], 256, rq[:, 3, :], ['z2'], 'rq3', cs, ck, sn, sk)
                        TABf = TAB[:].rearrange("p t h d -> p t (h d)")
                        P.dve(lambda e: e.tensor_tensor(out=rq[:, 1, :], in0=rq[:, 0, :], in1=TABf[:, 2, :], op=ALU.mult), ['rq0', 'TAB'], ['rq1'])
                        P.pool(lambda e: e.tensor_tensor(out=rq[:, 2, :], in0=rq[:, 0, :], in1=TABf[:, 3, :], op=ALU.mult), ['rq0', 'TAB'], ['rq2'])
                        P.dve(lambda e, rt=rt: e.tensor_tensor(out=rt[:, 0:256], in0=rq[:, 3, :], in1=TABf[:, 0, :], op=ALU.mult), ['rq3', 'TAB'], [rtk + 'f'])
                        P.pool(lambda e, rt=rt: e.tensor_tensor(out=rt[:, 256:512], in0=rq[:, 3, :], in1=TABf[:, 1, :], op=ALU.mult), ['rq3', 'TAB'], [rtk + 'b'])
                        P.dma('sp', RTOK[i * 128:(i + 1) * 128, :], rt[:], [rtk + 'v', rtk + 'f', rtk + 'b'], [('RTOK', i)], rtk)
                        rg_, rgk = rgt.next()
                        P.act(lambda e, rg_=rg_: e.activation(out=rg_[:], in_=psum[:, 4, 0:256], func=AF.Silu), ['z4'], [rgk])
                        P.dma('act', RG[i * 128:(i + 1) * 128, :], rg_[:], [rgk], [('RG', i)], rgk)
                        for t in range(4):
                            for c2 in range(2):
                                idx = t * 2 + c2
                                P.pe(lambda e, t=t, c2=c2, idx=idx: e.transpose(psum[:, 5 + idx // 4, (idx % 4) * 128:(idx % 4 + 1) * 128],
                                                                                rq[:, t, c2 * 128:(c2 + 1) * 128], ident[:]),
                                     ['rq%d' % t, 'ident'], ['pT%d' % idx])
                        rTt, rTk = rT.next()
                        for hh in range(2):
                            if hh == 0:
                                P.act(lambda e, rTt=rTt: e.activation(out=rTt[:, 0:4, :].rearrange("p k n -> p (k n)"), in_=psum[:, 5, :], func=AF.Copy),
                                      ['pT0', 'pT1', 'pT2', 'pT3'], [rTk + 'a'])
                            else:
                                P.act(lambda e, rTt=rTt: e.activation(out=rTt[:, 4:6, :].rearrange("p k n -> p (k n)"), in_=psum[:, 6, 0:256], func=AF.Copy),
                                      ['pT4', 'pT5'], [rTk + 'b'])
                                P.act(lambda e, rTt=rTt: e.activation(out=rTt[:, 6:8, :].rearrange("p k n -> p (k n)"), in_=psum[:, 6, 256:512], func=AF.Copy, scale=0.125),
                                      ['pT6', 'pT7'], [rTk + 'c'])
                        for t in range(3):
                            P.dma('act', RQT[t, :, :, i * 128:(i + 1) * 128].rearrange("(c q) d n -> (q d) c n", q=2),
                                  rTt[:, 2 * t:2 * t + 2, :], [rTk + 'a', rTk + 'b'], [('RQT', i, t)], rTk + 'q%d' % t)
                        P.dma('act', RKT[:, :, i * 128:(i + 1) * 128].rearrange("(c q) d n -> (q d) c n", q=2),
                              rTt[:, 6:8, :], [rTk + 'c'], [('RKT', i)], rTk + 'k')
                        P.act(lambda e: e.activation(out=sq[:], in_=psum[:, 3, :], func=AF.Square), ['z3'], ['sq'])
                        P.dve(lambda e: e.tensor_reduce(out=ss[:], in_=sq[:].rearrange("p (h d) -> p h d", d=64), axis=AX.X, op=ALU.add), ['sq'], ['ss'])
                        P.dve(lambda e: e.tensor_scalar(out=ss[:], in0=ss[:], scalar1=1.0 / 64, scalar2=EPS, op0=ALU.mult, op1=ALU.add), ['ss'], ['ss'])
                        P.act(lambda e: e.activation(out=ss[:], in_=ss[:], func=AF.Sqrt), ['ss'], ['ss'])
                        P.dve(lambda e: e.reciprocal(out=ss[:], in_=ss[:]), ['ss'], ['ss'])
                        P.dve(lambda e: e.tensor_tensor(out=aqn[:].rearrange("p (h d) -> p h d", d=64), in0=psum[:, 3, :].rearrange("p (h d) -> p h d", d=64),
                                                        in1=ss[:].unsqueeze(2).to_broadcast([128, 8, 64]), op=ALU.mult), ['z3', 'ss'], ['aqn'])
                        P.pool(lambda e: e.tensor_tensor(out=aqn[:], in0=aqn[:], in1=gq8[:].rearrange("p h d -> p (h d)"), op=ALU.mult), ['aqn', 'gq8'], ['aqn'])
                        if isctx:
                            aq_src, aq_key = aqn, 'aqn'
                        else:
                            rope(aqn[:], 512, aq[:], ['aqn'], 'aq', cs, ck, sn, sk)
                            aq_src, aq_key = aq, 'aq'
                        av_, avk = avt.next()
                        P.act(lambda e, av_=av_: e.activation(out=av_[:], in_=psum[:, 4, 384:512], func=AF.Copy), ['z4'], [avk])
                        P.dma('act', AV[i * 128:(i + 1) * 128, :], av_[:], [avk], [('AV', i)], avk)
                        P.act(lambda e: e.activation(out=sq[:, 0:128], in_=psum[:, 4, 256:384], func=AF.Square), ['z4'], ['sqk'])
                        P.dve(lambda e: e.tensor_reduce(out=ss[:, 0:2], in_=sq[:, 0:128].rearrange("p (h d) -> p h d", d=64), axis=AX.X, op=ALU.add), ['sqk'], ['ssk'])
                        P.dve(lambda e: e.tensor_scalar(out=ss[:, 0:2], in0=ss[:, 0:2], scalar1=1.0 / 64, scalar2=EPS, op0=ALU.mult, op1=ALU.add), ['ssk'], ['ssk'])
                        P.act(lambda e: e.activation(out=ss[:, 0:2], in_=ss[:, 0:2], func=AF.Sqrt), ['ssk'], ['ssk'])
                        P.dve(lambda e: e.reciprocal(out=ss[:, 0:2], in_=ss[:, 0:2]), ['ssk'], ['ssk'])
                        P.dve(lambda e: e.tensor_tensor(out=akn[:].rearrange("p (h d) -> p h d", d=64), in0=psum[:, 4, 256:384].rearrange("p (h d) -> p h d", d=64),
                                                        in1=ss[:, 0:2].unsqueeze(2).to_broadcast([128, 2, 64]), op=ALU.mult), ['z4', 'ssk'], ['akn'])
                        P.pool(lambda e: e.tensor_tensor(out=akn[:], in0=akn[:], in1=gk2[:].rearrange("p h d -> p (h d)"), op=ALU.mult), ['akn', 'gk2'], ['akn'])
                        if isctx:
                            ak_src, ak_key = akn, 'akn'
                        else:
                            rope(akn[:], 128, ak[:], ['akn'], 'ak', cs, ck, sn, sk)
                            ak_src, ak_key = ak, 'ak'
                        for c4 in range(4):
                            P.pe(lambda e, c4=c4, aq_src=aq_src: e.transpose(psum[:, 7, c4 * 128:(c4 + 1) * 128], aq_src[:, c4 * 128:(c4 + 1) * 128], ident[:]),
                                 [aq_key, 'ident'], ['pA%d' % c4])
                        P.pe(lambda e, ak_src=ak_src: e.transpose(psum[:, 5, 0:128], ak_src[:, 0:128], ident[:]), [ak_key, 'ident'], ['pT0'])
                        aTt, aTk = aT.next()
                        P.act(lambda e, aTt=aTt: e.activation(out=aTt[:, 0:4, :].rearrange("p k n -> p (k n)"), in_=psum[:, 7, :], func=AF.Copy),
                              ['pA0', 'pA1', 'pA2', 'pA3'], [aTk + 'q'])
                        P.act(lambda e, aTt=aTt: e.activation(out=aTt[:, 4, :], in_=psum[:, 5, 0:128], func=AF.Copy), ['pT0'], [aTk + 'k'])
                        P.dma('sp', AQT[:, :, i * 128:(i + 1) * 128].rearrange("(c q) d n -> (q d) c n", q=2), aTt[:, 0:4, :],
                              [aTk + 'q'], [('AQT', i)], aTk + 'q')
                        P.dma('sp', AKT[:, :, i * 128:(i + 1) * 128].rearrange("q d n -> (q d) n"), aTt[:, 4, :],
                              [aTk + 'k'], [('AKT', i)], aTk + 'k')
                    P.flush()


            if 'att' in phases:
                with contextlib.ExitStack() as st:
                    def sb(name, shape, dt=F32):
                        return st.enter_context(nc.sbuf_tensor(uname(name), list(shape), dt))
                    KT = sb("KT", [128, 2, N], BF16)
                    P.pool(lambda e: e.memset(KT[64:128, :, :], 0.0), [], ['KTz'])
                    P.dma('sp', KT[0:64, :, :], AKT.rearrange("k d n -> d k n"), [], ['KT'], 'KT')
                    V1 = sb("V1", [128, NT, 2, 65], BF16)
                    P.pool(lambda e: e.memset(V1[:], 1.0), [], ['V1'])
                    for kk_ in range(2):
                        P.dma('act', V1[:, :, kk_, 0:64], AV[:, kk_ * 64:(kk_ + 1) * 64].rearrange("(c p) d -> p c d", p=128), [], ['V1'], 'V1_%d' % kk_)
                    qring = Ring('QT', [sb("QT%d" % i, [128, N], BF16) for i in range(2)])
                    for qi_, qt_ in enumerate(qring.tiles):
                        P.pool(lambda e, qt_=qt_: e.memset(qt_[64:128, :], 0.0), [], ['QTz%d' % qi_])
                    ptring = Ring('PT', [sb("PT%d" % i, [128, 512], BF16) for i in range(4)])
                    aoring = Ring('AO', [sb("AO%d" % i, [128, 4, 64]) for i in range(2)])
                    rcring = Ring('rc', [sb("rc%d" % i, [128, 4]) for i in range(2)])
                    sbank = Ring('S', [0, 1, 2, 3])
                    obank = Ring('O', [4, 5])
                    groups = []
                    if not last:
                        groups.append((0, 2, [0, 1]))
                    for g in range(8):
                        groups.append((LC + g * 512, 4, list(range(NT))))
                    items = []
                    for hq in range(8):
                        for gi, (q0, nq, chunks) in enumerate(groups):
                            for ci, c in enumerate(chunks):
                                items.append((hq, gi, q0, nq, ci, c, len(chunks)))
                    qt_of = {}
                    st_of = {}

                    def get_qt(hq):
                        if hq not in qt_of:
                            QT, qk = qring.next()
                            P.dma('sp', QT[0:64, :], AQT[hq], [], [qk], qk)
                            qt_of[hq] = (QT, qk)
                        return qt_of[hq]

                    def emit_S(t):
                        hq, gi, q0, nq, ci, c, nch = items[t]
                        QT, qk = get_qt(hq)
                        kvh = hq // 4
                        W = nq * 128
                        sbk, sk = sbank.next()
                        P.pe(lambda e, sbk=sbk, c=c, QT=QT, q0=q0, W=W, kvh=kvh: e.matmul(psum[:, sbk, 0:W], KT[:, kvh, c * 128:(c + 1) * 128],
                                                                                         QT[:, q0:q0 + W], start=True, stop=True),
                             ['KT', 'KTz', 'QTz0', 'QTz1', qk], [sk])
                        st_of[t] = (sbk, sk)

                    cur_o = [None]

                    def emit_rest(t):
                        hq, gi, q0, nq, ci, c, nch = items[t]
                        kvh = hq // 4
                        W = nq * 128
                        sbk, sk = st_of.pop(t)
                        if ci == 0:
                            cur_o[0] = obank.next()
                        ob, ok = cur_o[0]
                        Ov = psum[:, ob, 0:260].rearrange("p (j e) -> p j e", e=65)
                        PT, pk = ptring.next()
                        P.act(lambda e, PT=PT, sbk=sbk, W=W: e.activation(out=PT[:, 0:W], in_=psum[:, sbk, 0:W], func=AF.Exp, scale=0.125),
                              [sk], [pk])
                        if t + 2 < len(items):
                            emit_S(t + 2)
                        for j in range(nq):
                            P.pe(lambda e, PT=PT, j=j, c=c, kvh=kvh, ci=ci, Ov=Ov, nch=nch: e.matmul(
                                Ov[:, j, :], PT[:, j * 128:(j + 1) * 128], V1[:, c, kvh, :],
                                start=(ci == 0 and j == 0), stop=(ci == nch - 1), skip_group_check=True),
                                [pk, 'V1'], [ok])
                        if ci == nch - 1:
                            rc, rk_ = rcring.next()
                            AO, ak_ = aoring.next()
                            P.dve(lambda e, rc=rc, Ov=Ov, nq=nq: e.reciprocal(out=rc[:, 0:nq], in_=Ov[:, 0:nq, 64]), [ok], [rk_])
                            P.dve(lambda e, rc=rc, Ov=Ov, nq=nq, AO=AO: e.tensor_tensor(out=AO[:, 0:nq, :], in0=Ov[:, 0:nq, 0:64],
                                                                                      in1=rc[:, 0:nq].unsqueeze(2).to_broadcast([128, nq, 64]), op=ALU.mult),
                                  [ok, rk_], [ak_])
                            P.dma('sp', MIX[q0:q0 + W, 512 + hq * 64:512 + (hq + 1) * 64].rearrange("(j p) d -> p j d", p=128), AO[:, 0:nq, :],
                                  [ak_], [('MIXa', q0, hq)], ak_)
                    emit_S(0)
                    emit_S(1)
                    for t in range(len(items)):
                        emit_rest(t)
                    P.flush()

            if 'ret' in phases:
                with contextlib.ExitStack() as st:
                    def sb(name, shape, dt=F32):
                        return st.enter_context(nc.sbuf_tensor(uname(name), list(shape), dt))
                    RT = sb("RT", [128, NT, 768], BF16)
                    P.dma('sp', RT[:], RTOK.rearrange("(c p) w -> p c w", p=128), [], ['RT'], 'RT')
                    RGt = sb("RGt", [128, NT, 256])
                    P.dma('act', RGt[:], RG.rearrange("(c p) w -> p c w", p=128), [], ['RGt'], 'RGt')
                    gng = sb("gng", [128, 256])
                    bload('act', gng[:], gn_g[l], 'gng')
                    lg = sb("lg", [128, 8])
                    bload('act', lg[:], rde[l].rearrange("a h -> (a h)"), 'lg')
                    P.act(lambda e: e.activation(out=lg[:], in_=lg[:], func=AF.Exp, scale=-math.log(2.0)), ['lg'], ['lg'])
                    P.act(lambda e: e.activation(out=lg[:], in_=lg[:], func=AF.Ln, scale=-1.0, bias=1.0), ['lg'], ['lg'])
                    dec = sb("dec", [128, 8])
                    P.act(lambda e: e.activation(out=dec[:], in_=lg[:], func=AF.Exp, scale=128.0), ['lg'], ['dec'])
                    dpos = sb("dpos", [128, 128]); dneg = sb("dneg", [128, 128]); mge = sb("mge", [128, 128])
                    P.dma('sp', dpos[:], dpos_in, [], ['dpos'], 'dpos')
                    P.dma('sp', dneg[:], dneg_in, [], ['dneg'], 'dneg')
                    P.dma('sp', mge[:], mge_in, [], ['mge'], 'mge')
                    DcT = sb("DcT", [128, 4, 128])
                    e1 = sb("e1", [128, 128])
                    for hh in range(4):
                        P.act(lambda e, hh=hh: e.activation(out=e1[:], in_=dpos[:], func=AF.Exp, scale=lg[:, hh:hh + 1]), ['dpos', 'lg'], ['e1'])
                        P.act(lambda e, hh=hh: e.activation(out=DcT[:, hh, :], in_=dneg[:], func=AF.Exp, scale=lg[:, 4 + hh:5 + hh]), ['dneg', 'lg'], ['DcT%d' % hh])
                        P.dve(lambda e, hh=hh: e.tensor_tensor(out=e1[:], in0=e1[:], in1=DcT[:, hh, :], op=ALU.subtract), ['e1', 'DcT%d' % hh], ['e1'])
                        P.dve(lambda e, hh=hh: e.tensor_tensor(out=e1[:], in0=e1[:], in1=mge[:], op=ALU.mult), ['e1', 'mge'], ['e1'])
                        P.dve(lambda e, hh=hh: e.tensor_tensor(out=DcT[:, hh, :], in0=DcT[:, hh, :], in1=e1[:], op=ALU.add), ['e1', 'DcT%d' % hh], ['DcT%d' % hh])
                    q3ring = Ring('Q3', [sb("Q3_%d" % i, [64, 3, N], BF16) for i in range(2)])
                    ktring = Ring('KTh', [sb("KTh%d" % i, [64, N], BF16) for i in range(2)])
                    Sf = sb("Sf", [64, NT + 1, 64], BF16)
                    Sb = sb("Sb", [64, NT + 1, 64], BF16)
                    srun = Ring('srun', [sb("srun%d" % i, [64, 64]) for i in range(2)])
                    ptr = Ring('PTr', [sb("PTr%d" % i, [128, 128], BF16) for i in range(2)])
                    st6 = sb("st6", [128, 6]); mv = sb("mv", [128, 2]); rs = sb("rs", [128, 1])
                    yo = Ring('yo', [sb("yo%d" % i, [128, 64]) for i in range(2)])
                    kvb = Ring('kv', [0, 1]); scb = Ring('sc', [2, 3]); yb = Ring('y', [4, 5])
                    ytiles = list(range(NT)) if not last else list(range(NT_C, NT))
                    for hh in range(4):
                        Q3, q3k = q3ring.next()
                        KTh, ktk = ktring.next()
                        P.dma('sp', Q3[:], RQT[:, hh].rearrange("t d n -> d t n"), [], [q3k], q3k)
                        P.dma('act', KTh[:], RKT[hh], [], [ktk], ktk)
                        vcol = 512 + hh * 64

                        def scan(order_tiles, S, skey, kcol, dcol, run_init_zero):
                            return None
                        run, runk = srun.next()
                        P.pool(lambda e, run=run: e.memset(run[:], 0.0), [], [runk])
                        for i in range(NT):
                            P.pool(lambda e, run=run, i=i: e.tensor_copy(out=Sf[:, i, :], in_=run[:]), [runk], [('Sf', i)])
                            kb, kk = kvb.next()
                            P.pe(lambda e, kb=kb, i=i, hh=hh, vcol=vcol: e.matmul(psum[0:64, kb, 0:64], RT[:, i, hh * 64:(hh + 1) * 64],
                                                                                 RT[:, i, vcol:vcol + 64], start=True, stop=True), ['RT'], [kk])
                            nrun, nrunk = srun.next()
                            P.dve(lambda e, run=run, nrun=nrun, kb=kb, hh=hh: e.scalar_tensor_tensor(out=nrun[:], in0=run[:], scalar=dec[0:64, hh:hh + 1],
                                                                                                     in1=psum[0:64, kb, 0:64], op0=ALU.mult, op1=ALU.add),
                                  [runk, kk, 'dec'], [nrunk])
                            run, runk = nrun, nrunk
                        run, runk = srun.next()
                        P.pool(lambda e, run=run: e.memset(run[:], 0.0), [], [runk])
                        for i in [1, 0] + list(range(NT - 1, NT_C - 1, -1)):
                            P.pool(lambda e, run=run, i=i: e.tensor_copy(out=Sb[:, i, :], in_=run[:]), [runk], [('Sb', i)])
                            kb, kk = kvb.next()
                            P.pe(lambda e, kb=kb, i=i, hh=hh, vcol=vcol: e.matmul(psum[0:64, kb, 0:64], RT[:, i, 256 + hh * 64:256 + (hh + 1) * 64],
                                                                                 RT[:, i, vcol:vcol + 64], start=True, stop=True), ['RT'], [kk])
                            nrun, nrunk = srun.next()
                            P.dve(lambda e, run=run, nrun=nrun, kb=kb, hh=hh: e.scalar_tensor_tensor(out=nrun[:], in0=run[:], scalar=dec[0:64, 4 + hh:5 + hh],
                                                                                                     in1=psum[0:64, kb, 0:64], op0=ALU.mult, op1=ALU.add),
                                  [runk, kk, 'dec'], [nrunk])
                            run, runk = nrun, nrunk
                        for i in ytiles:
                            sbk, sk = scb.next()
                            P.pe(lambda e, sbk=sbk, i=i, KTh=KTh, Q3=Q3: e.matmul(psum[:, sbk, 0:128], KTh[:, i * 128:(i + 1) * 128], Q3[:, 0, i * 128:(i + 1) * 128],
                                                                                start=True, stop=True), [ktk, q3k], [sk])
                            PTr, pk = ptr.next()
                            P.dve(lambda e, PTr=PTr, sbk=sbk, hh=hh: e.tensor_tensor(out=PTr[:], in0=psum[:, sbk, 0:128], in1=DcT[:, hh, :], op=ALU.mult),
                                  [sk, 'DcT%d' % hh], [pk])
                            ybk, yk = yb.next()
                            P.pe(lambda e, ybk=ybk, PTr=PTr, i=i, vcol=vcol: e.matmul(psum[:, ybk, 0:64], PTr[:], RT[:, i, vcol:vcol + 64], start=True, stop=False),
                                 [pk, 'RT'], [yk])
                            P.pe(lambda e, ybk=ybk, Q3=Q3, i=i: e.matmul(psum[:, ybk, 0:64], Q3[:, 1, i * 128:(i + 1) * 128], Sf[:, i, :], start=False, stop=False),
                                 [q3k, ('Sf', i)], [yk])
                            P.pe(lambda e, ybk=ybk, Q3=Q3, i=i: e.matmul(psum[:, ybk, 0:64], Q3[:, 2, i * 128:(i + 1) * 128], Sb[:, i, :], start=False, stop=True),
                                 [q3k, ('Sb', i)], [yk])
                            P.dve(lambda e, ybk=ybk: e.bn_stats(out=st6[:], in_=psum[:, ybk, 0:64]), [yk], ['st6'])
                            P.dve(lambda e: e.bn_aggr(out=mv[:], in_=st6[:]), ['st6'], ['mv'])
                            P.dve(lambda e: e.tensor_scalar_add(out=rs[:], in0=mv[:, 1:2], scalar1=EPS), ['mv'], ['rs'])
                            P.act(lambda e: e.activation(out=rs[:], in_=rs[:], func=AF.Sqrt), ['rs'], ['rs'])
                            P.dve(lambda e: e.reciprocal(out=rs[:], in_=rs[:]), ['rs'], ['rs'])
                            y_, yok = yo.next()
                            P.dve(lambda e, y_=y_, ybk=ybk: e.tensor_scalar(out=y_[:], in0=psum[:, ybk, 0:64], scalar1=mv[:, 0:1], scalar2=rs[:, 0:1],
                                                                           op0=ALU.subtract, op1=ALU.mult), [yk, 'mv', 'rs'], [yok])
                            P.pool(lambda e, y_=y_, hh=hh: e.tensor_tensor(out=y_[:], in0=y_[:], in1=gng[:, hh * 64:(hh + 1) * 64], op=ALU.mult), [yok, 'gng'], [yok])
                            P.pool(lambda e, y_=y_, hh=hh, i=i: e.tensor_tensor(out=y_[:], in0=y_[:], in1=RGt[:, i, hh * 64:(hh + 1) * 64], op=ALU.mult), [yok, 'RGt'], [yok])
                            P.dma('sp', MIX[i * 128:(i + 1) * 128, 256 + hh * 64:256 + (hh + 1) * 64], y_[:], [yok], [('MIXr', i, hh)], yok)
                    P.flush()

            if 'p3' in phases:
                with contextlib.ExitStack() as st:
                    def sb(name, shape, dt=F32):
                        return st.enter_context(nc.sbuf_tensor(uname(name), list(shape), dt))
                    wout = sb("wout", [128, 8, D], BF16)
                    for hf in range(2):
                        P.dma('pool', wout[:, :, hf * 512:(hf + 1) * 512], w_out[l, :, hf * 512:(hf + 1) * 512].rearrange("(k p) n -> p k n", p=128),
                              [], ['wout%d' % hf], 'wout%d' % hf)
                    CWr = sb("CWr", [128, 256, 3])
                    P.dma('act', CWr[:].rearrange("p c k -> p (c k)"), conv_w[l].rearrange("c k -> (c k)").partition_broadcast(128), [], ['CWr'], 'CWr')
                    CW = sb("CW", [128, 3, 256])
                    for k in range(3):
                        P.dve(lambda e, k=k: e.tensor_copy(out=CW[:, k, :], in_=CWr[:, :, k]), ['CWr'], ['CW%d' % k])
                    g1 = [sb("g1_%d" % s, [128, D]) for s in range(2)]
                    sc2 = [sb("sc2_%d" % s, [128, D]) for s in range(2)]
                    sh2 = [sb("sh2_%d" % s, [128, D]) for s in range(2)]
                    for s in range(2):
                        if last and s == 1:
                            continue
                        bload('sp', g1[s][:], modv[l, s, 2 * D:3 * D], 'g1_%d' % s)
                        bload('sp', sh2[s][:], modv[l, s, 3 * D:4 * D], 'sh2_%d' % s)
                        bload('sp', sc2[s][:], modv[l, s, 4 * D:5 * D], 'sc2_%d' % s)
                    lng = sb("lng", [128, D]); lnb = sb("lnb", [128, D])
                    bload('act', lng[:], ln_g[l, 0], 'lng')
                    bload('act', lnb[:], ln_b[l, 0], 'lnb')
                    wr = sb("wr", [128, 8, NE])
                    P.dma('act', wr[:], w_router[l].rearrange("(k p) e -> p k e", p=128), [], ['wr'], 'wr')
                    brt = sb("brt", [128, NE])
                    bload('act', brt[:], b_router[l], 'brt')
                    pmr = Ring('pm', [sb("pm%d" % i, [128, 3, 256]) for i in range(3)])
                    btr = Ring('bt', [sb("bt%d" % i, [128, 256]) for i in range(3)])
                    mixr = Ring('mix', [sb("mix%d" % i, [128, D]) for i in range(3)])
                    xr = Ring('x3', [sb("x3_%d" % i, [128, D]) for i in range(3)])
                    ca = sb("ca", [128, 256]); cb = sb("cb", [128, 256])
                    mTr = Ring('mT', [sb("mT%d" % i, [128, 8, 128], BF16) for i in range(2)])
                    rr = sb("rr", [128, D])
                    x1r = Ring('x1', [sb("x1_%d" % i, [128, D]) for i in range(2)])
                    h2 = sb("h2", [128, D])
                    h2br = Ring('h2b', [sb("h2b%d" % i, [128, D], BF16) for i in range(2)])
                    ix8 = sb("ix8", [128, 8], U32)
                    h2f = sb("h2f", [128, 8, 128])
                    st6 = sb("st6", [128, 2, 6]); mv = sb("mv", [128, 2]); rs = sb("rs", [128, 1])
                    lgt = sb("lgt", [128, NE]); mx8 = sb("mx8", [128, 8]); msk = sb("msk", [128, NE]); nmx = sb("nmx", [128, 1])
                    ex = sb("ex", [128, NE]); sm = sb("sm", [128, 1])
                    mwr = Ring('mw', [sb("mw%d" % i, [128, NE]) for i in range(2)])
                    ld3 = {}

                    def issue_loads3(i):
                        r0 = prow(i)
                        pm, pmk = pmr.next()
                        for k in range(3):
                            P.dma('sp', pm[:, k, :], PB[r0 - 1 + k:r0 + 127 + k, 0:256], [], [pmk + str(k)], pmk + str(k))
                        bt, btk = btr.next()
                        P.dma('sp', bt[:], PB[r0:r0 + 128, 256:512], [], [btk], btk)
                        mix, mixk = mixr.next()
                        P.dma('sp', mix[:, 256:D], MIX[i * 128:(i + 1) * 128, 256:D], [], [mixk + 'l'], mixk)
                        xt, xk = xr.next()
                        P.dma('sp', xt[:], xsrc(i), [], [xk], xk)
                        ld3[i] = (pm, pmk, bt, btk, mix, mixk, xt, xk)
                    issue_loads3(out_tiles[0])
                    for oi, i in enumerate(out_tiles):
                        s = 1 if i < NT_C else 0
                        if oi + 1 < len(out_tiles):
                            issue_loads3(out_tiles[oi + 1])
                        pm, pmk, bt, btk, mix, mixk, xt, xk = ld3.pop(i)
                        P.dve(lambda e, pm=pm: e.tensor_tensor(out=ca[:], in0=pm[:, 0, :], in1=CW[:, 0, :], op=ALU.mult), [pmk + '0', 'CW0'], ['ca'])
                        P.pool(lambda e, pm=pm: e.tensor_tensor(out=cb[:], in0=pm[:, 1, :], in1=CW[:, 1, :], op=ALU.mult), [pmk + '1', 'CW1'], ['cb'])
                        P.dve(lambda e: e.tensor_tensor(out=ca[:], in0=ca[:], in1=cb[:], op=ALU.add), ['ca', 'cb'], ['ca'])
                        P.pool(lambda e, pm=pm: e.tensor_tensor(out=cb[:], in0=pm[:, 2, :], in1=CW[:, 2, :], op=ALU.mult), [pmk + '2', 'CW2', 'ca'], ['cb'])
                        P.dve(lambda e: e.tensor_tensor(out=ca[:], in0=ca[:], in1=cb[:], op=ALU.add), ['ca', 'cb'], ['ca'])
                        P.dve(lambda e, mix=mix, bt=bt: e.tensor_tensor(out=mix[:, 0:256], in0=ca[:], in1=bt[:], op=ALU.mult), ['ca', btk], [mixk + 'c'])
                        if cut < 1:
                            continue
                        for k in range(8):
                            P.pe(lambda e, mix=mix, k=k: e.transpose(psum[:, k // 4, (k % 4) * 128:(k % 4 + 1) * 128], mix[:, k * 128:(k + 1) * 128], ident[:]),
                                 [mixk + 'l', mixk + 'c', 'ident'], ['pT%d' % k])
                        mT, mTk = mTr.next()
                        for hf in range(2):
                            P.act(lambda e, mT=mT, hf=hf: e.activation(out=mT[:, hf * 4:(hf + 1) * 4, :].rearrange("p k n -> p (k n)"), in_=psum[:, hf, :], func=AF.Copy),
                                  ['pT%d' % k for k in range(hf * 4, hf * 4 + 4)], [mTk + str(hf)])
                        for nh in range(2):
                            for k in range(8):
                                P.pe(lambda e, mT=mT, nh=nh, k=k: e.matmul(psum[:, 2 + nh, :], mT[:, k, :], wout[:, k, nh * 512:(nh + 1) * 512], start=(k == 0), stop=(k == 7)),
                                     [mTk + str(k // 4), 'wout%d' % nh], ['py%d' % nh])
                        if cut < 2:
                            continue
                        for nh in range(2):
                            P.dve(lambda e, nh=nh, s=s: e.tensor_tensor(out=rr[:, nh * 512:(nh + 1) * 512], in0=psum[:, 2 + nh, :], in1=g1[s][:, nh * 512:(nh + 1) * 512], op=ALU.mult),
                                  ['py%d' % nh, 'g1_%d' % s], ['rr%d' % nh])
                        P.dve(lambda e, xt=xt: e.scalar_tensor_tensor(out=rr[:], in0=xt[:], scalar=ALPHA, in1=rr[:], op0=ALU.mult, op1=ALU.add), [xk, 'rr0', 'rr1'], ['rr'])
                        for nh in range(2):
                            P.dve(lambda e, nh=nh: e.bn_stats(out=st6[:, nh, :], in_=rr[:, nh * 512:(nh + 1) * 512]), ['rr'], ['st6_%d' % nh])
                        P.dve(lambda e: e.bn_aggr(out=mv[:], in_=st6[:]), ['st6_0', 'st6_1'], ['mv'])
                        P.dve(lambda e: e.tensor_scalar_add(out=rs[:], in0=mv[:, 1:2], scalar1=EPS), ['mv'], ['rs'])
                        P.act(lambda e: e.activation(out=rs[:], in_=rs[:], func=AF.Sqrt), ['rs'], ['rs'])
                        P.dve(lambda e: e.reciprocal(out=rs[:], in_=rs[:]), ['rs'], ['rs'])
                        x1, x1k = x1r.next()
                        P.dve(lambda e, x1=x1: e.tensor_scalar(out=x1[:], in0=rr[:], scalar1=mv[:, 0:1], scalar2=rs[:, 0:1], op0=ALU.subtract, op1=ALU.mult), ['rr', 'mv', 'rs'], [x1k])
                        P.dve(lambda e, x1=x1: e.tensor_tensor(out=x1[:], in0=x1[:], in1=lng[:], op=ALU.mult), [x1k, 'lng'], [x1k])
                        P.dve(lambda e, x1=x1: e.tensor_tensor(out=x1[:], in0=x1[:], in1=lnb[:], op=ALU.add), [x1k, 'lnb'], [x1k])
                        P.dma('sp', XM[i * 128:(i + 1) * 128, :], x1[:], [x1k], [('XM', i)], x1k)
                        if cut < 3:
                            continue
                        P.dve(lambda e, x1=x1, s=s: e.tensor_tensor(out=h2[:], in0=x1[:], in1=sc2[s][:], op=ALU.mult), [x1k, 'sc2_%d' % s], ['h2'])
                        P.dve(lambda e, s=s: e.tensor_tensor(out=h2[:], in0=h2[:], in1=sh2[s][:], op=ALU.add), ['h2', 'sh2_%d' % s], ['h2'])
                        if cut < 3.2:
                            continue
                        for k in range(8):
                            P.pe(lambda e, k=k: e.transpose(psum[:, 4 + k // 4, (k % 4) * 128:(k % 4 + 1) * 128], h2[:, k * 128:(k + 1) * 128], ident[:]),
                                 ['h2', 'ident'], ['pU%d' % k])
                        if cut < 3.3:
                            continue
                        for hf in range(2):
                            P.dve(lambda e, hf=hf: e.tensor_copy(out=h2f[:, hf * 4:(hf + 1) * 4, :].rearrange("p k n -> p (k n)"), in_=psum[:, 4 + hf, :]),
                                  ['pU%d' % k for k in range(hf * 4, hf * 4 + 4)], ['h2f%d' % hf])
                        h2b, h2bk = h2br.next()
                        P.act(lambda e, h2b=h2b: e.activation(out=h2b[:], in_=h2[:], func=AF.Copy), ['h2'], [h2bk])
                        P.dma('sp', H2R[i * 128:(i + 1) * 128, :], h2b[:], [h2bk], [('H2R', i)], h2bk)
                        for k in range(8):
                            P.pe(lambda e, k=k: e.matmul(psum[:, 6, 0:NE], h2f[:, k, :], wr[:, k, :], start=(k == 0), stop=(k == 7)), ['h2f%d' % (k // 4), 'wr'], ['pl'])
                        P.dve(lambda e: e.tensor_tensor(out=lgt[:], in0=psum[:, 6, 0:NE], in1=brt[:], op=ALU.add), ['pl', 'brt'], ['lgt'])
                        P.dve(lambda e: e.max(out=mx8[:], in_=lgt[:]), ['lgt'], ['mx8'])
                        P.dve(lambda e: e.max_index(out=ix8[:], in_max=mx8[:], in_values=lgt[:]), ['lgt', 'mx8'], ['ix8'])
                        P.dve(lambda e: e.tensor_scalar_mul(out=nmx[:], in0=mx8[:, 0:1], scalar1=-1.0), ['mx8'], ['nmx'])
                        P.act(lambda e: e.activation(out=ex[:, 0:4], in_=mx8[:, 0:4], func=AF.Exp, bias=nmx[:, 0:1], scale=1.0), ['mx8', 'nmx'], ['ex'])
                        P.dve(lambda e: e.reduce_sum(out=sm[:], in_=ex[:, 0:4], axis=AX.X), ['ex'], ['sm'])
                        P.dve(lambda e: e.reciprocal(out=sm[:], in_=sm[:]), ['sm'], ['sm'])
                        mw_, mwk = mwr.next()
                        P.dve(lambda e, mw_=mw_: e.tensor_scalar_mul(out=mw_[:, 0:4], in0=ex[:, 0:4], scalar1=sm[:, 0:1]), ['ex', 'sm'], [mwk + 'w'])
                        P.dve(lambda e, mw_=mw_: e.tensor_copy(out=mw_[:, 4:8], in_=ix8[:, 0:4]), ['ix8'], [mwk + 'e'])
                        P.dma('sp', RW[i * 128:(i + 1) * 128, :], mw_[:, 0:4], [mwk + 'w'], [('RW', i)], mwk + 'w')
                        P.dma('sp', RE[i * 128:(i + 1) * 128, :], mw_[:, 4:8], [mwk + 'e'], [('RE', i)], mwk + 'e')
                    P.flush()

            if 'moe' in phases:
                tiles = out_tiles
                nt = len(tiles)
                T = nt * 128
                t0 = tiles[0] * 128
                NB = (4 * T + NE * (BS - 1) + BS - 1) // BS
                assert NB <= NBMAX
                w_gu_rows = w_gu.rearrange("l e r c -> (l e r) c")
                w_dn_rows = w_dn.rearrange("l e r c -> (l e r) c")
                with contextlib.ExitStack() as st:
                    def sb(name, shape, dt=F32):
                        return st.enter_context(nc.sbuf_tensor(uname(name), list(shape), dt))
                    DKi = sb("DKi", [128, nt, 4], I32)
                    IDXG = sb("IDXG", [128, NB, 8], I32)
                    EB = sb("EB", [128, NB])
                    iotap = sb("iotap", [128, 1])
                    P.dma('sp', iotap[:], iotap_in, [], ['iotap'], 'iotap')
                    with contextlib.ExitStack() as st2:
                        def sb2(name, shape, dt=F32):
                            return st2.enter_context(nc.sbuf_tensor(uname(name), list(shape), dt))
                        EF = sb2("EF", [128, nt, 4])
                        W4 = sb2("W4", [128, nt, 4])
                        P.dma('sp', EF[:], RE[t0:t0 + T, :].rearrange("(c p) k -> p c k", p=128), [], ['EF'], 'EF')
                        P.dma('sp', W4[:], RW[t0:t0 + T, :].rearrange("(c p) k -> p c k", p=128), [], ['W4'], 'W4')
                        iota32 = sb2("iota32", [128, NE])
                        P.dma('act', iota32[:], iota32_in, [], ['iota32'], 'iota32')
                        lts = sb2("lts", [128, 128])
                        P.dma('act', lts[:], lts_in, [], ['lts'], 'lts')
                        ones = sb2("ones", [128, 128])
                        P.pool(lambda e: e.memset(ones[:], 1.0), [], ['ones'])
                        bst = sb2("bst", [128, NBMAX])
                        P.dma('act', bst[:], bstart_in, [], ['bst'], 'bst')
                        kp = sb2("kp", [128, 8])
                        P.dma('act', kp[:], kp_in, [], ['kp'], 'kp')
                        OH = sb2("OH", [128, 4, nt, NE])
                        for k in range(4):
                            P.dve(lambda e, k=k: e.tensor_tensor(out=OH[:, k, :, :], in0=iota32[:].unsqueeze(1).to_broadcast([128, nt, NE]),
                                                                 in1=EF[:, :, k].unsqueeze(2).to_broadcast([128, nt, NE]), op=ALU.is_equal),
                                  ['iota32', 'EF'], ['OH%d' % k])
                        mask = sb2("mask", [128, nt, NE])
                        P.dve(lambda e: e.tensor_tensor(out=mask[:], in0=OH[:, 0, :, :], in1=OH[:, 1, :, :], op=ALU.add), ['OH0', 'OH1'], ['mask'])
                        P.dve(lambda e: e.tensor_tensor(out=mask[:], in0=mask[:], in1=OH[:, 2, :, :], op=ALU.add), ['mask', 'OH2'], ['mask'])
                        P.dve(lambda e: e.tensor_tensor(out=mask[:], in0=mask[:], in1=OH[:, 3, :, :], op=ALU.add), ['mask', 'OH3'], ['mask'])
                        for j in range(nt):
                            bk = j // 16
                            col = (j % 16) * NE
                            for m in range(j + 1):
                                P.pe(lambda e, j=j, m=m, bk=bk, col=col: e.matmul(psum[:, bk, col:col + NE], lts[:] if m == j else ones[:], mask[:, m, :],
                                                                                   start=(m == 0), stop=(m == j), skip_group_check=True),
                                     ['lts', 'ones', 'mask'], ['rk%d' % bk])
                        for m in range(nt):
                            P.pe(lambda e, m=m: e.matmul(psum[:, 3, 0:NE], ones[:], mask[:, m, :], start=(m == 0), stop=(m == nt - 1)), ['ones', 'mask'], ['cnt'])
                        c0 = sb2("c0", [128, NE]); c1 = sb2("c1", [128, NE]); padded = sb2("padded", [128, NE])
                        P.dve(lambda e: e.tensor_scalar_add(out=c0[:], in0=psum[:, 3, 0:NE], scalar1=float(BS - 1)), ['cnt'], ['c0'])
                        ci32 = sb2("ci32", [128, NE], I32)
                        P.dve(lambda e: e.tensor_scalar(out=c1[:], in0=c0[:], scalar1=1.0 / BS, scalar2=-0.5 + 0.5 / BS, op0=ALU.mult, op1=ALU.add), ['c0'], ['c1'])
                        P.dve(lambda e: e.tensor_copy(out=ci32[:], in_=c1[:]), ['c1'], ['ci32'])
                        P.dve(lambda e: e.tensor_copy(out=c1[:], in_=ci32[:]), ['ci32'], ['c1'])
                        P.dve(lambda e: e.tensor_scalar_mul(out=padded[:], in0=c1[:], scalar1=float(BS)), ['c1'], ['padded'])
                        P.dve(lambda e: e.tensor_copy(out=c0[:], in_=padded[:]), ['padded'], ['c0'])
                        cur, nxt_, ck, nk = c0, c1, 'c0', 'c1'
                        for sft in (1, 2, 4, 8, 16):
                            P.dve(lambda e, cur=cur, nxt_=nxt_, sft=sft: e.tensor_copy(out=nxt_[:, 0:sft], in_=cur[:, 0:sft]), [ck], [nk + 'a'])
                            P.dve(lambda e, cur=cur, nxt_=nxt_, sft=sft: e.tensor_tensor(out=nxt_[:, sft:NE], in0=cur[:, sft:NE], in1=cur[:, 0:NE - sft], op=ALU.add), [ck], [nk + 'b'])
                            P.dve(lambda e: e.engine_nop(), [nk + 'a', nk + 'b'], [nk])
                            cur, nxt_, ck, nk = nxt_, cur, nk, ck
                        pend, pendk = cur, ck
                        pstart = sb2("pstart", [128, NE])
                        P.dve(lambda e: e.tensor_tensor(out=pstart[:], in0=pend[:], in1=padded[:], op=ALU.subtract), [pendk, 'padded'], ['pstart'])
                        dfull = sb2("dfull", [128, nt, NE])
                        for bk in range((nt + 15) // 16):
                            j0 = bk * 16
                            nj = min(16, nt - j0)
                            P.dve(lambda e, bk=bk, j0=j0, nj=nj: e.tensor_tensor(out=dfull[:, j0:j0 + nj, :], in0=psum[:, bk, 0:nj * NE].rearrange("p (j e) -> p j e", e=NE),
                                                                                 in1=pstart[:].unsqueeze(1).to_broadcast([128, nj, NE]), op=ALU.add),
                                  ['rk%d' % bk, 'pstart'], ['dfull%d' % bk])
                        dkeys = ['dfull%d' % bk for bk in range((nt + 15) // 16)]
                        tmpd = sb2("tmpd", [128, nt, NE])
                        DKf = sb2("DKf", [128, nt, 4])
                        for k in range(4):
                            P.dve(lambda e, k=k: e.tensor_tensor(out=tmpd[:], in0=dfull[:], in1=OH[:, k, :, :], op=ALU.mult), dkeys + ['OH%d' % k], ['tmpd'])
                            P.dve(lambda e, k=k: e.tensor_reduce(out=DKf[:, :, k], in_=tmpd[:], axis=AX.X, op=ALU.add), ['tmpd'], ['DKf%d' % k])
                        P.dve(lambda e: e.tensor_copy(out=DKi[:], in_=DKf[:]), ['DKf%d' % k for k in range(4)], ['DKi'])
                        cmp_ = sb2("cmp", [128, NB, NE])
                        P.dve(lambda e: e.tensor_tensor(out=cmp_[:], in0=pend[:].unsqueeze(1).to_broadcast([128, NB, NE]),
                                                        in1=bst[:, 0:NB].unsqueeze(2).to_broadcast([128, NB, NE]), op=ALU.is_le), [pendk, 'bst'], ['cmp'])
                        P.dve(lambda e: e.tensor_reduce(out=EB[:], in_=cmp_[:], axis=AX.X, op=ALU.add), ['cmp'], ['EB'])
                        P.dve(lambda e: e.tensor_scalar_min(out=EB[:], in0=EB[:], scalar1=float(NE - 1)), ['EB'], ['EB'])
                        idxf = sb2("idxf", [128, NB, 8])
                        P.dve(lambda e: e.scalar_tensor_tensor(out=idxf[:], in0=EB[:].unsqueeze(2).to_broadcast([128, NB, 8]), scalar=float(D),
                                                               in1=kp[:].unsqueeze(1).to_broadcast([128, NB, 8]), op0=ALU.mult, op1=ALU.add), ['EB', 'kp'], ['idxf'])
                        P.dve(lambda e: e.tensor_scalar_add(out=idxf[:], in0=idxf[:], scalar1=float(l * NE * D)), ['idxf'], ['idxf'])
                        P.dve(lambda e: e.tensor_copy(out=IDXG[:], in_=idxf[:]), ['idxf'], ['IDXG'])
                        hr = Ring('hr', [sb2("hr%d" % i, [128, D], BF16) for i in range(3)])
                        for j, i in enumerate(tiles):
                            h_, hk_ = hr.next()
                            P.dma('sp', h_[:], H2R[i * 128:(i + 1) * 128, :], [], [hk_], hk_)
                            for k in range(4):
                                P.add('pool', lambda e, h_=h_, j=j, k=k: e.indirect_dma_start(out=XS[:, :], out_offset=bass.IndirectOffsetOnAxis(ap=DKi[:, j, k:k + 1], axis=0),
                                                                                          in_=h_[:, :], in_offset=None), [hk_, 'DKi'], [('XS', j, k)], 'xsc%d' % ((j * 4 + k) % 4))
                                P.add('pool', lambda e, j=j, k=k: e.indirect_dma_start(out=SW[:, :], out_offset=bass.IndirectOffsetOnAxis(ap=DKi[:, j, k:k + 1], axis=0),
                                                                                    in_=W4[:, j, k:k + 1], in_offset=None), ['W4', 'DKi'], [('SW', j, k)], 'swc%d' % ((j * 4 + k) % 4))
                        P.flush()
                    with contextlib.ExitStack() as st2:
                        def sb2(name, shape, dt=F32):
                            return st2.enter_context(nc.sbuf_tensor(uname(name), list(shape), dt))
                        identb = sb2("identb", [128, 128], BF16)
                        P.act(lambda e: e.activation(out=identb[:], in_=ident[:], func=AF.Copy), ['ident'], ['identb'])
                        bgr = sb2("bgr", [NE, 2 * D]); bgb = sb2("bgb", [NE, 2 * D], BF16)
                        P.dma('act', bgr[:], b_gu[l], [], ['bgr'], 'bgr')
                        P.act(lambda e: e.activation(out=bgb[:], in_=bgr[:], func=AF.Copy), ['bgr'], ['bgb'])
                        bdr = sb2("bdr", [NE, D]); bdb = sb2("bdb", [NE, D], BF16)
                        P.dma('act', bdr[:], b_dn[l], [], ['bdr'], 'bdr')
                        P.act(lambda e: e.activation(out=bdb[:], in_=bdr[:], func=AF.Copy), ['bdr'], ['bdb'])
                        wgr = Ring('WG', [sb2("WG%d" % i, [128, 8, 2 * D], BF16) for i in range(2)])
                        wdr = Ring('WD', [sb2("WD%d" % i, [128, 8, D], BF16) for i in range(2)])
                        xbr = Ring('XB', [sb2("XB%d" % i, [128, 4, D], BF16) for i in range(2)])
                        XT = sb2("XT", [128, 8, BS], BF16)
                        actr = Ring('act', [sb2("act%d" % i, [128, 8, BS], BF16) for i in range(2)])
                        Ar = Ring('A', [sb2("A%d" % i, [128, BS]) for i in range(2)])
                        Sr = Ring('Sg', [sb2("Sg%d" % i, [128, BS]) for i in range(2)])
                        Ur = Ring('U', [sb2("U%d" % i, [128, BS]) for i in range(2)])
                        ohr = Ring('ohb', [sb2("ohb%d" % i, [NE, BS], BF16) for i in range(2)])
                        swr = Ring('swb', [sb2("swb%d" % i, [128, 4]) for i in range(2)])
                        Yr = Ring('Y', [sb2("Y%d" % i, [128, 4, D]) for i in range(1)])
                        gbr = Ring('pg', [0, 1]); ubr = Ring('pu', [2, 3]); ybr = Ring('py', [4, 5])
                        psb = [psum[:, 6, :].bitcast(BF16), psum[:, 7, :].bitcast(BF16)]

                        def load_blk(b):
                            WG, wgk = wgr.next()
                            WD, wdk = wdr.next()
                            for k in range(8):
                                P.add('pool', lambda e, WG=WG, b=b, k=k: e.indirect_dma_start(out=WG[:, k, :], out_offset=None, in_=w_gu_rows[:, :],
                                                                                             in_offset=bass.IndirectOffsetOnAxis(ap=IDXG[:, b, k:k + 1], axis=0)),
                                      ['IDXG'], [wgk + str(k)], wgk + str(k))
                            for k in range(8):
                                P.add('pool', lambda e, WD=WD, b=b, k=k: e.indirect_dma_start(out=WD[:, k, :], out_offset=None, in_=w_dn_rows[:, :],
                                                                                             in_offset=bass.IndirectOffsetOnAxis(ap=IDXG[:, b, k:k + 1], axis=0)),
                                      ['IDXG'], [wdk + str(k)], wdk + str(k))
                            XB, xbk = xbr.next()
                            P.dma('sp', XB[:], XS[b * BS:(b + 1) * BS, :].rearrange("(s p) d -> p s d", p=128), [], [xbk], xbk)
                            swb, swk = swr.next()
                            P.dma('act', swb[:], SW[b * BS:(b + 1) * BS, :].rearrange("(s p) o -> p (s o)", p=128), [], [swk], swk, allow_slow_non_contiguous=True)
                            return WG, wgk, WD, wdk, XB, xbk, swb, swk
                        nxt = load_blk(0)
                        for b in range(NB):
                            WG, wgk, WD, wdk, XB, xbk, swb, swk = nxt
                            if b + 1 < NB:
                                nxt = load_blk(b + 1)
                            wgkeys = [wgk + str(k) for k in range(8)]
                            wdkeys = [wdk + str(k) for k in range(8)]
                            ohb, ohk = ohr.next()
                            P.dve(lambda e, ohb=ohb, b=b: e.tensor_scalar(out=ohb[:], in0=EB[0:NE, b:b + 1].to_broadcast([NE, BS]), scalar1=iotap[0:NE, 0:1], scalar2=None, op0=ALU.is_equal),
                                  ['EB', 'iotap'], [ohk])
                            for half in range(2):
                                for s2 in range(2):
                                    sidx = half * 2 + s2
                                    for k in range(8):
                                        P.pe(lambda e, XB=XB, sidx=sidx, k=k, s2=s2: e.transpose(psb[s2][:, k * 128:(k + 1) * 128], XB[:, sidx, k * 128:(k + 1) * 128], identb[:]),
                                             [xbk, 'identb'], ['pX%d' % s2])
                                    P.act(lambda e, sidx=sidx, s2=s2: e.activation(out=XT[:, :, sidx * 128:(sidx + 1) * 128], in_=psb[s2].rearrange("p (k n) -> p k n", n=128), func=AF.Copy),
                                          ['pX%d' % s2], ['XT%d' % sidx])
                            xtkeys = ['XT%d' % q for q in range(4)]
                            at, atk = actr.next()
                            for c in range(8):
                                gb, gbk = gbr.next()
                                ub, ubk = ubr.next()
                                for k in range(8):
                                    P.pe(lambda e, gb=gb, WG=WG, k=k, c=c: e.matmul(psum[:, gb, :], WG[:, k, c * 128:(c + 1) * 128], XT[:, k, :], start=(k == 0), stop=False),
                                         wgkeys + xtkeys, [gbk])
                                P.pe(lambda e, gb=gb, c=c, ohb=ohb: e.matmul(psum[:, gb, :], bgb[:, c * 128:(c + 1) * 128], ohb[:], start=False, stop=True), ['bgb', ohk], [gbk])
                                for k in range(8):
                                    P.pe(lambda e, ub=ub, WG=WG, k=k, c=c: e.matmul(psum[:, ub, :], WG[:, k, D + c * 128:D + (c + 1) * 128], XT[:, k, :], start=(k == 0), stop=False),
                                         wgkeys + xtkeys, [ubk])
                                P.pe(lambda e, ub=ub, c=c, ohb=ohb: e.matmul(psum[:, ub, :], bgb[:, D + c * 128:D + (c + 1) * 128], ohb[:], start=False, stop=True), ['bgb', ohk], [ubk])
                                A, Ak = Ar.next(); S_, Sk = Sr.next(); U, Uk = Ur.next()
                                P.dve(lambda e, A=A, gb=gb: e.tensor_scalar_min(out=A[:], in0=psum[:, gb, :], scalar1=7.0), [gbk], [Ak])
                                P.act(lambda e, A=A, S_=S_: e.activation(out=S_[:], in_=A[:], func=AF.Sigmoid, scale=1.702), [Ak], [Sk])
                                P.dve(lambda e, U=U, ub=ub: e.tensor_scalar(out=U[:], in0=psum[:, ub, :], scalar1=7.0, scalar2=-7.0, op0=ALU.min, op1=ALU.max), [ubk], [Uk])
                                P.pool(lambda e, A=A, S_=S_: e.tensor_tensor(out=A[:], in0=A[:], in1=S_[:], op=ALU.mult), [Ak, Sk], [Ak])
                                P.dve(lambda e, at=at, c=c, U=U, A=A: e.scalar_tensor_tensor(out=at[:, c, :], in0=U[:], scalar=1.0, in1=A[:], op0=ALU.add, op1=ALU.mult), [Uk, Ak], [atk + str(c)])
                            atkeys = [atk + str(c) for c in range(8)]
                            Y, Yk = Yr.next()
                            for s4 in range(4):
                                for nh in range(2):
                                    yb_, ybk = ybr.next()
                                    for c in range(8):
                                        P.pe(lambda e, yb_=yb_, at=at, c=c, s4=s4, WD=WD, nh=nh: e.matmul(psum[:, yb_, :], at[:, c, s4 * 128:(s4 + 1) * 128], WD[:, c, nh * 512:(nh + 1) * 512],
                                                                                                          start=(c == 0), stop=False), atkeys + wdkeys, [ybk])
                                    P.pe(lambda e, yb_=yb_, nh=nh, ohb=ohb: e.matmul(psum[:, yb_, :], ohb[:, 0:128], bdb[:, nh * 512:(nh + 1) * 512], start=False, stop=True), ['bdb', ohk], [ybk])
                                    if yb_ == 4:
                                        P.dve(lambda e, Y=Y, s4=s4, nh=nh, swb=swb: e.tensor_scalar_mul(out=Y[:, s4, nh * 512:(nh + 1) * 512], in0=psum[:, 4, :], scalar1=swb[:, s4:s4 + 1]),
                                              [ybk, swk], [Yk + '%d%d' % (s4, nh)])
                                    else:
                                        P.act(lambda e, Y=Y, s4=s4, nh=nh, swb=swb: e.activation(out=Y[:, s4, nh * 512:(nh + 1) * 512], in_=psum[:, 5, :], func=AF.Copy, scale=swb[:, s4:s4 + 1]),
                                              [ybk, swk], [Yk + '%d%d' % (s4, nh)])
                            P.dma('sp', YS[b * BS:(b + 1) * BS, :].rearrange("(s p) d -> p s d", p=128), Y[:], [Yk + '%d%d' % (q, r_) for q in range(4) for r_ in range(2)], [('YS', b)], Yk)
                        P.flush()
                    with contextlib.ExitStack() as st3:
                        def sb3(name, shape, dt=F32):
                            return st3.enter_context(nc.sbuf_tensor(uname(name), list(shape), dt))
                        g2 = [sb3("g2_%d" % s_, [128, D]) for s_ in range(2)]
                        for s_ in range(2):
                            bload('sp', g2[s_][:], modv[l, s_, 5 * D:6 * D], 'g2_%d' % s_)
                        lng = sb3("lng2", [128, D]); lnb = sb3("lnb2", [128, D])
                        bload('act', lng[:], ln_g[l, 1], 'lng')
                        bload('act', lnb[:], ln_b[l, 1], 'lnb')
                        x1r = Ring('xm', [sb3("xm%d" % i, [128, D]) for i in range(4)])
                        Gr = Ring('G', [sb3("G%d" % i, [128, 4, D]) for i in range(4)])
                        rr = sb3("rr2", [128, D])
                        xor_ = Ring('xo', [sb3("xo%d" % i, [128, D]) for i in range(2)])
                        st6 = sb3("st6b", [128, 2, 6]); mv = sb3("mvb", [128, 2]); rs = sb3("rsb", [128, 1])
                        for j, i in enumerate(tiles):
                            s_ = 1 if i < NT_C else 0
                            x1, x1k = x1r.next()
                            P.dma('sp', x1[:], XM[i * 128:(i + 1) * 128, :], [], [x1k], x1k)
                            G, Gk = Gr.next()
                            for k in range(4):
                                P.add('pool', lambda e, G=G, j=j, k=k: e.indirect_dma_start(out=G[:, k, :], out_offset=None, in_=YS[:, :],
                                                                                         in_offset=bass.IndirectOffsetOnAxis(ap=DKi[:, j, k:k + 1], axis=0)), ['DKi'], [Gk + str(k)], Gk + str(k))
                            P.dve(lambda e, G=G: e.tensor_tensor(out=G[:, 0, :], in0=G[:, 0, :], in1=G[:, 1, :], op=ALU.add), [Gk + '0', Gk + '1'], [Gk + '0'])
                            P.dve(lambda e, G=G: e.tensor_tensor(out=G[:, 2, :], in0=G[:, 2, :], in1=G[:, 3, :], op=ALU.add), [Gk + '2', Gk + '3'], [Gk + '2'])
                            P.dve(lambda e, G=G: e.tensor_tensor(out=G[:, 0, :], in0=G[:, 0, :], in1=G[:, 2, :], op=ALU.add), [Gk + '0', Gk + '2'], [Gk + '0'])
                            P.dve(lambda e, G=G, s_=s_: e.tensor_tensor(out=rr[:], in0=G[:, 0, :], in1=g2[s_][:], op=ALU.mult), [Gk + '0', 'g2_%d' % s_], ['rr'])
                            P.dve(lambda e, x1=x1: e.scalar_tensor_tensor(out=rr[:], in0=x1[:], scalar=ALPHA, in1=rr[:], op0=ALU.mult, op1=ALU.add), [x1k, 'rr'], ['rr'])
                            for nh in range(2):
                                P.dve(lambda e, nh=nh: e.bn_stats(out=st6[:, nh, :], in_=rr[:, nh * 512:(nh + 1) * 512]), ['rr'], ['st6_%d' % nh])
                            P.dve(lambda e: e.bn_aggr(out=mv[:], in_=st6[:]), ['st6_0', 'st6_1'], ['mv'])
                            P.dve(lambda e: e.tensor_scalar_add(out=rs[:], in0=mv[:, 1:2], scalar1=EPS), ['mv'], ['rs'])
                            P.act(lambda e: e.activation(out=rs[:], in_=rs[:], func=AF.Sqrt), ['rs'], ['rs'])
                            P.dve(lambda e: e.reciprocal(out=rs[:], in_=rs[:]), ['rs'], ['rs'])
                            xo, xok = xor_.next()
                            P.dve(lambda e, xo=xo: e.tensor_scalar(out=xo[:], in0=rr[:], scalar1=mv[:, 0:1], scalar2=rs[:, 0:1], op0=ALU.subtract, op1=ALU.mult), ['rr', 'mv', 'rs'], [xok])
                            P.dve(lambda e, xo=xo: e.tensor_tensor(out=xo[:], in0=xo[:], in1=lng[:], op=ALU.mult), [xok, 'lng'], [xok])
                            P.dve(lambda e, xo=xo: e.tensor_tensor(out=xo[:], in0=xo[:], in1=lnb[:], op=ALU.add), [xok, 'lnb'], [xok])
                            dst = XS0[i * 128:(i + 1) * 128, :] if not last else out[(i - NT_C) * 128:(i - NT_C + 1) * 128, :]
                            P.dma('sp', dst, xo[:], [xok], [('xout', i)], xok)
                        P.flush()
        P.flush()
    return nc


def make_consts():
    inv = (10000.0 ** (-np.arange(0, 32, 2, dtype=np.float32) / 32.0)).astype(np.float32)
    t = np.arange(L)
    row = (t // 64).astype(np.float32)
    col = (t % 64).astype(np.float32)
    ang = np.stack([row[:, None] * inv, col[:, None] * inv], axis=1)
    cos = np.cos(ang).astype(np.float32)
    sin = np.sin(ang).astype(np.float32)
    c64 = np.stack([cos, cos], axis=2).reshape(L, 64)
    s64 = np.stack([-sin, sin], axis=2).reshape(L, 64)
    p = np.arange(128, dtype=np.float32)
    pos = np.stack([127 - p, p, p + 1, 128 - p], axis=1)
    dif = p[None, :] - p[:, None]
    return dict(
        c_ident=np.eye(128, dtype=np.float32),
        c_cos=np.ascontiguousarray(np.tile(c64, (1, 8))),
        c_sin=np.ascontiguousarray(np.tile(s64, (1, 8))),
        c_pos=np.ascontiguousarray(pos.astype(np.float32)),
        c_dpos=np.maximum(dif, 0).astype(np.float32),
        c_dneg=np.maximum(-dif, 0).astype(np.float32),
        c_mge=(dif >= 0).astype(np.float32),
        c_zeros=np.zeros((2, 256), np.float32),
        c_iota32=np.tile(np.arange(NE, dtype=np.float32)[None, :], (128, 1)),
        c_lts=(p[:, None] < p[None, :]).astype(np.float32),
        c_bstart=np.tile((np.arange(NBMAX, dtype=np.float32) * BS)[None, :], (128, 1)),
        c_kp=(np.arange(8, dtype=np.float32)[None, :] * 128 + p[:, None]).astype(np.float32),
        c_iotap=p[:, None].astype(np.float32).copy(),
    )


WKEYS = ['w_mod', 'b_mod', 'w_in', 'conv_w', 'ret_decay_exp', 'ret_gn_g', 'q_norm_g', 'k_norm_g', 'w_out',
         'ln_g', 'ln_b', 'w_router', 'b_router', 'w_gate_up', 'b_gate_up', 'w_down', 'b_down']


def make_in_maps(inputs, cores, skip=()):
    consts = make_consts()
    shared = {k: np.ascontiguousarray(np.asarray(inputs[k], np.float32)) for k in WKEYS if k not in skip}
    shared['c_ctx'] = np.ascontiguousarray(np.asarray(inputs['c_ctx'], np.float32))
    shared.update(consts)
    maps = []
    for b in cores:
        m = dict(shared)
        m['x'] = np.ascontiguousarray(np.asarray(inputs['x'][b], np.float32))
        m['c'] = np.ascontiguousarray(np.asarray(inputs['c'][b], np.float32))
        m['ctx'] = np.ascontiguousarray(np.asarray(inputs['ctx'][b], np.float32))
        maps.append(m)
    return maps


def kernel(**inputs):
    nc = build()
    maps = make_in_maps(inputs, list(range(8)))
    res = run_bass_kernel_spmd(nc, maps, core_ids=list(range(8)))
    return np.stack([np.asarray(r["out"], np.float32) for r in res.results], axis=0)
```

```python
import contextlib
import math
import numpy as np
import concourse.bass as bass
import concourse.mybir as mybir
from concourse.bass_utils import run_bass_kernel_spmd

F32 = mybir.dt.float32
BF16 = mybir.dt.bfloat16
ALU = mybir.AluOpType
AF = mybir.ActivationFunctionType
AX = mybir.AxisListType

D = 1024
L = 4096
LC = 256
NT_C = 2
NT = 34
DEPTH = 2
NE = 32
BS = 512
NBMAX = 66
I32 = mybir.dt.int32
U32 = mybir.dt.uint32
ALPHA = (2.0 * DEPTH) ** 0.25
EPS = 1e-6


class Prog:
    ENGS = ('pe', 'act', 'dve', 'pool', 'sp')

    def __init__(self, nc, stack):
        self.nc = nc
        self.ops = []
        self.sems = {}
        self.stack = stack
        for eng in ('pe', 'act', 'dve', 'pool'):
            self.sems[('e', eng)] = stack.enter_context(nc.semaphore('sem_' + eng))
        self.cnt = {}
        self.streams = {}
        self.pool_cnt = []
        self.waited = {e: {} for e in self.ENGS}
        self.total_ops = 0

    def add(self, eng, fn, reads=(), writes=(), stream=None):
        self.ops.append((eng, fn, tuple(reads), tuple(writes), stream))

    def pe(self, fn, reads=(), writes=()):
        self.add('pe', fn, reads, writes)

    def act(self, fn, reads=(), writes=()):
        self.add('act', fn, reads, writes)

    def dve(self, fn, reads=(), writes=()):
        self.add('dve', fn, reads, writes)

    def pool(self, fn, reads=(), writes=()):
        self.add('pool', fn, reads, writes)

    def dma(self, q, out, in_, reads, writes, stream, **kw):
        self.add(q, lambda e: e.dma_start(out=out, in_=in_, **kw), reads, writes, stream)

    def flush(self):
        nc = self.nc
        ops = self.ops
        self.ops = []
        n = len(ops)
        if n == 0:
            return
        self.total_ops += n
        last_writer = {}
        readers = {}
        deps = [None] * n
        for i, (eng, fn, rd, wr, st) in enumerate(ops):
            d = set()
            for r in rd:
                j = last_writer.get(r)
                if j is not None:
                    d.add((j, 0))
            for w in wr:
                j = last_writer.get(w)
                if j is not None:
                    d.add((j, 1))
                for k in readers.get(w, ()):
                    if k != i:
                        d.add((k, 2))
            deps[i] = d
            for r in rd:
                readers.setdefault(r, []).append(i)
            for w in wr:
                last_writer[w] = i
                readers[w] = []
        sig = [False] * n
        need = [None] * n
        last_compute = {}
        for i in range(n):
            eng = ops[i][0]
            lst = set()
            for (j, kind) in deps[i]:
                jeng, _, _, _, jst = ops[j]
                if jst is None and jeng == eng:
                    if eng == 'pe' or eng == 'sp':
                        continue
                    if kind == 2:
                        continue
                if jst is None:
                    sig[j] = True
                lst.add(j)
            need[i] = lst
            if ops[i][4] is None and ops[i][1] is not None:
                last_compute[eng] = i
        for eng, i in last_compute.items():
            if eng != 'sp':
                sig[i] = True
        sval = [None] * n
        phase_map = {}
        for i, (eng, fn, rd, wr, st) in enumerate(ops):
            if st is not None:
                if st not in phase_map:
                    k = len(phase_map)
                    phase_map[st] = k
                    if k >= len(self.pool_cnt):
                        self.pool_cnt.append(0)
                        self.sems[('s', k)] = self.stack.enter_context(nc.semaphore('sd_%d' % k))
                k = phase_map[st]
                self.pool_cnt[k] += 1
                sval[i] = (('s', k), 16 * self.pool_cnt[k])
            elif sig[i]:
                self.cnt[eng] = self.cnt.get(eng, 0) + 1
                sval[i] = (('e', eng), self.cnt[eng])
        per_eng = {e: [] for e in self.ENGS}
        for i, op in enumerate(ops):
            per_eng[op[0]].append(i)
        sems = self.sems
        final = {}
        for eng in ('pe', 'act', 'dve', 'pool'):
            if self.cnt.get(eng, 0) > 0:
                final[('e', eng)] = self.cnt[eng]
        for k, c in enumerate(self.pool_cnt):
            final[('s', k)] = 16 * c

        def run(engname, e):
            waited = self.waited[engname]
            for i in per_eng[engname]:
                _, fn, rd, wr, st = ops[i]
                w = {}
                for j in need[i]:
                    key, val = sval[j]
                    if w.get(key, 0) < val:
                        w[key] = val
                for key, val in w.items():
                    if waited.get(key, 0) >= val:
                        continue
                    waited[key] = val
                    e.wait_ge(sems[key], val)
                if fn is None:
                    continue
                ins = fn(e)
                if sval[i] is not None:
                    key, val = sval[i]
                    ins.then_inc(sems[key], 16 if key[0] == 's' else 1)
            for key, val in final.items():
                if key == ('e', engname):
                    continue
                if waited.get(key, 0) >= val:
                    continue
                waited[key] = val
                e.wait_ge(sems[key], val)

        with nc.Block() as block:
            @block.tensor
            def _(e):
                run('pe', e)

            @block.scalar
            def _(e):
                run('act', e)

            @block.vector
            def _(e):
                run('dve', e)

            @block.gpsimd
            def _(e):
                run('pool', e)

            @block.sync
            def _(e):
                run('sp', e)


class Ring:
    def __init__(self, name, tiles):
        self.name = name
        self.tiles = tiles
        self.i = 0

    def next(self):
        k = self.i % len(self.tiles)
        self.i += 1
        return self.tiles[k], '%s%d' % (self.name, k)


SEC = dict(u=(0, 256), B=(256, 256), C=(512, 256), rq=(768, 256), rk=(1024, 256), rv=(1280, 256),
           rg=(1536, 256), aq=(1792, 512), ak=(2304, 128), av=(2432, 128))
MYCOL = dict(u=0, C=256, B=512, rv=768, rq=1024, rk=1280, aq=1536, rg=2048, ak=2304, av=2432)


def build(phases=('p0', 'p1', 'att', 'ret', 'p3', 'moe'), layers=(0, 1), debug=False, moe_experts=NE, cut=99):
    nc = bass.Bass("TRN2", target_bir_lowering=False)

    _uc = [0]

    def uname(name):
        _uc[0] += 1
        return '%s_u%d' % (name, _uc[0])

    def din(name, shape, dt=F32):
        return nc.dram_tensor(name, list(shape), dt, kind="ExternalInput").ap()

    def dscr(name, shape, dt=F32):
        return nc.dram_tensor(name, list(shape), dt, kind=("ExternalOutput" if debug else "Internal")).ap()

    x_in = din("x", [L, D])
    c_in = din("c", [D])
    ctx_in = din("ctx", [LC, D])
    cctx_in = din("c_ctx", [D])
    w_mod = din("w_mod", [DEPTH, D, 6 * D])
    b_mod = din("b_mod", [DEPTH, 6 * D])
    w_in = din("w_in", [DEPTH, D, 2560])
    conv_w = din("conv_w", [DEPTH, 256, 3])
    rde = din("ret_decay_exp", [DEPTH, 2, 4])
    gn_g = din("ret_gn_g", [DEPTH, 256])
    qn_g = din("q_norm_g", [DEPTH, 64])
    kn_g = din("k_norm_g", [DEPTH, 64])
    w_out = din("w_out", [DEPTH, D, D])
    ln_g = din("ln_g", [DEPTH, 2, D])
    ln_b = din("ln_b", [DEPTH, 2, D])
    w_router = din("w_router", [DEPTH, D, NE])
    b_router = din("b_router", [DEPTH, NE])
    if 'moe' in phases:
        w_gu = din("w_gate_up", [DEPTH, NE, D, 2 * D])
        b_gu = din("b_gate_up", [DEPTH, NE, 2 * D])
        w_dn = din("w_down", [DEPTH, NE, D, D])
        b_dn = din("b_down", [DEPTH, NE, D])
    ident_in = din("c_ident", [128, 128])
    cos_in = din("c_cos", [L, 512])
    sin_in = din("c_sin", [L, 512])
    pos_in = din("c_pos", [128, 4])
    dpos_in = din("c_dpos", [128, 128])
    dneg_in = din("c_dneg", [128, 128])
    mge_in = din("c_mge", [128, 128])
    zeros_in = din("c_zeros", [2, 256])
    iota32_in = din("c_iota32", [128, NE])
    lts_in = din("c_lts", [128, 128])
    bstart_in = din("c_bstart", [128, NBMAX])
    kp_in = din("c_kp", [128, 8])
    iotap_in = din("c_iotap", [128, 1])

    out = nc.dram_tensor("out", [L, D], F32, kind="ExternalOutput").ap()

    N = NT * 128
    modv = dscr("modv", [DEPTH, 2, 6 * D])
    PB = dscr("PB", [N + 4, 512])
    RQT = dscr("RQT", [3, 4, 64, N], BF16)
    RKT = dscr("RKT", [4, 64, N], BF16)
    RTOK = dscr("RTOK", [N, 768], BF16)
    RG = dscr("RG", [N, 256])
    AQT = dscr("AQT", [8, 64, N], BF16)
    AKT = dscr("AKT", [2, 64, N], BF16)
    AV = dscr("AV", [N, 128], BF16)
    MIX = dscr("MIX", [N, 1024])
    XS0 = dscr("XS0", [N, D])
    XM = dscr("XM", [N, D])
    H2R = dscr("H2R", [N, D], BF16)
    RW = dscr("RW", [N, 4])
    RE = dscr("RE", [N, 4])
    XS = dscr("XS", [NBMAX * BS, D], BF16)
    SW = dscr("SW", [NBMAX * BS, 1])
    YS = dscr("YS", [NBMAX * BS, D])
    BGT = dscr("BGT", [DEPTH * NE * 128, 16])

    def prow(i):
        return 1 + i * 128 if i < NT_C else 259 + (i - NT_C) * 128

    with contextlib.ExitStack() as gst:
        P = Prog(nc, gst)
        psum = gst.enter_context(nc.psum_tensor("psum", [128, 8, 512], F32))
        ident = gst.enter_context(nc.sbuf_tensor("ident", [128, 128], F32))
        P.dma('sp', ident[:], ident_in, [], ['ident'], 'ident')

        def bank(b):
            return psum[:, b, :]

        if 'p0' in phases:
            with contextlib.ExitStack() as st:
                def sb(name, shape, dt=F32):
                    return st.enter_context(nc.sbuf_tensor(uname(name), list(shape), dt))
                cnd = sb("cnd", [128, 2, 8])
                P.dma('sp', cnd[:, 0, :], c_in.rearrange("(k p) -> p k", p=128), [], ['cnd'], 'cnd0',
                      allow_slow_non_contiguous=True)
                P.dma('sp', cnd[:, 1, :], cctx_in.rearrange("(k p) -> p k", p=128), [], ['cnd'], 'cnd1',
                      allow_slow_non_contiguous=True)
                P.act(lambda e: e.activation(out=cnd[:], in_=cnd[:], func=AF.Silu), ['cnd'], ['cnd'])
                wring = Ring('wm', [sb("wm%d" % i, [128, 8, 512]) for i in range(6)])
                bring = Ring('bm', [sb("bm%d" % i, [1, 512]) for i in range(2)])
                oring = Ring('om', [sb("om%d" % i, [1, 2, 512]) for i in range(2)])
                for l in layers:
                    for n in range(12):
                        wt, wk = wring.next()
                        bt, bk = bring.next()
                        ot, ok = oring.next()
                        P.dma('sp' if n % 2 == 0 else 'act', wt[:], w_mod[l, :, n * 512:(n + 1) * 512].rearrange("(k p) n -> p k n", p=128),
                              [], [wk], wk)
                        P.dma('sp', bt[:], b_mod[l, n * 512:(n + 1) * 512].rearrange("(o n) -> o n", o=1), [], [bk], bk)
                        for s in range(2):
                            pb_ = 'ps0_%d' % s
                            for k in range(8):
                                P.pe(lambda e, s=s, k=k, wt=wt: e.matmul(psum[0:1, s, :], cnd[:, s, k:k + 1], wt[:, k, :],
                                                                     start=(k == 0), stop=(k == 7)),
                                     ['cnd', wk], [pb_])
                            P.dve(lambda e, s=s, ot=ot, bt=bt: e.tensor_tensor(out=ot[:, s, :], in0=psum[0:1, s, :],
                                                                               in1=bt[:], op=ALU.add),
                                  [pb_, bk], [ok])
                        if n in (2, 3, 8, 9):
                            P.dve(lambda e, ot=ot: e.tensor_scalar_add(out=ot[:], in0=ot[:], scalar1=1.0), [ok], [ok])
                        P.dma('sp', modv[l, :, n * 512:(n + 1) * 512].rearrange("(o s) n -> o s n", o=1), ot[:],
                              [ok], [('modv', l)], ok)
                P.flush()

        def bload(q, tile_ap, vec_ap, key, reads=()):
            P.dma(q, tile_ap, vec_ap.partition_broadcast(128), list(reads), [key], key)

        for l in layers:
            last = (l == DEPTH - 1)
            xsrc = (lambda i: (ctx_in[i * 128:(i + 1) * 128, :] if i < NT_C else x_in[(i - NT_C) * 128:(i - NT_C + 1) * 128, :])) \
                if l == 0 else (lambda i: XS0[i * 128:(i + 1) * 128, :])
            xs_key = (lambda i: ('xin', i)) if l == 0 else (lambda i: ('XS0', i))
            out_tiles = list(range(NT)) if not last else list(range(NT_C, NT))

            if 'p1' in phases:
                with contextlib.ExitStack() as st:
                    def sb(name, shape, dt=F32):
                        return st.enter_context(nc.sbuf_tensor(uname(name), list(shape), dt))
                    win = sb("win", [128, 8, 2560], BF16)
                    for name, (c0, w) in SEC.items():
                        m0 = MYCOL[name]
                        P.dma('pool', win[:, :, m0:m0 + w], w_in[l, :, c0:c0 + w].rearrange("(k p) n -> p k n", p=128),
                              [], ['win_' + name], 'win_' + name)
                    winkeys = ['win_' + k for k in SEC]
                    sc1 = [sb("sc1_%d" % s, [128, D]) for s in range(2)]
                    sh1 = [sb("sh1_%d" % s, [128, D]) for s in range(2)]
                    for s in range(2):
                        bload('sp', sc1[s][:], modv[l, s, D:2 * D], 'sc1_%d' % s, [('modv', l)])
                        bload('sp', sh1[s][:], modv[l, s, 0:D], 'sh1_%d' % s, [('modv', l)])
                    gq = sb("gq", [128, 64])
                    gk = sb("gk", [128, 64])
                    bload('act', gq[:], qn_g[l], 'gq')
                    bload('act', gk[:], kn_g[l], 'gk')
                    zpad = sb("zpad", [2, 256])
                    P.dma('act', zpad[:], zeros_in, [], ['zpad'], 'zpad')
                    for r0 in (0, 257):
                        P.dma('act', PB[r0:r0 + 2, 0:256] if r0 else PB[0:1, 0:256], zpad[0:2, :] if r0 else zpad[0:1, :],
                              ['zpad'], [('PBpad', r0)], 'zp%d' % r0)
                    P.dma('act', PB[N + 3:N + 4, 0:256], zpad[0:1, :], ['zpad'], [('PBpad', 3)], 'zp3')
                    posc = sb("posc", [128, 4])
                    P.dma('act', posc[:], pos_in, [], ['posc'], 'posc')
                    lg = sb("lg", [128, 8])
                    bload('act', lg[:], rde[l].rearrange("a h -> (a h)"), 'lg')
                    P.act(lambda e: e.activation(out=lg[:], in_=lg[:], func=AF.Exp, scale=-math.log(2.0)), ['lg'], ['lg'])
                    P.act(lambda e: e.activation(out=lg[:], in_=lg[:], func=AF.Ln, scale=-1.0, bias=1.0), ['lg'], ['lg'])
                    tab4 = sb("tab4", [128, 4, 4])
                    for ti, (di, pc) in enumerate(((0, 0), (1, 1), (0, 2), (1, 3))):
                        P.act(lambda e, ti=ti, di=di, pc=pc: e.activation(out=tab4[:, ti, :], in_=lg[:, di * 4:(di + 1) * 4],
                                                                          func=AF.Exp, scale=posc[:, pc:pc + 1]),
                              ['lg', 'posc'], ['tab4'])
                    P.dve(lambda e: e.tensor_scalar_mul(out=tab4[:, 0:2, :], in0=tab4[:, 0:2, :], scalar1=0.125), ['tab4'], ['tab4'])
                    TAB = sb("TAB", [128, 4, 4, 64])
                    P.dve(lambda e: e.tensor_copy(out=TAB[:].rearrange("p t h d -> p (t h) d"),
                                                  in_=tab4[:].rearrange("p t h -> p (t h)").unsqueeze(2).to_broadcast([128, 16, 64])),
                          ['tab4'], ['TAB'])
                    gq8 = sb("gq8", [128, 8, 64])
                    P.dve(lambda e: e.tensor_copy(out=gq8[:], in_=gq[:].unsqueeze(1).to_broadcast([128, 8, 64])), ['gq'], ['gq8'])
                    gk2 = sb("gk2", [128, 2, 64])
                    P.dve(lambda e: e.tensor_copy(out=gk2[:], in_=gk[:].unsqueeze(1).to_broadcast([128, 2, 64])), ['gk'], ['gk2'])

                    xring = Ring('xt', [sb("xt%d" % i, [128, D]) for i in range(3)])
                    csring = Ring('cs', [sb("cs%d" % i, [128, 512]) for i in range(3)])
                    snring = Ring('sn', [sb("sn%d" % i, [128, 512]) for i in range(3)])
                    hring = Ring('h', [sb("h%d" % i, [128, D]) for i in range(2)])
                    hTring = Ring('hT', [sb("hT%d" % i, [128, 8, 128], BF16) for i in range(2)])
                    usb = sb("usb", [128, 256])
                    pcb = Ring('pcb', [sb("pcb%d" % i, [128, 512]) for i in range(2)])
                    t1 = sb("t1", [128, 512])
                    t2 = sb("t2", [128, 512])
                    rq = sb("rq", [128, 4, 256])
                    rtok = Ring('rtok', [sb("rtok%d" % i, [128, 768], BF16) for i in range(2)])
                    rgt = Ring('rgt', [sb("rgt%d" % i, [128, 256]) for i in range(2)])
                    rT = Ring('rT', [sb("rT%d" % i, [128, 8, 128], BF16) for i in range(2)])
                    sq = sb("sq", [128, 512])
                    ss = sb("ss", [128, 8])
                    aq = sb("aq", [128, 512])
                    aqn = sb("aqn", [128, 512])
                    akn = sb("akn", [128, 128])
                    ak = sb("ak", [128, 128])
                    aT = Ring('aT', [sb("aT%d" % i, [128, 5, 128], BF16) for i in range(2)])
                    avt = Ring('avt', [sb("avt%d" % i, [128, 128], BF16) for i in range(2)])

                    def rope(src_ap, W, dst_ap, src_keys, dst_key, cs, ck, sn, sk):
                        g = W // 32
                        P.dve(lambda e: e.tensor_tensor(out=t1[:, :W], in0=src_ap, in1=cs[:, :W], op=ALU.mult),
                              src_keys + [ck], ['t1'])
                        s4 = src_ap.rearrange("p (g a f) -> p g a f", a=2, f=16)
                        t4 = t2[:, :W].rearrange("p (g a f) -> p g a f", a=2, f=16)
                        n4 = sn[:, :W].rearrange("p (g a f) -> p g a f", a=2, f=16)
                        P.dve(lambda e: e.tensor_tensor(out=t4[:, :, 0, :], in0=s4[:, :, 1, :], in1=n4[:, :, 0, :], op=ALU.mult),
                              src_keys + [sk], ['t2a'])
                        P.dve(lambda e: e.tensor_tensor(out=t4[:, :, 1, :], in0=s4[:, :, 0, :], in1=n4[:, :, 1, :], op=ALU.mult),
                              src_keys + [sk], ['t2b'])
                        P.pool(lambda e: e.tensor_tensor(out=dst_ap, in0=t1[:, :W], in1=t2[:, :W], op=ALU.add),
                               ['t1', 't2a', 't2b'], [dst_key])

                    ld = {}

                    def issue_loads(i):
                        xt, xk = xring.next()
                        P.dma('sp', xt[:], xsrc(i), [xs_key(i)], [xk], xk)
                        if i >= NT_C:
                            cs, ck = csring.next()
                            sn, sk = snring.next()
                            t0 = (i - NT_C) * 128
                            P.dma('sp', cs[:], cos_in[t0:t0 + 128, :], [], [ck], ck)
                            P.dma('sp', sn[:], sin_in[t0:t0 + 128, :], [], [sk], sk)
                            ld[i] = (xt, xk, cs, ck, sn, sk)
                        else:
                            ld[i] = (xt, xk, None, None, None, None)
                    issue_loads(0)
                    for i in range(NT):
                        isctx = i < NT_C
                        s = 1 if isctx else 0
                        if i + 1 < NT:
                            issue_loads(i + 1)
                        xt, xk, cs, ck, sn, sk = ld.pop(i)
                        h, hk = hring.next()
                        P.dve(lambda e, h=h, xt=xt, s=s: e.tensor_tensor(out=h[:], in0=xt[:], in1=sc1[s][:], op=ALU.mult),
                              [xk, 'sc1_%d' % s], [hk])
                        P.dve(lambda e, h=h, s=s: e.tensor_tensor(out=h[:], in0=h[:], in1=sh1[s][:], op=ALU.add),
                              [hk, 'sh1_%d' % s], [hk])
                        for k in range(8):
                            P.pe(lambda e, h=h, k=k: e.transpose(psum[:, 5 + k // 4, (k % 4) * 128:(k % 4 + 1) * 128],
                                                                 h[:, k * 128:(k + 1) * 128], ident[:]),
                                 [hk, 'ident'], ['pT%d' % k])
                        hT, hTk = hTring.next()
                        for hh in range(2):
                            P.act(lambda e, hT=hT, hh=hh: e.activation(out=hT[:, hh * 4:(hh + 1) * 4, :].rearrange("p k n -> p (k n)"),
                                                                       in_=psum[:, 5 + hh, :], func=AF.Copy),
                                  ['pT%d' % k for k in range(hh * 4, hh * 4 + 4)], [hTk + '_%d' % hh])
                        for b in range(5):
                            for k in range(8):
                                P.pe(lambda e, hT=hT, b=b, k=k: e.matmul(bank(b), hT[:, k, :], win[:, k, b * 512:(b + 1) * 512],
                                                                         start=(k == 0), stop=(k == 7)),
                                     [hTk + '_%d' % (k // 4)] + winkeys, ['z%d' % b])
                        pc, pck = pcb.next()
                        P.act(lambda e: e.activation(out=usb[:], in_=psum[:, 0, 0:256], func=AF.Copy), ['z0'], ['usb'])
                        P.dve(lambda e, pc=pc: e.tensor_tensor(out=pc[:, 0:256], in0=psum[:, 0, 256:512], in1=usb[:], op=ALU.mult),
                              ['z0', 'usb'], [pck + 'a'])
                        P.act(lambda e, pc=pc: e.activation(out=pc[:, 256:512], in_=psum[:, 1, 0:256], func=AF.Copy), ['z1'], [pck + 'b'])
                        r0 = prow(i)
                        P.dma('sp', PB[r0:r0 + 128, :], pc[:], [pck + 'a', pck + 'b'], [('PB', i)], pck)
                        rt, rtk = rtok.next()
                        P.act(lambda e, rt=rt: e.activation(out=rt[:, 512:768], in_=psum[:, 1, 256:512], func=AF.Copy), ['z1'], [rtk + 'v'])
                        if isctx:
                            P.act(lambda e: e.activation(out=rq[:, 0, :], in_=psum[:, 2, 0:256], func=AF.Copy), ['z2'], ['rq0'])
                            P.act(lambda e: e.activation(out=rq[:, 3, :], in_=psum[:, 2, 256:512], func=AF.Copy), ['z2'], ['rq3'])
                        else:
                            rope(psum[:, 2, 0:256], 256, rq[:, 0, :], ['z2'], 'rq0', cs, ck, sn, sk)
                            rope(psum[:, 2, 256:512], 256, rq[:, 3, :], ['z2'], 'rq3', cs, ck, sn, sk)
                        TABf = TAB[:].rearrange("p t h d -> p t (h d)")
                        P.dve(lambda e: e.tensor_tensor(out=rq[:, 1, :], in0=rq[:, 0, :], in1=TABf[:, 2, :], op=ALU.mult), ['rq0', 'TAB'], ['rq1'])
                        P.pool(lambda e: e.tensor_tensor(out=rq[:, 2, :], in0=rq[:, 0, :], in1=TABf[:, 3, :], op=ALU.mult), ['rq0', 'TAB'], ['rq2'])
                        P.dve(lambda e, rt=rt: e.tensor_tensor(out=rt[:, 0:256], in0=rq[:, 3, :], in1=TABf[:, 0, :], op=ALU.mult), ['rq3', 'TAB'], [rtk + 'f'])
                        P.pool(lambda e, rt=rt: e.tensor_tensor(out=rt[:, 256:512], in0=rq[:, 3, :], in1=TABf[:, 1, :], op=ALU.mult), ['rq3', 'TAB'], [rtk + 'b'])
                        P.dma('sp', RTOK[i * 128:(i + 1) * 128, :], rt[:], [rtk + 'v', rtk + 'f', rtk + 'b'], [('RTOK', i)], rtk)
                        rg_, rgk = rgt.next()
                        P.act(lambda e, rg_=rg_: e.activation(out=rg_[:], in_=psum[:, 4, 0:256], func=AF.Silu), ['z4'], [rgk])
                        P.dma('act', RG[i * 128:(i + 1) * 128, :], rg_[:], [rgk], [('RG', i)], rgk)
                        for t in range(4):
                            for c2 in range(2):
                                idx = t * 2 + c2
                                P.pe(lambda e, t=t, c2=c2, idx=idx: e.transpose(psum[:, 5 + idx // 4, (idx % 4) * 128:(idx % 4 + 1) * 128],
                                                                                rq[:, t, c2 * 128:(c2 + 1) * 128], ident[:]),
                                     ['rq%d' % t, 'ident'], ['pT%d' % idx])
                        rTt, rTk = rT.next()
                        for hh in range(2):
                            if hh == 0:
                                P.act(lambda e, rTt=rTt: e.activation(out=rTt[:, 0:4, :].rearrange("p k n -> p (k n)"), in_=psum[:, 5, :], func=AF.Copy),
                                      ['pT0', 'pT1', 'pT2', 'pT3'], [rTk + 'a'])
                            else:
                                P.act(lambda e, rTt=rTt: e.activation(out=rTt[:, 4:6, :].rearrange("p k n -> p (k n)"), in_=psum[:, 6, 0:256], func=AF.Copy),
                                      ['pT4', 'pT5'], [rTk + 'b'])
                                P.act(lambda e, rTt=rTt: e.activation(out=rTt[:, 6:8, :].rearrange("p k n -> p (k n)"), in_=psum[:, 6, 256:512], func=AF.Copy, scale=0.125),
                                      ['pT6', 'pT7'], [rTk + 'c'])
                        for t in range(3):
                            P.dma('act', RQT[t, :, :, i * 128:(i + 1) * 128].rearrange("(c q) d n -> (q d) c n", q=2),
                                  rTt[:, 2 * t:2 * t + 2, :], [rTk + 'a', rTk + 'b'], [('RQT', i, t)], rTk + 'q%d' % t)
                        P.dma('act', RKT[:, :, i * 128:(i + 1) * 128].rearrange("(c q) d n -> (q d) c n", q=2),
                              rTt[:, 6:8, :], [rTk + 'c'], [('RKT', i)], rTk + 'k')
                        P.act(lambda e: e.activation(out=sq[:], in_=psum[:, 3, :], func=AF.Square), ['z3'], ['sq'])
                        P.dve(lambda e: e.tensor_reduce(out=ss[:], in_=sq[:].rearrange("p (h d) -> p h d", d=64), axis=AX.X, op=ALU.add), ['sq'], ['ss'])
                        P.dve(lambda e: e.tensor_scalar(out=ss[:], in0=ss[:], scalar1=1.0 / 64, scalar2=EPS, op0=ALU.mult, op1=ALU.add), ['ss'], ['ss'])
                        P.act(lambda e: e.activation(out=ss[:], in_=ss[:], func=AF.Sqrt), ['ss'], ['ss'])
                        P.dve(lambda e: e.reciprocal(out=ss[:], in_=ss[:]), ['ss'], ['ss'])
                        P.dve(lambda e: e.tensor_tensor(out=aqn[:].rearrange("p (h d) -> p h d", d=64), in0=psum[:, 3, :].rearrange("p (h d) -> p h d", d=64),
                                                        in1=ss[:].unsqueeze(2).to_broadcast([128, 8, 64]), op=ALU.mult), ['z3', 'ss'], ['aqn'])
                        P.pool(lambda e: e.tensor_tensor(out=aqn[:], in0=aqn[:], in1=gq8[:].rearrange("p h d -> p (h d)"), op=ALU.mult), ['aqn', 'gq8'], ['aqn'])
                        if isctx:
                            aq_src, aq_key = aqn, 'aqn'
                        else:
                            rope(aqn[:], 512, aq[:], ['aqn'], 'aq', cs, ck, sn, sk)
                            aq_src, aq_key = aq, 'aq'
                        av_, avk = avt.next()
                        P.act(lambda e, av_=av_: e.activation(out=av_[:], in_=psum[:, 4, 384:512], func=AF.Copy), ['z4'], [avk])
                        P.dma('act', AV[i * 128:(i + 1) * 128, :], av_[:], [avk], [('AV', i)], avk)
                        P.act(lambda e: e.activation(out=sq[:, 0:128], in_=psum[:, 4, 256:384], func=AF.Square), ['z4'], ['sqk'])
                        P.dve(lambda e: e.tensor_reduce(out=ss[:, 0:2], in_=sq[:, 0:128].rearrange("p (h d) -> p h d", d=64), axis=AX.X, op=ALU.add), ['sqk'], ['ssk'])
                        P.dve(lambda e: e.tensor_scalar(out=ss[:, 0:2], in0=ss[:, 0:2], scalar1=1.0 / 64, scalar2=EPS, op0=ALU.mult, op1=ALU.add), ['ssk'], ['ssk'])
                        P.act(lambda e: e.activation(out=ss[:, 0:2], in_=ss[:, 0:2], func=AF.Sqrt), ['ssk'], ['ssk'])
                        P.dve(lambda e: e.reciprocal(out=ss[:, 0:2], in_=ss[:, 0:2]), ['ssk'], ['ssk'])
                        P.dve(lambda e: e.tensor_tensor(out=akn[:].rearrange("p (h d) -> p h d", d=64), in0=psum[:, 4, 256:384].rearrange("p (h d) -> p h d", d=64),
                                                        in1=ss[:, 0:2].unsqueeze(2).to_broadcast([128, 2, 64]), op=ALU.mult), ['z4', 'ssk'], ['akn'])
                        P.pool(lambda e: e.tensor_tensor(out=akn[:], in0=akn[:], in1=gk2[:].rearrange("p h d -> p (h d)"), op=ALU.mult), ['akn', 'gk2'], ['akn'])
                        if isctx:
                            ak_src, ak_key = akn, 'akn'
                        else:
                            rope(akn[:], 128, ak[:], ['akn'], 'ak', cs, ck, sn, sk)
                            ak_src, ak_key = ak, 'ak'
                        for c4 in range(4):
                            P.pe(lambda e, c4=c4, aq_src=aq_src: e.transpose(psum[:, 7, c4 * 128:(c4 + 1) * 128], aq_src[:, c4 * 128:(c4 + 1) * 128], ident[:]),
                                 [aq_key, 'ident'], ['pA%d' % c4])
                        P.pe(lambda e, ak_src=ak_src: e.transpose(psum[:, 5, 0:128], ak_src[:, 0:128], ident[:]), [ak_key, 'ident'], ['pT0'])
                        aTt, aTk = aT.next()
                        P.act(lambda e, aTt=aTt: e.activation(out=aTt[:, 0:4, :].rearrange("p k n -> p (k n)"), in_=psum[:, 7, :], func=AF.Copy),
                              ['pA0', 'pA1', 'pA2', 'pA3'], [aTk + 'q'])
                        P.act(lambda e, aTt=aTt: e.activation(out=aTt[:, 4, :], in_=psum[:, 5, 0:128], func=AF.Copy), ['pT0'], [aTk + 'k'])
                        P.dma('sp', AQT[:, :, i * 128:(i + 1) * 128].rearrange("(c q) d n -> (q d) c n", q=2), aTt[:, 0:4, :],
                              [aTk + 'q'], [('AQT', i)], aTk + 'q')
                        P.dma('sp', AKT[:, :, i * 128:(i + 1) * 128].rearrange("q d n -> (q d) n"), aTt[:, 4, :],
                              [aTk + 'k'], [('AKT', i)], aTk + 'k')
                    P.flush()


            if 'att' in phases:
                with contextlib.ExitStack() as st:
                    def sb(name, shape, dt=F32):
                        return st.enter_context(nc.sbuf_tensor(uname(name), list(shape), dt))
                    KT = sb("KT", [128, 2, N], BF16)
                    P.pool(lambda e: e.memset(KT[64:128, :, :], 0.0), [], ['KTz'])
                    P.dma('sp', KT[0:64, :, :], AKT.rearrange("k d n -> d k n"), [], ['KT'], 'KT')
                    V1 = sb("V1", [128, NT, 2, 65], BF16)
                    P.pool(lambda e: e.memset(V1[:], 1.0), [], ['V1'])
                    for kk_ in range(2):
                        P.dma('act', V1[:, :, kk_, 0:64], AV[:, kk_ * 64:(kk_ + 1) * 64].rearrange("(c p) d -> p c d", p=128), [], ['V1'], 'V1_%d' % kk_)
                    qring = Ring('QT', [sb("QT%d" % i, [128, N], BF16) for i in range(2)])
                    for qi_, qt_ in enumerate(qring.tiles):
                        P.pool(lambda e, qt_=qt_: e.memset(qt_[64:128, :], 0.0), [], ['QTz%d' % qi_])
                    ptring = Ring('PT', [sb("PT%d" % i, [128, 512], BF16) for i in range(4)])
                    aoring = Ring('AO', [sb("AO%d" % i, [128, 4, 64]) for i in range(2)])
                    rcring = Ring('rc', [sb("rc%d" % i, [128, 4]) for i in range(2)])
                    sbank = Ring('S', [0, 1, 2, 3])
                    obank = Ring('O', [4, 5])
                    groups = []
                    if not last:
                        groups.append((0, 2, [0, 1]))
                    for g in range(8):
                        groups.append((LC + g * 512, 4, list(range(NT))))
                    items = []
                    for hq in range(8):
                        for gi, (q0, nq, chunks) in enumerate(groups):
                            for ci, c in enumerate(chunks):
                                items.append((hq, gi, q0, nq, ci, c, len(chunks)))
                    qt_of = {}
                    st_of = {}

                    def get_qt(hq):
                        if hq not in qt_of:
                            QT, qk = qring.next()
                            P.dma('sp', QT[0:64, :], AQT[hq], [], [qk], qk)
                            qt_of[hq] = (QT, qk)
                        return qt_of[hq]

                    def emit_S(t):
                        hq, gi, q0, nq, ci, c, nch = items[t]
                        QT, qk = get_qt(hq)
                        kvh = hq // 4
                        W = nq * 128
                        sbk, sk = sbank.next()
                        P.pe(lambda e, sbk=sbk, c=c, QT=QT, q0=q0, W=W, kvh=kvh: e.matmul(psum[:, sbk, 0:W], KT[:, kvh, c * 128:(c + 1) * 128],
                                                                                         QT[:, q0:q0 + W], start=True, stop=True),
                             ['KT', 'KTz', 'QTz0', 'QTz1', qk], [sk])
                        st_of[t] = (sbk, sk)

                    cur_o = [None]

                    def emit_rest(t):
                        hq, gi, q0, nq, ci, c, nch = items[t]
                        kvh = hq // 4
                        W = nq * 128
                        sbk, sk = st_of.pop(t)
                        if ci == 0:
                            cur_o[0] = obank.next()
                        ob, ok = cur_o[0]
                        Ov = psum[:, ob, 0:260].rearrange("p (j e) -> p j e", e=65)
                        PT, pk = ptring.next()
                        P.act(lambda e, PT=PT, sbk=sbk, W=W: e.activation(out=PT[:, 0:W], in_=psum[:, sbk, 0:W], func=AF.Exp, scale=0.125),
                              [sk], [pk])
                        if t + 2 < len(items):
                            emit_S(t + 2)
                        for j in range(nq):
                            P.pe(lambda e, PT=PT, j=j, c=c, kvh=kvh, ci=ci, Ov=Ov, nch=nch: e.matmul(
                                Ov[:, j, :], PT[:, j * 128:(j + 1) * 128], V1[:, c, kvh, :],
                                start=(ci == 0 and j == 0), stop=(ci == nch - 1), skip_group_check=True),
                                [pk, 'V1'], [ok])
                        if ci == nch - 1:
                            rc, rk_ = rcring.next()
                            AO, ak_ = aoring.next()
                            P.dve(lambda e, rc=rc, Ov=Ov, nq=nq: e.reciprocal(out=rc[:, 0:nq], in_=Ov[:, 0:nq, 64]), [ok], [rk_])
                            P.dve(lambda e, rc=rc, Ov=Ov, nq=nq, AO=AO: e.tensor_tensor(out=AO[:, 0:nq, :], in0=Ov[:, 0:nq, 0:64],
                                                                                      in1=rc[:, 0:nq].unsqueeze(2).to_broadcast([128, nq, 64]), op=ALU.mult),
                                  [ok, rk_], [ak_])
                            P.dma('sp', MIX[q0:q0 + W, 512 + hq * 64:512 + (hq + 1) * 64].rearrange("(j p) d -> p j d", p=128), AO[:, 0:nq, :],
                                  [ak_], [('MIXa', q0, hq)], ak_)
                    emit_S(0)
                    emit_S(1)
                    for t in range(len(items)):
                        emit_rest(t)
                    P.flush()

            if 'ret' in phases:
                with contextlib.ExitStack() as st:
                    def sb(name, shape, dt=F32):
                        return st.enter_context(nc.sbuf_tensor(uname(name), list(shape), dt))
                    RT = sb("RT", [128, NT, 768], BF16)
                    P.dma('sp', RT[:], RTOK.rearrange("(c p) w -> p c w", p=128), [], ['RT'], 'RT')
                    RGt = sb("RGt", [128, NT, 256])
                    P.dma('act', RGt[:], RG.rearrange("(c p) w -> p c w", p=128), [], ['RGt'], 'RGt')
                    gng = sb("gng", [128, 256])
                    bload('act', gng[:], gn_g[l], 'gng')
                    lg = sb("lg", [128, 8])
                    bload('act', lg[:], rde[l].rearrange("a h -> (a h)"), 'lg')
                    P.act(lambda e: e.activation(out=lg[:], in_=lg[:], func=AF.Exp, scale=-math.log(2.0)), ['lg'], ['lg'])
                    P.act(lambda e: e.activation(out=lg[:], in_=lg[:], func=AF.Ln, scale=-1.0, bias=1.0), ['lg'], ['lg'])
                    dec = sb("dec", [128, 8])
                    P.act(lambda e: e.activation(out=dec[:], in_=lg[:], func=AF.Exp, scale=128.0), ['lg'], ['dec'])
                    dpos = sb("dpos", [128, 128]); dneg = sb("dneg", [128, 128]); mge = sb("mge", [128, 128])
                    P.dma('sp', dpos[:], dpos_in, [], ['dpos'], 'dpos')
                    P.dma('sp', dneg[:], dneg_in, [], ['dneg'], 'dneg')
                    P.dma('sp', mge[:], mge_in, [], ['mge'], 'mge')
                    DcT = sb("DcT", [128, 4, 128])
                    e1 = sb("e1", [128, 128])
                    for hh in range(4):
                        P.act(lambda e, hh=hh: e.activation(out=e1[:], in_=dpos[:], func=AF.Exp, scale=lg[:, hh:hh + 1]), ['dpos', 'lg'], ['e1'])
                        P.act(lambda e, hh=hh: e.activation(out=DcT[:, hh, :], in_=dneg[:], func=AF.Exp, scale=lg[:, 4 + hh:5 + hh]), ['dneg', 'lg'], ['DcT%d' % hh])
                        P.dve(lambda e, hh=hh: e.tensor_tensor(out=e1[:], in0=e1[:], in1=DcT[:, hh, :], op=ALU.subtract), ['e1', 'DcT%d' % hh], ['e1'])
                        P.dve(lambda e, hh=hh: e.tensor_tensor(out=e1[:], in0=e1[:], in1=mge[:], op=ALU.mult), ['e1', 'mge'], ['e1'])
                        P.dve(lambda e, hh=hh: e.tensor_tensor(out=DcT[:, hh, :], in0=DcT[:, hh, :], in1=e1[:], op=ALU.add), ['e1', 'DcT%d' % hh], ['DcT%d' % hh])
                    q3ring = Ring('Q3', [sb("Q3_%d" % i, [64, 3, N], BF16) for i in range(2)])
                    ktring = Ring('KTh', [sb("KTh%d" % i, [64, N], BF16) for i in range(2)])
                    Sf = sb("Sf", [64, NT + 1, 64], BF16)
                    Sb = sb("Sb", [64, NT + 1, 64], BF16)
                    srun = Ring('srun', [sb("srun%d" % i, [64, 64]) for i in range(2)])
                    ptr = Ring('PTr', [sb("PTr%d" % i, [128, 128], BF16) for i in range(2)])
                    st6 = sb("st6", [128, 6]); mv = sb("mv", [128, 2]); rs = sb("rs", [128, 1])
                    yo = Ring('yo', [sb("yo%d" % i, [128, 64]) for i in range(2)])
                    kvb = Ring('kv', [0, 1]); scb = Ring('sc', [2, 3]); yb = Ring('y', [4, 5])
                    ytiles = list(range(NT)) if not last else list(range(NT_C, NT))
                    for hh in range(4):
                        Q3, q3k = q3ring.next()
                        KTh, ktk = ktring.next()
                        P.dma('sp', Q3[:], RQT[:, hh].rearrange("t d n -> d t n"), [], [q3k], q3k)
                        P.dma('act', KTh[:], RKT[hh], [], [ktk], ktk)
                        vcol = 512 + hh * 64

                        def scan(order_tiles, S, skey, kcol, dcol, run_init_zero):
                            return None
                        run, runk = srun.next()
                        P.pool(lambda e, run=run: e.memset(run[:], 0.0), [], [runk])
                        for i in range(NT):
                            P.pool(lambda e, run=run, i=i: e.tensor_copy(out=Sf[:, i, :], in_=run[:]), [runk], [('Sf', i)])
                            kb, kk = kvb.next()
                            P.pe(lambda e, kb=kb, i=i, hh=hh, vcol=vcol: e.matmul(psum[0:64, kb, 0:64], RT[:, i, hh * 64:(hh + 1) * 64],
                                                                                 RT[:, i, vcol:vcol + 64], start=True, stop=True), ['RT'], [kk])
                            nrun, nrunk = srun.next()
                            P.dve(lambda e, run=run, nrun=nrun, kb=kb, hh=hh: e.scalar_tensor_tensor(out=nrun[:], in0=run[:], scalar=dec[0:64, hh:hh + 1],
                                                                                                     in1=psum[0:64, kb, 0:64], op0=ALU.mult, op1=ALU.add),
                                  [runk, kk, 'dec'], [nrunk])
                            run, runk = nrun, nrunk
                        run, runk = srun.next()
                        P.pool(lambda e, run=run: e.memset(run[:], 0.0), [], [runk])
                        for i in [1, 0] + list(range(NT - 1, NT_C - 1, -1)):
                            P.pool(lambda e, run=run, i=i: e.tensor_copy(out=Sb[:, i, :], in_=run[:]), [runk], [('Sb', i)])
                            kb, kk = kvb.next()
                            P.pe(lambda e, kb=kb, i=i, hh=hh, vcol=vcol: e.matmul(psum[0:64, kb, 0:64], RT[:, i, 256 + hh * 64:256 + (hh + 1) * 64],
                                                                                 RT[:, i, vcol:vcol + 64], start=True, stop=True), ['RT'], [kk])
                            nrun, nrunk = srun.next()
                            P.dve(lambda e, run=run, nrun=nrun, kb=kb, hh=hh: e.scalar_tensor_tensor(out=nrun[:], in0=run[:], scalar=dec[0:64, 4 + hh:5 + hh],
                                                                                                     in1=psum[0:64, kb, 0:64], op0=ALU.mult, op1=ALU.add),
                                  [runk, kk, 'dec'], [nrunk])
                            run, runk = nrun, nrunk
                        for i in ytiles:
                            sbk, sk = scb.next()
                            P.pe(lambda e, sbk=sbk, i=i, KTh=KTh, Q3=Q3: e.matmul(psum[:, sbk, 0:128], KTh[:, i * 128:(i + 1) * 128], Q3[:, 0, i * 128:(i + 1) * 128],
                                                                                start=True, stop=True), [ktk, q3k], [sk])
                            PTr, pk = ptr.next()
                            P.dve(lambda e, PTr=PTr, sbk=sbk, hh=hh: e.tensor_tensor(out=PTr[:], in0=psum[:, sbk, 0:128], in1=DcT[:, hh, :], op=ALU.mult),
                                  [sk, 'DcT%d' % hh], [pk])
                            ybk, yk = yb.next()
                            P.pe(lambda e, ybk=ybk, PTr=PTr, i=i, vcol=vcol: e.matmul(psum[:, ybk, 0:64], PTr[:], RT[:, i, vcol:vcol + 64], start=True, stop=False),
                                 [pk, 'RT'], [yk])
                            P.pe(lambda e, ybk=ybk, Q3=Q3, i=i: e.matmul(psum[:, ybk, 0:64], Q3[:, 1, i * 128:(i + 1) * 128], Sf[:, i, :], start=False, stop=False),
                                 [q3k, ('Sf', i)], [yk])
                            P.pe(lambda e, ybk=ybk, Q3=Q3, i=i: e.matmul(psum[:, ybk, 0:64], Q3[:, 2, i * 128:(i + 1) * 128], Sb[:, i, :], start=False, stop=True),
                                 [q3k, ('Sb', i)], [yk])
                            P.dve(lambda e, ybk=ybk: e.bn_stats(out=st6[:], in_=psum[:, ybk, 0:64]), [yk], ['st6'])
                            P.dve(lambda e: e.bn_aggr(out=mv[:], in_=st6[:]), ['st6'], ['mv'])
                            P.dve(lambda e: e.tensor_scalar_add(out=rs[:], in0=mv[:, 1:2], scalar1=EPS), ['mv'], ['rs'])
                            P.act(lambda e: e.activation(out=rs[:], in_=rs[:], func=AF.Sqrt), ['rs'], ['rs'])
                            P.dve(lambda e: e.reciprocal(out=rs[:], in_=rs[:]), ['rs'], ['rs'])
                            y_, yok = yo.next()
                            P.dve(lambda e, y_=y_, ybk=ybk: e.tensor_scalar(out=y_[:], in0=psum[:, ybk, 0:64], scalar1=mv[:, 0:1], scalar2=rs[:, 0:1],
                                                                           op0=ALU.subtract, op1=ALU.mult), [yk, 'mv', 'rs'], [yok])
                            P.pool(lambda e, y_=y_, hh=hh: e.tensor_tensor(out=y_[:], in0=y_[:], in1=gng[:, hh * 64:(hh + 1) * 64], op=ALU.mult), [yok, 'gng'], [yok])
                            P.pool(lambda e, y_=y_, hh=hh, i=i: e.tensor_tensor(out=y_[:], in0=y_[:], in1=RGt[:, i, hh * 64:(hh + 1) * 64], op=ALU.mult), [yok, 'RGt'], [yok])
                            P.dma('sp', MIX[i * 128:(i + 1) * 128, 256 + hh * 64:256 + (hh + 1) * 64], y_[:], [yok], [('MIXr', i, hh)], yok)
                    P.flush()

            if 'p3' in phases:
                with contextlib.ExitStack() as st:
                    def sb(name, shape, dt=F32):
                        return st.enter_context(nc.sbuf_tensor(uname(name), list(shape), dt))
                    wout = sb("wout", [128, 8, D], BF16)
                    for hf in range(2):
                        P.dma('pool', wout[:, :, hf * 512:(hf + 1) * 512], w_out[l, :, hf * 512:(hf + 1) * 512].rearrange("(k p) n -> p k n", p=128),
                              [], ['wout%d' % hf], 'wout%d' % hf)
                    CWr = sb("CWr", [128, 256, 3])
                    P.dma('act', CWr[:].rearrange("p c k -> p (c k)"), conv_w[l].rearrange("c k -> (c k)").partition_broadcast(128), [], ['CWr'], 'CWr')
                    CW = sb("CW", [128, 3, 256])
                    for k in range(3):
                        P.dve(lambda e, k=k: e.tensor_copy(out=CW[:, k, :], in_=CWr[:, :, k]), ['CWr'], ['CW%d' % k])
                    g1 = [sb("g1_%d" % s, [128, D]) for s in range(2)]
                    sc2 = [sb("sc2_%d" % s, [128, D]) for s in range(2)]
                    sh2 = [sb("sh2_%d" % s, [128, D]) for s in range(2)]
                    for s in range(2):
                        if last and s == 1:
                            continue
                        bload('sp', g1[s][:], modv[l, s, 2 * D:3 * D], 'g1_%d' % s)
                        bload('sp', sh2[s][:], modv[l, s, 3 * D:4 * D], 'sh2_%d' % s)
                        bload('sp', sc2[s][:], modv[l, s, 4 * D:5 * D], 'sc2_%d' % s)
                    lng = sb("lng", [128, D]); lnb = sb("lnb", [128, D])
                    bload('act', lng[:], ln_g[l, 0], 'lng')
                    bload('act', lnb[:], ln_b[l, 0], 'lnb')
                    wr = sb("wr", [128, 8, NE])
                    P.dma('act', wr[:], w_router[l].rearrange("(k p) e -> p k e", p=128), [], ['wr'], 'wr')
                    brt = sb("brt", [128, NE])
                    bload('act', brt[:], b_router[l], 'brt')
                    pmr = Ring('pm', [sb("pm%d" % i, [128, 3, 256]) for i in range(3)])
                    btr = Ring('bt', [sb("bt%d" % i, [128, 256]) for i in range(3)])
                    mixr = Ring('mix', [sb("mix%d" % i, [128, D]) for i in range(3)])
                    xr = Ring('x3', [sb("x3_%d" % i, [128, D]) for i in range(3)])
                    ca = sb("ca", [128, 256]); cb = sb("cb", [128, 256])
                    mTr = Ring('mT', [sb("mT%d" % i, [128, 8, 128], BF16) for i in range(2)])
                    rr = sb("rr", [128, D])
                    x1r = Ring('x1', [sb("x1_%d" % i, [128, D]) for i in range(2)])
                    h2 = sb("h2", [128, D])
                    h2br = Ring('h2b', [sb("h2b%d" % i, [128, D], BF16) for i in range(2)])
                    ix8 = sb("ix8", [128, 8], U32)
                    h2f = sb("h2f", [128, 8, 128])
                    st6 = sb("st6", [128, 2, 6]); mv = sb("mv", [128, 2]); rs = sb("rs", [128, 1])
                    lgt = sb("lgt", [128, NE]); mx8 = sb("mx8", [128, 8]); msk = sb("msk", [128, NE]); nmx = sb("nmx", [128, 1])
                    ex = sb("ex", [128, NE]); sm = sb("sm", [128, 1])
                    mwr = Ring('mw', [sb("mw%d" % i, [128, NE]) for i in range(2)])
                    ld3 = {}

                    def issue_loads3(i):
                        r0 = prow(i)
                        pm, pmk = pmr.next()
                        for k in range(3):
                            P.dma('sp', pm[:, k, :], PB[r0 - 1 + k:r0 + 127 + k, 0:256], [], [pmk + str(k)], pmk + str(k))
                        bt, btk = btr.next()
                        P.dma('sp', bt[:], PB[r0:r0 + 128, 256:512], [], [btk], btk)
                        mix, mixk = mixr.next()
                        P.dma('sp', mix[:, 256:D], MIX[i * 128:(i + 1) * 128, 256:D], [], [mixk + 'l'], mixk)
                        xt, xk = xr.next()
                        P.dma('sp', xt[:], xsrc(i), [], [xk], xk)
                        ld3[i] = (pm, pmk, bt, btk, mix, mixk, xt, xk)
                    issue_loads3(out_tiles[0])
                    for oi, i in enumerate(out_tiles):
                        s = 1 if i < NT_C else 0
                        if oi + 1 < len(out_tiles):
                            issue_loads3(out_tiles[oi + 1])
                        pm, pmk, bt, btk, mix, mixk, xt, xk = ld3.pop(i)
                        P.dve(lambda e, pm=pm: e.tensor_tensor(out=ca[:], in0=pm[:, 0, :], in1=CW[:, 0, :], op=ALU.mult), [pmk + '0', 'CW0'], ['ca'])
                        P.pool(lambda e, pm=pm: e.tensor_tensor(out=cb[:], in0=pm[:, 1, :], in1=CW[:, 1, :], op=ALU.mult), [pmk + '1', 'CW1'], ['cb'])
                        P.dve(lambda e: e.tensor_tensor(out=ca[:], in0=ca[:], in1=cb[:], op=ALU.add), ['ca', 'cb'], ['ca'])
                        P.pool(lambda e, pm=pm: e.tensor_tensor(out=cb[:], in0=pm[:, 2, :], in1=CW[:, 2, :], op=ALU.mult), [pmk + '2', 'CW2', 'ca'], ['cb'])
                        P.dve(lambda e: e.tensor_tensor(out=ca[:], in0=ca[:], in1=cb[:], op=ALU.add), ['ca', 'cb'], ['ca'])
                        P.dve(lambda e, mix=mix, bt=bt: e.tensor_tensor(out=mix[:, 0:256], in0=ca[:], in1=bt[:], op=ALU.mult), ['ca', btk], [mixk + 'c'])
                        if cut < 1:
                            continue
                        for k in range(8):
                            P.pe(lambda e, mix=mix, k=k: e.transpose(psum[:, k // 4, (k % 4) * 128:(k % 4 + 1) * 128], mix[:, k * 128:(k + 1) * 128], ident[:]),
                                 [mixk + 'l', mixk + 'c', 'ident'], ['pT%d' % k])
                        mT, mTk = mTr.next()
                        for hf in range(2):
                            P.act(lambda e, mT=mT, hf=hf: e.activation(out=mT[:, hf * 4:(hf + 1) * 4, :].rearrange("p k n -> p (k n)"), in_=psum[:, hf, :], func=AF.Copy),
                                  ['pT%d' % k for k in range(hf * 4, hf * 4 + 4)], [mTk + str(hf)])
                        for nh in range(2):
                            for k in range(8):
                                P.pe(lambda e, mT=mT, nh=nh, k=k: e.matmul(psum[:, 2 + nh, :], mT[:, k, :], wout[:, k, nh * 512:(nh + 1) * 512], start=(k == 0), stop=(k == 7)),
                                     [mTk + str(k // 4), 'wout%d' % nh], ['py%d' % nh])
                        if cut < 2:
                            continue
                        for nh in range(2):
                            P.dve(lambda e, nh=nh, s=s: e.tensor_tensor(out=rr[:, nh * 512:(nh + 1) * 512], in0=psum[:, 2 + nh, :], in1=g1[s][:, nh * 512:(nh + 1) * 512], op=ALU.mult),
                                  ['py%d' % nh, 'g1_%d' % s], ['rr%d' % nh])
                        P.dve(lambda e, xt=xt: e.scalar_tensor_tensor(out=rr[:], in0=xt[:], scalar=ALPHA, in1=rr[:], op0=ALU.mult, op1=ALU.add), [xk, 'rr0', 'rr1'], ['rr'])
                        for nh in range(2):
                            P.dve(lambda e, nh=nh: e.bn_stats(out=st6[:, nh, :], in_=rr[:, nh * 512:(nh + 1) * 512]), ['rr'], ['st6_%d' % nh])
                        P.dve(lambda e: e.bn_aggr(out=mv[:], in_=st6[:]), ['st6_0', 'st6_1'], ['mv'])
                        P.dve(lambda e: e.tensor_scalar_add(out=rs[:], in0=mv[:, 1:2], scalar1=EPS), ['mv'], ['rs'])
                        P.act(lambda e: e.activation(out=rs[:], in_=rs[:], func=AF.Sqrt), ['rs'], ['rs'])
                        P.dve(lambda e: e.reciprocal(out=rs[:], in_=rs[:]), ['rs'], ['rs'])
                        x1, x1k = x1r.next()
                        P.dve(lambda e, x1=x1: e.tensor_scalar(out=x1[:], in0=rr[:], scalar1=mv[:, 0:1], scalar2=rs[:, 0:1], op0=ALU.subtract, op1=ALU.mult), ['rr', 'mv', 'rs'], [x1k])
                        P.dve(lambda e, x1=x1: e.tensor_tensor(out=x1[:], in0=x1[:], in1=lng[:], op=ALU.mult), [x1k, 'lng'], [x1k])
                        P.dve(lambda e, x1=x1: e.tensor_tensor(out=x1[:], in0=x1[:], in1=lnb[:], op=ALU.add), [x1k, 'lnb'], [x1k])
                        P.dma('sp', XM[i * 128:(i + 1) * 128, :], x1[:], [x1k], [('XM', i)], x1k)
                        if cut < 3:
                            continue
                        P.dve(lambda e, x1=x1, s=s: e.tensor_tensor(out=h2[:], in0=x1[:], in1=sc2[s][:], op=ALU.mult), [x1k, 'sc2_%d' % s], ['h2'])
                        P.dve(lambda e, s=s: e.tensor_tensor(out=h2[:], in0=h2[:], in1=sh2[s][:], op=ALU.add), ['h2', 'sh2_%d' % s], ['h2'])
                        if cut < 3.2:
                            continue
                        for k in range(8):
                            P.pe(lambda e, k=k: e.transpose(psum[:, 4 + k // 4, (k % 4) * 128:(k % 4 + 1) * 128], h2[:, k * 128:(k + 1) * 128], ident[:]),
                                 ['h2', 'ident'], ['pU%d' % k])
                        if cut < 3.3:
                            continue
                        for hf in range(2):
                            P.dve(lambda e, hf=hf: e.tensor_copy(out=h2f[:, hf * 4:(hf + 1) * 4, :].rearrange("p k n -> p (k n)"), in_=psum[:, 4 + hf, :]),
                                  ['pU%d' % k for k in range(hf * 4, hf * 4 + 4)], ['h2f%d' % hf])
                        h2b, h2bk = h2br.next()
                        P.act(lambda e, h2b=h2b: e.activation(out=h2b[:], in_=h2[:], func=AF.Copy), ['h2'], [h2bk])
                        P.dma('sp', H2R[i * 128:(i + 1) * 128, :], h2b[:], [h2bk], [('H2R', i)], h2bk)
                        for k in range(8):
                            P.pe(lambda e, k=k: e.matmul(psum[:, 6, 0:NE], h2f[:, k, :], wr[:, k, :], start=(k == 0), stop=(k == 7)), ['h2f%d' % (k // 4), 'wr'], ['pl'])
                        P.dve(lambda e: e.tensor_tensor(out=lgt[:], in0=psum[:, 6, 0:NE], in1=brt[:], op=ALU.add), ['pl', 'brt'], ['lgt'])
                        P.dve(lambda e: e.max(out=mx8[:], in_=lgt[:]), ['lgt'], ['mx8'])
                        P.dve(lambda e: e.max_index(out=ix8[:], in_max=mx8[:], in_values=lgt[:]), ['lgt', 'mx8'], ['ix8'])
                        P.dve(lambda e: e.tensor_scalar_mul(out=nmx[:], in0=mx8[:, 0:1], scalar1=-1.0), ['mx8'], ['nmx'])
                        P.act(lambda e: e.activation(out=ex[:, 0:4], in_=mx8[:, 0:4], func=AF.Exp, bias=nmx[:, 0:1], scale=1.0), ['mx8', 'nmx'], ['ex'])
                        P.dve(lambda e: e.reduce_sum(out=sm[:], in_=ex[:, 0:4], axis=AX.X), ['ex'], ['sm'])
                        P.dve(lambda e: e.reciprocal(out=sm[:], in_=sm[:]), ['sm'], ['sm'])
                        mw_, mwk = mwr.next()
                        P.dve(lambda e, mw_=mw_: e.tensor_scalar_mul(out=mw_[:, 0:4], in0=ex[:, 0:4], scalar1=sm[:, 0:1]), ['ex', 'sm'], [mwk + 'w'])
                        P.dve(lambda e, mw_=mw_: e.tensor_copy(out=mw_[:, 4:8], in_=ix8[:, 0:4]), ['ix8'], [mwk + 'e'])
                        P.dma('sp', RW[i * 128:(i + 1) * 128, :], mw_[:, 0:4], [mwk + 'w'], [('RW', i)], mwk + 'w')
                        P.dma('sp', RE[i * 128:(i + 1) * 128, :], mw_[:, 4:8], [mwk + 'e'], [('RE', i)], mwk + 'e')
                    P.flush()

            if 'moe' in phases:
                tiles = out_tiles
                nt = len(tiles)
                T = nt * 128
                t0 = tiles[0] * 128
                NB = (4 * T + NE * (BS - 1) + BS - 1) // BS
                assert NB <= NBMAX
                w_gu_rows = w_gu.rearrange("l e r c -> (l e r) c")
                w_dn_rows = w_dn.rearrange("l e r c -> (l e r) c")
                with contextlib.ExitStack() as st:
                    def sb(name, shape, dt=F32):
                        return st.enter_context(nc.sbuf_tensor(uname(name), list(shape), dt))
                    DKi = sb("DKi", [128, nt, 4], I32)
                    IDXG = sb("IDXG", [128, NB, 8], I32)
                    EB = sb("EB", [128, NB])
                    IDXB = sb("IDXB", [128, NB], I32)
                    mwd = sb("mwd", [128, nt, NE])
                    iotap = sb("iotap", [128, 1])
                    P.dma('sp', iotap[:], iotap_in, [], ['iotap'], 'iotap')
                    with contextlib.ExitStack() as st2:
                        def sb2(name, shape, dt=F32):
                            return st2.enter_context(nc.sbuf_tensor(uname(name), list(shape), dt))
                        EF = sb2("EF", [128, nt, 4])
                        W4 = sb2("W4", [128, nt, 4])
                        P.dma('sp', EF[:], RE[t0:t0 + T, :].rearrange("(c p) k -> p c k", p=128), [], ['EF'], 'EF')
                        P.dma('sp', W4[:], RW[t0:t0 + T, :].rearrange("(c p) k -> p c k", p=128), [], ['W4'], 'W4')
                        iota32 = sb2("iota32", [128, NE])
                        P.dma('act', iota32[:], iota32_in, [], ['iota32'], 'iota32')
                        lts = sb2("lts", [128, 128])
                        P.dma('act', lts[:], lts_in, [], ['lts'], 'lts')
                        ones = sb2("ones", [128, 128])
                        P.pool(lambda e: e.memset(ones[:], 1.0), [], ['ones'])
                        bst = sb2("bst", [128, NBMAX])
                        P.dma('act', bst[:], bstart_in, [], ['bst'], 'bst')
                        kp = sb2("kp", [128, 8])
                        P.dma('act', kp[:], kp_in, [], ['kp'], 'kp')
                        OH = sb2("OH", [128, 4, nt, NE])
                        for k in range(4):
                            P.dve(lambda e, k=k: e.tensor_tensor(out=OH[:, k, :, :], in0=iota32[:].unsqueeze(1).to_broadcast([128, nt, NE]),
                                                                 in1=EF[:, :, k].unsqueeze(2).to_broadcast([128, nt, NE]), op=ALU.is_equal),
                                  ['iota32', 'EF'], ['OH%d' % k])
                        mask = sb2("mask", [128, nt, NE])
                        P.dve(lambda e: e.tensor_tensor(out=mask[:], in0=OH[:, 0, :, :], in1=OH[:, 1, :, :], op=ALU.add), ['OH0', 'OH1'], ['mask'])
                        P.dve(lambda e: e.tensor_tensor(out=mask[:], in0=mask[:], in1=OH[:, 2, :, :], op=ALU.add), ['mask', 'OH2'], ['mask'])
                        P.dve(lambda e: e.tensor_tensor(out=mask[:], in0=mask[:], in1=OH[:, 3, :, :], op=ALU.add), ['mask', 'OH3'], ['mask'])
                        for j in range(nt):
                            bk = j // 16
                            col = (j % 16) * NE
                            P.pe(lambda e, j=j, bk=bk, col=col: e.matmul(psum[:, bk, col:col + NE], lts[:], mask[:, j, :], start=True, stop=True, skip_group_check=True),
                                 ['lts', 'mask'], ['rk%d' % bk])
                            P.pe(lambda e, j=j, bk=bk, col=col: e.matmul(psum[:, 4 + bk, col:col + NE], ones[:], mask[:, j, :], start=True, stop=True, skip_group_check=True),
                                 ['ones', 'mask'], ['tt%d' % bk])
                        for m in range(nt):
                            P.pe(lambda e, m=m: e.matmul(psum[:, 3, 0:NE], ones[:], mask[:, m, :], start=(m == 0), stop=(m == nt - 1)), ['ones', 'mask'], ['cnt'])
                        TOT = sb2("TOT", [128, nt, NE])
                        PRE = sb2("PRE", [128, nt, NE])
                        for bk in range((nt + 15) // 16):
                            j0 = bk * 16
                            nj = min(16, nt - j0)
                            P.act(lambda e, bk=bk, j0=j0, nj=nj: e.activation(out=TOT[:, j0:j0 + nj, :].rearrange("p j e -> p (j e)"), in_=psum[:, 4 + bk, 0:nj * NE], func=AF.Copy),
                                  ['tt%d' % bk], ['TOT%d' % bk])
                        P.pool(lambda e: e.memset(PRE[:, 0, :], 0.0), [], ['PRE'])
                        for j in range(1, nt):
                            P.dve(lambda e, j=j: e.tensor_tensor(out=PRE[:, j, :], in0=PRE[:, j - 1, :], in1=TOT[:, j - 1, :], op=ALU.add),
                                  ['PRE'] + ['TOT%d' % bk for bk in range((nt + 15) // 16)], ['PRE'])
                        c0 = sb2("c0", [128, NE]); c1 = sb2("c1", [128, NE]); padded = sb2("padded", [128, NE])
                        P.dve(lambda e: e.tensor_scalar_add(out=c0[:], in0=psum[:, 3, 0:NE], scalar1=float(BS - 1)), ['cnt'], ['c0'])
                        ci32 = sb2("ci32", [128, NE], I32)
                        P.dve(lambda e: e.tensor_scalar(out=c1[:], in0=c0[:], scalar1=1.0 / BS, scalar2=-0.5 + 0.5 / BS, op0=ALU.mult, op1=ALU.add), ['c0'], ['c1'])
                        P.dve(lambda e: e.tensor_copy(out=ci32[:], in_=c1[:]), ['c1'], ['ci32'])
                        P.dve(lambda e: e.tensor_copy(out=c1[:], in_=ci32[:]), ['ci32'], ['c1'])
                        P.dve(lambda e: e.tensor_scalar_mul(out=padded[:], in0=c1[:], scalar1=float(BS)), ['c1'], ['padded'])
                        P.dve(lambda e: e.tensor_copy(out=c0[:], in_=padded[:]), ['padded'], ['c0'])
                        cur, nxt_, ck, nk = c0, c1, 'c0', 'c1'
                        for sft in (1, 2, 4, 8, 16):
                            P.dve(lambda e, cur=cur, nxt_=nxt_, sft=sft: e.tensor_copy(out=nxt_[:, 0:sft], in_=cur[:, 0:sft]), [ck], [nk + 'a'])
                            P.dve(lambda e, cur=cur, nxt_=nxt_, sft=sft: e.tensor_tensor(out=nxt_[:, sft:NE], in0=cur[:, sft:NE], in1=cur[:, 0:NE - sft], op=ALU.add), [ck], [nk + 'b'])
                            P.dve(lambda e: e.engine_nop(), [nk + 'a', nk + 'b'], [nk])
                            cur, nxt_, ck, nk = nxt_, cur, nk, ck
                        pend, pendk = cur, ck
                        pstart = sb2("pstart", [128, NE])
                        P.dve(lambda e: e.tensor_tensor(out=pstart[:], in0=pend[:], in1=padded[:], op=ALU.subtract), [pendk, 'padded'], ['pstart'])
                        dfull = sb2("dfull", [128, nt, NE])
                        for bk in range((nt + 15) // 16):
                            j0 = bk * 16
                            nj = min(16, nt - j0)
                            P.dve(lambda e, bk=bk, j0=j0, nj=nj: e.tensor_tensor(out=dfull[:, j0:j0 + nj, :], in0=psum[:, bk, 0:nj * NE].rearrange("p (j e) -> p j e", e=NE),
                                                                                 in1=pstart[:].unsqueeze(1).to_broadcast([128, nj, NE]), op=ALU.add),
                                  ['rk%d' % bk, 'pstart'], ['dfull%d' % bk])
                            P.dve(lambda e, j0=j0, nj=nj: e.tensor_tensor(out=dfull[:, j0:j0 + nj, :], in0=dfull[:, j0:j0 + nj, :], in1=PRE[:, j0:j0 + nj, :], op=ALU.add),
                                  ['dfull%d' % bk, 'PRE'], ['dfull%d' % bk])
                        dkeys = ['dfull%d' % bk for bk in range((nt + 15) // 16)]
                        tmpd = sb2("tmpd", [128, nt, NE])
                        DKf = sb2("DKf", [128, nt, 4])
                        for k in range(4):
                            P.dve(lambda e, k=k: e.tensor_tensor(out=tmpd[:], in0=dfull[:], in1=OH[:, k, :, :], op=ALU.mult), dkeys + ['OH%d' % k], ['tmpd'])
                            P.dve(lambda e, k=k: e.tensor_reduce(out=DKf[:, :, k], in_=tmpd[:], axis=AX.X, op=ALU.add), ['tmpd'], ['DKf%d' % k])
                        P.dve(lambda e: e.tensor_copy(out=DKi[:], in_=DKf[:]), ['DKf%d' % k for k in range(4)], ['DKi'])
                        cmp_ = sb2("cmp", [128, NB, NE])
                        P.dve(lambda e: e.tensor_tensor(out=cmp_[:], in0=pend[:].unsqueeze(1).to_broadcast([128, NB, NE]),
                                                        in1=bst[:, 0:NB].unsqueeze(2).to_broadcast([128, NB, NE]), op=ALU.is_le), [pendk, 'bst'], ['cmp'])
                        P.dve(lambda e: e.tensor_reduce(out=EB[:], in_=cmp_[:], axis=AX.X, op=ALU.add), ['cmp'], ['EB'])
                        P.dve(lambda e: e.tensor_scalar_min(out=EB[:], in0=EB[:], scalar1=float(NE - 1)), ['EB'], ['EB'])
                        idxf = sb2("idxf", [128, NB, 8])
                        P.dve(lambda e: e.scalar_tensor_tensor(out=idxf[:], in0=EB[:].unsqueeze(2).to_broadcast([128, NB, 8]), scalar=float(D),
                                                               in1=kp[:].unsqueeze(1).to_broadcast([128, NB, 8]), op0=ALU.mult, op1=ALU.add), ['EB', 'kp'], ['idxf'])
                        P.dve(lambda e: e.tensor_scalar_add(out=idxf[:], in0=idxf[:], scalar1=float(l * NE * D)), ['idxf'], ['idxf'])
                        P.dve(lambda e: e.tensor_copy(out=IDXG[:], in_=idxf[:]), ['idxf'], ['IDXG'])
                        idxbf = sb2("idxbf", [128, NB])
                        P.dve(lambda e: e.tensor_scalar(out=idxbf[:], in0=EB[:], scalar1=128.0, scalar2=float(l * NE * 128), op0=ALU.mult, op1=ALU.add), ['EB'], ['idxbf'])
                        P.dve(lambda e: e.tensor_scalar(out=idxbf[:], in0=idxbf[:], scalar1=iotap[:, 0:1], scalar2=None, op0=ALU.add), ['idxbf', 'iotap'], ['idxbf'])
                        P.dve(lambda e: e.tensor_copy(out=IDXB[:], in_=idxbf[:]), ['idxbf'], ['IDXB'])
                        for k in range(4):
                            dst_ = mwd if k == 0 else tmpd
                            P.dve(lambda e, k=k, dst_=dst_: e.tensor_tensor(out=dst_[:], in0=OH[:, k, :, :], in1=W4[:, :, k].unsqueeze(2).to_broadcast([128, nt, NE]), op=ALU.mult),
                                  ['OH%d' % k, 'W4', 'DKf0', 'DKf1', 'DKf2', 'DKf3'], ['mwd' if k == 0 else 'tmpd'])
                            if k > 0:
                                P.dve(lambda e: e.tensor_tensor(out=mwd[:], in0=mwd[:], in1=tmpd[:], op=ALU.add), ['mwd', 'tmpd'], ['mwd'])
                        hr = Ring('hr', [sb2("hr%d" % i, [128, D], BF16) for i in range(3)])
                        for j, i in enumerate(tiles):
                            h_, hk_ = hr.next()
                            P.dma('sp', h_[:], H2R[i * 128:(i + 1) * 128, :], [], [hk_], hk_)
                            for k in range(4):
                                P.add('pool', lambda e, h_=h_, j=j, k=k: e.indirect_dma_start(out=XS[:, :], out_offset=bass.IndirectOffsetOnAxis(ap=DKi[:, j, k:k + 1], axis=0),
                                                                                          in_=h_[:, :], in_offset=None), [hk_, 'DKi'], [('XS', j, k)], 'xsc%d' % ((j * 4 + k) % 4))
                                P.add('pool', lambda e, j=j, k=k: e.indirect_dma_start(out=SW[:, :], out_offset=bass.IndirectOffsetOnAxis(ap=DKi[:, j, k:k + 1], axis=0),
                                                                                    in_=W4[:, j, k:k + 1], in_offset=None), ['W4', 'DKi'], [('SW', j, k)], 'swc%d' % ((j * 4 + k) % 4))
                        P.flush()
                    with contextlib.ExitStack() as st2:
                        def sb2(name, shape, dt=F32):
                            return st2.enter_context(nc.sbuf_tensor(uname(name), list(shape), dt))
                        identb = sb2("identb", [128, 128], BF16)
                        P.act(lambda e: e.activation(out=identb[:], in_=ident[:], func=AF.Copy), ['ident'], ['identb'])
                        bgr = sb2("bgr", [NE, 2 * D])
                        P.dma('act', bgr[:], b_gu[l], [], ['bgr'], 'bgr')
                        for c in range(16):
                            P.pe(lambda e, c=c: e.transpose(psum[:, 6, c * NE:(c + 1) * NE], bgr[:, c * 128:(c + 1) * 128], ident[0:NE, 0:NE]), ['bgr', 'ident'], ['pX0'])
                        X2 = sb2("X2", [128, NE, 16])
                        P.dve(lambda e: e.tensor_copy(out=X2[:], in_=psum[:, 6, :].rearrange("p (c e) -> p e c", e=NE)), ['pX0'], ['X2'])
                        P.dve(lambda e: e.tensor_scalar_add(out=X2[:, :, 8:16], in0=X2[:, :, 8:16], scalar1=1.0), ['X2'], ['X2'])
                        P.dma('sp', BGT[l * NE * 128:(l + 1) * NE * 128, :].rearrange("(e p) c -> p e c", p=128), X2[:], ['X2'], ['BGT'], 'BGT')
                        bbr = Ring('bb', [sb2("bb%d" % i, [128, 16]) for i in range(2)])
                        wgr = Ring('WG', [sb2("WG%d" % i, [128, 8, 2 * D], BF16) for i in range(2)])
                        wdr = Ring('WD', [sb2("WD%d" % i, [128, 8, D], BF16) for i in range(2)])
                        xbr = Ring('XB', [sb2("XB%d" % i, [128, 4, D], BF16) for i in range(2)])
                        XT = sb2("XT", [128, 8, BS], BF16)
                        actr = Ring('act', [sb2("act%d" % i, [128, 8, BS], BF16) for i in range(2)])
                        Ar = Ring('A', [sb2("A%d" % i, [128, BS]) for i in range(2)])
                        Sr = Ring('Sg', [sb2("Sg%d" % i, [128, BS]) for i in range(2)])
                        Ur = Ring('U', [sb2("U%d" % i, [128, BS]) for i in range(2)])
                        swr = Ring('swb', [sb2("swb%d" % i, [128, 4]) for i in range(2)])
                        Yr = Ring('Y', [sb2("Y%d" % i, [128, 4, D]) for i in range(1)])
                        gbr = Ring('pg', [0, 1]); ubr = Ring('pu', [2, 3]); ybr = Ring('py', [4, 5])
                        psb = [psum[:, 6, :].bitcast(BF16), psum[:, 7, :].bitcast(BF16)]

                        def load_blk(b):
                            WG, wgk = wgr.next()
                            WD, wdk = wdr.next()
                            for k in range(8):
                                P.add('pool', lambda e, WG=WG, b=b, k=k: e.indirect_dma_start(out=WG[:, k, :], out_offset=None, in_=w_gu_rows[:, :],
                                                                                             in_offset=bass.IndirectOffsetOnAxis(ap=IDXG[:, b, k:k + 1], axis=0)),
                                      ['IDXG'], [wgk + str(k)], wgk + str(k))
                            for k in range(8):
                                P.add('pool', lambda e, WD=WD, b=b, k=k: e.indirect_dma_start(out=WD[:, k, :], out_offset=None, in_=w_dn_rows[:, :],
                                                                                             in_offset=bass.IndirectOffsetOnAxis(ap=IDXG[:, b, k:k + 1], axis=0)),
                                      ['IDXG'], [wdk + str(k)], wdk + str(k))
                            XB, xbk = xbr.next()
                            P.dma('sp', XB[:], XS[b * BS:(b + 1) * BS, :].rearrange("(s p) d -> p s d", p=128), [], [xbk], xbk)
                            swb, swk = swr.next()
                            P.dma('act', swb[:], SW[b * BS:(b + 1) * BS, :].rearrange("(s p) o -> p (s o)", p=128), [], [swk], swk, allow_slow_non_contiguous=True)
                            bb, bbk = bbr.next()
                            P.add('pool', lambda e, bb=bb, b=b: e.indirect_dma_start(out=bb[:, :], out_offset=None, in_=BGT[:, :],
                                                                                   in_offset=bass.IndirectOffsetOnAxis(ap=IDXB[:, b:b + 1], axis=0)),
                                  ['IDXB', 'BGT'], [bbk], bbk)
                            return WG, wgk, WD, wdk, XB, xbk, swb, swk, bb, bbk
                        nxt = load_blk(0)
                        for b in range(NB):
                            WG, wgk, WD, wdk, XB, xbk, swb, swk, bb, bbk = nxt
                            if b + 1 < NB:
                                nxt = load_blk(b + 1)
                            wgkeys = [wgk + str(k) for k in range(8)]
                            wdkeys = [wdk + str(k) for k in range(8)]
                            for half in range(2):
                                for s2 in range(2):
                                    sidx = half * 2 + s2
                                    for k in range(8):
                                        P.pe(lambda e, XB=XB, sidx=sidx, k=k, s2=s2: e.transpose(psb[s2][:, k * 128:(k + 1) * 128], XB[:, sidx, k * 128:(k + 1) * 128], identb[:]),
                                             [xbk, 'identb'], ['pX%d' % s2])
                                    P.act(lambda e, sidx=sidx, s2=s2: e.activation(out=XT[:, :, sidx * 128:(sidx + 1) * 128], in_=psb[s2].rearrange("p (k n) -> p k n", n=128), func=AF.Copy),
                                          ['pX%d' % s2], ['XT%d' % sidx])
                            xtkeys = ['XT%d' % q for q in range(4)]
                            at, atk = actr.next()
                            for c in range(8):
                                gb, gbk = gbr.next()
                                ub, ubk = ubr.next()
                                for k in range(8):
                                    P.pe(lambda e, gb=gb, WG=WG, k=k, c=c: e.matmul(psum[:, gb, :], WG[:, k, c * 128:(c + 1) * 128], XT[:, k, :], start=(k == 0), stop=(k == 7)),
                                         wgkeys + xtkeys, [gbk])
                                for k in range(8):
                                    P.pe(lambda e, ub=ub, WG=WG, k=k, c=c: e.matmul(psum[:, ub, :], WG[:, k, D + c * 128:D + (c + 1) * 128], XT[:, k, :], start=(k == 0), stop=(k == 7)),
                                         wgkeys + xtkeys, [ubk])
                                A, Ak = Ar.next(); S_, Sk = Sr.next(); U, Uk = Ur.next()
                                P.dve(lambda e, A=A, gb=gb, bb=bb, c=c: e.tensor_scalar(out=A[:], in0=psum[:, gb, :], scalar1=bb[:, c:c + 1], scalar2=7.0, op0=ALU.add, op1=ALU.min), [gbk, bbk], [Ak])
                                P.act(lambda e, A=A, S_=S_: e.activation(out=S_[:], in_=A[:], func=AF.Sigmoid, scale=1.702), [Ak], [Sk])
                                P.dve(lambda e, U=U, ub=ub, bb=bb, c=c: e.tensor_scalar(out=U[:], in0=psum[:, ub, :], scalar1=bb[:, 8 + c:9 + c], scalar2=8.0, op0=ALU.add, op1=ALU.min), [ubk, bbk], [Uk])
                                P.pool(lambda e, A=A, S_=S_: e.tensor_tensor(out=A[:], in0=A[:], in1=S_[:], op=ALU.mult), [Ak, Sk], [Ak])
                                P.dve(lambda e, at=at, c=c, U=U, A=A: e.scalar_tensor_tensor(out=at[:, c, :], in0=U[:], scalar=-6.0, in1=A[:], op0=ALU.max, op1=ALU.mult), [Uk, Ak], [atk + str(c)])
                            atkeys = [atk + str(c) for c in range(8)]
                            Y, Yk = Yr.next()
                            for s4 in range(4):
                                for nh in range(2):
                                    yb_, ybk = ybr.next()
                                    for c in range(8):
                                        P.pe(lambda e, yb_=yb_, at=at, c=c, s4=s4, WD=WD, nh=nh: e.matmul(psum[:, yb_, :], at[:, c, s4 * 128:(s4 + 1) * 128], WD[:, c, nh * 512:(nh + 1) * 512],
                                                                                                          start=(c == 0), stop=(c == 7)), atkeys + wdkeys, [ybk])
                                    if yb_ == 4:
                                        P.dve(lambda e, Y=Y, s4=s4, nh=nh, swb=swb: e.tensor_scalar_mul(out=Y[:, s4, nh * 512:(nh + 1) * 512], in0=psum[:, 4, :], scalar1=swb[:, s4:s4 + 1]),
                                              [ybk, swk], [Yk + '%d%d' % (s4, nh)])
                                    else:
                                        P.act(lambda e, Y=Y, s4=s4, nh=nh, swb=swb: e.activation(out=Y[:, s4, nh * 512:(nh + 1) * 512], in_=psum[:, 5, :], func=AF.Copy, scale=swb[:, s4:s4 + 1]),
                                              [ybk, swk], [Yk + '%d%d' % (s4, nh)])
                            P.dma('sp', YS[b * BS:(b + 1) * BS, :].rearrange("(s p) d -> p s d", p=128), Y[:], [Yk + '%d%d' % (q, r_) for q in range(4) for r_ in range(2)], [('YS', b)], Yk)
                        P.flush()
                    with contextlib.ExitStack() as st3:
                        def sb3(name, shape, dt=F32):
                            return st3.enter_context(nc.sbuf_tensor(uname(name), list(shape), dt))
                        g2 = [sb3("g2_%d" % s_, [128, D]) for s_ in range(2)]
                        for s_ in range(2):
                            bload('sp', g2[s_][:], modv[l, s_, 5 * D:6 * D], 'g2_%d' % s_)
                        lng = sb3("lng2", [128, D]); lnb = sb3("lnb2", [128, D])
                        bload('act', lng[:], ln_g[l, 1], 'lng')
                        bload('act', lnb[:], ln_b[l, 1], 'lnb')
                        x1r = Ring('xm', [sb3("xm%d" % i, [128, D]) for i in range(4)])
                        Gr = Ring('G', [sb3("G%d" % i, [128, 4, D]) for i in range(4)])
                        rr = sb3("rr2", [128, D])
                        xor_ = Ring('xo', [sb3("xo%d" % i, [128, D]) for i in range(2)])
                        st6 = sb3("st6b", [128, 2, 6]); mv = sb3("mvb", [128, 2]); rs = sb3("rsb", [128, 1])
                        bdr = sb3("bdr", [NE, D])
                        P.dma('act', bdr[:], b_dn[l], [], ['bdr'], 'bdr')
                        mwTr = Ring('mwT', [sb3("mwT%d" % i, [NE, 128]) for i in range(2)])
                        tbr = Ring('tb', [0, 1]); bbk2 = Ring('bd', [(2, 3), (4, 5)])
                        for j, i in enumerate(tiles):
                            s_ = 1 if i < NT_C else 0
                            tb_, tbk = tbr.next()
                            P.pe(lambda e, j=j, tb_=tb_: e.transpose(psum[0:NE, tb_, 0:128], mwd[:, j, :], ident[:]), ['ident'], [tbk])
                            mwT, mwTk = mwTr.next()
                            P.act(lambda e, mwT=mwT, tb_=tb_: e.activation(out=mwT[:], in_=psum[0:NE, tb_, 0:128], func=AF.Copy), [tbk], [mwTk])
                            (bd0, bd1), bdk = bbk2.next()
                            for nh, bdb_ in enumerate((bd0, bd1)):
                                P.pe(lambda e, mwT=mwT, nh=nh, bdb_=bdb_: e.matmul(psum[:, bdb_, :], mwT[:], bdr[:, nh * 512:(nh + 1) * 512], start=True, stop=True), [mwTk, 'bdr'], [bdk + str(nh)])
                            x1, x1k = x1r.next()
                            P.dma('sp', x1[:], XM[i * 128:(i + 1) * 128, :], [], [x1k], x1k)
                            G, Gk = Gr.next()
                            for k in range(4):
                                P.add('pool', lambda e, G=G, j=j, k=k: e.indirect_dma_start(out=G[:, k, :], out_offset=None, in_=YS[:, :],
                                                                                         in_offset=bass.IndirectOffsetOnAxis(ap=DKi[:, j, k:k + 1], axis=0)), ['DKi'], [Gk + str(k)], Gk + str(k))
                            P.dve(lambda e, G=G: e.tensor_tensor(out=G[:, 0, :], in0=G[:, 0, :], in1=G[:, 1, :], op=ALU.add), [Gk + '0', Gk + '1'], [Gk + '0'])
                            P.dve(lambda e, G=G: e.tensor_tensor(out=G[:, 2, :], in0=G[:, 2, :], in1=G[:, 3, :], op=ALU.add), [Gk + '2', Gk + '3'], [Gk + '2'])
                            P.dve(lambda e, G=G: e.tensor_tensor(out=G[:, 0, :], in0=G[:, 0, :], in1=G[:, 2, :], op=ALU.add), [Gk + '0', Gk + '2'], [Gk + '0'])
                            for nh, bdb_ in enumerate((bd0, bd1)):
                                P.dve(lambda e, G=G, nh=nh, bdb_=bdb_: e.tensor_tensor(out=G[:, 0, nh * 512:(nh + 1) * 512], in0=G[:, 0, nh * 512:(nh + 1) * 512], in1=psum[:, bdb_, :], op=ALU.add),
                                      [Gk + '0', bdk + str(nh)], [Gk + '0'])
                            P.dve(lambda e, G=G, s_=s_: e.tensor_tensor(out=rr[:], in0=G[:, 0, :], in1=g2[s_][:], op=ALU.mult), [Gk + '0', 'g2_%d' % s_], ['rr'])
                            P.dve(lambda e, x1=x1: e.scalar_tensor_tensor(out=rr[:], in0=x1[:], scalar=ALPHA, in1=rr[:], op0=ALU.mult, op1=ALU.add), [x1k, 'rr'], ['rr'])
                            for nh in range(2):
                                P.dve(lambda e, nh=nh: e.bn_stats(out=st6[:, nh, :], in_=rr[:, nh * 512:(nh + 1) * 512]), ['rr'], ['st6_%d' % nh])
                            P.dve(lambda e: e.bn_aggr(out=mv[:], in_=st6[:]), ['st6_0', 'st6_1'], ['mv'])
                            P.dve(lambda e: e.tensor_scalar_add(out=rs[:], in0=mv[:, 1:2], scalar1=EPS), ['mv'], ['rs'])
                            P.act(lambda e: e.activation(out=rs[:], in_=rs[:], func=AF.Sqrt), ['rs'], ['rs'])
                            P.dve(lambda e: e.reciprocal(out=rs[:], in_=rs[:]), ['rs'], ['rs'])
                            xo, xok = xor_.next()
                            P.dve(lambda e, xo=xo: e.tensor_scalar(out=xo[:], in0=rr[:], scalar1=mv[:, 0:1], scalar2=rs[:, 0:1], op0=ALU.subtract, op1=ALU.mult), ['rr', 'mv', 'rs'], [xok])
                            P.dve(lambda e, xo=xo: e.tensor_tensor(out=xo[:], in0=xo[:], in1=lng[:], op=ALU.mult), [xok, 'lng'], [xok])
                            P.dve(lambda e, xo=xo: e.tensor_tensor(out=xo[:], in0=xo[:], in1=lnb[:], op=ALU.add), [xok, 'lnb'], [xok])
                            dst = XS0[i * 128:(i + 1) * 128, :] if not last else out[(i - NT_C) * 128:(i - NT_C + 1) * 128, :]
                            P.dma('sp', dst, xo[:], [xok], [('xout', i)], xok)
                        P.flush()
        P.flush()
    return nc


def make_consts():
    inv = (10000.0 ** (-np.arange(0, 32, 2, dtype=np.float32) / 32.0)).astype(np.float32)
    t = np.arange(L)
    row = (t // 64).astype(np.float32)
    col = (t % 64).astype(np.float32)
    ang = np.stack([row[:, None] * inv, col[:, None] * inv], axis=1)
    cos = np.cos(ang).astype(np.float32)
    sin = np.sin(ang).astype(np.float32)
    c64 = np.stack([cos, cos], axis=2).reshape(L, 64)
    s64 = np.stack([-sin, sin], axis=2).reshape(L, 64)
    p = np.arange(128, dtype=np.float32)
    pos = np.stack([127 - p, p, p + 1, 128 - p], axis=1)
    dif = p[None, :] - p[:, None]
    return dict(
        c_ident=np.eye(128, dtype=np.float32),
        c_cos=np.ascontiguousarray(np.tile(c64, (1, 8))),
        c_sin=np.ascontiguousarray(np.tile(s64, (1, 8))),
        c_pos=np.ascontiguousarray(pos.astype(np.float32)),
        c_dpos=np.maximum(dif, 0).astype(np.float32),
        c_dneg=np.maximum(-dif, 0).astype(np.float32),
        c_mge=(dif >= 0).astype(np.float32),
        c_zeros=np.zeros((2, 256), np.float32),
        c_iota32=np.tile(np.arange(NE, dtype=np.float32)[None, :], (128, 1)),
        c_lts=(p[:, None] < p[None, :]).astype(np.float32),
        c_bstart=np.tile((np.arange(NBMAX, dtype=np.float32) * BS)[None, :], (128, 1)),
        c_kp=(np.arange(8, dtype=np.float32)[None, :] * 128 + p[:, None]).astype(np.float32),
        c_iotap=p[:, None].astype(np.float32).copy(),
    )


WKEYS = ['w_mod', 'b_mod', 'w_in', 'conv_w', 'ret_decay_exp', 'ret_gn_g', 'q_norm_g', 'k_norm_g', 'w_out',
         'ln_g', 'ln_b', 'w_router', 'b_router', 'w_gate_up', 'b_gate_up', 'w_down', 'b_down']


def make_in_maps(inputs, cores, skip=()):
    consts = make_consts()
    shared = {k: np.ascontiguousarray(np.asarray(inputs[k], np.float32)) for k in WKEYS if k not in skip}
    shared['c_ctx'] = np.ascontiguousarray(np.asarray(inputs['c_ctx'], np.float32))
    shared.update(consts)
    maps = []
    for b in cores:
        m = dict(shared)
        m['x'] = np.ascontiguousarray(np.asarray(inputs['x'][b], np.float32))
        m['c'] = np.ascontiguousarray(np.asarray(inputs['c'][b], np.float32))
        m['ctx'] = np.ascontiguousarray(np.asarray(inputs['ctx'][b], np.float32))
        maps.append(m)
    return maps


def kernel(**inputs):
    nc = build()
    maps = make_in_maps(inputs, list(range(8)))
    res = run_bass_kernel_spmd(nc, maps, core_ids=list(range(8)))
    return np.stack([np.asarray(r["out"], np.float32) for r in res.results], axis=0)
```

```python
import contextlib
import math
import numpy as np
import concourse.bass as bass
import concourse.mybir as mybir
from concourse.bass_utils import run_bass_kernel_spmd

F32 = mybir.dt.float32
BF16 = mybir.dt.bfloat16
ALU = mybir.AluOpType
AF = mybir.ActivationFunctionType
AX = mybir.AxisListType

D = 1024
L = 4096
LC = 256
NT_C = 2
NT = 34
DEPTH = 2
NE = 32
BS = 512
NBMAX = 66
I32 = mybir.dt.int32
U32 = mybir.dt.uint32
ALPHA = (2.0 * DEPTH) ** 0.25
EPS = 1e-6


class Prog:
    ENGS = ('pe', 'act', 'dve', 'pool', 'sp')

    def __init__(self, nc, stack):
        self.nc = nc
        self.ops = []
        self.sems = {}
        self.stack = stack
        for eng in ('pe', 'act', 'dve', 'pool'):
            self.sems[('e', eng)] = stack.enter_context(nc.semaphore('sem_' + eng))
        self.cnt = {}
        self.streams = {}
        self.pool_cnt = []
        self.waited = {e: {} for e in self.ENGS}
        self.total_ops = 0

    def add(self, eng, fn, reads=(), writes=(), stream=None):
        self.ops.append((eng, fn, tuple(reads), tuple(writes), stream))

    def pe(self, fn, reads=(), writes=()):
        self.add('pe', fn, reads, writes)

    def act(self, fn, reads=(), writes=()):
        self.add('act', fn, reads, writes)

    def dve(self, fn, reads=(), writes=()):
        self.add('dve', fn, reads, writes)

    def pool(self, fn, reads=(), writes=()):
        self.add('pool', fn, reads, writes)

    def dma(self, q, out, in_, reads, writes, stream, **kw):
        self.add(q, lambda e: e.dma_start(out=out, in_=in_, **kw), reads, writes, stream)

    def flush(self):
        nc = self.nc
        ops = self.ops
        self.ops = []
        n = len(ops)
        if n == 0:
            return
        self.total_ops += n
        last_writer = {}
        readers = {}
        deps = [None] * n
        for i, (eng, fn, rd, wr, st) in enumerate(ops):
            d = set()
            for r in rd:
                j = last_writer.get(r)
                if j is not None:
                    d.add((j, 0))
            for w in wr:
                j = last_writer.get(w)
                if j is not None:
                    d.add((j, 1))
                for k in readers.get(w, ()):
                    if k != i:
                        d.add((k, 2))
            deps[i] = d
            for r in rd:
                readers.setdefault(r, []).append(i)
            for w in wr:
                last_writer[w] = i
                readers[w] = []
        sig = [False] * n
        need = [None] * n
        last_compute = {}
        for i in range(n):
            eng = ops[i][0]
            lst = set()
            for (j, kind) in deps[i]:
                jeng, _, _, _, jst = ops[j]
                if jst is None and jeng == eng:
                    if eng == 'pe' or eng == 'sp':
                        continue
                    if kind == 2:
                        continue
                if jst is None:
                    sig[j] = True
                lst.add(j)
            need[i] = lst
            if ops[i][4] is None and ops[i][1] is not None:
                last_compute[eng] = i
        for eng, i in last_compute.items():
            if eng != 'sp':
                sig[i] = True
        sval = [None] * n
        phase_map = {}
        for i, (eng, fn, rd, wr, st) in enumerate(ops):
            if st is not None:
                if st not in phase_map:
                    k = len(phase_map)
                    phase_map[st] = k
                    if k >= len(self.pool_cnt):
                        self.pool_cnt.append(0)
                        self.sems[('s', k)] = self.stack.enter_context(nc.semaphore('sd_%d' % k))
                k = phase_map[st]
                self.pool_cnt[k] += 1
                sval[i] = (('s', k), 16 * self.pool_cnt[k])
            elif sig[i]:
                self.cnt[eng] = self.cnt.get(eng, 0) + 1
                sval[i] = (('e', eng), self.cnt[eng])
        per_eng = {e: [] for e in self.ENGS}
        for i, op in enumerate(ops):
            per_eng[op[0]].append(i)
        sems = self.sems
        final = {}
        for eng in ('pe', 'act', 'dve', 'pool'):
            if self.cnt.get(eng, 0) > 0:
                final[('e', eng)] = self.cnt[eng]
        for k, c in enumerate(self.pool_cnt):
            final[('s', k)] = 16 * c

        def run(engname, e):
            waited = self.waited[engname]
            for i in per_eng[engname]:
                _, fn, rd, wr, st = ops[i]
                w = {}
                for j in need[i]:
                    key, val = sval[j]
                    if w.get(key, 0) < val:
                        w[key] = val
                for key, val in w.items():
                    if waited.get(key, 0) >= val:
                        continue
                    waited[key] = val
                    e.wait_ge(sems[key], val)
                if fn is None:
                    continue
                ins = fn(e)
                if sval[i] is not None:
                    key, val = sval[i]
                    ins.then_inc(sems[key], 16 if key[0] == 's' else 1)
            for key, val in final.items():
                if key == ('e', engname):
                    continue
                if waited.get(key, 0) >= val:
                    continue
                waited[key] = val
                e.wait_ge(sems[key], val)

        with nc.Block() as block:
            @block.tensor
            def _(e):
                run('pe', e)

            @block.scalar
            def _(e):
                run('act', e)

            @block.vector
            def _(e):
                run('dve', e)

            @block.gpsimd
            def _(e):
                run('pool', e)

            @block.sync
            def _(e):
                run('sp', e)


class Ring:
    def __init__(self, name, tiles):
        self.name = name
        self.tiles = tiles
        self.i = 0

    def next(self):
        k = self.i % len(self.tiles)
        self.i += 1
        return self.tiles[k], '%s%d' % (self.name, k)


SEC = dict(u=(0, 256), B=(256, 256), C=(512, 256), rq=(768, 256), rk=(1024, 256), rv=(1280, 256),
           rg=(1536, 256), aq=(1792, 512), ak=(2304, 128), av=(2432, 128))
MYCOL = dict(u=0, C=256, B=512, rv=768, rq=1024, rk=1280, aq=1536, rg=2048, ak=2304, av=2432)


def build(phases=('p0', 'p1', 'att', 'ret', 'p3', 'moe'), layers=(0, 1), debug=False, moe_experts=NE, cut=99):
    nc = bass.Bass("TRN2", target_bir_lowering=False)

    _uc = [0]

    def uname(name):
        _uc[0] += 1
        return '%s_u%d' % (name, _uc[0])

    def din(name, shape, dt=F32):
        return nc.dram_tensor(name, list(shape), dt, kind="ExternalInput").ap()

    def dscr(name, shape, dt=F32):
        return nc.dram_tensor(name, list(shape), dt, kind=("ExternalOutput" if debug else "Internal")).ap()

    x_in = din("x", [L, D])
    c_in = din("c", [D])
    ctx_in = din("ctx", [LC, D])
    cctx_in = din("c_ctx", [D])
    w_mod = din("w_mod", [DEPTH, D, 6 * D])
    b_mod = din("b_mod", [DEPTH, 6 * D])
    w_in = din("w_in", [DEPTH, D, 2560])
    conv_w = din("conv_w", [DEPTH, 256, 3])
    rde = din("ret_decay_exp", [DEPTH, 2, 4])
    gn_g = din("ret_gn_g", [DEPTH, 256])
    qn_g = din("q_norm_g", [DEPTH, 64])
    kn_g = din("k_norm_g", [DEPTH, 64])
    w_out = din("w_out", [DEPTH, D, D])
    ln_g = din("ln_g", [DEPTH, 2, D])
    ln_b = din("ln_b", [DEPTH, 2, D])
    w_router = din("w_router", [DEPTH, D, NE])
    b_router = din("b_router", [DEPTH, NE])
    if 'moe' in phases:
        w_gu = din("w_gate_up", [DEPTH, NE, D, 2 * D])
        b_gu = din("b_gate_up", [DEPTH, NE, 2 * D])
        w_dn = din("w_down", [DEPTH, NE, D, D])
        b_dn = din("b_down", [DEPTH, NE, D])
    ident_in = din("c_ident", [128, 128])
    cos_in = din("c_cos", [L, 512])
    sin_in = din("c_sin", [L, 512])
    pos_in = din("c_pos", [128, 4])
    dpos_in = din("c_dpos", [128, 128])
    dneg_in = din("c_dneg", [128, 128])
    mge_in = din("c_mge", [128, 128])
    zeros_in = din("c_zeros", [2, 256])
    iota32_in = din("c_iota32", [128, NE])
    lts_in = din("c_lts", [128, 128])
    bstart_in = din("c_bstart", [128, NBMAX])
    kp_in = din("c_kp", [128, 8])
    iotap_in = din("c_iotap", [128, 1])

    out = nc.dram_tensor("out", [L, D], F32, kind="ExternalOutput").ap()

    N = NT * 128
    modv = dscr("modv", [DEPTH, 2, 6 * D])
    PB = dscr("PB", [N + 4, 512])
    RQT = dscr("RQT", [3, 4, 64, N], BF16)
    RKT = dscr("RKT", [4, 64, N], BF16)
    RTOK = dscr("RTOK", [N, 768], BF16)
    RG = dscr("RG", [N, 256])
    AQT = dscr("AQT", [8, 64, N], BF16)
    AKT = dscr("AKT", [2, 64, N], BF16)
    AV = dscr("AV", [N, 128], BF16)
    MIX = dscr("MIX", [N, 1024])
    XS0 = dscr("XS0", [N, D])
    XM = dscr("XM", [N, D])
    H2R = dscr("H2R", [N, D], BF16)
    RW = dscr("RW", [N, 4])
    RE = dscr("RE", [N, 4])
    XS = dscr("XS", [NBMAX * BS, D], BF16)
    SW = dscr("SW", [NBMAX * BS, 1])
    YS = dscr("YS", [NBMAX * BS, D])
    BGT = dscr("BGT", [DEPTH * NE * 128, 16])

    def prow(i):
        return 1 + i * 128 if i < NT_C else 259 + (i - NT_C) * 128

    with contextlib.ExitStack() as gst:
        P = Prog(nc, gst)
        psum = gst.enter_context(nc.psum_tensor("psum", [128, 8, 512], F32))
        ident = gst.enter_context(nc.sbuf_tensor("ident", [128, 128], F32))
        P.dma('sp', ident[:], ident_in, [], ['ident'], 'ident')

        def bank(b):
            return psum[:, b, :]

        if 'p0' in phases:
            with contextlib.ExitStack() as st:
                def sb(name, shape, dt=F32):
                    return st.enter_context(nc.sbuf_tensor(uname(name), list(shape), dt))
                cnd = sb("cnd", [128, 2, 8])
                P.dma('sp', cnd[:, 0, :], c_in.rearrange("(k p) -> p k", p=128), [], ['cnd'], 'cnd0',
                      allow_slow_non_contiguous=True)
                P.dma('sp', cnd[:, 1, :], cctx_in.rearrange("(k p) -> p k", p=128), [], ['cnd'], 'cnd1',
                      allow_slow_non_contiguous=True)
                P.act(lambda e: e.activation(out=cnd[:], in_=cnd[:], func=AF.Silu), ['cnd'], ['cnd'])
                wring = Ring('wm', [sb("wm%d" % i, [128, 8, 512]) for i in range(6)])
                bring = Ring('bm', [sb("bm%d" % i, [1, 512]) for i in range(2)])
                oring = Ring('om', [sb("om%d" % i, [1, 2, 512]) for i in range(2)])
                for l in layers:
                    for n in range(12):
                        wt, wk = wring.next()
                        bt, bk = bring.next()
                        ot, ok = oring.next()
                        P.dma('sp' if n % 2 == 0 else 'act', wt[:], w_mod[l, :, n * 512:(n + 1) * 512].rearrange("(k p) n -> p k n", p=128),
                              [], [wk], wk)
                        P.dma('sp', bt[:], b_mod[l, n * 512:(n + 1) * 512].rearrange("(o n) -> o n", o=1), [], [bk], bk)
                        for s in range(2):
                            pb_ = 'ps0_%d' % s
                            for k in range(8):
                                P.pe(lambda e, s=s, k=k, wt=wt: e.matmul(psum[0:1, s, :], cnd[:, s, k:k + 1], wt[:, k, :],
                                                                     start=(k == 0), stop=(k == 7)),
                                     ['cnd', wk], [pb_])
                            P.dve(lambda e, s=s, ot=ot, bt=bt: e.tensor_tensor(out=ot[:, s, :], in0=psum[0:1, s, :],
                                                                               in1=bt[:], op=ALU.add),
                                  [pb_, bk], [ok])
                        if n in (2, 3, 8, 9):
                            P.dve(lambda e, ot=ot: e.tensor_scalar_add(out=ot[:], in0=ot[:], scalar1=1.0), [ok], [ok])
                        P.dma('sp', modv[l, :, n * 512:(n + 1) * 512].rearrange("(o s) n -> o s n", o=1), ot[:],
                              [ok], [('modv', l)], ok)
                P.flush()

        def bload(q, tile_ap, vec_ap, key, reads=()):
            P.dma(q, tile_ap, vec_ap.partition_broadcast(128), list(reads), [key], key)

        for l in layers:
            last = (l == DEPTH - 1)
            xsrc = (lambda i: (ctx_in[i * 128:(i + 1) * 128, :] if i < NT_C else x_in[(i - NT_C) * 128:(i - NT_C + 1) * 128, :])) \
                if l == 0 else (lambda i: XS0[i * 128:(i + 1) * 128, :])
            xs_key = (lambda i: ('xin', i)) if l == 0 else (lambda i: ('XS0', i))
            out_tiles = list(range(NT)) if not last else list(range(NT_C, NT))

            if 'p1' in phases:
                with contextlib.ExitStack() as st:
                    def sb(name, shape, dt=F32):
                        return st.enter_context(nc.sbuf_tensor(uname(name), list(shape), dt))
                    win = sb("win", [128, 8, 2560], BF16)
                    for name, (c0, w) in SEC.items():
                        m0 = MYCOL[name]
                        P.dma('pool', win[:, :, m0:m0 + w], w_in[l, :, c0:c0 + w].rearrange("(k p) n -> p k n", p=128),
                              [], ['win_' + name], 'win_' + name)
                    winkeys = ['win_' + k for k in SEC]
                    sc1 = [sb("sc1_%d" % s, [128, D]) for s in range(2)]
                    sh1 = [sb("sh1_%d" % s, [128, D]) for s in range(2)]
                    for s in range(2):
                        bload('sp', sc1[s][:], modv[l, s, D:2 * D], 'sc1_%d' % s, [('modv', l)])
                        bload('sp', sh1[s][:], modv[l, s, 0:D], 'sh1_%d' % s, [('modv', l)])
                    gq = sb("gq", [128, 64])
                    gk = sb("gk", [128, 64])
                    bload('act', gq[:], qn_g[l], 'gq')
                    bload('act', gk[:], kn_g[l], 'gk')
                    zpad = sb("zpad", [2, 256])
                    P.dma('act', zpad[:], zeros_in, [], ['zpad'], 'zpad')
                    for r0 in (0, 257):
                        P.dma('act', PB[r0:r0 + 2, 0:256] if r0 else PB[0:1, 0:256], zpad[0:2, :] if r0 else zpad[0:1, :],
                              ['zpad'], [('PBpad', r0)], 'zp%d' % r0)
                    P.dma('act', PB[N + 3:N + 4, 0:256], zpad[0:1, :], ['zpad'], [('PBpad', 3)], 'zp3')
                    posc = sb("posc", [128, 4])
                    P.dma('act', posc[:], pos_in, [], ['posc'], 'posc')
                    lg = sb("lg", [128, 8])
                    bload('act', lg[:], rde[l].rearrange("a h -> (a h)"), 'lg')
                    P.act(lambda e: e.activation(out=lg[:], in_=lg[:], func=AF.Exp, scale=-math.log(2.0)), ['lg'], ['lg'])
                    P.act(lambda e: e.activation(out=lg[:], in_=lg[:], func=AF.Ln, scale=-1.0, bias=1.0), ['lg'], ['lg'])
                    tab4 = sb("tab4", [128, 4, 4])
                    for ti, (di, pc) in enumerate(((0, 0), (1, 1), (0, 2), (1, 3))):
                        P.act(lambda e, ti=ti, di=di, pc=pc: e.activation(out=tab4[:, ti, :], in_=lg[:, di * 4:(di + 1) * 4],
                                                                          func=AF.Exp, scale=posc[:, pc:pc + 1]),
                              ['lg', 'posc'], ['tab4'])
                    P.dve(lambda e: e.tensor_scalar_mul(out=tab4[:, 0:2, :], in0=tab4[:, 0:2, :], scalar1=0.125), ['tab4'], ['tab4'])
                    TAB = sb("TAB", [128, 4, 4, 64])
                    P.dve(lambda e: e.tensor_copy(out=TAB[:].rearrange("p t h d -> p (t h) d"),
                                                  in_=tab4[:].rearrange("p t h -> p (t h)").unsqueeze(2).to_broadcast([128, 16, 64])),
                          ['tab4'], ['TAB'])
                    gq8 = sb("gq8", [128, 8, 64])
                    P.dve(lambda e: e.tensor_copy(out=gq8[:], in_=gq[:].unsqueeze(1).to_broadcast([128, 8, 64])), ['gq'], ['gq8'])
                    gk2 = sb("gk2", [128, 2, 64])
                    P.dve(lambda e: e.tensor_copy(out=gk2[:], in_=gk[:].unsqueeze(1).to_broadcast([128, 2, 64])), ['gk'], ['gk2'])

                    xring = Ring('xt', [sb("xt%d" % i, [128, D]) for i in range(3)])
                    csring = Ring('cs', [sb("cs%d" % i, [128, 512]) for i in range(3)])
                    snring = Ring('sn', [sb("sn%d" % i, [128, 512]) for i in range(3)])
                    hring = Ring('h', [sb("h%d" % i, [128, D]) for i in range(2)])
                    hTring = Ring('hT', [sb("hT%d" % i, [128, 8, 128], BF16) for i in range(2)])
                    usb = sb("usb", [128, 256])
                    pcb = Ring('pcb', [sb("pcb%d" % i, [128, 512]) for i in range(2)])
                    t1 = sb("t1", [128, 512])
                    t2 = sb("t2", [128, 512])
                    rq = sb("rq", [128, 4, 256])
                    rtok = Ring('rtok', [sb("rtok%d" % i, [128, 768], BF16) for i in range(2)])
                    rgt = Ring('rgt', [sb("rgt%d" % i, [128, 256]) for i in range(2)])
                    rT = Ring('rT', [sb("rT%d" % i, [128, 8, 128], BF16) for i in range(2)])
                    sq = sb("sq", [128, 512])
                    ss = sb("ss", [128, 8])
                    aq = sb("aq", [128, 512])
                    aqn = sb("aqn", [128, 512])
                    akn = sb("akn", [128, 128])
                    ak = sb("ak", [128, 128])
                    aT = Ring('aT', [sb("aT%d" % i, [128, 5, 128], BF16) for i in range(2)])
                    avt = Ring('avt', [sb("avt%d" % i, [128, 128], BF16) for i in range(2)])

                    def rope(src_ap, W, dst_ap, src_keys, dst_key, cs, ck, sn, sk):
                        g = W // 32
                        P.dve(lambda e: e.tensor_tensor(out=t1[:, :W], in0=src_ap, in1=cs[:, :W], op=ALU.mult),
                              src_keys + [ck], ['t1'])
                        s4 = src_ap.rearrange("p (g a f) -> p g a f", a=2, f=16)
                        t4 = t2[:, :W].rearrange("p (g a f) -> p g a f", a=2, f=16)
                        n4 = sn[:, :W].rearrange("p (g a f) -> p g a f", a=2, f=16)
                        P.dve(lambda e: e.tensor_tensor(out=t4[:, :, 0, :], in0=s4[:, :, 1, :], in1=n4[:, :, 0, :], op=ALU.mult),
                              src_keys + [sk], ['t2a'])
                        P.dve(lambda e: e.tensor_tensor(out=t4[:, :, 1, :], in0=s4[:, :, 0, :], in1=n4[:, :, 1, :], op=ALU.mult),
                              src_keys + [sk], ['t2b'])
                        P.pool(lambda e: e.tensor_tensor(out=dst_ap, in0=t1[:, :W], in1=t2[:, :W], op=ALU.add),
                               ['t1', 't2a', 't2b'], [dst_key])

                    ld = {}

                    def issue_loads(i):
                        xt, xk = xring.next()
                        P.dma('sp', xt[:], xsrc(i), [xs_key(i)], [xk], xk)
                        if i >= NT_C:
                            cs, ck = csring.next()
                            sn, sk = snring.next()
                            t0 = (i - NT_C) * 128
                            P.dma('sp', cs[:], cos_in[t0:t0 + 128, :], [], [ck], ck)
                            P.dma('sp', sn[:], sin_in[t0:t0 + 128, :], [], [sk], sk)
                            ld[i] = (xt, xk, cs, ck, sn, sk)
                        else:
                            ld[i] = (xt, xk, None, None, None, None)
                    issue_loads(0)
                    for i in range(NT):
                        isctx = i < NT_C
                        s = 1 if isctx else 0
                        if i + 1 < NT:
                            issue_loads(i + 1)
                        xt, xk, cs, ck, sn, sk = ld.pop(i)
                        h, hk = hring.next()
                        P.dve(lambda e, h=h, xt=xt, s=s: e.tensor_tensor(out=h[:], in0=xt[:], in1=sc1[s][:], op=ALU.mult),
                              [xk, 'sc1_%d' % s], [hk])
                        P.dve(lambda e, h=h, s=s: e.tensor_tensor(out=h[:], in0=h[:], in1=sh1[s][:], op=ALU.add),
                              [hk, 'sh1_%d' % s], [hk])
                        for k in range(8):
                            P.pe(lambda e, h=h, k=k: e.transpose(psum[:, 5 + k // 4, (k % 4) * 128:(k % 4 + 1) * 128],
                                                                 h[:, k * 128:(k + 1) * 128], ident[:]),
                                 [hk, 'ident'], ['pT%d' % k])
                        hT, hTk = hTring.next()
                        for hh in range(2):
                            P.act(lambda e, hT=hT, hh=hh: e.activation(out=hT[:, hh * 4:(hh + 1) * 4, :].rearrange("p k n -> p (k n)"),
                                                                       in_=psum[:, 5 + hh, :], func=AF.Copy),
                                  ['pT%d' % k for k in range(hh * 4, hh * 4 + 4)], [hTk + '_%d' % hh])
                        for b in range(5):
                            for k in range(8):
                                P.pe(lambda e, hT=hT, b=b, k=k: e.matmul(bank(b), hT[:, k, :], win[:, k, b * 512:(b + 1) * 512],
                                                                         start=(k == 0), stop=(k == 7)),
                                     [hTk + '_%d' % (k // 4)] + winkeys, ['z%d' % b])
                        pc, pck = pcb.next()
                        P.act(lambda e: e.activation(out=usb[:], in_=psum[:, 0, 0:256], func=AF.Copy), ['z0'], ['usb'])
                        P.dve(lambda e, pc=pc: e.tensor_tensor(out=pc[:, 0:256], in0=psum[:, 0, 256:512], in1=usb[:], op=ALU.mult),
                              ['z0', 'usb'], [pck + 'a'])
                        P.act(lambda e, pc=pc: e.activation(out=pc[:, 256:512], in_=psum[:, 1, 0:256], func=AF.Copy), ['z1'], [pck + 'b'])
                        r0 = prow(i)
                        P.dma('sp', PB[r0:r0 + 128, :], pc[:], [pck + 'a', pck + 'b'], [('PB', i)], pck)
                        rt, rtk = rtok.next()
                        P.act(lambda e, rt=rt: e.activation(out=rt[:, 512:768], in_=psum[:, 1, 256:512], func=AF.Copy), ['z1'], [rtk + 'v'])
                        if isctx:
                            P.act(lambda e: e.activation(out=rq[:, 0, :], in_=psum[:, 2, 0:256], func=AF.Copy), ['z2'], ['rq0'])
                            P.act(lambda e: e.activation(out=rq[:, 3, :], in_=psum[:, 2, 256:512], func=AF.Copy), ['z2'], ['rq3'])
                        else:
                            rope(psum[:, 2, 0:256], 256, rq[:, 0, :], ['z2'], 'rq0', cs, ck, sn, sk)
                            rope(psum[:, 2, 256:512], 256, rq[:, 3, :], ['z2'], 'rq3', cs, ck, sn, sk)
                        TABf = TAB[:].rearrange("p t h d -> p t (h d)")
                        P.dve(lambda e: e.tensor_tensor(out=rq[:, 1, :], in0=rq[:, 0, :], in1=TABf[:, 2, :], op=ALU.mult), ['rq0', 'TAB'], ['rq1'])
                        P.pool(lambda e: e.tensor_tensor(out=rq[:, 2, :], in0=rq[:, 0, :], in1=TABf[:, 3, :], op=ALU.mult), ['rq0', 'TAB'], ['rq2'])
                        P.dve(lambda e, rt=rt: e.tensor_tensor(out=rt[:, 0:256], in0=rq[:, 3, :], in1=TABf[:, 0, :], op=ALU.mult), ['rq3', 'TAB'], [rtk + 'f'])
                        P.pool(lambda e, rt=rt: e.tensor_tensor(out=rt[:, 256:512], in0=rq[:, 3, :], in1=TABf[:, 1, :], op=ALU.mult), ['rq3', 'TAB'], [rtk + 'b'])
                        P.dma('sp', RTOK[i * 128:(i + 1) * 128, :], rt[:], [rtk + 'v', rtk + 'f', rtk + 'b'], [('RTOK', i)], rtk)
                        rg_, rgk = rgt.next()
                        P.act(lambda e, rg_=rg_: e.activation(out=rg_[:], in_=psum[:, 4, 0:256], func=AF.Silu), ['z4'], [rgk])
                        P.dma('act', RG[i * 128:(i + 1) * 128, :], rg_[:], [rgk], [('RG', i)], rgk)
                        for t in range(4):
                            for c2 in range(2):
                                idx = t * 2 + c2
                                P.pe(lambda e, t=t, c2=c2, idx=idx: e.transpose(psum[:, 5 + idx // 4, (idx % 4) * 128:(idx % 4 + 1) * 128],
                                                                                rq[:, t, c2 * 128:(c2 + 1) * 128], ident[:]),
                                     ['rq%d' % t, 'ident'], ['pT%d' % idx])
                        rTt, rTk = rT.next()
                        for hh in range(2):
                            if hh == 0:
                                P.act(lambda e, rTt=rTt: e.activation(out=rTt[:, 0:4, :].rearrange("p k n -> p (k n)"), in_=psum[:, 5, :], func=AF.Copy),
                                      ['pT0', 'pT1', 'pT2', 'pT3'], [rTk + 'a'])
                            else:
                                P.act(lambda e, rTt=rTt: e.activation(out=rTt[:, 4:6, :].rearrange("p k n -> p (k n)"), in_=psum[:, 6, 0:256], func=AF.Copy),
                                      ['pT4', 'pT5'], [rTk + 'b'])
                                P.act(lambda e, rTt=rTt: e.activation(out=rTt[:, 6:8, :].rearrange("p k n -> p (k n)"), in_=psum[:, 6, 256:512], func=AF.Copy, scale=0.125),
                                      ['pT6', 'pT7'], [rTk + 'c'])
                        for t in range(3):
                            P.dma('act', RQT[t, :, :, i * 128:(i + 1) * 128].rearrange("(c q) d n -> (q d) c n", q=2),
                                  rTt[:, 2 * t:2 * t + 2, :], [rTk + 'a', rTk + 'b'], [('RQT', i, t)], rTk + 'q%d' % t)
                        P.dma('act', RKT[:, :, i * 128:(i + 1) * 128].rearrange("(c q) d n -> (q d) c n", q=2),
                              rTt[:, 6:8, :], [rTk + 'c'], [('RKT', i)], rTk + 'k')
                        P.act(lambda e: e.activation(out=sq[:], in_=psum[:, 3, :], func=AF.Square), ['z3'], ['sq'])
                        P.dve(lambda e: e.tensor_reduce(out=ss[:], in_=sq[:].rearrange("p (h d) -> p h d", d=64), axis=AX.X, op=ALU.add), ['sq'], ['ss'])
                        P.dve(lambda e: e.tensor_scalar(out=ss[:], in0=ss[:], scalar1=1.0 / 64, scalar2=EPS, op0=ALU.mult, op1=ALU.add), ['ss'], ['ss'])
                        P.act(lambda e: e.activation(out=ss[:], in_=ss[:], func=AF.Sqrt), ['ss'], ['ss'])
                        P.dve(lambda e: e.reciprocal(out=ss[:], in_=ss[:]), ['ss'], ['ss'])
                        P.dve(lambda e: e.tensor_tensor(out=aqn[:].rearrange("p (h d) -> p h d", d=64), in0=psum[:, 3, :].rearrange("p (h d) -> p h d", d=64),
                                                        in1=ss[:].unsqueeze(2).to_broadcast([128, 8, 64]), op=ALU.mult), ['z3', 'ss'], ['aqn'])
                        P.pool(lambda e: e.tensor_tensor(out=aqn[:], in0=aqn[:], in1=gq8[:].rearrange("p h d -> p (h d)"), op=ALU.mult), ['aqn', 'gq8'], ['aqn'])
                        if isctx:
                            aq_src, aq_key = aqn, 'aqn'
                        else:
                            rope(aqn[:], 512, aq[:], ['aqn'], 'aq', cs, ck, sn, sk)
                            aq_src, aq_key = aq, 'aq'
                        av_, avk = avt.next()
                        P.act(lambda e, av_=av_: e.activation(out=av_[:], in_=psum[:, 4, 384:512], func=AF.Copy), ['z4'], [avk])
                        P.dma('act', AV[i * 128:(i + 1) * 128, :], av_[:], [avk], [('AV', i)], avk)
                        P.act(lambda e: e.activation(out=sq[:, 0:128], in_=psum[:, 4, 256:384], func=AF.Square), ['z4'], ['sqk'])
                        P.dve(lambda e: e.tensor_reduce(out=ss[:, 0:2], in_=sq[:, 0:128].rearrange("p (h d) -> p h d", d=64), axis=AX.X, op=ALU.add), ['sqk'], ['ssk'])
                        P.dve(lambda e: e.tensor_scalar(out=ss[:, 0:2], in0=ss[:, 0:2], scalar1=1.0 / 64, scalar2=EPS, op0=ALU.mult, op1=ALU.add), ['ssk'], ['ssk'])
                        P.act(lambda e: e.activation(out=ss[:, 0:2], in_=ss[:, 0:2], func=AF.Sqrt), ['ssk'], ['ssk'])
                        P.dve(lambda e: e.reciprocal(out=ss[:, 0:2], in_=ss[:, 0:2]), ['ssk'], ['ssk'])
                        P.dve(lambda e: e.tensor_tensor(out=akn[:].rearrange("p (h d) -> p h d", d=64), in0=psum[:, 4, 256:384].rearrange("p (h d) -> p h d", d=64),
                                                        in1=ss[:, 0:2].unsqueeze(2).to_broadcast([128, 2, 64]), op=ALU.mult), ['z4', 'ssk'], ['akn'])
                        P.pool(lambda e: e.tensor_tensor(out=akn[:], in0=akn[:], in1=gk2[:].rearrange("p h d -> p (h d)"), op=ALU.mult), ['akn', 'gk2'], ['akn'])
                        if isctx:
                            ak_src, ak_key = akn, 'akn'
                        else:
                            rope(akn[:], 128, ak[:], ['akn'], 'ak', cs, ck, sn, sk)
                            ak_src, ak_key = ak, 'ak'
                        for c4 in range(4):
                            P.pe(lambda e, c4=c4, aq_src=aq_src: e.transpose(psum[:, 7, c4 * 128:(c4 + 1) * 128], aq_src[:, c4 * 128:(c4 + 1) * 128], ident[:]),
                                 [aq_key, 'ident'], ['pA%d' % c4])
                        P.pe(lambda e, ak_src=ak_src: e.transpose(psum[:, 5, 0:128], ak_src[:, 0:128], ident[:]), [ak_key, 'ident'], ['pT0'])
                        aTt, aTk = aT.next()
                        P.act(lambda e, aTt=aTt: e.activation(out=aTt[:, 0:4, :].rearrange("p k n -> p (k n)"), in_=psum[:, 7, :], func=AF.Copy),
                              ['pA0', 'pA1', 'pA2', 'pA3'], [aTk + 'q'])
                        P.act(lambda e, aTt=aTt: e.activation(out=aTt[:, 4, :], in_=psum[:, 5, 0:128], func=AF.Copy), ['pT0'], [aTk + 'k'])
                        P.dma('sp', AQT[:, :, i * 128:(i + 1) * 128].rearrange("(c q) d n -> (q d) c n", q=2), aTt[:, 0:4, :],
                              [aTk + 'q'], [('AQT', i)], aTk + 'q')
                        P.dma('sp', AKT[:, :, i * 128:(i + 1) * 128].rearrange("q d n -> (q d) n"), aTt[:, 4, :],
                              [aTk + 'k'], [('AKT', i)], aTk + 'k')
                    P.flush()


            if 'att' in phases:
                with contextlib.ExitStack() as st:
                    def sb(name, shape, dt=F32):
                        return st.enter_context(nc.sbuf_tensor(uname(name), list(shape), dt))
                    KT = sb("KT", [128, 2, N], BF16)
                    P.pool(lambda e: e.memset(KT[64:128, :, :], 0.0), [], ['KTz'])
                    P.dma('sp', KT[0:64, :, :], AKT.rearrange("k d n -> d k n"), [], ['KT'], 'KT')
                    V1 = sb("V1", [128, NT, 2, 65], BF16)
                    P.pool(lambda e: e.memset(V1[:], 1.0), [], ['V1'])
                    for kk_ in range(2):
                        P.dma('act', V1[:, :, kk_, 0:64], AV[:, kk_ * 64:(kk_ + 1) * 64].rearrange("(c p) d -> p c d", p=128), [], ['V1'], 'V1_%d' % kk_)
                    qring = Ring('QT', [sb("QT%d" % i, [128, N], BF16) for i in range(2)])
                    for qi_, qt_ in enumerate(qring.tiles):
                        P.pool(lambda e, qt_=qt_: e.memset(qt_[64:128, :], 0.0), [], ['QTz%d' % qi_])
                    ptring = Ring('PT', [sb("PT%d" % i, [128, 512], BF16) for i in range(4)])
                    aoring = Ring('AO', [sb("AO%d" % i, [128, 4, 64]) for i in range(2)])
                    rcring = Ring('rc', [sb("rc%d" % i, [128, 4]) for i in range(2)])
                    sbank = Ring('S', [0, 1, 2, 3])
                    obank = Ring('O', [4, 5])
                    groups = []
                    if not last:
                        groups.append((0, 2, [0, 1]))
                    for g in range(8):
                        groups.append((LC + g * 512, 4, list(range(NT))))
                    items = []
                    for hq in range(8):
                        for gi, (q0, nq, chunks) in enumerate(groups):
                            for ci, c in enumerate(chunks):
                                items.append((hq, gi, q0, nq, ci, c, len(chunks)))
                    qt_of = {}
                    st_of = {}

                    def get_qt(hq):
                        if hq not in qt_of:
                            QT, qk = qring.next()
                            P.dma('sp', QT[0:64, :], AQT[hq], [], [qk], qk)
                            qt_of[hq] = (QT, qk)
                        return qt_of[hq]

                    def emit_S(t):
                        hq, gi, q0, nq, ci, c, nch = items[t]
                        QT, qk = get_qt(hq)
                        kvh = hq // 4
                        W = nq * 128
                        sbk, sk = sbank.next()
                        P.pe(lambda e, sbk=sbk, c=c, QT=QT, q0=q0, W=W, kvh=kvh: e.matmul(psum[:, sbk, 0:W], KT[:, kvh, c * 128:(c + 1) * 128],
                                                                                         QT[:, q0:q0 + W], start=True, stop=True),
                             ['KT', 'KTz', 'QTz0', 'QTz1', qk], [sk])
                        st_of[t] = (sbk, sk)

                    cur_o = [None]

                    def emit_rest(t):
                        hq, gi, q0, nq, ci, c, nch = items[t]
                        kvh = hq // 4
                        W = nq * 128
                        sbk, sk = st_of.pop(t)
                        if ci == 0:
                            cur_o[0] = obank.next()
                        ob, ok = cur_o[0]
                        Ov = psum[:, ob, 0:260].rearrange("p (j e) -> p j e", e=65)
                        PT, pk = ptring.next()
                        P.act(lambda e, PT=PT, sbk=sbk, W=W: e.activation(out=PT[:, 0:W], in_=psum[:, sbk, 0:W], func=AF.Exp, scale=0.125),
                              [sk], [pk])
                        if t + 2 < len(items):
                            emit_S(t + 2)
                        for j in range(nq):
                            P.pe(lambda e, PT=PT, j=j, c=c, kvh=kvh, ci=ci, Ov=Ov, nch=nch: e.matmul(
                                Ov[:, j, :], PT[:, j * 128:(j + 1) * 128], V1[:, c, kvh, :],
                                start=(ci == 0 and j == 0), stop=(ci == nch - 1), skip_group_check=True),
                                [pk, 'V1'], [ok])
                        if ci == nch - 1:
                            rc, rk_ = rcring.next()
                            AO, ak_ = aoring.next()
                            P.dve(lambda e, rc=rc, Ov=Ov, nq=nq: e.reciprocal(out=rc[:, 0:nq], in_=Ov[:, 0:nq, 64]), [ok], [rk_])
                            P.dve(lambda e, rc=rc, Ov=Ov, nq=nq, AO=AO: e.tensor_tensor(out=AO[:, 0:nq, :], in0=Ov[:, 0:nq, 0:64],
                                                                                      in1=rc[:, 0:nq].unsqueeze(2).to_broadcast([128, nq, 64]), op=ALU.mult),
                                  [ok, rk_], [ak_])
                            P.dma('sp', MIX[q0:q0 + W, 512 + hq * 64:512 + (hq + 1) * 64].rearrange("(j p) d -> p j d", p=128), AO[:, 0:nq, :],
                                  [ak_], [('MIXa', q0, hq)], ak_)
                    emit_S(0)
                    emit_S(1)
                    for t in range(len(items)):
                        emit_rest(t)
                    P.flush()

            if 'ret' in phases:
                with contextlib.ExitStack() as st:
                    def sb(name, shape, dt=F32):
                        return st.enter_context(nc.sbuf_tensor(uname(name), list(shape), dt))
                    RT = sb("RT", [128, NT, 768], BF16)
                    P.dma('sp', RT[:], RTOK.rearrange("(c p) w -> p c w", p=128), [], ['RT'], 'RT')
                    RGt = sb("RGt", [128, NT, 256])
                    P.dma('act', RGt[:], RG.rearrange("(c p) w -> p c w", p=128), [], ['RGt'], 'RGt')
                    gng = sb("gng", [128, 256])
                    bload('act', gng[:], gn_g[l], 'gng')
                    lg = sb("lg", [128, 8])
                    bload('act', lg[:], rde[l].rearrange("a h -> (a h)"), 'lg')
                    P.act(lambda e: e.activation(out=lg[:], in_=lg[:], func=AF.Exp, scale=-math.log(2.0)), ['lg'], ['lg'])
                    P.act(lambda e: e.activation(out=lg[:], in_=lg[:], func=AF.Ln, scale=-1.0, bias=1.0), ['lg'], ['lg'])
                    dec = sb("dec", [128, 8])
                    P.act(lambda e: e.activation(out=dec[:], in_=lg[:], func=AF.Exp, scale=128.0), ['lg'], ['dec'])
                    dpos = sb("dpos", [128, 128]); dneg = sb("dneg", [128, 128]); mge = sb("mge", [128, 128])
                    P.dma('sp', dpos[:], dpos_in, [], ['dpos'], 'dpos')
                    P.dma('sp', dneg[:], dneg_in, [], ['dneg'], 'dneg')
                    P.dma('sp', mge[:], mge_in, [], ['mge'], 'mge')
                    DcT = sb("DcT", [128, 4, 128])
                    e1 = sb("e1", [128, 128])
                    for hh in range(4):
                        P.act(lambda e, hh=hh: e.activation(out=e1[:], in_=dpos[:], func=AF.Exp, scale=lg[:, hh:hh + 1]), ['dpos', 'lg'], ['e1'])
                        P.act(lambda e, hh=hh: e.activation(out=DcT[:, hh, :], in_=dneg[:], func=AF.Exp, scale=lg[:, 4 + hh:5 + hh]), ['dneg', 'lg'], ['DcT%d' % hh])
                        P.dve(lambda e, hh=hh: e.tensor_tensor(out=e1[:], in0=e1[:], in1=DcT[:, hh, :], op=ALU.subtract), ['e1', 'DcT%d' % hh], ['e1'])
                        P.dve(lambda e, hh=hh: e.tensor_tensor(out=e1[:], in0=e1[:], in1=mge[:], op=ALU.mult), ['e1', 'mge'], ['e1'])
                        P.dve(lambda e, hh=hh: e.tensor_tensor(out=DcT[:, hh, :], in0=DcT[:, hh, :], in1=e1[:], op=ALU.add), ['e1', 'DcT%d' % hh], ['DcT%d' % hh])
                    q3ring = Ring('Q3', [sb("Q3_%d" % i, [64, 3, N], BF16) for i in range(2)])
                    ktring = Ring('KTh', [sb("KTh%d" % i, [64, N], BF16) for i in range(2)])
                    Sf = sb("Sf", [64, NT + 1, 64], BF16)
                    Sb = sb("Sb", [64, NT + 1, 64], BF16)
                    srun = Ring('srun', [sb("srun%d" % i, [64, 64]) for i in range(2)])
                    ptr = Ring('PTr', [sb("PTr%d" % i, [128, 128], BF16) for i in range(2)])
                    st6 = sb("st6", [128, 6]); mv = sb("mv", [128, 2]); rs = sb("rs", [128, 1])
                    yo = Ring('yo', [sb("yo%d" % i, [128, 64]) for i in range(2)])
                    kvb = Ring('kv', [0, 1]); scb = Ring('sc', [2, 3]); yb = Ring('y', [4, 5])
                    ytiles = list(range(NT)) if not last else list(range(NT_C, NT))
                    for hh in range(4):
                        Q3, q3k = q3ring.next()
                        KTh, ktk = ktring.next()
                        P.dma('sp', Q3[:], RQT[:, hh].rearrange("t d n -> d t n"), [], [q3k], q3k)
                        P.dma('act', KTh[:], RKT[hh], [], [ktk], ktk)
                        vcol = 512 + hh * 64

                        def scan(order_tiles, S, skey, kcol, dcol, run_init_zero):
                            return None
                        run, runk = srun.next()
                        P.pool(lambda e, run=run: e.memset(run[:], 0.0), [], [runk])
                        for i in range(NT):
                            P.pool(lambda e, run=run, i=i: e.tensor_copy(out=Sf[:, i, :], in_=run[:]), [runk], [('Sf', i)])
                            kb, kk = kvb.next()
                            P.pe(lambda e, kb=kb, i=i, hh=hh, vcol=vcol: e.matmul(psum[0:64, kb, 0:64], RT[:, i, hh * 64:(hh + 1) * 64],
                                                                                 RT[:, i, vcol:vcol + 64], start=True, stop=True), ['RT'], [kk])
                            nrun, nrunk = srun.next()
                            P.dve(lambda e, run=run, nrun=nrun, kb=kb, hh=hh: e.scalar_tensor_tensor(out=nrun[:], in0=run[:], scalar=dec[0:64, hh:hh + 1],
                                                                                                     in1=psum[0:64, kb, 0:64], op0=ALU.mult, op1=ALU.add),
                                  [runk, kk, 'dec'], [nrunk])
                            run, runk = nrun, nrunk
                        run, runk = srun.next()
                        P.pool(lambda e, run=run: e.memset(run[:], 0.0), [], [runk])
                        for i in [1, 0] + list(range(NT - 1, NT_C - 1, -1)):
                            P.pool(lambda e, run=run, i=i: e.tensor_copy(out=Sb[:, i, :], in_=run[:]), [runk], [('Sb', i)])
                            kb, kk = kvb.next()
                            P.pe(lambda e, kb=kb, i=i, hh=hh, vcol=vcol: e.matmul(psum[0:64, kb, 0:64], RT[:, i, 256 + hh * 64:256 + (hh + 1) * 64],
                                                                                 RT[:, i, vcol:vcol + 64], start=True, stop=True), ['RT'], [kk])
                            nrun, nrunk = srun.next()
                            P.dve(lambda e, run=run, nrun=nrun, kb=kb, hh=hh: e.scalar_tensor_tensor(out=nrun[:], in0=run[:], scalar=dec[0:64, 4 + hh:5 + hh],
                                                                                                     in1=psum[0:64, kb, 0:64], op0=ALU.mult, op1=ALU.add),
                                  [runk, kk, 'dec'], [nrunk])
                            run, runk = nrun, nrunk
                        for i in ytiles:
                            sbk, sk = scb.next()
                            P.pe(lambda e, sbk=sbk, i=i, KTh=KTh, Q3=Q3: e.matmul(psum[:, sbk, 0:128], KTh[:, i * 128:(i + 1) * 128], Q3[:, 0, i * 128:(i + 1) * 128],
                                                                                start=True, stop=True), [ktk, q3k], [sk])
                            PTr, pk = ptr.next()
                            P.dve(lambda e, PTr=PTr, sbk=sbk, hh=hh: e.tensor_tensor(out=PTr[:], in0=psum[:, sbk, 0:128], in1=DcT[:, hh, :], op=ALU.mult),
                                  [sk, 'DcT%d' % hh], [pk])
                            ybk, yk = yb.next()
                            P.pe(lambda e, ybk=ybk, PTr=PTr, i=i, vcol=vcol: e.matmul(psum[:, ybk, 0:64], PTr[:], RT[:, i, vcol:vcol + 64], start=True, stop=False),
                                 [pk, 'RT'], [yk])
                            P.pe(lambda e, ybk=ybk, Q3=Q3, i=i: e.matmul(psum[:, ybk, 0:64], Q3[:, 1, i * 128:(i + 1) * 128], Sf[:, i, :], start=False, stop=False),
                                 [q3k, ('Sf', i)], [yk])
                            P.pe(lambda e, ybk=ybk, Q3=Q3, i=i: e.matmul(psum[:, ybk, 0:64], Q3[:, 2, i * 128:(i + 1) * 128], Sb[:, i, :], start=False, stop=True),
                                 [q3k, ('Sb', i)], [yk])
                            P.dve(lambda e, ybk=ybk: e.bn_stats(out=st6[:], in_=psum[:, ybk, 0:64]), [yk], ['st6'])
                            P.dve(lambda e: e.bn_aggr(out=mv[:], in_=st6[:]), ['st6'], ['mv'])
                            P.dve(lambda e: e.tensor_scalar_add(out=rs[:], in0=mv[:, 1:2], scalar1=EPS), ['mv'], ['rs'])
                            P.act(lambda e: e.activation(out=rs[:], in_=rs[:], func=AF.Sqrt), ['rs'], ['rs'])
                            P.dve(lambda e: e.reciprocal(out=rs[:], in_=rs[:]), ['rs'], ['rs'])
                            y_, yok = yo.next()
                            P.dve(lambda e, y_=y_, ybk=ybk: e.tensor_scalar(out=y_[:], in0=psum[:, ybk, 0:64], scalar1=mv[:, 0:1], scalar2=rs[:, 0:1],
                                                                           op0=ALU.subtract, op1=ALU.mult), [yk, 'mv', 'rs'], [yok])
                            P.pool(lambda e, y_=y_, hh=hh: e.tensor_tensor(out=y_[:], in0=y_[:], in1=gng[:, hh * 64:(hh + 1) * 64], op=ALU.mult), [yok, 'gng'], [yok])
                            P.pool(lambda e, y_=y_, hh=hh, i=i: e.tensor_tensor(out=y_[:], in0=y_[:], in1=RGt[:, i, hh * 64:(hh + 1) * 64], op=ALU.mult), [yok, 'RGt'], [yok])
                            P.dma('sp', MIX[i * 128:(i + 1) * 128, 256 + hh * 64:256 + (hh + 1) * 64], y_[:], [yok], [('MIXr', i, hh)], yok)
                    P.flush()

            if 'p3' in phases:
                with contextlib.ExitStack() as st:
                    def sb(name, shape, dt=F32):
                        return st.enter_context(nc.sbuf_tensor(uname(name), list(shape), dt))
                    wout = sb("wout", [128, 8, D], BF16)
                    for hf in range(2):
                        P.dma('pool', wout[:, :, hf * 512:(hf + 1) * 512], w_out[l, :, hf * 512:(hf + 1) * 512].rearrange("(k p) n -> p k n", p=128),
                              [], ['wout%d' % hf], 'wout%d' % hf)
                    CWr = sb("CWr", [128, 256, 3])
                    P.dma('act', CWr[:].rearrange("p c k -> p (c k)"), conv_w[l].rearrange("c k -> (c k)").partition_broadcast(128), [], ['CWr'], 'CWr')
                    CW = sb("CW", [128, 3, 256])
                    for k in range(3):
                        P.dve(lambda e, k=k: e.tensor_copy(out=CW[:, k, :], in_=CWr[:, :, k]), ['CWr'], ['CW%d' % k])
                    g1 = [sb("g1_%d" % s, [128, D]) for s in range(2)]
                    sc2 = [sb("sc2_%d" % s, [128, D]) for s in range(2)]
                    sh2 = [sb("sh2_%d" % s, [128, D]) for s in range(2)]
                    for s in range(2):
                        if last and s == 1:
                            continue
                        bload('sp', g1[s][:], modv[l, s, 2 * D:3 * D], 'g1_%d' % s)
                        bload('sp', sh2[s][:], modv[l, s, 3 * D:4 * D], 'sh2_%d' % s)
                        bload('sp', sc2[s][:], modv[l, s, 4 * D:5 * D], 'sc2_%d' % s)
                    lng = sb("lng", [128, D]); lnb = sb("lnb", [128, D])
                    bload('act', lng[:], ln_g[l, 0], 'lng')
                    bload('act', lnb[:], ln_b[l, 0], 'lnb')
                    wr = sb("wr", [128, 8, NE])
                    P.dma('act', wr[:], w_router[l].rearrange("(k p) e -> p k e", p=128), [], ['wr'], 'wr')
                    brt = sb("brt", [128, NE])
                    bload('act', brt[:], b_router[l], 'brt')
                    pmr = Ring('pm', [sb("pm%d" % i, [128, 3, 256]) for i in range(3)])
                    btr = Ring('bt', [sb("bt%d" % i, [128, 256]) for i in range(3)])
                    mixr = Ring('mix', [sb("mix%d" % i, [128, D]) for i in range(3)])
                    xr = Ring('x3', [sb("x3_%d" % i, [128, D]) for i in range(3)])
                    ca = sb("ca", [128, 256]); cb = sb("cb", [128, 256])
                    mTr = Ring('mT', [sb("mT%d" % i, [128, 8, 128], BF16) for i in range(2)])
                    rr = sb("rr", [128, D])
                    x1r = Ring('x1', [sb("x1_%d" % i, [128, D]) for i in range(2)])
                    h2 = sb("h2", [128, D])
                    h2br = Ring('h2b', [sb("h2b%d" % i, [128, D], BF16) for i in range(2)])
                    ix8 = sb("ix8", [128, 8], U32)
                    h2f = sb("h2f", [128, 8, 128])
                    st6 = sb("st6", [128, 2, 6]); mv = sb("mv", [128, 2]); rs = sb("rs", [128, 1])
                    lgt = sb("lgt", [128, NE]); mx8 = sb("mx8", [128, 8]); msk = sb("msk", [128, NE]); nmx = sb("nmx", [128, 1])
                    ex = sb("ex", [128, NE]); sm = sb("sm", [128, 1])
                    mwr = Ring('mw', [sb("mw%d" % i, [128, NE]) for i in range(2)])
                    ld3 = {}

                    def issue_loads3(i):
                        r0 = prow(i)
                        pm, pmk = pmr.next()
                        for k in range(3):
                            P.dma('sp', pm[:, k, :], PB[r0 - 1 + k:r0 + 127 + k, 0:256], [], [pmk + str(k)], pmk + str(k))
                        bt, btk = btr.next()
                        P.dma('sp', bt[:], PB[r0:r0 + 128, 256:512], [], [btk], btk)
                        mix, mixk = mixr.next()
                        P.dma('sp', mix[:, 256:D], MIX[i * 128:(i + 1) * 128, 256:D], [], [mixk + 'l'], mixk)
                        xt, xk = xr.next()
                        P.dma('sp', xt[:], xsrc(i), [], [xk], xk)
                        ld3[i] = (pm, pmk, bt, btk, mix, mixk, xt, xk)
                    issue_loads3(out_tiles[0])
                    for oi, i in enumerate(out_tiles):
                        s = 1 if i < NT_C else 0
                        if oi + 1 < len(out_tiles):
                            issue_loads3(out_tiles[oi + 1])
                        pm, pmk, bt, btk, mix, mixk, xt, xk = ld3.pop(i)
                        P.dve(lambda e, pm=pm: e.tensor_tensor(out=ca[:], in0=pm[:, 0, :], in1=CW[:, 0, :], op=ALU.mult), [pmk + '0', 'CW0'], ['ca'])
                        P.pool(lambda e, pm=pm: e.tensor_tensor(out=cb[:], in0=pm[:, 1, :], in1=CW[:, 1, :], op=ALU.mult), [pmk + '1', 'CW1'], ['cb'])
                        P.dve(lambda e: e.tensor_tensor(out=ca[:], in0=ca[:], in1=cb[:], op=ALU.add), ['ca', 'cb'], ['ca'])
                        P.pool(lambda e, pm=pm: e.tensor_tensor(out=cb[:], in0=pm[:, 2, :], in1=CW[:, 2, :], op=ALU.mult), [pmk + '2', 'CW2', 'ca'], ['cb'])
                        P.dve(lambda e: e.tensor_tensor(out=ca[:], in0=ca[:], in1=cb[:], op=ALU.add), ['ca', 'cb'], ['ca'])
                        P.dve(lambda e, mix=mix, bt=bt: e.tensor_tensor(out=mix[:, 0:256], in0=ca[:], in1=bt[:], op=ALU.mult), ['ca', btk], [mixk + 'c'])
                        if cut < 1:
                            continue
                        for k in range(8):
                            P.pe(lambda e, mix=mix, k=k: e.transpose(psum[:, k // 4, (k % 4) * 128:(k % 4 + 1) * 128], mix[:, k * 128:(k + 1) * 128], ident[:]),
                                 [mixk + 'l', mixk + 'c', 'ident'], ['pT%d' % k])
                        mT, mTk = mTr.next()
                        for hf in range(2):
                            P.act(lambda e, mT=mT, hf=hf: e.activation(out=mT[:, hf * 4:(hf + 1) * 4, :].rearrange("p k n -> p (k n)"), in_=psum[:, hf, :], func=AF.Copy),
                                  ['pT%d' % k for k in range(hf * 4, hf * 4 + 4)], [mTk + str(hf)])
                        for nh in range(2):
                            for k in range(8):
                                P.pe(lambda e, mT=mT, nh=nh, k=k: e.matmul(psum[:, 2 + nh, :], mT[:, k, :], wout[:, k, nh * 512:(nh + 1) * 512], start=(k == 0), stop=(k == 7)),
                                     [mTk + str(k // 4), 'wout%d' % nh], ['py%d' % nh])
                        if cut < 2:
                            continue
                        for nh in range(2):
                            P.dve(lambda e, nh=nh, s=s: e.tensor_tensor(out=rr[:, nh * 512:(nh + 1) * 512], in0=psum[:, 2 + nh, :], in1=g1[s][:, nh * 512:(nh + 1) * 512], op=ALU.mult),
                                  ['py%d' % nh, 'g1_%d' % s], ['rr%d' % nh])
                        P.dve(lambda e, xt=xt: e.scalar_tensor_tensor(out=rr[:], in0=xt[:], scalar=ALPHA, in1=rr[:], op0=ALU.mult, op1=ALU.add), [xk, 'rr0', 'rr1'], ['rr'])
                        for nh in range(2):
                            P.dve(lambda e, nh=nh: e.bn_stats(out=st6[:, nh, :], in_=rr[:, nh * 512:(nh + 1) * 512]), ['rr'], ['st6_%d' % nh])
                        P.dve(lambda e: e.bn_aggr(out=mv[:], in_=st6[:]), ['st6_0', 'st6_1'], ['mv'])
                        P.dve(lambda e: e.tensor_scalar_add(out=rs[:], in0=mv[:, 1:2], scalar1=EPS), ['mv'], ['rs'])
                        P.act(lambda e: e.activation(out=rs[:], in_=rs[:], func=AF.Sqrt), ['rs'], ['rs'])
                        P.dve(lambda e: e.reciprocal(out=rs[:], in_=rs[:]), ['rs'], ['rs'])
                        x1, x1k = x1r.next()
                        P.dve(lambda e, x1=x1: e.tensor_scalar(out=x1[:], in0=rr[:], scalar1=mv[:, 0:1], scalar2=rs[:, 0:1], op0=ALU.subtract, op1=ALU.mult), ['rr', 'mv', 'rs'], [x1k])
                        P.dve(lambda e, x1=x1: e.tensor_tensor(out=x1[:], in0=x1[:], in1=lng[:], op=ALU.mult), [x1k, 'lng'], [x1k])
                        P.dve(lambda e, x1=x1: e.tensor_tensor(out=x1[:], in0=x1[:], in1=lnb[:], op=ALU.add), [x1k, 'lnb'], [x1k])
                        P.dma('sp', XM[i * 128:(i + 1) * 128, :], x1[:], [x1k], [('XM', i)], x1k)
                        if cut < 3:
                            continue
                        P.dve(lambda e, x1=x1, s=s: e.tensor_tensor(out=h2[:], in0=x1[:], in1=sc2[s][:], op=ALU.mult), [x1k, 'sc2_%d' % s], ['h2'])
                        P.dve(lambda e, s=s: e.tensor_tensor(out=h2[:], in0=h2[:], in1=sh2[s][:], op=ALU.add), ['h2', 'sh2_%d' % s], ['h2'])
                        if cut < 3.2:
                            continue
                        for k in range(8):
                            P.pe(lambda e, k=k: e.transpose(psum[:, 4 + k // 4, (k % 4) * 128:(k % 4 + 1) * 128], h2[:, k * 128:(k + 1) * 128], ident[:]),
                                 ['h2', 'ident'], ['pU%d' % k])
                        if cut < 3.3:
                            continue
                        for hf in range(2):
                            P.dve(lambda e, hf=hf: e.tensor_copy(out=h2f[:, hf * 4:(hf + 1) * 4, :].rearrange("p k n -> p (k n)"), in_=psum[:, 4 + hf, :]),
                                  ['pU%d' % k for k in range(hf * 4, hf * 4 + 4)], ['h2f%d' % hf])
                        h2b, h2bk = h2br.next()
                        P.act(lambda e, h2b=h2b: e.activation(out=h2b[:], in_=h2[:], func=AF.Copy), ['h2'], [h2bk])
                        P.dma('sp', H2R[i * 128:(i + 1) * 128, :], h2b[:], [h2bk], [('H2R', i)], h2bk)
                        for k in range(8):
                            P.pe(lambda e, k=k: e.matmul(psum[:, 6, 0:NE], h2f[:, k, :], wr[:, k, :], start=(k == 0), stop=(k == 7)), ['h2f%d' % (k // 4), 'wr'], ['pl'])
                        P.dve(lambda e: e.tensor_tensor(out=lgt[:], in0=psum[:, 6, 0:NE], in1=brt[:], op=ALU.add), ['pl', 'brt'], ['lgt'])
                        P.dve(lambda e: e.max(out=mx8[:], in_=lgt[:]), ['lgt'], ['mx8'])
                        P.dve(lambda e: e.max_index(out=ix8[:], in_max=mx8[:], in_values=lgt[:]), ['lgt', 'mx8'], ['ix8'])
                        P.dve(lambda e: e.tensor_scalar_mul(out=nmx[:], in0=mx8[:, 0:1], scalar1=-1.0), ['mx8'], ['nmx'])
                        P.act(lambda e: e.activation(out=ex[:, 0:4], in_=mx8[:, 0:4], func=AF.Exp, bias=nmx[:, 0:1], scale=1.0), ['mx8', 'nmx'], ['ex'])
                        P.dve(lambda e: e.reduce_sum(out=sm[:], in_=ex[:, 0:4], axis=AX.X), ['ex'], ['sm'])
                        P.dve(lambda e: e.reciprocal(out=sm[:], in_=sm[:]), ['sm'], ['sm'])
                        mw_, mwk = mwr.next()
                        P.dve(lambda e, mw_=mw_: e.tensor_scalar_mul(out=mw_[:, 0:4], in0=ex[:, 0:4], scalar1=sm[:, 0:1]), ['ex', 'sm'], [mwk + 'w'])
                        P.dve(lambda e, mw_=mw_: e.tensor_copy(out=mw_[:, 4:8], in_=ix8[:, 0:4]), ['ix8'], [mwk + 'e'])
                        P.dma('sp', RW[i * 128:(i + 1) * 128, :], mw_[:, 0:4], [mwk + 'w'], [('RW', i)], mwk + 'w')
                        P.dma('sp', RE[i * 128:(i + 1) * 128, :], mw_[:, 4:8], [mwk + 'e'], [('RE', i)], mwk + 'e')
                    P.flush()

            if 'moe' in phases:
                tiles = out_tiles
                nt = len(tiles)
                T = nt * 128
                t0 = tiles[0] * 128
                NB = (4 * T + NE * (BS - 1) + BS - 1) // BS
                assert NB <= NBMAX
                w_gu_rows = w_gu.rearrange("l e r c -> (l e r) c")
                w_dn_rows = w_dn.rearrange("l e r c -> (l e r) c")
                with contextlib.ExitStack() as st:
                    def sb(name, shape, dt=F32):
                        return st.enter_context(nc.sbuf_tensor(uname(name), list(shape), dt))
                    DKi = sb("DKi", [128, nt, 4], I32)
                    IDXG = sb("IDXG", [128, NB, 8], I32)
                    EB = sb("EB", [128, NB])
                    IDXB = sb("IDXB", [128, NB], I32)
                    mwd = sb("mwd", [128, nt, NE])
                    iotap = sb("iotap", [128, 1])
                    P.dma('sp', iotap[:], iotap_in, [], ['iotap'], 'iotap')
                    with contextlib.ExitStack() as st2:
                        def sb2(name, shape, dt=F32):
                            return st2.enter_context(nc.sbuf_tensor(uname(name), list(shape), dt))
                        EF = sb2("EF", [128, nt, 4])
                        W4 = sb2("W4", [128, nt, 4])
                        P.dma('sp', EF[:], RE[t0:t0 + T, :].rearrange("(c p) k -> p c k", p=128), [], ['EF'], 'EF')
                        P.dma('sp', W4[:], RW[t0:t0 + T, :].rearrange("(c p) k -> p c k", p=128), [], ['W4'], 'W4')
                        iota32 = sb2("iota32", [128, NE])
                        P.dma('act', iota32[:], iota32_in, [], ['iota32'], 'iota32')
                        lts = sb2("lts", [128, 128])
                        P.dma('act', lts[:], lts_in, [], ['lts'], 'lts')
                        ones = sb2("ones", [128, 128])
                        P.pool(lambda e: e.memset(ones[:], 1.0), [], ['ones'])
                        bst = sb2("bst", [128, NBMAX])
                        P.dma('act', bst[:], bstart_in, [], ['bst'], 'bst')
                        kp = sb2("kp", [128, 8])
                        P.dma('act', kp[:], kp_in, [], ['kp'], 'kp')
                        OH = sb2("OH", [128, 4, nt, NE])
                        for k in range(4):
                            P.dve(lambda e, k=k: e.tensor_tensor(out=OH[:, k, :, :], in0=iota32[:].unsqueeze(1).to_broadcast([128, nt, NE]),
                                                                 in1=EF[:, :, k].unsqueeze(2).to_broadcast([128, nt, NE]), op=ALU.is_equal),
                                  ['iota32', 'EF'], ['OH%d' % k])
                        mask = sb2("mask", [128, nt, NE])
                        P.dve(lambda e: e.tensor_tensor(out=mask[:], in0=OH[:, 0, :, :], in1=OH[:, 1, :, :], op=ALU.add), ['OH0', 'OH1'], ['mask'])
                        P.dve(lambda e: e.tensor_tensor(out=mask[:], in0=mask[:], in1=OH[:, 2, :, :], op=ALU.add), ['mask', 'OH2'], ['mask'])
                        P.dve(lambda e: e.tensor_tensor(out=mask[:], in0=mask[:], in1=OH[:, 3, :, :], op=ALU.add), ['mask', 'OH3'], ['mask'])
                        for j in range(nt):
                            bk = j // 16
                            col = (j % 16) * NE
                            P.pe(lambda e, j=j, bk=bk, col=col: e.matmul(psum[:, bk, col:col + NE], lts[:], mask[:, j, :], start=True, stop=True, skip_group_check=True),
                                 ['lts', 'mask'], ['rk%d' % bk])
                            P.pe(lambda e, j=j, bk=bk, col=col: e.matmul(psum[:, 4 + bk, col:col + NE], ones[:], mask[:, j, :], start=True, stop=True, skip_group_check=True),
                                 ['ones', 'mask'], ['tt%d' % bk])
                        for m in range(nt):
                            P.pe(lambda e, m=m: e.matmul(psum[:, 3, 0:NE], ones[:], mask[:, m, :], start=(m == 0), stop=(m == nt - 1)), ['ones', 'mask'], ['cnt'])
                        TOT = sb2("TOT", [128, nt, NE])
                        PRE = sb2("PRE", [128, nt, NE])
                        for bk in range((nt + 15) // 16):
                            j0 = bk * 16
                            nj = min(16, nt - j0)
                            P.act(lambda e, bk=bk, j0=j0, nj=nj: e.activation(out=TOT[:, j0:j0 + nj, :].rearrange("p j e -> p (j e)"), in_=psum[:, 4 + bk, 0:nj * NE], func=AF.Copy),
                                  ['tt%d' % bk], ['TOT%d' % bk])
                        P.pool(lambda e: e.memset(PRE[:, 0, :], 0.0), [], ['PRE'])
                        for j in range(1, nt):
                            P.dve(lambda e, j=j: e.tensor_tensor(out=PRE[:, j, :], in0=PRE[:, j - 1, :], in1=TOT[:, j - 1, :], op=ALU.add),
                                  ['PRE'] + ['TOT%d' % bk for bk in range((nt + 15) // 16)], ['PRE'])
                        c0 = sb2("c0", [128, NE]); c1 = sb2("c1", [128, NE]); padded = sb2("padded", [128, NE])
                        P.dve(lambda e: e.tensor_scalar_add(out=c0[:], in0=psum[:, 3, 0:NE], scalar1=float(BS - 1)), ['cnt'], ['c0'])
                        ci32 = sb2("ci32", [128, NE], I32)
                        P.dve(lambda e: e.tensor_scalar(out=c1[:], in0=c0[:], scalar1=1.0 / BS, scalar2=-0.5 + 0.5 / BS, op0=ALU.mult, op1=ALU.add), ['c0'], ['c1'])
                        P.dve(lambda e: e.tensor_copy(out=ci32[:], in_=c1[:]), ['c1'], ['ci32'])
                        P.dve(lambda e: e.tensor_copy(out=c1[:], in_=ci32[:]), ['ci32'], ['c1'])
                        P.dve(lambda e: e.tensor_scalar_mul(out=padded[:], in0=c1[:], scalar1=float(BS)), ['c1'], ['padded'])
                        P.dve(lambda e: e.tensor_copy(out=c0[:], in_=padded[:]), ['padded'], ['c0'])
                        cur, nxt_, ck, nk = c0, c1, 'c0', 'c1'
                        for sft in (1, 2, 4, 8, 16):
                            P.dve(lambda e, cur=cur, nxt_=nxt_, sft=sft: e.tensor_copy(out=nxt_[:, 0:sft], in_=cur[:, 0:sft]), [ck], [nk + 'a'])
                            P.dve(lambda e, cur=cur, nxt_=nxt_, sft=sft: e.tensor_tensor(out=nxt_[:, sft:NE], in0=cur[:, sft:NE], in1=cur[:, 0:NE - sft], op=ALU.add), [ck], [nk + 'b'])
                            P.dve(lambda e: e.engine_nop(), [nk + 'a', nk + 'b'], [nk])
                            cur, nxt_, ck, nk = nxt_, cur, nk, ck
                        pend, pendk = cur, ck
                        pstart = sb2("pstart", [128, NE])
                        P.dve(lambda e: e.tensor_tensor(out=pstart[:], in0=pend[:], in1=padded[:], op=ALU.subtract), [pendk, 'padded'], ['pstart'])
                        dfull = sb2("dfull", [128, nt, NE])
                        for bk in range((nt + 15) // 16):
                            j0 = bk * 16
                            nj = min(16, nt - j0)
                            P.dve(lambda e, bk=bk, j0=j0, nj=nj: e.tensor_tensor(out=dfull[:, j0:j0 + nj, :], in0=psum[:, bk, 0:nj * NE].rearrange("p (j e) -> p j e", e=NE),
                                                                                 in1=pstart[:].unsqueeze(1).to_broadcast([128, nj, NE]), op=ALU.add),
                                  ['rk%d' % bk, 'pstart'], ['dfull%d' % bk])
                            P.dve(lambda e, j0=j0, nj=nj: e.tensor_tensor(out=dfull[:, j0:j0 + nj, :], in0=dfull[:, j0:j0 + nj, :], in1=PRE[:, j0:j0 + nj, :], op=ALU.add),
                                  ['dfull%d' % bk, 'PRE'], ['dfull%d' % bk])
                        dkeys = ['dfull%d' % bk for bk in range((nt + 15) // 16)]
                        tmpd = sb2("tmpd", [128, nt, NE])
                        DKf = sb2("DKf", [128, nt, 4])
                        for k in range(4):
                            P.dve(lambda e, k=k: e.tensor_tensor(out=tmpd[:], in0=dfull[:], in1=OH[:, k, :, :], op=ALU.mult), dkeys + ['OH%d' % k], ['tmpd'])
                            P.dve(lambda e, k=k: e.tensor_reduce(out=DKf[:, :, k], in_=tmpd[:], axis=AX.X, op=ALU.add), ['tmpd'], ['DKf%d' % k])
                        P.dve(lambda e: e.tensor_copy(out=DKi[:], in_=DKf[:]), ['DKf%d' % k for k in range(4)], ['DKi'])
                        cmp_ = sb2("cmp", [128, NB, NE])
                        P.dve(lambda e: e.tensor_tensor(out=cmp_[:], in0=pend[:].unsqueeze(1).to_broadcast([128, NB, NE]),
                                                        in1=bst[:, 0:NB].unsqueeze(2).to_broadcast([128, NB, NE]), op=ALU.is_le), [pendk, 'bst'], ['cmp'])
                        P.dve(lambda e: e.tensor_reduce(out=EB[:], in_=cmp_[:], axis=AX.X, op=ALU.add), ['cmp'], ['EB'])
                        P.dve(lambda e: e.tensor_scalar_min(out=EB[:], in0=EB[:], scalar1=float(NE - 1)), ['EB'], ['EB'])
                        idxf = sb2("idxf", [128, NB, 8])
                        P.dve(lambda e: e.scalar_tensor_tensor(out=idxf[:], in0=EB[:].unsqueeze(2).to_broadcast([128, NB, 8]), scalar=float(D),
                                                               in1=kp[:].unsqueeze(1).to_broadcast([128, NB, 8]), op0=ALU.mult, op1=ALU.add), ['EB', 'kp'], ['idxf'])
                        P.dve(lambda e: e.tensor_scalar_add(out=idxf[:], in0=idxf[:], scalar1=float(l * NE * D)), ['idxf'], ['idxf'])
                        P.dve(lambda e: e.tensor_copy(out=IDXG[:], in_=idxf[:]), ['idxf'], ['IDXG'])
                        idxbf = sb2("idxbf", [128, NB])
                        P.dve(lambda e: e.tensor_scalar(out=idxbf[:], in0=EB[:], scalar1=128.0, scalar2=float(l * NE * 128), op0=ALU.mult, op1=ALU.add), ['EB'], ['idxbf'])
                        P.dve(lambda e: e.tensor_scalar(out=idxbf[:], in0=idxbf[:], scalar1=iotap[:, 0:1], scalar2=None, op0=ALU.add), ['idxbf', 'iotap'], ['idxbf'])
                        P.dve(lambda e: e.tensor_copy(out=IDXB[:], in_=idxbf[:]), ['idxbf'], ['IDXB'])
                        for k in range(4):
                            dst_ = mwd if k == 0 else tmpd
                            P.dve(lambda e, k=k, dst_=dst_: e.tensor_tensor(out=dst_[:], in0=OH[:, k, :, :], in1=W4[:, :, k].unsqueeze(2).to_broadcast([128, nt, NE]), op=ALU.mult),
                                  ['OH%d' % k, 'W4', 'DKf0', 'DKf1', 'DKf2', 'DKf3'], ['mwd' if k == 0 else 'tmpd'])
                            if k > 0:
                                P.dve(lambda e: e.tensor_tensor(out=mwd[:], in0=mwd[:], in1=tmpd[:], op=ALU.add), ['mwd', 'tmpd'], ['mwd'])
                        hr = Ring('hr', [sb2("hr%d" % i, [128, D], BF16) for i in range(3)])
                        for j, i in enumerate(tiles):
                            h_, hk_ = hr.next()
                            P.dma('sp', h_[:], H2R[i * 128:(i + 1) * 128, :], [], [hk_], hk_)
                            for k in range(4):
                                P.add('pool', lambda e, h_=h_, j=j, k=k: e.indirect_dma_start(out=XS[:, :], out_offset=bass.IndirectOffsetOnAxis(ap=DKi[:, j, k:k + 1], axis=0),
                                                                                          in_=h_[:, :], in_offset=None), [hk_, 'DKi'], [('XS', j, k)], 'xsc%d' % ((j * 4 + k) % 4))
                                P.add('pool', lambda e, j=j, k=k: e.indirect_dma_start(out=SW[:, :], out_offset=bass.IndirectOffsetOnAxis(ap=DKi[:, j, k:k + 1], axis=0),
                                                                                    in_=W4[:, j, k:k + 1], in_offset=None), ['W4', 'DKi'], [('SW', j, k)], 'swc%d' % ((j * 4 + k) % 4))
                        P.flush()
                    with contextlib.ExitStack() as st2:
                        def sb2(name, shape, dt=F32):
                            return st2.enter_context(nc.sbuf_tensor(uname(name), list(shape), dt))
                        identb = sb2("identb", [128, 128], BF16)
                        P.act(lambda e: e.activation(out=identb[:], in_=ident[:], func=AF.Copy), ['ident'], ['identb'])
                        bgr = sb2("bgr", [NE, 2 * D])
                        P.dma('act', bgr[:], b_gu[l], [], ['bgr'], 'bgr')
                        for c in range(16):
                            P.pe(lambda e, c=c: e.transpose(psum[:, 6, c * NE:(c + 1) * NE], bgr[:, c * 128:(c + 1) * 128], ident[0:NE, 0:NE]), ['bgr', 'ident'], ['pX0'])
                        X2 = sb2("X2", [128, NE, 16])
                        P.dve(lambda e: e.tensor_copy(out=X2[:], in_=psum[:, 6, :].rearrange("p (c e) -> p e c", e=NE)), ['pX0'], ['X2'])
                        P.dve(lambda e: e.tensor_scalar_add(out=X2[:, :, 8:16], in0=X2[:, :, 8:16], scalar1=1.0), ['X2'], ['X2'])
                        P.dma('sp', BGT[l * NE * 128:(l + 1) * NE * 128, :].rearrange("(e p) c -> p e c", p=128), X2[:], ['X2'], ['BGT'], 'BGT')
                        bbr = Ring('bb', [sb2("bb%d" % i, [128, 16]) for i in range(2)])
                        wgr = Ring('WG', [sb2("WG%d" % i, [128, 8, 2 * D], BF16) for i in range(2)])
                        wdr = Ring('WD', [sb2("WD%d" % i, [128, 8, D], BF16) for i in range(2)])
                        xbr = Ring('XB', [sb2("XB%d" % i, [128, 4, D], BF16) for i in range(2)])
                        XTr = [sb2("XT%d" % i, [128, 8, BS], BF16) for i in range(2)]
                        actr = Ring('act', [sb2("act%d" % i, [128, 8, BS], BF16) for i in range(2)])
                        Ar = Ring('A', [sb2("A%d" % i, [128, BS]) for i in range(2)])
                        Sr = Ring('Sg', [sb2("Sg%d" % i, [128, BS]) for i in range(2)])
                        Ur = Ring('U', [sb2("U%d" % i, [128, BS]) for i in range(2)])
                        swr = Ring('swb', [sb2("swb%d" % i, [128, 4]) for i in range(2)])
                        Yr = Ring('Y', [sb2("Y%d" % i, [128, 4, D]) for i in range(1)])
                        gbr = Ring('pg', [0, 1]); ubr = Ring('pu', [2, 3]); ybr = Ring('py', [4, 5])
                        psb = [psum[:, 6, :].bitcast(BF16), psum[:, 7, :].bitcast(BF16)]

                        def load_blk(b):
                            WG, wgk = wgr.next()
                            WD, wdk = wdr.next()
                            for k in range(8):
                                P.add('pool', lambda e, WG=WG, b=b, k=k: e.indirect_dma_start(out=WG[:, k, :], out_offset=None, in_=w_gu_rows[:, :],
                                                                                             in_offset=bass.IndirectOffsetOnAxis(ap=IDXG[:, b, k:k + 1], axis=0)),
                                      ['IDXG'], [wgk + str(k)], wgk + str(k))
                            for k in range(8):
                                P.add('pool', lambda e, WD=WD, b=b, k=k: e.indirect_dma_start(out=WD[:, k, :], out_offset=None, in_=w_dn_rows[:, :],
                                                                                             in_offset=bass.IndirectOffsetOnAxis(ap=IDXG[:, b, k:k + 1], axis=0)),
                                      ['IDXG'], [wdk + str(k)], wdk + str(k))
                            XB, xbk = xbr.next()
                            P.dma('sp', XB[:], XS[b * BS:(b + 1) * BS, :].rearrange("(s p) d -> p s d", p=128), [], [xbk], xbk)
                            swb, swk = swr.next()
                            P.dma('act', swb[:], SW[b * BS:(b + 1) * BS, :].rearrange("(s p) o -> p (s o)", p=128), [], [swk], swk, allow_slow_non_contiguous=True)
                            bb, bbk = bbr.next()
                            P.add('pool', lambda e, bb=bb, b=b: e.indirect_dma_start(out=bb[:, :], out_offset=None, in_=BGT[:, :],
                                                                                   in_offset=bass.IndirectOffsetOnAxis(ap=IDXB[:, b:b + 1], axis=0)),
                                  ['IDXB', 'BGT'], [bbk], bbk)
                            return WG, wgk, WD, wdk, XB, xbk, swb, swk, bb, bbk
                        def transposes(b, XB, xbk):
                            XT = XTr[b % 2]
                            for half in range(2):
                                for s2 in range(2):
                                    sidx = half * 2 + s2
                                    for k in range(8):
                                        P.pe(lambda e, XB=XB, sidx=sidx, k=k, s2=s2: e.transpose(psb[s2][:, k * 128:(k + 1) * 128], XB[:, sidx, k * 128:(k + 1) * 128], identb[:]),
                                             [xbk, 'identb'], ['pX%d' % s2])
                                    P.act(lambda e, sidx=sidx, s2=s2, XT=XT: e.activation(out=XT[:, :, sidx * 128:(sidx + 1) * 128], in_=psb[s2].rearrange("p (k n) -> p k n", n=128), func=AF.Copy),
                                          ['pX%d' % s2], ['XT%d_%d' % (b % 2, sidx)])
                        nxt = load_blk(0)
                        transposes(0, nxt[4], nxt[5])
                        for b in range(NB):
                            WG, wgk, WD, wdk, XB, xbk, swb, swk, bb, bbk = nxt
                            if b + 1 < NB:
                                nxt = load_blk(b + 1)
                            wgkeys = [wgk + str(k) for k in range(8)]
                            wdkeys = [wdk + str(k) for k in range(8)]
                            XT = XTr[b % 2]
                            xtkeys = ['XT%d_%d' % (b % 2, q) for q in range(4)]
                            at, atk = actr.next()
                            for c in range(8):
                                gb, gbk = gbr.next()
                                ub, ubk = ubr.next()
                                for k in range(8):
                                    P.pe(lambda e, gb=gb, WG=WG, k=k, c=c, XT=XT: e.matmul(psum[:, gb, :], WG[:, k, c * 128:(c + 1) * 128], XT[:, k, :], start=(k == 0), stop=(k == 7)),
                                         wgkeys + xtkeys, [gbk])
                                for k in range(8):
                                    P.pe(lambda e, ub=ub, WG=WG, k=k, c=c, XT=XT: e.matmul(psum[:, ub, :], WG[:, k, D + c * 128:D + (c + 1) * 128], XT[:, k, :], start=(k == 0), stop=(k == 7)),
                                         wgkeys + xtkeys, [ubk])
                                A, Ak = Ar.next(); S_, Sk = Sr.next(); U, Uk = Ur.next()
                                P.dve(lambda e, A=A, gb=gb, bb=bb, c=c: e.tensor_scalar(out=A[:], in0=psum[:, gb, :], scalar1=bb[:, c:c + 1], scalar2=7.0, op0=ALU.add, op1=ALU.min), [gbk, bbk], [Ak])
                                P.act(lambda e, A=A, S_=S_: e.activation(out=S_[:], in_=A[:], func=AF.Sigmoid, scale=1.702), [Ak], [Sk])
                                P.dve(lambda e, U=U, ub=ub, bb=bb, c=c: e.tensor_scalar(out=U[:], in0=psum[:, ub, :], scalar1=bb[:, 8 + c:9 + c], scalar2=8.0, op0=ALU.add, op1=ALU.min), [ubk, bbk], [Uk])
                                P.pool(lambda e, A=A, S_=S_: e.tensor_tensor(out=A[:], in0=A[:], in1=S_[:], op=ALU.mult), [Ak, Sk], [Ak])
                                P.dve(lambda e, at=at, c=c, U=U, A=A: e.scalar_tensor_tensor(out=at[:, c, :], in0=U[:], scalar=-6.0, in1=A[:], op0=ALU.max, op1=ALU.mult), [Uk, Ak], [atk + str(c)])
                            atkeys = [atk + str(c) for c in range(8)]
                            if b + 1 < NB:
                                transposes(b + 1, nxt[4], nxt[5])
                            Y, Yk = Yr.next()
                            for s4 in range(4):
                                for nh in range(2):
                                    yb_, ybk = ybr.next()
                                    for c in range(8):
                                        P.pe(lambda e, yb_=yb_, at=at, c=c, s4=s4, WD=WD, nh=nh: e.matmul(psum[:, yb_, :], at[:, c, s4 * 128:(s4 + 1) * 128], WD[:, c, nh * 512:(nh + 1) * 512],
                                                                                                          start=(c == 0), stop=(c == 7)), atkeys + wdkeys, [ybk])
                                    if yb_ == 4:
                                        P.dve(lambda e, Y=Y, s4=s4, nh=nh, swb=swb: e.tensor_scalar_mul(out=Y[:, s4, nh * 512:(nh + 1) * 512], in0=psum[:, 4, :], scalar1=swb[:, s4:s4 + 1]),
                                              [ybk, swk], [Yk + '%d%d' % (s4, nh)])
                                    else:
                                        P.act(lambda e, Y=Y, s4=s4, nh=nh, swb=swb: e.activation(out=Y[:, s4, nh * 512:(nh + 1) * 512], in_=psum[:, 5, :], func=AF.Copy, scale=swb[:, s4:s4 + 1]),
                                              [ybk, swk], [Yk + '%d%d' % (s4, nh)])
                            P.dma('sp', YS[b * BS:(b + 1) * BS, :].rearrange("(s p) d -> p s d", p=128), Y[:], [Yk + '%d%d' % (q, r_) for q in range(4) for r_ in range(2)], [('YS', b)], Yk)
                        P.flush()
                    with contextlib.ExitStack() as st3:
                        def sb3(name, shape, dt=F32):
                            return st3.enter_context(nc.sbuf_tensor(uname(name), list(shape), dt))
                        g2 = [sb3("g2_%d" % s_, [128, D]) for s_ in range(2)]
                        for s_ in range(2):
                            bload('sp', g2[s_][:], modv[l, s_, 5 * D:6 * D], 'g2_%d' % s_)
                        lng = sb3("lng2", [128, D]); lnb = sb3("lnb2", [128, D])
                        bload('act', lng[:], ln_g[l, 1], 'lng')
                        bload('act', lnb[:], ln_b[l, 1], 'lnb')
                        x1r = Ring('xm', [sb3("xm%d" % i, [128, D]) for i in range(4)])
                        Gr = Ring('G', [sb3("G%d" % i, [128, 4, D]) for i in range(4)])
                        rr = sb3("rr2", [128, D])
                        xor_ = Ring('xo', [sb3("xo%d" % i, [128, D]) for i in range(2)])
                        st6 = sb3("st6b", [128, 2, 6]); mv = sb3("mvb", [128, 2]); rs = sb3("rsb", [128, 1])
                        bdr = sb3("bdr", [NE, D])
                        P.dma('act', bdr[:], b_dn[l], [], ['bdr'], 'bdr')
                        mwTr = Ring('mwT', [sb3("mwT%d" % i, [NE, 128]) for i in range(2)])
                        tbr = Ring('tb', [0, 1]); bbk2 = Ring('bd', [(2, 3), (4, 5)])
                        for j, i in enumerate(tiles):
                            s_ = 1 if i < NT_C else 0
                            tb_, tbk = tbr.next()
                            P.pe(lambda e, j=j, tb_=tb_: e.transpose(psum[0:NE, tb_, 0:128], mwd[:, j, :], ident[:]), ['ident'], [tbk])
                            mwT, mwTk = mwTr.next()
                            P.act(lambda e, mwT=mwT, tb_=tb_: e.activation(out=mwT[:], in_=psum[0:NE, tb_, 0:128], func=AF.Copy), [tbk], [mwTk])
                            (bd0, bd1), bdk = bbk2.next()
                            for nh, bdb_ in enumerate((bd0, bd1)):
                                P.pe(lambda e, mwT=mwT, nh=nh, bdb_=bdb_: e.matmul(psum[:, bdb_, :], mwT[:], bdr[:, nh * 512:(nh + 1) * 512], start=True, stop=True), [mwTk, 'bdr'], [bdk + str(nh)])
                            x1, x1k = x1r.next()
                            P.dma('sp', x1[:], XM[i * 128:(i + 1) * 128, :], [], [x1k], x1k)
                            G, Gk = Gr.next()
                            for k in range(4):
                                P.add('pool', lambda e, G=G, j=j, k=k: e.indirect_dma_start(out=G[:, k, :], out_offset=None, in_=YS[:, :],
                                                                                         in_offset=bass.IndirectOffsetOnAxis(ap=DKi[:, j, k:k + 1], axis=0)), ['DKi'], [Gk + str(k)], Gk + str(k))
                            P.dve(lambda e, G=G: e.tensor_tensor(out=G[:, 0, :], in0=G[:, 0, :], in1=G[:, 1, :], op=ALU.add), [Gk + '0', Gk + '1'], [Gk + '0'])
                            P.dve(lambda e, G=G: e.tensor_tensor(out=G[:, 2, :], in0=G[:, 2, :], in1=G[:, 3, :], op=ALU.add), [Gk + '2', Gk + '3'], [Gk + '2'])
                            P.dve(lambda e, G=G: e.tensor_tensor(out=G[:, 0, :], in0=G[:, 0, :], in1=G[:, 2, :], op=ALU.add), [Gk + '0', Gk + '2'], [Gk + '0'])
                            for nh, bdb_ in enumerate((bd0, bd1)):
                                P.dve(lambda e, G=G, nh=nh, bdb_=bdb_: e.tensor_tensor(out=G[:, 0, nh * 512:(nh + 1) * 512], in0=G[:, 0, nh * 512:(nh + 1) * 512], in1=psum[:, bdb_, :], op=ALU.add),
                                      [Gk + '0', bdk + str(nh)], [Gk + '0'])
                            P.dve(lambda e, G=G, s_=s_: e.tensor_tensor(out=rr[:], in0=G[:, 0, :], in1=g2[s_][:], op=ALU.mult), [Gk + '0', 'g2_%d' % s_], ['rr'])
                            P.dve(lambda e, x1=x1: e.scalar_tensor_tensor(out=rr[:], in0=x1[:], scalar=ALPHA, in1=rr[:], op0=ALU.mult, op1=ALU.add), [x1k, 'rr'], ['rr'])
                            for nh in range(2):
                                P.dve(lambda e, nh=nh: e.bn_stats(out=st6[:, nh, :], in_=rr[:, nh * 512:(nh + 1) * 512]), ['rr'], ['st6_%d' % nh])
                            P.dve(lambda e: e.bn_aggr(out=mv[:], in_=st6[:]), ['st6_0', 'st6_1'], ['mv'])
                            P.dve(lambda e: e.tensor_scalar_add(out=rs[:], in0=mv[:, 1:2], scalar1=EPS), ['mv'], ['rs'])
                            P.act(lambda e: e.activation(out=rs[:], in_=rs[:], func=AF.Sqrt), ['rs'], ['rs'])
                            P.dve(lambda e: e.reciprocal(out=rs[:], in_=rs[:]), ['rs'], ['rs'])
                            xo, xok = xor_.next()
                            P.dve(lambda e, xo=xo: e.tensor_scalar(out=xo[:], in0=rr[:], scalar1=mv[:, 0:1], scalar2=rs[:, 0:1], op0=ALU.subtract, op1=ALU.mult), ['rr', 'mv', 'rs'], [xok])
                            P.dve(lambda e, xo=xo: e.tensor_tensor(out=xo[:], in0=xo[:], in1=lng[:], op=ALU.mult), [xok, 'lng'], [xok])
                            P.dve(lambda e, xo=xo: e.tensor_tensor(out=xo[:], in0=xo[:], in1=lnb[:], op=ALU.add), [xok, 'lnb'], [xok])
                            dst = XS0[i * 128:(i + 1) * 128, :] if not last else out[(i - NT_C) * 128:(i - NT_C + 1) * 128, :]
                            P.dma('sp', dst, xo[:], [xok], [('xout', i)], xok)
                        P.flush()
        P.flush()
    return nc


def make_consts():
    inv = (10000.0 ** (-np.arange(0, 32, 2, dtype=np.float32) / 32.0)).astype(np.float32)
    t = np.arange(L)
    row = (t // 64).astype(np.float32)
    col = (t % 64).astype(np.float32)
    ang = np.stack([row[:, None] * inv, col[:, None] * inv], axis=1)
    cos = np.cos(ang).astype(np.float32)
    sin = np.sin(ang).astype(np.float32)
    c64 = np.stack([cos, cos], axis=2).reshape(L, 64)
    s64 = np.stack([-sin, sin], axis=2).reshape(L, 64)
    p = np.arange(128, dtype=np.float32)
    pos = np.stack([127 - p, p, p + 1, 128 - p], axis=1)
    dif = p[None, :] - p[:, None]
    return dict(
        c_ident=np.eye(128, dtype=np.float32),
        c_cos=np.ascontiguousarray(np.tile(c64, (1, 8))),
        c_sin=np.ascontiguousarray(np.tile(s64, (1, 8))),
        c_pos=np.ascontiguousarray(pos.astype(np.float32)),
        c_dpos=np.maximum(dif, 0).astype(np.float32),
        c_dneg=np.maximum(-dif, 0).astype(np.float32),
        c_mge=(dif >= 0).astype(np.float32),
        c_zeros=np.zeros((2, 256), np.float32),
        c_iota32=np.tile(np.arange(NE, dtype=np.float32)[None, :], (128, 1)),
        c_lts=(p[:, None] < p[None, :]).astype(np.float32),
        c_bstart=np.tile((np.arange(NBMAX, dtype=np.float32) * BS)[None, :], (128, 1)),
        c_kp=(np.arange(8, dtype=np.float32)[None, :] * 128 + p[:, None]).astype(np.float32),
        c_iotap=p[:, None].astype(np.float32).copy(),
    )


WKEYS = ['w_mod', 'b_mod', 'w_in', 'conv_w', 'ret_decay_exp', 'ret_gn_g', 'q_norm_g', 'k_norm_g', 'w_out',
         'ln_g', 'ln_b', 'w_router', 'b_router', 'w_gate_up', 'b_gate_up', 'w_down', 'b_down']


def make_in_maps(inputs, cores, skip=()):
    consts = make_consts()
    shared = {k: np.ascontiguousarray(np.asarray(inputs[k], np.float32)) for k in WKEYS if k not in skip}
    shared['c_ctx'] = np.ascontiguousarray(np.asarray(inputs['c_ctx'], np.float32))
    shared.update(consts)
    maps = []
    for b in cores:
        m = dict(shared)
        m['x'] = np.ascontiguousarray(np.asarray(inputs['x'][b], np.float32))
        m['c'] = np.ascontiguousarray(np.asarray(inputs['c'][b], np.float32))
        m['ctx'] = np.ascontiguousarray(np.asarray(inputs['ctx'][b], np.float32))
        maps.append(m)
    return maps


def kernel(**inputs):
    nc = build()
    maps = make_in_maps(inputs, list(range(8)))
    res = run_bass_kernel_spmd(nc, maps, core_ids=list(range(8)))
    return np.stack([np.asarray(r["out"], np.float32) for r in res.results], axis=0)
```

```python
import contextlib
import math
import numpy as np
import concourse.bass as bass
import concourse.mybir as mybir
from concourse.bass_utils import run_bass_kernel_spmd

F32 = mybir.dt.float32
BF16 = mybir.dt.bfloat16
ALU = mybir.AluOpType
AF = mybir.ActivationFunctionType
AX = mybir.AxisListType

D = 1024
L = 4096
LC = 256
NT_C = 2
NT = 34
DEPTH = 2
NE = 32
BS = 512
NBMAX = 66
I32 = mybir.dt.int32
U32 = mybir.dt.uint32
ALPHA = (2.0 * DEPTH) ** 0.25
EPS = 1e-6


class Prog:
    ENGS = ('pe', 'act', 'dve', 'pool', 'sp')

    def __init__(self, nc, stack):
        self.nc = nc
        self.ops = []
        self.sems = {}
        self.stack = stack
        for eng in ('pe', 'act', 'dve', 'pool'):
            self.sems[('e', eng)] = stack.enter_context(nc.semaphore('sem_' + eng))
        self.cnt = {}
        self.streams = {}
        self.pool_cnt = []
        self.waited = {e: {} for e in self.ENGS}
        self.total_ops = 0

    def add(self, eng, fn, reads=(), writes=(), stream=None):
        self.ops.append((eng, fn, tuple(reads), tuple(writes), stream))

    def pe(self, fn, reads=(), writes=()):
        self.add('pe', fn, reads, writes)

    def act(self, fn, reads=(), writes=()):
        self.add('act', fn, reads, writes)

    def dve(self, fn, reads=(), writes=()):
        self.add('dve', fn, reads, writes)

    def pool(self, fn, reads=(), writes=()):
        self.add('pool', fn, reads, writes)

    def dma(self, q, out, in_, reads, writes, stream, **kw):
        self.add(q, lambda e: e.dma_start(out=out, in_=in_, **kw), reads, writes, stream)

    def flush(self):
        nc = self.nc
        ops = self.ops
        self.ops = []
        n = len(ops)
        if n == 0:
            return
        self.total_ops += n
        last_writer = {}
        readers = {}
        deps = [None] * n
        for i, (eng, fn, rd, wr, st) in enumerate(ops):
            d = set()
            for r in rd:
                j = last_writer.get(r)
                if j is not None:
                    d.add((j, 0))
            for w in wr:
                j = last_writer.get(w)
                if j is not None:
                    d.add((j, 1))
                for k in readers.get(w, ()):
                    if k != i:
                        d.add((k, 2))
            deps[i] = d
            for r in rd:
                readers.setdefault(r, []).append(i)
            for w in wr:
                last_writer[w] = i
                readers[w] = []
        sig = [False] * n
        need = [None] * n
        last_compute = {}
        for i in range(n):
            eng = ops[i][0]
            lst = set()
            for (j, kind) in deps[i]:
                jeng, _, _, _, jst = ops[j]
                if jst is None and jeng == eng:
                    if eng == 'pe' or eng == 'sp':
                        continue
                    if kind == 2:
                        continue
                if jst is None:
                    sig[j] = True
                lst.add(j)
            need[i] = lst
            if ops[i][4] is None and ops[i][1] is not None:
                last_compute[eng] = i
        for eng, i in last_compute.items():
            if eng != 'sp':
                sig[i] = True
        sval = [None] * n
        phase_map = {}
        for i, (eng, fn, rd, wr, st) in enumerate(ops):
            if st is not None:
                if st not in phase_map:
                    k = len(phase_map)
                    phase_map[st] = k
                    if k >= len(self.pool_cnt):
                        self.pool_cnt.append(0)
                        self.sems[('s', k)] = self.stack.enter_context(nc.semaphore('sd_%d' % k))
                k = phase_map[st]
                self.pool_cnt[k] += 1
                sval[i] = (('s', k), 16 * self.pool_cnt[k])
            elif sig[i]:
                self.cnt[eng] = self.cnt.get(eng, 0) + 1
                sval[i] = (('e', eng), self.cnt[eng])
        per_eng = {e: [] for e in self.ENGS}
        for i, op in enumerate(ops):
            per_eng[op[0]].append(i)
        sems = self.sems
        final = {}
        for eng in ('pe', 'act', 'dve', 'pool'):
            if self.cnt.get(eng, 0) > 0:
                final[('e', eng)] = self.cnt[eng]
        for k, c in enumerate(self.pool_cnt):
            final[('s', k)] = 16 * c

        def run(engname, e):
            waited = self.waited[engname]
            for i in per_eng[engname]:
                _, fn, rd, wr, st = ops[i]
                w = {}
                for j in need[i]:
                    key, val = sval[j]
                    if w.get(key, 0) < val:
                        w[key] = val
                for key, val in w.items():
                    if waited.get(key, 0) >= val:
                        continue
                    waited[key] = val
                    e.wait_ge(sems[key], val)
                if fn is None:
                    continue
                ins = fn(e)
                if sval[i] is not None:
                    key, val = sval[i]
                    ins.then_inc(sems[key], 16 if key[0] == 's' else 1)
            for key, val in final.items():
                if key == ('e', engname):
                    continue
                if waited.get(key, 0) >= val:
                    continue
                waited[key] = val
                e.wait_ge(sems[key], val)

        with nc.Block() as block:
            @block.tensor
            def _(e):
                run('pe', e)

            @block.scalar
            def _(e):
                run('act', e)

            @block.vector
            def _(e):
                run('dve', e)

            @block.gpsimd
            def _(e):
                run('pool', e)

            @block.sync
            def _(e):
                run('sp', e)


def pipeline_reorder(P, base, marks):
    ops = P.ops
    H = [ops[a:b] for (a, b, c) in marks]
    T = [ops[b:c] for (a, b, c) in marks]
    new = list(ops[:base]) + H[0]
    for i in range(len(marks)):
        if i + 1 < len(marks):
            new += H[i + 1]
        new += T[i]
    new += list(ops[marks[-1][2]:])
    assert len(new) == len(ops)
    P.ops = new


class Ring:
    def __init__(self, name, tiles):
        self.name = name
        self.tiles = tiles
        self.i = 0

    def next(self):
        k = self.i % len(self.tiles)
        self.i += 1
        return self.tiles[k], '%s%d' % (self.name, k)


SEC = dict(u=(0, 256), B=(256, 256), C=(512, 256), rq=(768, 256), rk=(1024, 256), rv=(1280, 256),
           rg=(1536, 256), aq=(1792, 512), ak=(2304, 128), av=(2432, 128))
MYCOL = dict(u=0, C=256, B=512, rv=768, rq=1024, rk=1280, aq=1536, rg=2048, ak=2304, av=2432)


def build(phases=('p0', 'p1', 'att', 'ret', 'p3', 'moe'), layers=(0, 1), debug=False, moe_experts=NE, cut=99):
    nc = bass.Bass("TRN2", target_bir_lowering=False)

    _uc = [0]

    def uname(name):
        _uc[0] += 1
        return '%s_u%d' % (name, _uc[0])

    def din(name, shape, dt=F32):
        return nc.dram_tensor(name, list(shape), dt, kind="ExternalInput").ap()

    def dscr(name, shape, dt=F32):
        return nc.dram_tensor(name, list(shape), dt, kind=("ExternalOutput" if debug else "Internal")).ap()

    x_in = din("x", [L, D])
    c_in = din("c", [D])
    ctx_in = din("ctx", [LC, D])
    cctx_in = din("c_ctx", [D])
    w_mod = din("w_mod", [DEPTH, D, 6 * D])
    b_mod = din("b_mod", [DEPTH, 6 * D])
    w_in = din("w_in", [DEPTH, D, 2560])
    conv_w = din("conv_w", [DEPTH, 256, 3])
    rde = din("ret_decay_exp", [DEPTH, 2, 4])
    gn_g = din("ret_gn_g", [DEPTH, 256])
    qn_g = din("q_norm_g", [DEPTH, 64])
    kn_g = din("k_norm_g", [DEPTH, 64])
    w_out = din("w_out", [DEPTH, D, D])
    ln_g = din("ln_g", [DEPTH, 2, D])
    ln_b = din("ln_b", [DEPTH, 2, D])
    w_router = din("w_router", [DEPTH, D, NE])
    b_router = din("b_router", [DEPTH, NE])
    if 'moe' in phases:
        w_gu = din("w_gate_up", [DEPTH, NE, D, 2 * D])
        b_gu = din("b_gate_up", [DEPTH, NE, 2 * D])
        w_dn = din("w_down", [DEPTH, NE, D, D])
        b_dn = din("b_down", [DEPTH, NE, D])
    ident_in = din("c_ident", [128, 128])
    cos_in = din("c_cos", [L, 512])
    sin_in = din("c_sin", [L, 512])
    pos_in = din("c_pos", [128, 4])
    dpos_in = din("c_dpos", [128, 128])
    dneg_in = din("c_dneg", [128, 128])
    mge_in = din("c_mge", [128, 128])
    zeros_in = din("c_zeros", [2, 256])
    iota32_in = din("c_iota32", [128, NE])
    lts_in = din("c_lts", [128, 128])
    bstart_in = din("c_bstart", [128, NBMAX])
    kp_in = din("c_kp", [128, 8])
    iotap_in = din("c_iotap", [128, 1])

    out = nc.dram_tensor("out", [L, D], F32, kind="ExternalOutput").ap()

    N = NT * 128
    modv = dscr("modv", [DEPTH, 2, 6 * D])
    PB = dscr("PB", [N + 4, 512])
    RQT = dscr("RQT", [3, 4, 64, N], BF16)
    RKT = dscr("RKT", [4, 64, N], BF16)
    RTOK = dscr("RTOK", [N, 768], BF16)
    RG = dscr("RG", [N, 256])
    AQT = dscr("AQT", [8, 64, N], BF16)
    AKT = dscr("AKT", [2, 64, N], BF16)
    AV = dscr("AV", [N, 128], BF16)
    MIX = dscr("MIX", [N, 1024])
    XS0 = dscr("XS0", [N, D])
    XM = dscr("XM", [N, D])
    H2R = dscr("H2R", [N, D], BF16)
    RW = dscr("RW", [N, 4])
    RE = dscr("RE", [N, 4])
    XS = dscr("XS", [NBMAX * BS, D], BF16)
    SW = dscr("SW", [NBMAX * BS, 1])
    YS = dscr("YS", [NBMAX * BS, D])
    BGT = dscr("BGT", [DEPTH * NE * 128, 16])

    def prow(i):
        return 1 + i * 128 if i < NT_C else 259 + (i - NT_C) * 128

    with contextlib.ExitStack() as gst:
        P = Prog(nc, gst)
        psum = gst.enter_context(nc.psum_tensor("psum", [128, 8, 512], F32))
        ident = gst.enter_context(nc.sbuf_tensor("ident", [128, 128], F32))
        P.dma('sp', ident[:], ident_in, [], ['ident'], 'ident')

        def bank(b):
            return psum[:, b, :]

        if 'p0' in phases:
            with contextlib.ExitStack() as st:
                def sb(name, shape, dt=F32):
                    return st.enter_context(nc.sbuf_tensor(uname(name), list(shape), dt))
                cnd = sb("cnd", [128, 2, 8])
                P.dma('sp', cnd[:, 0, :], c_in.rearrange("(k p) -> p k", p=128), [], ['cnd'], 'cnd0',
                      allow_slow_non_contiguous=True)
                P.dma('sp', cnd[:, 1, :], cctx_in.rearrange("(k p) -> p k", p=128), [], ['cnd'], 'cnd1',
                      allow_slow_non_contiguous=True)
                P.act(lambda e: e.activation(out=cnd[:], in_=cnd[:], func=AF.Silu), ['cnd'], ['cnd'])
                wring = Ring('wm', [sb("wm%d" % i, [128, 8, 512]) for i in range(6)])
                bring = Ring('bm', [sb("bm%d" % i, [1, 512]) for i in range(2)])
                oring = Ring('om', [sb("om%d" % i, [1, 2, 512]) for i in range(2)])
                for l in layers:
                    for n in range(12):
                        wt, wk = wring.next()
                        bt, bk = bring.next()
                        ot, ok = oring.next()
                        P.dma('sp' if n % 2 == 0 else 'act', wt[:], w_mod[l, :, n * 512:(n + 1) * 512].rearrange("(k p) n -> p k n", p=128),
                              [], [wk], wk)
                        P.dma('sp', bt[:], b_mod[l, n * 512:(n + 1) * 512].rearrange("(o n) -> o n", o=1), [], [bk], bk)
                        for s in range(2):
                            pb_ = 'ps0_%d' % s
                            for k in range(8):
                                P.pe(lambda e, s=s, k=k, wt=wt: e.matmul(psum[0:1, s, :], cnd[:, s, k:k + 1], wt[:, k, :],
                                                                     start=(k == 0), stop=(k == 7)),
                                     ['cnd', wk], [pb_])
                            P.dve(lambda e, s=s, ot=ot, bt=bt: e.tensor_tensor(out=ot[:, s, :], in0=psum[0:1, s, :],
                                                                               in1=bt[:], op=ALU.add),
                                  [pb_, bk], [ok])
                        if n in (2, 3, 8, 9):
                            P.dve(lambda e, ot=ot: e.tensor_scalar_add(out=ot[:], in0=ot[:], scalar1=1.0), [ok], [ok])
                        P.dma('sp', modv[l, :, n * 512:(n + 1) * 512].rearrange("(o s) n -> o s n", o=1), ot[:],
                              [ok], [('modv', l)], ok)
                P.flush()

        def bload(q, tile_ap, vec_ap, key, reads=()):
            P.dma(q, tile_ap, vec_ap.partition_broadcast(128), list(reads), [key], key)

        for l in layers:
            last = (l == DEPTH - 1)
            xsrc = (lambda i: (ctx_in[i * 128:(i + 1) * 128, :] if i < NT_C else x_in[(i - NT_C) * 128:(i - NT_C + 1) * 128, :])) \
                if l == 0 else (lambda i: XS0[i * 128:(i + 1) * 128, :])
            xs_key = (lambda i: ('xin', i)) if l == 0 else (lambda i: ('XS0', i))
            out_tiles = list(range(NT)) if not last else list(range(NT_C, NT))

            if 'p1' in phases:
                with contextlib.ExitStack() as st:
                    def sb(name, shape, dt=F32):
                        return st.enter_context(nc.sbuf_tensor(uname(name), list(shape), dt))
                    win = sb("win", [128, 8, 2560], BF16)
                    for name, (c0, w) in SEC.items():
                        m0 = MYCOL[name]
                        P.dma('pool', win[:, :, m0:m0 + w], w_in[l, :, c0:c0 + w].rearrange("(k p) n -> p k n", p=128),
                              [], ['win_' + name], 'win_' + name)
                    winkeys = ['win_' + k for k in SEC]
                    sc1 = [sb("sc1_%d" % s, [128, D]) for s in range(2)]
                    sh1 = [sb("sh1_%d" % s, [128, D]) for s in range(2)]
                    for s in range(2):
                        bload('sp', sc1[s][:], modv[l, s, D:2 * D], 'sc1_%d' % s, [('modv', l)])
                        bload('sp', sh1[s][:], modv[l, s, 0:D], 'sh1_%d' % s, [('modv', l)])
                    gq = sb("gq", [128, 64])
                    gk = sb("gk", [128, 64])
                    bload('act', gq[:], qn_g[l], 'gq')
                    bload('act', gk[:], kn_g[l], 'gk')
                    zpad = sb("zpad", [2, 256])
                    P.dma('act', zpad[:], zeros_in, [], ['zpad'], 'zpad')
                    for r0 in (0, 257):
                        P.dma('act', PB[r0:r0 + 2, 0:256] if r0 else PB[0:1, 0:256], zpad[0:2, :] if r0 else zpad[0:1, :],
                              ['zpad'], [('PBpad', r0)], 'zp%d' % r0)
                    P.dma('act', PB[N + 3:N + 4, 0:256], zpad[0:1, :], ['zpad'], [('PBpad', 3)], 'zp3')
                    posc = sb("posc", [128, 4])
                    P.dma('act', posc[:], pos_in, [], ['posc'], 'posc')
                    lg = sb("lg", [128, 8])
                    bload('act', lg[:], rde[l].rearrange("a h -> (a h)"), 'lg')
                    P.act(lambda e: e.activation(out=lg[:], in_=lg[:], func=AF.Exp, scale=-math.log(2.0)), ['lg'], ['lg'])
                    P.act(lambda e: e.activation(out=lg[:], in_=lg[:], func=AF.Ln, scale=-1.0, bias=1.0), ['lg'], ['lg'])
                    tab4 = sb("tab4", [128, 4, 4])
                    for ti, (di, pc) in enumerate(((0, 0), (1, 1), (0, 2), (1, 3))):
                        P.act(lambda e, ti=ti, di=di, pc=pc: e.activation(out=tab4[:, ti, :], in_=lg[:, di * 4:(di + 1) * 4],
                                                                          func=AF.Exp, scale=posc[:, pc:pc + 1]),
                              ['lg', 'posc'], ['tab4'])
                    P.dve(lambda e: e.tensor_scalar_mul(out=tab4[:, 0:2, :], in0=tab4[:, 0:2, :], scalar1=0.125), ['tab4'], ['tab4'])
                    TAB = sb("TAB", [128, 4, 4, 64])
                    P.dve(lambda e: e.tensor_copy(out=TAB[:].rearrange("p t h d -> p (t h) d"),
                                                  in_=tab4[:].rearrange("p t h -> p (t h)").unsqueeze(2).to_broadcast([128, 16, 64])),
                          ['tab4'], ['TAB'])
                    gq8 = sb("gq8", [128, 8, 64])
                    P.dve(lambda e: e.tensor_copy(out=gq8[:], in_=gq[:].unsqueeze(1).to_broadcast([128, 8, 64])), ['gq'], ['gq8'])
                    gk2 = sb("gk2", [128, 2, 64])
                    P.dve(lambda e: e.tensor_copy(out=gk2[:], in_=gk[:].unsqueeze(1).to_broadcast([128, 2, 64])), ['gk'], ['gk2'])

                    xring = Ring('xt', [sb("xt%d" % i, [128, D]) for i in range(3)])
                    csring = Ring('cs', [sb("cs%d" % i, [128, 512]) for i in range(3)])
                    snring = Ring('sn', [sb("sn%d" % i, [128, 512]) for i in range(3)])
                    hring = Ring('h', [sb("h%d" % i, [128, D]) for i in range(2)])
                    hTring = Ring('hT', [sb("hT%d" % i, [128, 8, 128], BF16) for i in range(2)])
                    usb = sb("usb", [128, 256])
                    pcb = Ring('pcb', [sb("pcb%d" % i, [128, 512]) for i in range(2)])
                    t1 = sb("t1", [128, 512])
                    t2 = sb("t2", [128, 512])
                    rq = sb("rq", [128, 4, 256])
                    rtok = Ring('rtok', [sb("rtok%d" % i, [128, 768], BF16) for i in range(2)])
                    rgt = Ring('rgt', [sb("rgt%d" % i, [128, 256]) for i in range(2)])
                    rT = Ring('rT', [sb("rT%d" % i, [128, 8, 128], BF16) for i in range(2)])
                    sq = sb("sq", [128, 512])
                    ss = sb("ss", [128, 8])
                    aq = sb("aq", [128, 512])
                    aqn = sb("aqn", [128, 512])
                    akn = sb("akn", [128, 128])
                    ak = sb("ak", [128, 128])
                    aT = Ring('aT', [sb("aT%d" % i, [128, 5, 128], BF16) for i in range(2)])
                    avt = Ring('avt', [sb("avt%d" % i, [128, 128], BF16) for i in range(2)])

                    def rope(src_ap, W, dst_ap, src_keys, dst_key, cs, ck, sn, sk):
                        g = W // 32
                        P.dve(lambda e: e.tensor_tensor(out=t1[:, :W], in0=src_ap, in1=cs[:, :W], op=ALU.mult),
                              src_keys + [ck], ['t1'])
                        s4 = src_ap.rearrange("p (g a f) -> p g a f", a=2, f=16)
                        t4 = t2[:, :W].rearrange("p (g a f) -> p g a f", a=2, f=16)
                        n4 = sn[:, :W].rearrange("p (g a f) -> p g a f", a=2, f=16)
                        P.dve(lambda e: e.tensor_tensor(out=t4[:, :, 0, :], in0=s4[:, :, 1, :], in1=n4[:, :, 0, :], op=ALU.mult),
                              src_keys + [sk], ['t2a'])
                        P.dve(lambda e: e.tensor_tensor(out=t4[:, :, 1, :], in0=s4[:, :, 0, :], in1=n4[:, :, 1, :], op=ALU.mult),
                              src_keys + [sk], ['t2b'])
                        P.pool(lambda e: e.tensor_tensor(out=dst_ap, in0=t1[:, :W], in1=t2[:, :W], op=ALU.add),
                               ['t1', 't2a', 't2b'], [dst_key])

                    ld = {}

                    def issue_loads(i):
                        xt, xk = xring.next()
                        P.dma('sp', xt[:], xsrc(i), [xs_key(i)], [xk], xk)
                        if i >= NT_C:
                            cs, ck = csring.next()
                            sn, sk = snring.next()
                            t0 = (i - NT_C) * 128
                            P.dma('sp', cs[:], cos_in[t0:t0 + 128, :], [], [ck], ck)
                            P.dma('sp', sn[:], sin_in[t0:t0 + 128, :], [], [sk], sk)
                            ld[i] = (xt, xk, cs, ck, sn, sk)
                        else:
                            ld[i] = (xt, xk, None, None, None, None)
                    issue_loads(0)
                    p1_base = len(P.ops)
                    p1_marks = []
                    for i in range(NT):
                        isctx = i < NT_C
                        s = 1 if isctx else 0
                        m_a = len(P.ops)
                        if i + 1 < NT:
                            issue_loads(i + 1)
                        xt, xk, cs, ck, sn, sk = ld.pop(i)
                        h, hk = hring.next()
                        P.dve(lambda e, h=h, xt=xt, s=s: e.tensor_tensor(out=h[:], in0=xt[:], in1=sc1[s][:], op=ALU.mult),
                              [xk, 'sc1_%d' % s], [hk])
                        P.dve(lambda e, h=h, s=s: e.tensor_tensor(out=h[:], in0=h[:], in1=sh1[s][:], op=ALU.add),
                              [hk, 'sh1_%d' % s], [hk])
                        for k in range(8):
                            P.pe(lambda e, h=h, k=k: e.transpose(psum[:, 5 + k // 4, (k % 4) * 128:(k % 4 + 1) * 128],
                                                                 h[:, k * 128:(k + 1) * 128], ident[:]),
                                 [hk, 'ident'], ['pT%d' % k])
                        hT, hTk = hTring.next()
                        for hh in range(2):
                            P.act(lambda e, hT=hT, hh=hh: e.activation(out=hT[:, hh * 4:(hh + 1) * 4, :].rearrange("p k n -> p (k n)"),
                                                                       in_=psum[:, 5 + hh, :], func=AF.Copy),
                                  ['pT%d' % k for k in range(hh * 4, hh * 4 + 4)], [hTk + '_%d' % hh])
                        m_b = len(P.ops)
                        for b in range(5):
                            for k in range(8):
                                P.pe(lambda e, hT=hT, b=b, k=k: e.matmul(bank(b), hT[:, k, :], win[:, k, b * 512:(b + 1) * 512],
                                                                         start=(k == 0), stop=(k == 7)),
                                     [hTk + '_%d' % (k // 4)] + winkeys, ['z%d' % b])
                        pc, pck = pcb.next()
                        P.act(lambda e: e.activation(out=usb[:], in_=psum[:, 0, 0:256], func=AF.Copy), ['z0'], ['usb'])
                        P.dve(lambda e, pc=pc: e.tensor_tensor(out=pc[:, 0:256], in0=psum[:, 0, 256:512], in1=usb[:], op=ALU.mult),
                              ['z0', 'usb'], [pck + 'a'])
                        P.act(lambda e, pc=pc: e.activation(out=pc[:, 256:512], in_=psum[:, 1, 0:256], func=AF.Copy), ['z1'], [pck + 'b'])
                        r0 = prow(i)
                        P.dma('sp', PB[r0:r0 + 128, :], pc[:], [pck + 'a', pck + 'b'], [('PB', i)], pck)
                        rt, rtk = rtok.next()
                        P.act(lambda e, rt=rt: e.activation(out=rt[:, 512:768], in_=psum[:, 1, 256:512], func=AF.Copy), ['z1'], [rtk + 'v'])
                        if isctx:
                            P.act(lambda e: e.activation(out=rq[:, 0, :], in_=psum[:, 2, 0:256], func=AF.Copy), ['z2'], ['rq0'])
                            P.act(lambda e: e.activation(out=rq[:, 3, :], in_=psum[:, 2, 256:512], func=AF.Copy), ['z2'], ['rq3'])
                        else:
                            rope(psum[:, 2, 0:256], 256, rq[:, 0, :], ['z2'], 'rq0', cs, ck, sn, sk)
                            rope(psum[:, 2, 256:512], 256, rq[:, 3, :], ['z2'], 'rq3', cs, ck, sn, sk)
                        TABf = TAB[:].rearrange("p t h d -> p t (h d)")
                        P.dve(lambda e: e.tensor_tensor(out=rq[:, 1, :], in0=rq[:, 0, :], in1=TABf[:, 2, :], op=ALU.mult), ['rq0', 'TAB'], ['rq1'])
                        P.pool(lambda e: e.tensor_tensor(out=rq[:, 2, :], in0=rq[:, 0, :], in1=TABf[:, 3, :], op=ALU.mult), ['rq0', 'TAB'], ['rq2'])
                        P.dve(lambda e, rt=rt: e.tensor_tensor(out=rt[:, 0:256], in0=rq[:, 3, :], in1=TABf[:, 0, :], op=ALU.mult), ['rq3', 'TAB'], [rtk + 'f'])
                        P.pool(lambda e, rt=rt: e.tensor_tensor(out=rt[:, 256:512], in0=rq[:, 3, :], in1=TABf[:, 1, :], op=ALU.mult), ['rq3', 'TAB'], [rtk + 'b'])
                        P.dma('sp', RTOK[i * 128:(i + 1) * 128, :], rt[:], [rtk + 'v', rtk + 'f', rtk + 'b'], [('RTOK', i)], rtk)
                        rg_, rgk = rgt.next()
                        P.act(lambda e, rg_=rg_: e.activation(out=rg_[:], in_=psum[:, 4, 0:256], func=AF.Silu), ['z4'], [rgk])
                        P.dma('act', RG[i * 128:(i + 1) * 128, :], rg_[:], [rgk], [('RG', i)], rgk)
                        for t in range(4):
                            for c2 in range(2):
                                idx = t * 2 + c2
                                P.pe(lambda e, t=t, c2=c2, idx=idx: e.transpose(psum[:, 5 + idx // 4, (idx % 4) * 128:(idx % 4 + 1) * 128],
                                                                                rq[:, t, c2 * 128:(c2 + 1) * 128], ident[:]),
                                     ['rq%d' % t, 'ident'], ['pT%d' % idx])
                        rTt, rTk = rT.next()
                        for hh in range(2):
                            if hh == 0:
                                P.act(lambda e, rTt=rTt: e.activation(out=rTt[:, 0:4, :].rearrange("p k n -> p (k n)"), in_=psum[:, 5, :], func=AF.Copy),
                                      ['pT0', 'pT1', 'pT2', 'pT3'], [rTk + 'a'])
                            else:
                                P.act(lambda e, rTt=rTt: e.activation(out=rTt[:, 4:6, :].rearrange("p k n -> p (k n)"), in_=psum[:, 6, 0:256], func=AF.Copy),
                                      ['pT4', 'pT5'], [rTk + 'b'])
                                P.act(lambda e, rTt=rTt: e.activation(out=rTt[:, 6:8, :].rearrange("p k n -> p (k n)"), in_=psum[:, 6, 256:512], func=AF.Copy, scale=0.125),
                                      ['pT6', 'pT7'], [rTk + 'c'])
                        for t in range(3):
                            P.dma('act', RQT[t, :, :, i * 128:(i + 1) * 128].rearrange("(c q) d n -> (q d) c n", q=2),
                                  rTt[:, 2 * t:2 * t + 2, :], [rTk + 'a', rTk + 'b'], [('RQT', i, t)], rTk + 'q%d' % t)
                        P.dma('act', RKT[:, :, i * 128:(i + 1) * 128].rearrange("(c q) d n -> (q d) c n", q=2),
                              rTt[:, 6:8, :], [rTk + 'c'], [('RKT', i)], rTk + 'k')
                        P.act(lambda e: e.activation(out=sq[:], in_=psum[:, 3, :], func=AF.Square), ['z3'], ['sq'])
                        P.dve(lambda e: e.tensor_reduce(out=ss[:], in_=sq[:].rearrange("p (h d) -> p h d", d=64), axis=AX.X, op=ALU.add), ['sq'], ['ss'])
                        P.dve(lambda e: e.tensor_scalar(out=ss[:], in0=ss[:], scalar1=1.0 / 64, scalar2=EPS, op0=ALU.mult, op1=ALU.add), ['ss'], ['ss'])
                        P.act(lambda e: e.activation(out=ss[:], in_=ss[:], func=AF.Sqrt), ['ss'], ['ss'])
                        P.dve(lambda e: e.reciprocal(out=ss[:], in_=ss[:]), ['ss'], ['ss'])
                        P.dve(lambda e: e.tensor_tensor(out=aqn[:].rearrange("p (h d) -> p h d", d=64), in0=psum[:, 3, :].rearrange("p (h d) -> p h d", d=64),
                                                        in1=ss[:].unsqueeze(2).to_broadcast([128, 8, 64]), op=ALU.mult), ['z3', 'ss'], ['aqn'])
                        P.pool(lambda e: e.tensor_tensor(out=aqn[:], in0=aqn[:], in1=gq8[:].rearrange("p h d -> p (h d)"), op=ALU.mult), ['aqn', 'gq8'], ['aqn'])
                        if isctx:
                            aq_src, aq_key = aqn, 'aqn'
                        else:
                            rope(aqn[:], 512, aq[:], ['aqn'], 'aq', cs, ck, sn, sk)
                            aq_src, aq_key = aq, 'aq'
                        av_, avk = avt.next()
                        P.act(lambda e, av_=av_: e.activation(out=av_[:], in_=psum[:, 4, 384:512], func=AF.Copy), ['z4'], [avk])
                        P.dma('act', AV[i * 128:(i + 1) * 128, :], av_[:], [avk], [('AV', i)], avk)
                        P.act(lambda e: e.activation(out=sq[:, 0:128], in_=psum[:, 4, 256:384], func=AF.Square), ['z4'], ['sqk'])
                        P.dve(lambda e: e.tensor_reduce(out=ss[:, 0:2], in_=sq[:, 0:128].rearrange("p (h d) -> p h d", d=64), axis=AX.X, op=ALU.add), ['sqk'], ['ssk'])
                        P.dve(lambda e: e.tensor_scalar(out=ss[:, 0:2], in0=ss[:, 0:2], scalar1=1.0 / 64, scalar2=EPS, op0=ALU.mult, op1=ALU.add), ['ssk'], ['ssk'])
                        P.act(lambda e: e.activation(out=ss[:, 0:2], in_=ss[:, 0:2], func=AF.Sqrt), ['ssk'], ['ssk'])
                        P.dve(lambda e: e.reciprocal(out=ss[:, 0:2], in_=ss[:, 0:2]), ['ssk'], ['ssk'])
                        P.dve(lambda e: e.tensor_tensor(out=akn[:].rearrange("p (h d) -> p h d", d=64), in0=psum[:, 4, 256:384].rearrange("p (h d) -> p h d", d=64),
                                                        in1=ss[:, 0:2].unsqueeze(2).to_broadcast([128, 2, 64]), op=ALU.mult), ['z4', 'ssk'], ['akn'])
                        P.pool(lambda e: e.tensor_tensor(out=akn[:], in0=akn[:], in1=gk2[:].rearrange("p h d -> p (h d)"), op=ALU.mult), ['akn', 'gk2'], ['akn'])
                        if isctx:
                            ak_src, ak_key = akn, 'akn'
                        else:
                            rope(akn[:], 128, ak[:], ['akn'], 'ak', cs, ck, sn, sk)
                            ak_src, ak_key = ak, 'ak'
                        for c4 in range(4):
                            P.pe(lambda e, c4=c4, aq_src=aq_src: e.transpose(psum[:, 7, c4 * 128:(c4 + 1) * 128], aq_src[:, c4 * 128:(c4 + 1) * 128], ident[:]),
                                 [aq_key, 'ident'], ['pA%d' % c4])
                        P.pe(lambda e, ak_src=ak_src: e.transpose(psum[:, 5, 0:128], ak_src[:, 0:128], ident[:]), [ak_key, 'ident'], ['pT0'])
                        aTt, aTk = aT.next()
                        P.act(lambda e, aTt=aTt: e.activation(out=aTt[:, 0:4, :].rearrange("p k n -> p (k n)"), in_=psum[:, 7, :], func=AF.Copy),
                              ['pA0', 'pA1', 'pA2', 'pA3'], [aTk + 'q'])
                        P.act(lambda e, aTt=aTt: e.activation(out=aTt[:, 4, :], in_=psum[:, 5, 0:128], func=AF.Copy), ['pT0'], [aTk + 'k'])
                        P.dma('sp', AQT[:, :, i * 128:(i + 1) * 128].rearrange("(c q) d n -> (q d) c n", q=2), aTt[:, 0:4, :],
                              [aTk + 'q'], [('AQT', i)], aTk + 'q')
                        P.dma('sp', AKT[:, :, i * 128:(i + 1) * 128].rearrange("q d n -> (q d) n"), aTt[:, 4, :],
                              [aTk + 'k'], [('AKT', i)], aTk + 'k')
                        p1_marks.append((m_a, m_b, len(P.ops)))
                    pipeline_reorder(P, p1_base, p1_marks)
                    P.flush()


            if 'att' in phases:
                with contextlib.ExitStack() as st:
                    def sb(name, shape, dt=F32):
                        return st.enter_context(nc.sbuf_tensor(uname(name), list(shape), dt))
                    KT = sb("KT", [128, 2, N], BF16)
                    P.pool(lambda e: e.memset(KT[64:128, :, :], 0.0), [], ['KTz'])
                    P.dma('sp', KT[0:64, :, :], AKT.rearrange("k d n -> d k n"), [], ['KT'], 'KT')
                    V1 = sb("V1", [128, NT, 2, 65], BF16)
                    P.pool(lambda e: e.memset(V1[:], 1.0), [], ['V1'])
                    for kk_ in range(2):
                        P.dma('act', V1[:, :, kk_, 0:64], AV[:, kk_ * 64:(kk_ + 1) * 64].rearrange("(c p) d -> p c d", p=128), [], ['V1'], 'V1_%d' % kk_)
                    qring = Ring('QT', [sb("QT%d" % i, [128, N], BF16) for i in range(2)])
                    for qi_, qt_ in enumerate(qring.tiles):
                        P.pool(lambda e, qt_=qt_: e.memset(qt_[64:128, :], 0.0), [], ['QTz%d' % qi_])
                    ptring = Ring('PT', [sb("PT%d" % i, [128, 512], BF16) for i in range(4)])
                    aoring = Ring('AO', [sb("AO%d" % i, [128, 4, 64]) for i in range(2)])
                    rcring = Ring('rc', [sb("rc%d" % i, [128, 4]) for i in range(2)])
                    sbank = Ring('S', [0, 1, 2, 3])
                    obank = Ring('O', [4, 5])
                    groups = []
                    if not last:
                        groups.append((0, 2, [0, 1]))
                    for g in range(8):
                        groups.append((LC + g * 512, 4, list(range(NT))))
                    items = []
                    for hq in range(8):
                        for gi, (q0, nq, chunks) in enumerate(groups):
                            for ci, c in enumerate(chunks):
                                items.append((hq, gi, q0, nq, ci, c, len(chunks)))
                    qt_of = {}
                    st_of = {}

                    def get_qt(hq):
                        if hq not in qt_of:
                            QT, qk = qring.next()
                            P.dma('sp', QT[0:64, :], AQT[hq], [], [qk], qk)
                            qt_of[hq] = (QT, qk)
                        return qt_of[hq]

                    def emit_S(t):
                        hq, gi, q0, nq, ci, c, nch = items[t]
                        QT, qk = get_qt(hq)
                        kvh = hq // 4
                        W = nq * 128
                        sbk, sk = sbank.next()
                        P.pe(lambda e, sbk=sbk, c=c, QT=QT, q0=q0, W=W, kvh=kvh: e.matmul(psum[:, sbk, 0:W], KT[:, kvh, c * 128:(c + 1) * 128],
                                                                                         QT[:, q0:q0 + W], start=True, stop=True),
                             ['KT', 'KTz', 'QTz0', 'QTz1', qk], [sk])
                        st_of[t] = (sbk, sk)

                    cur_o = [None]

                    def emit_rest(t):
                        hq, gi, q0, nq, ci, c, nch = items[t]
                        kvh = hq // 4
                        W = nq * 128
                        sbk, sk = st_of.pop(t)
                        if ci == 0:
                            cur_o[0] = obank.next()
                        ob, ok = cur_o[0]
                        Ov = psum[:, ob, 0:260].rearrange("p (j e) -> p j e", e=65)
                        PT, pk = ptring.next()
                        P.act(lambda e, PT=PT, sbk=sbk, W=W: e.activation(out=PT[:, 0:W], in_=psum[:, sbk, 0:W], func=AF.Exp, scale=0.125),
                              [sk], [pk])
                        if t + 2 < len(items):
                            emit_S(t + 2)
                        for j in range(nq):
                            P.pe(lambda e, PT=PT, j=j, c=c, kvh=kvh, ci=ci, Ov=Ov, nch=nch: e.matmul(
                                Ov[:, j, :], PT[:, j * 128:(j + 1) * 128], V1[:, c, kvh, :],
                                start=(ci == 0 and j == 0), stop=(ci == nch - 1), skip_group_check=True),
                                [pk, 'V1'], [ok])
                        if ci == nch - 1:
                            rc, rk_ = rcring.next()
                            AO, ak_ = aoring.next()
                            P.dve(lambda e, rc=rc, Ov=Ov, nq=nq: e.reciprocal(out=rc[:, 0:nq], in_=Ov[:, 0:nq, 64]), [ok], [rk_])
                            P.dve(lambda e, rc=rc, Ov=Ov, nq=nq, AO=AO: e.tensor_tensor(out=AO[:, 0:nq, :], in0=Ov[:, 0:nq, 0:64],
                                                                                      in1=rc[:, 0:nq].unsqueeze(2).to_broadcast([128, nq, 64]), op=ALU.mult),
                                  [ok, rk_], [ak_])
                            P.dma('sp', MIX[q0:q0 + W, 512 + hq * 64:512 + (hq + 1) * 64].rearrange("(j p) d -> p j d", p=128), AO[:, 0:nq, :],
                                  [ak_], [('MIXa', q0, hq)], ak_)
                    emit_S(0)
                    emit_S(1)
                    for t in range(len(items)):
                        emit_rest(t)
                    P.flush()

            if 'ret' in phases:
                with contextlib.ExitStack() as st:
                    def sb(name, shape, dt=F32):
                        return st.enter_context(nc.sbuf_tensor(uname(name), list(shape), dt))
                    RT = sb("RT", [128, NT, 768], BF16)
                    P.dma('sp', RT[:], RTOK.rearrange("(c p) w -> p c w", p=128), [], ['RT'], 'RT')
                    RGt = sb("RGt", [128, NT, 256])
                    P.dma('act', RGt[:], RG.rearrange("(c p) w -> p c w", p=128), [], ['RGt'], 'RGt')
                    gng = sb("gng", [128, 256])
                    bload('act', gng[:], gn_g[l], 'gng')
                    lg = sb("lg", [128, 8])
                    bload('act', lg[:], rde[l].rearrange("a h -> (a h)"), 'lg')
                    P.act(lambda e: e.activation(out=lg[:], in_=lg[:], func=AF.Exp, scale=-math.log(2.0)), ['lg'], ['lg'])
                    P.act(lambda e: e.activation(out=lg[:], in_=lg[:], func=AF.Ln, scale=-1.0, bias=1.0), ['lg'], ['lg'])
                    dec = sb("dec", [128, 8])
                    P.act(lambda e: e.activation(out=dec[:], in_=lg[:], func=AF.Exp, scale=128.0), ['lg'], ['dec'])
                    dpos = sb("dpos", [128, 128]); dneg = sb("dneg", [128, 128]); mge = sb("mge", [128, 128])
                    P.dma('sp', dpos[:], dpos_in, [], ['dpos'], 'dpos')
                    P.dma('sp', dneg[:], dneg_in, [], ['dneg'], 'dneg')
                    P.dma('sp', mge[:], mge_in, [], ['mge'], 'mge')
                    DcT = sb("DcT", [128, 4, 128])
                    e1 = sb("e1", [128, 128])
                    for hh in range(4):
                        P.act(lambda e, hh=hh: e.activation(out=e1[:], in_=dpos[:], func=AF.Exp, scale=lg[:, hh:hh + 1]), ['dpos', 'lg'], ['e1'])
                        P.act(lambda e, hh=hh: e.activation(out=DcT[:, hh, :], in_=dneg[:], func=AF.Exp, scale=lg[:, 4 + hh:5 + hh]), ['dneg', 'lg'], ['DcT%d' % hh])
                        P.dve(lambda e, hh=hh: e.tensor_tensor(out=e1[:], in0=e1[:], in1=DcT[:, hh, :], op=ALU.subtract), ['e1', 'DcT%d' % hh], ['e1'])
                        P.dve(lambda e, hh=hh: e.tensor_tensor(out=e1[:], in0=e1[:], in1=mge[:], op=ALU.mult), ['e1', 'mge'], ['e1'])
                        P.dve(lambda e, hh=hh: e.tensor_tensor(out=DcT[:, hh, :], in0=DcT[:, hh, :], in1=e1[:], op=ALU.add), ['e1', 'DcT%d' % hh], ['DcT%d' % hh])
                    q3ring = Ring('Q3', [sb("Q3_%d" % i, [64, 3, N], BF16) for i in range(2)])
                    ktring = Ring('KTh', [sb("KTh%d" % i, [64, N], BF16) for i in range(2)])
                    Sf = sb("Sf", [64, NT + 1, 64], BF16)
                    Sb = sb("Sb", [64, NT + 1, 64], BF16)
                    srun = Ring('srun', [sb("srun%d" % i, [64, 64]) for i in range(2)])
                    ptr = Ring('PTr', [sb("PTr%d" % i, [128, 128], BF16) for i in range(2)])
                    st6 = sb("st6", [128, 6]); mv = sb("mv", [128, 2]); rs = sb("rs", [128, 1])
                    yo = Ring('yo', [sb("yo%d" % i, [128, 64]) for i in range(2)])
                    kvb = Ring('kv', [0, 1]); scb = Ring('sc', [2, 3]); yb = Ring('y', [4, 5])
                    ytiles = list(range(NT)) if not last else list(range(NT_C, NT))
                    for hh in range(4):
                        Q3, q3k = q3ring.next()
                        KTh, ktk = ktring.next()
                        P.dma('sp', Q3[:], RQT[:, hh].rearrange("t d n -> d t n"), [], [q3k], q3k)
                        P.dma('act', KTh[:], RKT[hh], [], [ktk], ktk)
                        vcol = 512 + hh * 64

                        def scan(order_tiles, S, skey, kcol, dcol, run_init_zero):
                            return None
                        run, runk = srun.next()
                        P.pool(lambda e, run=run: e.memset(run[:], 0.0), [], [runk])
                        for i in range(NT):
                            P.pool(lambda e, run=run, i=i: e.tensor_copy(out=Sf[:, i, :], in_=run[:]), [runk], [('Sf', i)])
                            kb, kk = kvb.next()
                            P.pe(lambda e, kb=kb, i=i, hh=hh, vcol=vcol: e.matmul(psum[0:64, kb, 0:64], RT[:, i, hh * 64:(hh + 1) * 64],
                                                                                 RT[:, i, vcol:vcol + 64], start=True, stop=True), ['RT'], [kk])
                            nrun, nrunk = srun.next()
                            P.dve(lambda e, run=run, nrun=nrun, kb=kb, hh=hh: e.scalar_tensor_tensor(out=nrun[:], in0=run[:], scalar=dec[0:64, hh:hh + 1],
                                                                                                     in1=psum[0:64, kb, 0:64], op0=ALU.mult, op1=ALU.add),
                                  [runk, kk, 'dec'], [nrunk])
                            run, runk = nrun, nrunk
                        run, runk = srun.next()
                        P.pool(lambda e, run=run: e.memset(run[:], 0.0), [], [runk])
                        for i in [1, 0] + list(range(NT - 1, NT_C - 1, -1)):
                            P.pool(lambda e, run=run, i=i: e.tensor_copy(out=Sb[:, i, :], in_=run[:]), [runk], [('Sb', i)])
                            kb, kk = kvb.next()
                            P.pe(lambda e, kb=kb, i=i, hh=hh, vcol=vcol: e.matmul(psum[0:64, kb, 0:64], RT[:, i, 256 + hh * 64:256 + (hh + 1) * 64],
                                                                                 RT[:, i, vcol:vcol + 64], start=True, stop=True), ['RT'], [kk])
                            nrun, nrunk = srun.next()
                            P.dve(lambda e, run=run, nrun=nrun, kb=kb, hh=hh: e.scalar_tensor_tensor(out=nrun[:], in0=run[:], scalar=dec[0:64, 4 + hh:5 + hh],
                                                                                                     in1=psum[0:64, kb, 0:64], op0=ALU.mult, op1=ALU.add),
                                  [runk, kk, 'dec'], [nrunk])
                            run, runk = nrun, nrunk
                        for i in ytiles:
                            sbk, sk = scb.next()
                            P.pe(lambda e, sbk=sbk, i=i, KTh=KTh, Q3=Q3: e.matmul(psum[:, sbk, 0:128], KTh[:, i * 128:(i + 1) * 128], Q3[:, 0, i * 128:(i + 1) * 128],
                                                                                start=True, stop=True), [ktk, q3k], [sk])
                            PTr, pk = ptr.next()
                            P.dve(lambda e, PTr=PTr, sbk=sbk, hh=hh: e.tensor_tensor(out=PTr[:], in0=psum[:, sbk, 0:128], in1=DcT[:, hh, :], op=ALU.mult),
                                  [sk, 'DcT%d' % hh], [pk])
                            ybk, yk = yb.next()
                            P.pe(lambda e, ybk=ybk, PTr=PTr, i=i, vcol=vcol: e.matmul(psum[:, ybk, 0:64], PTr[:], RT[:, i, vcol:vcol + 64], start=True, stop=False),
                                 [pk, 'RT'], [yk])
                            P.pe(lambda e, ybk=ybk, Q3=Q3, i=i: e.matmul(psum[:, ybk, 0:64], Q3[:, 1, i * 128:(i + 1) * 128], Sf[:, i, :], start=False, stop=False),
                                 [q3k, ('Sf', i)], [yk])
                            P.pe(lambda e, ybk=ybk, Q3=Q3, i=i: e.matmul(psum[:, ybk, 0:64], Q3[:, 2, i * 128:(i + 1) * 128], Sb[:, i, :], start=False, stop=True),
                                 [q3k, ('Sb', i)], [yk])
                            P.dve(lambda e, ybk=ybk: e.bn_stats(out=st6[:], in_=psum[:, ybk, 0:64]), [yk], ['st6'])
                            P.dve(lambda e: e.bn_aggr(out=mv[:], in_=st6[:]), ['st6'], ['mv'])
                            P.dve(lambda e: e.tensor_scalar_add(out=rs[:], in0=mv[:, 1:2], scalar1=EPS), ['mv'], ['rs'])
                            P.act(lambda e: e.activation(out=rs[:], in_=rs[:], func=AF.Sqrt), ['rs'], ['rs'])
                            P.dve(lambda e: e.reciprocal(out=rs[:], in_=rs[:]), ['rs'], ['rs'])
                            y_, yok = yo.next()
                            P.dve(lambda e, y_=y_, ybk=ybk: e.tensor_scalar(out=y_[:], in0=psum[:, ybk, 0:64], scalar1=mv[:, 0:1], scalar2=rs[:, 0:1],
                                                                           op0=ALU.subtract, op1=ALU.mult), [yk, 'mv', 'rs'], [yok])
                            P.pool(lambda e, y_=y_, hh=hh: e.tensor_tensor(out=y_[:], in0=y_[:], in1=gng[:, hh * 64:(hh + 1) * 64], op=ALU.mult), [yok, 'gng'], [yok])
                            P.pool(lambda e, y_=y_, hh=hh, i=i: e.tensor_tensor(out=y_[:], in0=y_[:], in1=RGt[:, i, hh * 64:(hh + 1) * 64], op=ALU.mult), [yok, 'RGt'], [yok])
                            P.dma('sp', MIX[i * 128:(i + 1) * 128, 256 + hh * 64:256 + (hh + 1) * 64], y_[:], [yok], [('MIXr', i, hh)], yok)
                    P.flush()

            if 'p3' in phases:
                with contextlib.ExitStack() as st:
                    def sb(name, shape, dt=F32):
                        return st.enter_context(nc.sbuf_tensor(uname(name), list(shape), dt))
                    wout = sb("wout", [128, 8, D], BF16)
                    for hf in range(2):
                        P.dma('pool', wout[:, :, hf * 512:(hf + 1) * 512], w_out[l, :, hf * 512:(hf + 1) * 512].rearrange("(k p) n -> p k n", p=128),
                              [], ['wout%d' % hf], 'wout%d' % hf)
                    CWr = sb("CWr", [128, 256, 3])
                    P.dma('act', CWr[:].rearrange("p c k -> p (c k)"), conv_w[l].rearrange("c k -> (c k)").partition_broadcast(128), [], ['CWr'], 'CWr')
                    CW = sb("CW", [128, 3, 256])
                    for k in range(3):
                        P.dve(lambda e, k=k: e.tensor_copy(out=CW[:, k, :], in_=CWr[:, :, k]), ['CWr'], ['CW%d' % k])
                    g1 = [sb("g1_%d" % s, [128, D]) for s in range(2)]
                    sc2 = [sb("sc2_%d" % s, [128, D]) for s in range(2)]
                    sh2 = [sb("sh2_%d" % s, [128, D]) for s in range(2)]
                    for s in range(2):
                        if last and s == 1:
                            continue
                        bload('sp', g1[s][:], modv[l, s, 2 * D:3 * D], 'g1_%d' % s)
                        bload('sp', sh2[s][:], modv[l, s, 3 * D:4 * D], 'sh2_%d' % s)
                        bload('sp', sc2[s][:], modv[l, s, 4 * D:5 * D], 'sc2_%d' % s)
                    lng = sb("lng", [128, D]); lnb = sb("lnb", [128, D])
                    bload('act', lng[:], ln_g[l, 0], 'lng')
                    bload('act', lnb[:], ln_b[l, 0], 'lnb')
                    wr = sb("wr", [128, 8, NE])
                    P.dma('act', wr[:], w_router[l].rearrange("(k p) e -> p k e", p=128), [], ['wr'], 'wr')
                    brt = sb("brt", [128, NE])
                    bload('act', brt[:], b_router[l], 'brt')
                    pmr = Ring('pm', [sb("pm%d" % i, [128, 3, 256]) for i in range(3)])
                    btr = Ring('bt', [sb("bt%d" % i, [128, 256]) for i in range(3)])
                    mixr = Ring('mix', [sb("mix%d" % i, [128, D]) for i in range(3)])
                    xr = Ring('x3', [sb("x3_%d" % i, [128, D]) for i in range(3)])
                    ca = sb("ca", [128, 256]); cb = sb("cb", [128, 256])
                    mTr = Ring('mT', [sb("mT%d" % i, [128, 8, 128], BF16) for i in range(2)])
                    rr = sb("rr", [128, D])
                    x1r = Ring('x1', [sb("x1_%d" % i, [128, D]) for i in range(2)])
                    h2 = sb("h2", [128, D])
                    h2br = Ring('h2b', [sb("h2b%d" % i, [128, D], BF16) for i in range(2)])
                    ix8 = sb("ix8", [128, 8], U32)
                    h2f = sb("h2f", [128, 8, 128])
                    st6 = sb("st6", [128, 2, 6]); mv = sb("mv", [128, 2]); rs = sb("rs", [128, 1])
                    lgt = sb("lgt", [128, NE]); mx8 = sb("mx8", [128, 8]); msk = sb("msk", [128, NE]); nmx = sb("nmx", [128, 1])
                    ex = sb("ex", [128, NE]); sm = sb("sm", [128, 1])
                    mwr = Ring('mw', [sb("mw%d" % i, [128, NE]) for i in range(2)])
                    ld3 = {}

                    def issue_loads3(i):
                        r0 = prow(i)
                        pm, pmk = pmr.next()
                        for k in range(3):
                            P.dma('sp', pm[:, k, :], PB[r0 - 1 + k:r0 + 127 + k, 0:256], [], [pmk + str(k)], pmk + str(k))
                        bt, btk = btr.next()
                        P.dma('sp', bt[:], PB[r0:r0 + 128, 256:512], [], [btk], btk)
                        mix, mixk = mixr.next()
                        P.dma('sp', mix[:, 256:D], MIX[i * 128:(i + 1) * 128, 256:D], [], [mixk + 'l'], mixk)
                        xt, xk = xr.next()
                        P.dma('sp', xt[:], xsrc(i), [], [xk], xk)
                        ld3[i] = (pm, pmk, bt, btk, mix, mixk, xt, xk)
                    issue_loads3(out_tiles[0])
                    p3_base = len(P.ops)
                    p3_marks = []
                    for oi, i in enumerate(out_tiles):
                        s = 1 if i < NT_C else 0
                        m_a = len(P.ops)
                        if oi + 1 < len(out_tiles):
                            issue_loads3(out_tiles[oi + 1])
                        pm, pmk, bt, btk, mix, mixk, xt, xk = ld3.pop(i)
                        P.dve(lambda e, pm=pm: e.tensor_tensor(out=ca[:], in0=pm[:, 0, :], in1=CW[:, 0, :], op=ALU.mult), [pmk + '0', 'CW0'], ['ca'])
                        P.pool(lambda e, pm=pm: e.tensor_tensor(out=cb[:], in0=pm[:, 1, :], in1=CW[:, 1, :], op=ALU.mult), [pmk + '1', 'CW1'], ['cb'])
                        P.dve(lambda e: e.tensor_tensor(out=ca[:], in0=ca[:], in1=cb[:], op=ALU.add), ['ca', 'cb'], ['ca'])
                        P.pool(lambda e, pm=pm: e.tensor_tensor(out=cb[:], in0=pm[:, 2, :], in1=CW[:, 2, :], op=ALU.mult), [pmk + '2', 'CW2', 'ca'], ['cb'])
                        P.dve(lambda e: e.tensor_tensor(out=ca[:], in0=ca[:], in1=cb[:], op=ALU.add), ['ca', 'cb'], ['ca'])
                        P.dve(lambda e, mix=mix, bt=bt: e.tensor_tensor(out=mix[:, 0:256], in0=ca[:], in1=bt[:], op=ALU.mult), ['ca', btk], [mixk + 'c'])
                        for k in range(8):
                            P.pe(lambda e, mix=mix, k=k: e.transpose(psum[:, k // 4, (k % 4) * 128:(k % 4 + 1) * 128], mix[:, k * 128:(k + 1) * 128], ident[:]),
                                 [mixk + 'l', mixk + 'c', 'ident'], ['pT%d' % k])
                        mT, mTk = mTr.next()
                        for hf in range(2):
                            P.act(lambda e, mT=mT, hf=hf: e.activation(out=mT[:, hf * 4:(hf + 1) * 4, :].rearrange("p k n -> p (k n)"), in_=psum[:, hf, :], func=AF.Copy),
                                  ['pT%d' % k for k in range(hf * 4, hf * 4 + 4)], [mTk + str(hf)])
                        pyb = 2 + 2 * (oi % 2)
                        for nh in range(2):
                            for k in range(8):
                                P.pe(lambda e, mT=mT, nh=nh, k=k, pyb=pyb: e.matmul(psum[:, pyb + nh, :], mT[:, k, :], wout[:, k, nh * 512:(nh + 1) * 512], start=(k == 0), stop=(k == 7)),
                                     [mTk + str(k // 4), 'wout%d' % nh], ['py%d' % (pyb + nh)])
                        m_b = len(P.ops)
                        for nh in range(2):
                            P.dve(lambda e, nh=nh, s=s, pyb=pyb: e.tensor_tensor(out=rr[:, nh * 512:(nh + 1) * 512], in0=psum[:, pyb + nh, :], in1=g1[s][:, nh * 512:(nh + 1) * 512], op=ALU.mult),
                                  ['py%d' % (pyb + nh), 'g1_%d' % s], ['rr%d' % nh])
                        P.dve(lambda e, xt=xt: e.scalar_tensor_tensor(out=rr[:], in0=xt[:], scalar=ALPHA, in1=rr[:], op0=ALU.mult, op1=ALU.add), [xk, 'rr0', 'rr1'], ['rr'])
                        for nh in range(2):
                            P.dve(lambda e, nh=nh: e.bn_stats(out=st6[:, nh, :], in_=rr[:, nh * 512:(nh + 1) * 512]), ['rr'], ['st6_%d' % nh])
                        P.dve(lambda e: e.bn_aggr(out=mv[:], in_=st6[:]), ['st6_0', 'st6_1'], ['mv'])
                        P.dve(lambda e: e.tensor_scalar_add(out=rs[:], in0=mv[:, 1:2], scalar1=EPS), ['mv'], ['rs'])
                        P.act(lambda e: e.activation(out=rs[:], in_=rs[:], func=AF.Sqrt), ['rs'], ['rs'])
                        P.dve(lambda e: e.reciprocal(out=rs[:], in_=rs[:]), ['rs'], ['rs'])
                        x1, x1k = x1r.next()
                        P.dve(lambda e, x1=x1: e.tensor_scalar(out=x1[:], in0=rr[:], scalar1=mv[:, 0:1], scalar2=rs[:, 0:1], op0=ALU.subtract, op1=ALU.mult), ['rr', 'mv', 'rs'], [x1k])
                        P.dve(lambda e, x1=x1: e.tensor_tensor(out=x1[:], in0=x1[:], in1=lng[:], op=ALU.mult), [x1k, 'lng'], [x1k])
                        P.dve(lambda e, x1=x1: e.tensor_tensor(out=x1[:], in0=x1[:], in1=lnb[:], op=ALU.add), [x1k, 'lnb'], [x1k])
                        P.dma('sp', XM[i * 128:(i + 1) * 128, :], x1[:], [x1k], [('XM', i)], x1k)
                        P.dve(lambda e, x1=x1, s=s: e.tensor_tensor(out=h2[:], in0=x1[:], in1=sc2[s][:], op=ALU.mult), [x1k, 'sc2_%d' % s], ['h2'])
                        P.dve(lambda e, s=s: e.tensor_tensor(out=h2[:], in0=h2[:], in1=sh2[s][:], op=ALU.add), ['h2', 'sh2_%d' % s], ['h2'])
                        for k in range(8):
                            P.pe(lambda e, k=k: e.transpose(psum[:, k // 4, (k % 4) * 128:(k % 4 + 1) * 128], h2[:, k * 128:(k + 1) * 128], ident[:]),
                                 ['h2', 'ident'], ['pT%d' % k])
                        for hf in range(2):
                            P.dve(lambda e, hf=hf: e.tensor_copy(out=h2f[:, hf * 4:(hf + 1) * 4, :].rearrange("p k n -> p (k n)"), in_=psum[:, hf, :]),
                                  ['pT%d' % k for k in range(hf * 4, hf * 4 + 4)], ['h2f%d' % hf])
                        h2b, h2bk = h2br.next()
                        P.act(lambda e, h2b=h2b: e.activation(out=h2b[:], in_=h2[:], func=AF.Copy), ['h2'], [h2bk])
                        P.dma('sp', H2R[i * 128:(i + 1) * 128, :], h2b[:], [h2bk], [('H2R', i)], h2bk)
                        for k in range(8):
                            P.pe(lambda e, k=k: e.matmul(psum[:, 6, 0:NE], h2f[:, k, :], wr[:, k, :], start=(k == 0), stop=(k == 7)), ['h2f%d' % (k // 4), 'wr'], ['pl'])
                        P.dve(lambda e: e.tensor_tensor(out=lgt[:], in0=psum[:, 6, 0:NE], in1=brt[:], op=ALU.add), ['pl', 'brt'], ['lgt'])
                        P.dve(lambda e: e.max(out=mx8[:], in_=lgt[:]), ['lgt'], ['mx8'])
                        P.dve(lambda e: e.max_index(out=ix8[:], in_max=mx8[:], in_values=lgt[:]), ['lgt', 'mx8'], ['ix8'])
                        P.dve(lambda e: e.tensor_scalar_mul(out=nmx[:], in0=mx8[:, 0:1], scalar1=-1.0), ['mx8'], ['nmx'])
                        P.act(lambda e: e.activation(out=ex[:, 0:4], in_=mx8[:, 0:4], func=AF.Exp, bias=nmx[:, 0:1], scale=1.0), ['mx8', 'nmx'], ['ex'])
                        P.dve(lambda e: e.reduce_sum(out=sm[:], in_=ex[:, 0:4], axis=AX.X), ['ex'], ['sm'])
                        P.dve(lambda e: e.reciprocal(out=sm[:], in_=sm[:]), ['sm'], ['sm'])
                        mw_, mwk = mwr.next()
                        P.dve(lambda e, mw_=mw_: e.tensor_scalar_mul(out=mw_[:, 0:4], in0=ex[:, 0:4], scalar1=sm[:, 0:1]), ['ex', 'sm'], [mwk + 'w'])
                        P.dve(lambda e, mw_=mw_: e.tensor_copy(out=mw_[:, 4:8], in_=ix8[:, 0:4]), ['ix8'], [mwk + 'e'])
                        P.dma('sp', RW[i * 128:(i + 1) * 128, :], mw_[:, 0:4], [mwk + 'w'], [('RW', i)], mwk + 'w')
                        P.dma('sp', RE[i * 128:(i + 1) * 128, :], mw_[:, 4:8], [mwk + 'e'], [('RE', i)], mwk + 'e')
                        p3_marks.append((m_a, m_b, len(P.ops)))
                    pipeline_reorder(P, p3_base, p3_marks)
                    P.flush()

            if 'moe' in phases:
                tiles = out_tiles
                nt = len(tiles)
                T = nt * 128
                t0 = tiles[0] * 128
                NB = (4 * T + NE * (BS - 1) + BS - 1) // BS
                assert NB <= NBMAX
                w_gu_rows = w_gu.rearrange("l e r c -> (l e r) c")
                w_dn_rows = w_dn.rearrange("l e r c -> (l e r) c")
                with contextlib.ExitStack() as st:
                    def sb(name, shape, dt=F32):
                        return st.enter_context(nc.sbuf_tensor(uname(name), list(shape), dt))
                    DKi = sb("DKi", [128, nt, 4], I32)
                    IDXG = sb("IDXG", [128, NB, 8], I32)
                    EB = sb("EB", [128, NB])
                    IDXB = sb("IDXB", [128, NB], I32)
                    mwd = sb("mwd", [128, nt, NE])
                    iotap = sb("iotap", [128, 1])
                    P.dma('sp', iotap[:], iotap_in, [], ['iotap'], 'iotap')
                    with contextlib.ExitStack() as st2:
                        def sb2(name, shape, dt=F32):
                            return st2.enter_context(nc.sbuf_tensor(uname(name), list(shape), dt))
                        EF = sb2("EF", [128, nt, 4])
                        W4 = sb2("W4", [128, nt, 4])
                        P.dma('sp', EF[:], RE[t0:t0 + T, :].rearrange("(c p) k -> p c k", p=128), [], ['EF'], 'EF')
                        P.dma('sp', W4[:], RW[t0:t0 + T, :].rearrange("(c p) k -> p c k", p=128), [], ['W4'], 'W4')
                        iota32 = sb2("iota32", [128, NE])
                        P.dma('act', iota32[:], iota32_in, [], ['iota32'], 'iota32')
                        lts = sb2("lts", [128, 128])
                        P.dma('act', lts[:], lts_in, [], ['lts'], 'lts')
                        ones = sb2("ones", [128, 128])
                        P.pool(lambda e: e.memset(ones[:], 1.0), [], ['ones'])
                        bst = sb2("bst", [128, NBMAX])
                        P.dma('act', bst[:], bstart_in, [], ['bst'], 'bst')
                        kp = sb2("kp", [128, 8])
                        P.dma('act', kp[:], kp_in, [], ['kp'], 'kp')
                        OH = sb2("OH", [128, 4, nt, NE])
                        for k in range(4):
                            P.dve(lambda e, k=k: e.tensor_tensor(out=OH[:, k, :, :], in0=iota32[:].unsqueeze(1).to_broadcast([128, nt, NE]),
                                                                 in1=EF[:, :, k].unsqueeze(2).to_broadcast([128, nt, NE]), op=ALU.is_equal),
                                  ['iota32', 'EF'], ['OH%d' % k])
                        mask = sb2("mask", [128, nt, NE])
                        P.dve(lambda e: e.tensor_tensor(out=mask[:], in0=OH[:, 0, :, :], in1=OH[:, 1, :, :], op=ALU.add), ['OH0', 'OH1'], ['mask'])
                        P.dve(lambda e: e.tensor_tensor(out=mask[:], in0=mask[:], in1=OH[:, 2, :, :], op=ALU.add), ['mask', 'OH2'], ['mask'])
                        P.dve(lambda e: e.tensor_tensor(out=mask[:], in0=mask[:], in1=OH[:, 3, :, :], op=ALU.add), ['mask', 'OH3'], ['mask'])
                        for j in range(nt):
                            bk = j // 16
                            col = (j % 16) * NE
                            P.pe(lambda e, j=j, bk=bk, col=col: e.matmul(psum[:, bk, col:col + NE], lts[:], mask[:, j, :], start=True, stop=True, skip_group_check=True),
                                 ['lts', 'mask'], ['rk%d' % bk])
                            P.pe(lambda e, j=j, bk=bk, col=col: e.matmul(psum[:, 4 + bk, col:col + NE], ones[:], mask[:, j, :], start=True, stop=True, skip_group_check=True),
                                 ['ones', 'mask'], ['tt%d' % bk])
                        for m in range(nt):
                            P.pe(lambda e, m=m: e.matmul(psum[:, 3, 0:NE], ones[:], mask[:, m, :], start=(m == 0), stop=(m == nt - 1)), ['ones', 'mask'], ['cnt'])
                        TOT = sb2("TOT", [128, nt, NE])
                        PRE = sb2("PRE", [128, nt, NE])
                        for bk in range((nt + 15) // 16):
                            j0 = bk * 16
                            nj = min(16, nt - j0)
                            P.act(lambda e, bk=bk, j0=j0, nj=nj: e.activation(out=TOT[:, j0:j0 + nj, :].rearrange("p j e -> p (j e)"), in_=psum[:, 4 + bk, 0:nj * NE], func=AF.Copy),
                                  ['tt%d' % bk], ['TOT%d' % bk])
                        P.pool(lambda e: e.memset(PRE[:, 0, :], 0.0), [], ['PRE'])
                        for j in range(1, nt):
                            P.dve(lambda e, j=j: e.tensor_tensor(out=PRE[:, j, :], in0=PRE[:, j - 1, :], in1=TOT[:, j - 1, :], op=ALU.add),
                                  ['PRE'] + ['TOT%d' % bk for bk in range((nt + 15) // 16)], ['PRE'])
                        c0 = sb2("c0", [128, NE]); c1 = sb2("c1", [128, NE]); padded = sb2("padded", [128, NE])
                        P.dve(lambda e: e.tensor_scalar_add(out=c0[:], in0=psum[:, 3, 0:NE], scalar1=float(BS - 1)), ['cnt'], ['c0'])
                        ci32 = sb2("ci32", [128, NE], I32)
                        P.dve(lambda e: e.tensor_scalar(out=c1[:], in0=c0[:], scalar1=1.0 / BS, scalar2=-0.5 + 0.5 / BS, op0=ALU.mult, op1=ALU.add), ['c0'], ['c1'])
                        P.dve(lambda e: e.tensor_copy(out=ci32[:], in_=c1[:]), ['c1'], ['ci32'])
                        P.dve(lambda e: e.tensor_copy(out=c1[:], in_=ci32[:]), ['ci32'], ['c1'])
                        P.dve(lambda e: e.tensor_scalar_mul(out=padded[:], in0=c1[:], scalar1=float(BS)), ['c1'], ['padded'])
                        P.dve(lambda e: e.tensor_copy(out=c0[:], in_=padded[:]), ['padded'], ['c0'])
                        cur, nxt_, ck, nk = c0, c1, 'c0', 'c1'
                        for sft in (1, 2, 4, 8, 16):
                            P.dve(lambda e, cur=cur, nxt_=nxt_, sft=sft: e.tensor_copy(out=nxt_[:, 0:sft], in_=cur[:, 0:sft]), [ck], [nk + 'a'])
                            P.dve(lambda e, cur=cur, nxt_=nxt_, sft=sft: e.tensor_tensor(out=nxt_[:, sft:NE], in0=cur[:, sft:NE], in1=cur[:, 0:NE - sft], op=ALU.add), [ck], [nk + 'b'])
                            P.dve(lambda e: e.engine_nop(), [nk + 'a', nk + 'b'], [nk])
                            cur, nxt_, ck, nk = nxt_, cur, nk, ck
                        pend, pendk = cur, ck
                        pstart = sb2("pstart", [128, NE])
                        P.dve(lambda e: e.tensor_tensor(out=pstart[:], in0=pend[:], in1=padded[:], op=ALU.subtract), [pendk, 'padded'], ['pstart'])
                        dfull = sb2("dfull", [128, nt, NE])
                        for bk in range((nt + 15) // 16):
                            j0 = bk * 16
                            nj = min(16, nt - j0)
                            P.dve(lambda e, bk=bk, j0=j0, nj=nj: e.tensor_tensor(out=dfull[:, j0:j0 + nj, :], in0=psum[:, bk, 0:nj * NE].rearrange("p (j e) -> p j e", e=NE),
                                                                                 in1=pstart[:].unsqueeze(1).to_broadcast([128, nj, NE]), op=ALU.add),
                                  ['rk%d' % bk, 'pstart'], ['dfull%d' % bk])
                            P.dve(lambda e, j0=j0, nj=nj: e.tensor_tensor(out=dfull[:, j0:j0 + nj, :], in0=dfull[:, j0:j0 + nj, :], in1=PRE[:, j0:j0 + nj, :], op=ALU.add),
                                  ['dfull%d' % bk, 'PRE'], ['dfull%d' % bk])
                        dkeys = ['dfull%d' % bk for bk in range((nt + 15) // 16)]
                        tmpd = sb2("tmpd", [128, nt, NE])
                        DKf = sb2("DKf", [128, nt, 4])
                        for k in range(4):
                            P.dve(lambda e, k=k: e.tensor_tensor(out=tmpd[:], in0=dfull[:], in1=OH[:, k, :, :], op=ALU.mult), dkeys + ['OH%d' % k], ['tmpd'])
                            P.dve(lambda e, k=k: e.tensor_reduce(out=DKf[:, :, k], in_=tmpd[:], axis=AX.X, op=ALU.add), ['tmpd'], ['DKf%d' % k])
                        P.dve(lambda e: e.tensor_copy(out=DKi[:], in_=DKf[:]), ['DKf%d' % k for k in range(4)], ['DKi'])
                        cmp_ = sb2("cmp", [128, NB, NE])
                        P.dve(lambda e: e.tensor_tensor(out=cmp_[:], in0=pend[:].unsqueeze(1).to_broadcast([128, NB, NE]),
                                                        in1=bst[:, 0:NB].unsqueeze(2).to_broadcast([128, NB, NE]), op=ALU.is_le), [pendk, 'bst'], ['cmp'])
                        P.dve(lambda e: e.tensor_reduce(out=EB[:], in_=cmp_[:], axis=AX.X, op=ALU.add), ['cmp'], ['EB'])
                        P.dve(lambda e: e.tensor_scalar_min(out=EB[:], in0=EB[:], scalar1=float(NE - 1)), ['EB'], ['EB'])
                        idxf = sb2("idxf", [128, NB, 8])
                        P.dve(lambda e: e.scalar_tensor_tensor(out=idxf[:], in0=EB[:].unsqueeze(2).to_broadcast([128, NB, 8]), scalar=float(D),
                                                               in1=kp[:].unsqueeze(1).to_broadcast([128, NB, 8]), op0=ALU.mult, op1=ALU.add), ['EB', 'kp'], ['idxf'])
                        P.dve(lambda e: e.tensor_scalar_add(out=idxf[:], in0=idxf[:], scalar1=float(l * NE * D)), ['idxf'], ['idxf'])
                        P.dve(lambda e: e.tensor_copy(out=IDXG[:], in_=idxf[:]), ['idxf'], ['IDXG'])
                        idxbf = sb2("idxbf", [128, NB])
                        P.dve(lambda e: e.tensor_scalar(out=idxbf[:], in0=EB[:], scalar1=128.0, scalar2=float(l * NE * 128), op0=ALU.mult, op1=ALU.add), ['EB'], ['idxbf'])
                        P.dve(lambda e: e.tensor_scalar(out=idxbf[:], in0=idxbf[:], scalar1=iotap[:, 0:1], scalar2=None, op0=ALU.add), ['idxbf', 'iotap'], ['idxbf'])
                        P.dve(lambda e: e.tensor_copy(out=IDXB[:], in_=idxbf[:]), ['idxbf'], ['IDXB'])
                        for k in range(4):
                            dst_ = mwd if k == 0 else tmpd
                            P.dve(lambda e, k=k, dst_=dst_: e.tensor_tensor(out=dst_[:], in0=OH[:, k, :, :], in1=W4[:, :, k].unsqueeze(2).to_broadcast([128, nt, NE]), op=ALU.mult),
                                  ['OH%d' % k, 'W4', 'DKf0', 'DKf1', 'DKf2', 'DKf3'], ['mwd' if k == 0 else 'tmpd'])
                            if k > 0:
                                P.dve(lambda e: e.tensor_tensor(out=mwd[:], in0=mwd[:], in1=tmpd[:], op=ALU.add), ['mwd', 'tmpd'], ['mwd'])
                        hr = Ring('hr', [sb2("hr%d" % i, [128, D], BF16) for i in range(3)])
                        for j, i in enumerate(tiles):
                            h_, hk_ = hr.next()
                            P.dma('sp', h_[:], H2R[i * 128:(i + 1) * 128, :], [], [hk_], hk_)
                            for k in range(4):
                                P.add('pool', lambda e, h_=h_, j=j, k=k: e.indirect_dma_start(out=XS[:, :], out_offset=bass.IndirectOffsetOnAxis(ap=DKi[:, j, k:k + 1], axis=0),
                                                                                          in_=h_[:, :], in_offset=None), [hk_, 'DKi'], [('XS', j, k)], 'xsc%d' % ((j * 4 + k) % 4))
                                P.add('pool', lambda e, j=j, k=k: e.indirect_dma_start(out=SW[:, :], out_offset=bass.IndirectOffsetOnAxis(ap=DKi[:, j, k:k + 1], axis=0),
                                                                                    in_=W4[:, j, k:k + 1], in_offset=None), ['W4', 'DKi'], [('SW', j, k)], 'swc%d' % ((j * 4 + k) % 4))
                        P.flush()
                    with contextlib.ExitStack() as st2:
                        def sb2(name, shape, dt=F32):
                            return st2.enter_context(nc.sbuf_tensor(uname(name), list(shape), dt))
                        identb = sb2("identb", [128, 128], BF16)
                        P.act(lambda e: e.activation(out=identb[:], in_=ident[:], func=AF.Copy), ['ident'], ['identb'])
                        bgr = sb2("bgr", [NE, 2 * D])
                        P.dma('act', bgr[:], b_gu[l], [], ['bgr'], 'bgr')
                        for c in range(16):
                            P.pe(lambda e, c=c: e.transpose(psum[:, 6, c * NE:(c + 1) * NE], bgr[:, c * 128:(c + 1) * 128], ident[0:NE, 0:NE]), ['bgr', 'ident'], ['pX0'])
                        X2 = sb2("X2", [128, NE, 16])
                        P.dve(lambda e: e.tensor_copy(out=X2[:], in_=psum[:, 6, :].rearrange("p (c e) -> p e c", e=NE)), ['pX0'], ['X2'])
                        P.dve(lambda e: e.tensor_scalar_add(out=X2[:, :, 8:16], in0=X2[:, :, 8:16], scalar1=1.0), ['X2'], ['X2'])
                        P.dma('sp', BGT[l * NE * 128:(l + 1) * NE * 128, :].rearrange("(e p) c -> p e c", p=128), X2[:], ['X2'], ['BGT'], 'BGT')
                        bbr = Ring('bb', [sb2("bb%d" % i, [128, 16]) for i in range(2)])
                        wgr = Ring('WG', [sb2("WG%d" % i, [128, 8, 2 * D], BF16) for i in range(2)])
                        wdr = Ring('WD', [sb2("WD%d" % i, [128, 8, D], BF16) for i in range(2)])
                        xbr = Ring('XB', [sb2("XB%d" % i, [128, 4, D], BF16) for i in range(2)])
                        XTr = [sb2("XT%d" % i, [128, 8, BS], BF16) for i in range(2)]
                        actr = Ring('act', [sb2("act%d" % i, [128, 8, BS], BF16) for i in range(2)])
                        Ar = Ring('A', [sb2("A%d" % i, [128, BS]) for i in range(2)])
                        Sr = Ring('Sg', [sb2("Sg%d" % i, [128, BS]) for i in range(2)])
                        Ur = Ring('U', [sb2("U%d" % i, [128, BS]) for i in range(2)])
                        swr = Ring('swb', [sb2("swb%d" % i, [128, 4]) for i in range(2)])
                        Yr = Ring('Y', [sb2("Y%d" % i, [128, 4, D]) for i in range(1)])
                        gbr = Ring('pg', [0, 1]); ubr = Ring('pu', [2, 3]); ybr = Ring('py', [4, 5])
                        psb = [psum[:, 6, :].bitcast(BF16), psum[:, 7, :].bitcast(BF16)]

                        def load_blk(b):
                            WG, wgk = wgr.next()
                            WD, wdk = wdr.next()
                            for k in range(8):
                                P.add('pool', lambda e, WG=WG, b=b, k=k: e.indirect_dma_start(out=WG[:, k, :], out_offset=None, in_=w_gu_rows[:, :],
                                                                                             in_offset=bass.IndirectOffsetOnAxis(ap=IDXG[:, b, k:k + 1], axis=0)),
                                      ['IDXG'], [wgk + str(k)], wgk + str(k))
                            for k in range(8):
                                P.add('pool', lambda e, WD=WD, b=b, k=k: e.indirect_dma_start(out=WD[:, k, :], out_offset=None, in_=w_dn_rows[:, :],
                                                                                             in_offset=bass.IndirectOffsetOnAxis(ap=IDXG[:, b, k:k + 1], axis=0)),
                                      ['IDXG'], [wdk + str(k)], wdk + str(k))
                            XB, xbk = xbr.next()
                            P.dma('sp', XB[:], XS[b * BS:(b + 1) * BS, :].rearrange("(s p) d -> p s d", p=128), [], [xbk], xbk)
                            swb, swk = swr.next()
                            P.dma('act', swb[:], SW[b * BS:(b + 1) * BS, :].rearrange("(s p) o -> p (s o)", p=128), [], [swk], swk, allow_slow_non_contiguous=True)
                            bb, bbk = bbr.next()
                            P.add('pool', lambda e, bb=bb, b=b: e.indirect_dma_start(out=bb[:, :], out_offset=None, in_=BGT[:, :],
                                                                                   in_offset=bass.IndirectOffsetOnAxis(ap=IDXB[:, b:b + 1], axis=0)),
                                  ['IDXB', 'BGT'], [bbk], bbk)
                            return WG, wgk, WD, wdk, XB, xbk, swb, swk, bb, bbk
                        def transposes(b, XB, xbk):
                            XT = XTr[b % 2]
                            for half in range(2):
                                for s2 in range(2):
                                    sidx = half * 2 + s2
                                    for k in range(8):
                                        P.pe(lambda e, XB=XB, sidx=sidx, k=k, s2=s2: e.transpose(psb[s2][:, k * 128:(k + 1) * 128], XB[:, sidx, k * 128:(k + 1) * 128], identb[:]),
                                             [xbk, 'identb'], ['pX%d' % s2])
                                    P.act(lambda e, sidx=sidx, s2=s2, XT=XT: e.activation(out=XT[:, :, sidx * 128:(sidx + 1) * 128], in_=psb[s2].rearrange("p (k n) -> p k n", n=128), func=AF.Copy),
                                          ['pX%d' % s2], ['XT%d_%d' % (b % 2, sidx)])
                        nxt = load_blk(0)
                        transposes(0, nxt[4], nxt[5])
                        for b in range(NB):
                            WG, wgk, WD, wdk, XB, xbk, swb, swk, bb, bbk = nxt
                            if b + 1 < NB:
                                nxt = load_blk(b + 1)
                            wgkeys = [wgk + str(k) for k in range(8)]
                            wdkeys = [wdk + str(k) for k in range(8)]
                            XT = XTr[b % 2]
                            xtkeys = ['XT%d_%d' % (b % 2, q) for q in range(4)]
                            at, atk = actr.next()
                            for c in range(8):
                                gb, gbk = gbr.next()
                                ub, ubk = ubr.next()
                                for k in range(8):
                                    P.pe(lambda e, gb=gb, WG=WG, k=k, c=c, XT=XT: e.matmul(psum[:, gb, :], WG[:, k, c * 128:(c + 1) * 128], XT[:, k, :], start=(k == 0), stop=(k == 7)),
                                         wgkeys + xtkeys, [gbk])
                                for k in range(8):
                                    P.pe(lambda e, ub=ub, WG=WG, k=k, c=c, XT=XT: e.matmul(psum[:, ub, :], WG[:, k, D + c * 128:D + (c + 1) * 128], XT[:, k, :], start=(k == 0), stop=(k == 7)),
                                         wgkeys + xtkeys, [ubk])
                                A, Ak = Ar.next(); S_, Sk = Sr.next(); U, Uk = Ur.next()
                                P.dve(lambda e, A=A, gb=gb, bb=bb, c=c: e.tensor_scalar(out=A[:], in0=psum[:, gb, :], scalar1=bb[:, c:c + 1], scalar2=7.0, op0=ALU.add, op1=ALU.min), [gbk, bbk], [Ak])
                                P.act(lambda e, A=A, S_=S_: e.activation(out=S_[:], in_=A[:], func=AF.Sigmoid, scale=1.702), [Ak], [Sk])
                                P.dve(lambda e, U=U, ub=ub, bb=bb, c=c: e.tensor_scalar(out=U[:], in0=psum[:, ub, :], scalar1=bb[:, 8 + c:9 + c], scalar2=8.0, op0=ALU.add, op1=ALU.min), [ubk, bbk], [Uk])
                                P.pool(lambda e, A=A, S_=S_: e.tensor_tensor(out=A[:], in0=A[:], in1=S_[:], op=ALU.mult), [Ak, Sk], [Ak])
                                P.dve(lambda e, at=at, c=c, U=U, A=A: e.scalar_tensor_tensor(out=at[:, c, :], in0=U[:], scalar=-6.0, in1=A[:], op0=ALU.max, op1=ALU.mult), [Uk, Ak], [atk + str(c)])
                            atkeys = [atk + str(c) for c in range(8)]
                            if b + 1 < NB:
                                transposes(b + 1, nxt[4], nxt[5])
                            Y, Yk = Yr.next()
                            for s4 in range(4):
                                for nh in range(2):
                                    yb_, ybk = ybr.next()
                                    for c in range(8):
                                        P.pe(lambda e, yb_=yb_, at=at, c=c, s4=s4, WD=WD, nh=nh: e.matmul(psum[:, yb_, :], at[:, c, s4 * 128:(s4 + 1) * 128], WD[:, c, nh * 512:(nh + 1) * 512],
                                                                                                          start=(c == 0), stop=(c == 7)), atkeys + wdkeys, [ybk])
                                    if yb_ == 4:
                                        P.dve(lambda e, Y=Y, s4=s4, nh=nh, swb=swb: e.tensor_scalar_mul(out=Y[:, s4, nh * 512:(nh + 1) * 512], in0=psum[:, 4, :], scalar1=swb[:, s4:s4 + 1]),
                                              [ybk, swk], [Yk + '%d%d' % (s4, nh)])
                                    else:
                                        P.act(lambda e, Y=Y, s4=s4, nh=nh, swb=swb: e.activation(out=Y[:, s4, nh * 512:(nh + 1) * 512], in_=psum[:, 5, :], func=AF.Copy, scale=swb[:, s4:s4 + 1]),
                                              [ybk, swk], [Yk + '%d%d' % (s4, nh)])
                            P.dma('sp', YS[b * BS:(b + 1) * BS, :].rearrange("(s p) d -> p s d", p=128), Y[:], [Yk + '%d%d' % (q, r_) for q in range(4) for r_ in range(2)], [('YS', b)], Yk)
                        P.flush()
                    with contextlib.ExitStack() as st3:
                        def sb3(name, shape, dt=F32):
                            return st3.enter_context(nc.sbuf_tensor(uname(name), list(shape), dt))
                        g2 = [sb3("g2_%d" % s_, [128, D]) for s_ in range(2)]
                        for s_ in range(2):
                            bload('sp', g2[s_][:], modv[l, s_, 5 * D:6 * D], 'g2_%d' % s_)
                        lng = sb3("lng2", [128, D]); lnb = sb3("lnb2", [128, D])
                        bload('act', lng[:], ln_g[l, 1], 'lng')
                        bload('act', lnb[:], ln_b[l, 1], 'lnb')
                        x1r = Ring('xm', [sb3("xm%d" % i, [128, D]) for i in range(4)])
                        Gr = Ring('G', [sb3("G%d" % i, [128, 4, D]) for i in range(4)])
                        rr = sb3("rr2", [128, D])
                        xor_ = Ring('xo', [sb3("xo%d" % i, [128, D]) for i in range(2)])
                        st6 = sb3("st6b", [128, 2, 6]); mv = sb3("mvb", [128, 2]); rs = sb3("rsb", [128, 1])
                        bdr = sb3("bdr", [NE, D])
                        P.dma('act', bdr[:], b_dn[l], [], ['bdr'], 'bdr')
                        mwTr = Ring('mwT', [sb3("mwT%d" % i, [NE, 128]) for i in range(2)])
                        tbr = Ring('tb', [0, 1]); bbk2 = Ring('bd', [(2, 3), (4, 5)])
                        for j, i in enumerate(tiles):
                            s_ = 1 if i < NT_C else 0
                            tb_, tbk = tbr.next()
                            P.pe(lambda e, j=j, tb_=tb_: e.transpose(psum[0:NE, tb_, 0:128], mwd[:, j, :], ident[:]), ['ident'], [tbk])
                            mwT, mwTk = mwTr.next()
                            P.act(lambda e, mwT=mwT, tb_=tb_: e.activation(out=mwT[:], in_=psum[0:NE, tb_, 0:128], func=AF.Copy), [tbk], [mwTk])
                            (bd0, bd1), bdk = bbk2.next()
                            for nh, bdb_ in enumerate((bd0, bd1)):
                                P.pe(lambda e, mwT=mwT, nh=nh, bdb_=bdb_: e.matmul(psum[:, bdb_, :], mwT[:], bdr[:, nh * 512:(nh + 1) * 512], start=True, stop=True), [mwTk, 'bdr'], [bdk + str(nh)])
                            x1, x1k = x1r.next()
                            P.dma('sp', x1[:], XM[i * 128:(i + 1) * 128, :], [], [x1k], x1k)
                            G, Gk = Gr.next()
                            for k in range(4):
                                P.add('pool', lambda e, G=G, j=j, k=k: e.indirect_dma_start(out=G[:, k, :], out_offset=None, in_=YS[:, :],
                                                                                         in_offset=bass.IndirectOffsetOnAxis(ap=DKi[:, j, k:k + 1], axis=0)), ['DKi'], [Gk + str(k)], Gk + str(k))
                            P.dve(lambda e, G=G: e.tensor_tensor(out=G[:, 0, :], in0=G[:, 0, :], in1=G[:, 1, :], op=ALU.add), [Gk + '0', Gk + '1'], [Gk + '0'])
                            P.dve(lambda e, G=G: e.tensor_tensor(out=G[:, 2, :], in0=G[:, 2, :], in1=G[:, 3, :], op=ALU.add), [Gk + '2', Gk + '3'], [Gk + '2'])
                            P.dve(lambda e, G=G: e.tensor_tensor(out=G[:, 0, :], in0=G[:, 0, :], in1=G[:, 2, :], op=ALU.add), [Gk + '0', Gk + '2'], [Gk + '0'])
                            for nh, bdb_ in enumerate((bd0, bd1)):
                                P.dve(lambda e, G=G, nh=nh, bdb_=bdb_: e.tensor_tensor(out=G[:, 0, nh * 512:(nh + 1) * 512], in0=G[:, 0, nh * 512:(nh + 1) * 512], in1=psum[:, bdb_, :], op=ALU.add),
                                      [Gk + '0', bdk + str(nh)], [Gk + '0'])
                            P.dve(lambda e, G=G, s_=s_: e.tensor_tensor(out=rr[:], in0=G[:, 0, :], in1=g2[s_][:], op=ALU.mult), [Gk + '0', 'g2_%d' % s_], ['rr'])
                            P.dve(lambda e, x1=x1: e.scalar_tensor_tensor(out=rr[:], in0=x1[:], scalar=ALPHA, in1=rr[:], op0=ALU.mult, op1=ALU.add), [x1k, 'rr'], ['rr'])
                            for nh in range(2):
                                P.dve(lambda e, nh=nh: e.bn_stats(out=st6[:, nh, :], in_=rr[:, nh * 512:(nh + 1) * 512]), ['rr'], ['st6_%d' % nh])
                            P.dve(lambda e: e.bn_aggr(out=mv[:], in_=st6[:]), ['st6_0', 'st6_1'], ['mv'])
                            P.dve(lambda e: e.tensor_scalar_add(out=rs[:], in0=mv[:, 1:2], scalar1=EPS), ['mv'], ['rs'])
                            P.act(lambda e: e.activation(out=rs[:], in_=rs[:], func=AF.Sqrt), ['rs'], ['rs'])
                            P.dve(lambda e: e.reciprocal(out=rs[:], in_=rs[:]), ['rs'], ['rs'])
                            xo, xok = xor_.next()
                            P.dve(lambda e, xo=xo: e.tensor_scalar(out=xo[:], in0=rr[:], scalar1=mv[:, 0:1], scalar2=rs[:, 0:1], op0=ALU.subtract, op1=ALU.mult), ['rr', 'mv', 'rs'], [xok])
                            P.dve(lambda e, xo=xo: e.tensor_tensor(out=xo[:], in0=xo[:], in1=lng[:], op=ALU.mult), [xok, 'lng'], [xok])
                            P.dve(lambda e, xo=xo: e.tensor_tensor(out=xo[:], in0=xo[:], in1=lnb[:], op=ALU.add), [xok, 'lnb'], [xok])
                            dst = XS0[i * 128:(i + 1) * 128, :] if not last else out[(i - NT_C) * 128:(i - NT_C + 1) * 128, :]
                            P.dma('sp', dst, xo[:], [xok], [('xout', i)], xok)
                        P.flush()
        P.flush()
    return nc


def make_consts():
    inv = (10000.0 ** (-np.arange(0, 32, 2, dtype=np.float32) / 32.0)).astype(np.float32)
    t = np.arange(L)
    row = (t // 64).astype(np.float32)
    col = (t % 64).astype(np.float32)
    ang = np.stack([row[:, None] * inv, col[:, None] * inv], axis=1)
    cos = np.cos(ang).astype(np.float32)
    sin = np.sin(ang).astype(np.float32)
    c64 = np.stack([cos, cos], axis=2).reshape(L, 64)
    s64 = np.stack([-sin, sin], axis=2).reshape(L, 64)
    p = np.arange(128, dtype=np.float32)
    pos = np.stack([127 - p, p, p + 1, 128 - p], axis=1)
    dif = p[None, :] - p[:, None]
    return dict(
        c_ident=np.eye(128, dtype=np.float32),
        c_cos=np.ascontiguousarray(np.tile(c64, (1, 8))),
        c_sin=np.ascontiguousarray(np.tile(s64, (1, 8))),
        c_pos=np.ascontiguousarray(pos.astype(np.float32)),
        c_dpos=np.maximum(dif, 0).astype(np.float32),
        c_dneg=np.maximum(-dif, 0).astype(np.float32),
        c_mge=(dif >= 0).astype(np.float32),
        c_zeros=np.zeros((2, 256), np.float32),
        c_iota32=np.tile(np.arange(NE, dtype=np.float32)[None, :], (128, 1)),
        c_lts=(p[:, None] < p[None, :]).astype(np.float32),
        c_bstart=np.tile((np.arange(NBMAX, dtype=np.float32) * BS)[None, :], (128, 1)),
        c_kp=(np.arange(8, dtype=np.float32)[None, :] * 128 + p[:, None]).astype(np.float32),
        c_iotap=p[:, None].astype(np.float32).copy(),
    )


WKEYS = ['w_mod', 'b_mod', 'w_in', 'conv_w', 'ret_decay_exp', 'ret_gn_g', 'q_norm_g', 'k_norm_g', 'w_out',
         'ln_g', 'ln_b', 'w_router', 'b_router', 'w_gate_up', 'b_gate_up', 'w_down', 'b_down']


def make_in_maps(inputs, cores, skip=()):
    consts = make_consts()
    shared = {k: np.ascontiguousarray(np.asarray(inputs[k], np.float32)) for k in WKEYS if k not in skip}
    shared['c_ctx'] = np.ascontiguousarray(np.asarray(inputs['c_ctx'], np.float32))
    shared.update(consts)
    maps = []
    for b in cores:
        m = dict(shared)
        m['x'] = np.ascontiguousarray(np.asarray(inputs['x'][b], np.float32))
        m['c'] = np.ascontiguousarray(np.asarray(inputs['c'][b], np.float32))
        m['ctx'] = np.ascontiguousarray(np.asarray(inputs['ctx'][b], np.float32))
        maps.append(m)
    return maps


def kernel(**inputs):
    nc = build()
    maps = make_in_maps(inputs, list(range(8)))
    res = run_bass_kernel_spmd(nc, maps, core_ids=list(range(8)))
    return np.stack([np.asarray(r["out"], np.float32) for r in res.results], axis=0)
```

```python
import contextlib
import math
import numpy as np
import concourse.bass as bass
import concourse.mybir as mybir
from concourse.bass_utils import run_bass_kernel_spmd

F32 = mybir.dt.float32
BF16 = mybir.dt.bfloat16
ALU = mybir.AluOpType
AF = mybir.ActivationFunctionType
AX = mybir.AxisListType

D = 1024
L = 4096
LC = 256
NT_C = 2
NT = 34
DEPTH = 2
NE = 32
BS = 512
NBMAX = 66
I32 = mybir.dt.int32
U32 = mybir.dt.uint32
ALPHA = (2.0 * DEPTH) ** 0.25
EPS = 1e-6


class Prog:
    ENGS = ('pe', 'act', 'dve', 'pool', 'sp')

    def __init__(self, nc, stack):
        self.nc = nc
        self.ops = []
        self.sems = {}
        self.stack = stack
        for eng in ('pe', 'act', 'dve', 'pool'):
            self.sems[('e', eng)] = stack.enter_context(nc.semaphore('sem_' + eng))
        self.cnt = {}
        self.streams = {}
        self.pool_cnt = []
        self.waited = {e: {} for e in self.ENGS}
        self.total_ops = 0

    def add(self, eng, fn, reads=(), writes=(), stream=None):
        self.ops.append((eng, fn, tuple(reads), tuple(writes), stream))

    def pe(self, fn, reads=(), writes=()):
        self.add('pe', fn, reads, writes)

    def act(self, fn, reads=(), writes=()):
        self.add('act', fn, reads, writes)

    def dve(self, fn, reads=(), writes=()):
        self.add('dve', fn, reads, writes)

    def pool(self, fn, reads=(), writes=()):
        self.add('pool', fn, reads, writes)

    def dma(self, q, out, in_, reads, writes, stream, **kw):
        self.add(q, lambda e: e.dma_start(out=out, in_=in_, **kw), reads, writes, stream)

    def flush(self):
        nc = self.nc
        ops = self.ops
        self.ops = []
        n = len(ops)
        if n == 0:
            return
        self.total_ops += n
        last_writer = {}
        readers = {}
        deps = [None] * n
        for i, (eng, fn, rd, wr, st) in enumerate(ops):
            d = set()
            for r in rd:
                j = last_writer.get(r)
                if j is not None:
                    d.add((j, 0))
            for w in wr:
                j = last_writer.get(w)
                if j is not None:
                    d.add((j, 1))
                for k in readers.get(w, ()):
                    if k != i:
                        d.add((k, 2))
            deps[i] = d
            for r in rd:
                readers.setdefault(r, []).append(i)
            for w in wr:
                last_writer[w] = i
                readers[w] = []
        sig = [False] * n
        need = [None] * n
        last_compute = {}
        for i in range(n):
            eng = ops[i][0]
            lst = set()
            for (j, kind) in deps[i]:
                jeng, _, _, _, jst = ops[j]
                if jst is None and jeng == eng:
                    if eng == 'pe' or eng == 'sp':
                        continue
                    if kind == 2:
                        continue
                if jst is None:
                    sig[j] = True
                lst.add(j)
            need[i] = lst
            if ops[i][4] is None and ops[i][1] is not None:
                last_compute[eng] = i
        for eng, i in last_compute.items():
            if eng != 'sp':
                sig[i] = True
        sval = [None] * n
        phase_map = {}
        for i, (eng, fn, rd, wr, st) in enumerate(ops):
            if st is not None:
                if st not in phase_map:
                    k = len(phase_map)
                    phase_map[st] = k
                    if k >= len(self.pool_cnt):
                        self.pool_cnt.append(0)
                        self.sems[('s', k)] = self.stack.enter_context(nc.semaphore('sd_%d' % k))
                k = phase_map[st]
                self.pool_cnt[k] += 1
                sval[i] = (('s', k), 16 * self.pool_cnt[k])
            elif sig[i]:
                self.cnt[eng] = self.cnt.get(eng, 0) + 1
                sval[i] = (('e', eng), self.cnt[eng])
        per_eng = {e: [] for e in self.ENGS}
        for i, op in enumerate(ops):
            per_eng[op[0]].append(i)
        sems = self.sems
        final = {}
        for eng in ('pe', 'act', 'dve', 'pool'):
            if self.cnt.get(eng, 0) > 0:
                final[('e', eng)] = self.cnt[eng]
        for k, c in enumerate(self.pool_cnt):
            final[('s', k)] = 16 * c

        def run(engname, e):
            waited = self.waited[engname]
            for i in per_eng[engname]:
                _, fn, rd, wr, st = ops[i]
                w = {}
                for j in need[i]:
                    key, val = sval[j]
                    if w.get(key, 0) < val:
                        w[key] = val
                for key, val in w.items():
                    if waited.get(key, 0) >= val:
                        continue
                    waited[key] = val
                    e.wait_ge(sems[key], val)
                if fn is None:
                    continue
                ins = fn(e)
                if sval[i] is not None:
                    key, val = sval[i]
                    ins.then_inc(sems[key], 16 if key[0] == 's' else 1)
            for key, val in final.items():
                if key == ('e', engname):
                    continue
                if waited.get(key, 0) >= val:
                    continue
                waited[key] = val
                e.wait_ge(sems[key], val)

        with nc.Block() as block:
            @block.tensor
            def _(e):
                run('pe', e)

            @block.scalar
            def _(e):
                run('act', e)

            @block.vector
            def _(e):
                run('dve', e)

            @block.gpsimd
            def _(e):
                run('pool', e)

            @block.sync
            def _(e):
                run('sp', e)


def pipeline_reorder(P, base, marks):
    ops = P.ops
    H = [ops[a:b] for (a, b, c) in marks]
    T = [ops[b:c] for (a, b, c) in marks]
    new = list(ops[:base]) + H[0]
    for i in range(len(marks)):
        if i + 1 < len(marks):
            new += H[i + 1]
        new += T[i]
    new += list(ops[marks[-1][2]:])
    assert len(new) == len(ops)
    P.ops = new


class Ring:
    def __init__(self, name, tiles):
        self.name = name
        self.tiles = tiles
        self.i = 0

    def next(self):
        k = self.i % len(self.tiles)
        self.i += 1
        return self.tiles[k], '%s%d' % (self.name, k)


SEC = dict(u=(0, 256), B=(256, 256), C=(512, 256), rq=(768, 256), rk=(1024, 256), rv=(1280, 256),
           rg=(1536, 256), aq=(1792, 512), ak=(2304, 128), av=(2432, 128))
MYCOL = dict(u=0, C=256, B=512, rv=768, rq=1024, rk=1280, aq=1536, rg=2048, ak=2304, av=2432)


def build(phases=('p0', 'p1', 'att', 'ret', 'p3', 'moe'), layers=(0, 1), debug=False, moe_experts=NE, cut=99):
    nc = bass.Bass("TRN2", target_bir_lowering=False)

    _uc = [0]

    def uname(name):
        _uc[0] += 1
        return '%s_u%d' % (name, _uc[0])

    def din(name, shape, dt=F32):
        return nc.dram_tensor(name, list(shape), dt, kind="ExternalInput").ap()

    def dscr(name, shape, dt=F32):
        return nc.dram_tensor(name, list(shape), dt, kind=("ExternalOutput" if debug else "Internal")).ap()

    x_in = din("x", [L, D])
    c_in = din("c", [D])
    ctx_in = din("ctx", [LC, D])
    cctx_in = din("c_ctx", [D])
    w_mod = din("w_mod", [DEPTH, D, 6 * D])
    b_mod = din("b_mod", [DEPTH, 6 * D])
    w_in = din("w_in", [DEPTH, D, 2560])
    conv_w = din("conv_w", [DEPTH, 256, 3])
    rde = din("ret_decay_exp", [DEPTH, 2, 4])
    gn_g = din("ret_gn_g", [DEPTH, 256])
    qn_g = din("q_norm_g", [DEPTH, 64])
    kn_g = din("k_norm_g", [DEPTH, 64])
    w_out = din("w_out", [DEPTH, D, D])
    ln_g = din("ln_g", [DEPTH, 2, D])
    ln_b = din("ln_b", [DEPTH, 2, D])
    w_router = din("w_router", [DEPTH, D, NE])
    b_router = din("b_router", [DEPTH, NE])
    if 'moe' in phases:
        w_gu = din("w_gate_up", [DEPTH, NE, D, 2 * D])
        b_gu = din("b_gate_up", [DEPTH, NE, 2 * D])
        w_dn = din("w_down", [DEPTH, NE, D, D])
        b_dn = din("b_down", [DEPTH, NE, D])
    ident_in = din("c_ident", [128, 128])
    cos_in = din("c_cos", [L, 512])
    sin_in = din("c_sin", [L, 512])
    pos_in = din("c_pos", [128, 4])
    dpos_in = din("c_dpos", [128, 128])
    dneg_in = din("c_dneg", [128, 128])
    mge_in = din("c_mge", [128, 128])
    zeros_in = din("c_zeros", [2, 256])
    iota32_in = din("c_iota32", [128, NE])
    lts_in = din("c_lts", [128, 128])
    bstart_in = din("c_bstart", [128, NBMAX])
    kp_in = din("c_kp", [128, 8])
    iotap_in = din("c_iotap", [128, 1])

    out = nc.dram_tensor("out", [L, D], F32, kind="ExternalOutput").ap()

    N = NT * 128
    modv = dscr("modv", [DEPTH, 2, 6 * D])
    PB = dscr("PB", [N + 4, 512])
    RQT = dscr("RQT", [3, 4, 64, N], BF16)
    RKT = dscr("RKT", [4, 64, N], BF16)
    RTOK = dscr("RTOK", [N, 768], BF16)
    RG = dscr("RG", [N, 256])
    AQT = dscr("AQT", [8, 64, N], BF16)
    AKT = dscr("AKT", [2, 64, N], BF16)
    AV = dscr("AV", [N, 128], BF16)
    MIX = dscr("MIX", [N, 1024])
    XS0 = dscr("XS0", [N, D])
    XM = dscr("XM", [N, D])
    H2R = dscr("H2R", [N, D], BF16)
    RW = dscr("RW", [N, 4])
    RE = dscr("RE", [N, 4])
    XS = dscr("XS", [NBMAX * BS, D], BF16)
    SW = dscr("SW", [NBMAX * BS, 1])
    YS = dscr("YS", [NBMAX * BS, D])
    BGT = dscr("BGT", [DEPTH * NE * 128, 16])

    def prow(i):
        return 1 + i * 128 if i < NT_C else 259 + (i - NT_C) * 128

    with contextlib.ExitStack() as gst:
        P = Prog(nc, gst)
        psum = gst.enter_context(nc.psum_tensor("psum", [128, 8, 512], F32))
        ident = gst.enter_context(nc.sbuf_tensor("ident", [128, 128], F32))
        P.dma('sp', ident[:], ident_in, [], ['ident'], 'ident')

        def bank(b):
            return psum[:, b, :]

        if 'p0' in phases:
            with contextlib.ExitStack() as st:
                def sb(name, shape, dt=F32):
                    return st.enter_context(nc.sbuf_tensor(uname(name), list(shape), dt))
                cnd = sb("cnd", [128, 2, 8])
                P.dma('sp', cnd[:, 0, :], c_in.rearrange("(k p) -> p k", p=128), [], ['cnd'], 'cnd0',
                      allow_slow_non_contiguous=True)
                P.dma('sp', cnd[:, 1, :], cctx_in.rearrange("(k p) -> p k", p=128), [], ['cnd'], 'cnd1',
                      allow_slow_non_contiguous=True)
                P.act(lambda e: e.activation(out=cnd[:], in_=cnd[:], func=AF.Silu), ['cnd'], ['cnd'])
                wring = Ring('wm', [sb("wm%d" % i, [128, 8, 512]) for i in range(6)])
                bring = Ring('bm', [sb("bm%d" % i, [2, 512]) for i in range(2)])
                oring = Ring('om', [sb("om%d" % i, [2, 512]) for i in range(2)])
                for l in layers:
                    for n in range(12):
                        wt, wk = wring.next()
                        bt, bk = bring.next()
                        ot, ok = oring.next()
                        P.dma('sp' if n % 2 == 0 else 'act', wt[:], w_mod[l, :, n * 512:(n + 1) * 512].rearrange("(k p) n -> p k n", p=128),
                              [], [wk], wk)
                        P.dma('sp', bt[:], b_mod[l, n * 512:(n + 1) * 512].partition_broadcast(2), [], [bk], bk)
                        pbank = n % 2
                        pb_ = 'ps0_%d' % pbank
                        for k in range(8):
                            P.pe(lambda e, k=k, wt=wt, pbank=pbank: e.matmul(psum[0:2, pbank, :], cnd[:, :, k], wt[:, k, :],
                                                                            start=(k == 0), stop=(k == 7)),
                                 ['cnd', wk], [pb_])
                        P.dve(lambda e, ot=ot, bt=bt, pbank=pbank: e.tensor_tensor(out=ot[:], in0=psum[0:2, pbank, :], in1=bt[:], op=ALU.add),
                              [pb_, bk], [ok])
                        if n in (2, 3, 8, 9):
                            P.dve(lambda e, ot=ot: e.tensor_scalar_add(out=ot[:], in0=ot[:], scalar1=1.0), [ok], [ok])
                        P.dma('sp', modv[l, :, n * 512:(n + 1) * 512], ot[:], [ok], [('modv', l)], ok)
                P.flush()

        def bload(q, tile_ap, vec_ap, key, reads=()):
            P.dma(q, tile_ap, vec_ap.partition_broadcast(128), list(reads), [key], key)

        for l in layers:
            last = (l == DEPTH - 1)
            xsrc = (lambda i: (ctx_in[i * 128:(i + 1) * 128, :] if i < NT_C else x_in[(i - NT_C) * 128:(i - NT_C + 1) * 128, :])) \
                if l == 0 else (lambda i: XS0[i * 128:(i + 1) * 128, :])
            xs_key = (lambda i: ('xin', i)) if l == 0 else (lambda i: ('XS0', i))
            out_tiles = list(range(NT)) if not last else list(range(NT_C, NT))

            if 'p1' in phases:
                with contextlib.ExitStack() as st:
                    def sb(name, shape, dt=F32):
                        return st.enter_context(nc.sbuf_tensor(uname(name), list(shape), dt))
                    win = sb("win", [128, 8, 2560], BF16)
                    for name, (c0, w) in SEC.items():
                        m0 = MYCOL[name]
                        P.dma('pool', win[:, :, m0:m0 + w], w_in[l, :, c0:c0 + w].rearrange("(k p) n -> p k n", p=128),
                              [], ['win_' + name], 'win_' + name)
                    winkeys = ['win_' + k for k in SEC]
                    sc1 = [sb("sc1_%d" % s, [128, D]) for s in range(2)]
                    sh1 = [sb("sh1_%d" % s, [128, D]) for s in range(2)]
                    for s in range(2):
                        bload('sp', sc1[s][:], modv[l, s, D:2 * D], 'sc1_%d' % s, [('modv', l)])
                        bload('sp', sh1[s][:], modv[l, s, 0:D], 'sh1_%d' % s, [('modv', l)])
                    gq = sb("gq", [128, 64])
                    gk = sb("gk", [128, 64])
                    bload('act', gq[:], qn_g[l], 'gq')
                    bload('act', gk[:], kn_g[l], 'gk')
                    zpad = sb("zpad", [2, 256])
                    P.dma('act', zpad[:], zeros_in, [], ['zpad'], 'zpad')
                    for r0 in (0, 257):
                        P.dma('act', PB[r0:r0 + 2, 0:256] if r0 else PB[0:1, 0:256], zpad[0:2, :] if r0 else zpad[0:1, :],
                              ['zpad'], [('PBpad', r0)], 'zp%d' % r0)
                    P.dma('act', PB[N + 3:N + 4, 0:256], zpad[0:1, :], ['zpad'], [('PBpad', 3)], 'zp3')
                    posc = sb("posc", [128, 4])
                    P.dma('act', posc[:], pos_in, [], ['posc'], 'posc')
                    lg = sb("lg", [128, 8])
                    bload('act', lg[:], rde[l].rearrange("a h -> (a h)"), 'lg')
                    P.act(lambda e: e.activation(out=lg[:], in_=lg[:], func=AF.Exp, scale=-math.log(2.0)), ['lg'], ['lg'])
                    P.act(lambda e: e.activation(out=lg[:], in_=lg[:], func=AF.Ln, scale=-1.0, bias=1.0), ['lg'], ['lg'])
                    tab4 = sb("tab4", [128, 4, 4])
                    for ti, (di, pc) in enumerate(((0, 0), (1, 1), (0, 2), (1, 3))):
                        P.act(lambda e, ti=ti, di=di, pc=pc: e.activation(out=tab4[:, ti, :], in_=lg[:, di * 4:(di + 1) * 4],
                                                                          func=AF.Exp, scale=posc[:, pc:pc + 1]),
                              ['lg', 'posc'], ['tab4'])
                    P.dve(lambda e: e.tensor_scalar_mul(out=tab4[:, 0:2, :], in0=tab4[:, 0:2, :], scalar1=0.125), ['tab4'], ['tab4'])
                    TAB = sb("TAB", [128, 4, 4, 64])
                    P.dve(lambda e: e.tensor_copy(out=TAB[:].rearrange("p t h d -> p (t h) d"),
                                                  in_=tab4[:].rearrange("p t h -> p (t h)").unsqueeze(2).to_broadcast([128, 16, 64])),
                          ['tab4'], ['TAB'])
                    gq8 = sb("gq8", [128, 8, 64])
                    P.dve(lambda e: e.tensor_copy(out=gq8[:], in_=gq[:].unsqueeze(1).to_broadcast([128, 8, 64])), ['gq'], ['gq8'])
                    gk2 = sb("gk2", [128, 2, 64])
                    P.dve(lambda e: e.tensor_copy(out=gk2[:], in_=gk[:].unsqueeze(1).to_broadcast([128, 2, 64])), ['gk'], ['gk2'])

                    xring = Ring('xt', [sb("xt%d" % i, [128, D]) for i in range(3)])
                    csring = Ring('cs', [sb("cs%d" % i, [128, 512]) for i in range(3)])
                    snring = Ring('sn', [sb("sn%d" % i, [128, 512]) for i in range(3)])
                    hring = Ring('h', [sb("h%d" % i, [128, D]) for i in range(2)])
                    hTring = Ring('hT', [sb("hT%d" % i, [128, 8, 128], BF16) for i in range(2)])
                    usb = sb("usb", [128, 256])
                    pcb = Ring('pcb', [sb("pcb%d" % i, [128, 512]) for i in range(2)])
                    t1 = sb("t1", [128, 512])
                    t2 = sb("t2", [128, 512])
                    rq = sb("rq", [128, 4, 256])
                    rtok = Ring('rtok', [sb("rtok%d" % i, [128, 768], BF16) for i in range(2)])
                    rgt = Ring('rgt', [sb("rgt%d" % i, [128, 256]) for i in range(2)])
                    rT = Ring('rT', [sb("rT%d" % i, [128, 8, 128], BF16) for i in range(2)])
                    sq = sb("sq", [128, 512])
                    ss = sb("ss", [128, 8])
                    aq = sb("aq", [128, 512])
                    aqn = sb("aqn", [128, 512])
                    akn = sb("akn", [128, 128])
                    ak = sb("ak", [128, 128])
                    aT = Ring('aT', [sb("aT%d" % i, [128, 5, 128], BF16) for i in range(2)])
                    avt = Ring('avt', [sb("avt%d" % i, [128, 128], BF16) for i in range(2)])

                    def rope(src_ap, W, dst_ap, src_keys, dst_key, cs, ck, sn, sk):
                        g = W // 32
                        P.dve(lambda e: e.tensor_tensor(out=t1[:, :W], in0=src_ap, in1=cs[:, :W], op=ALU.mult),
                              src_keys + [ck], ['t1'])
                        s4 = src_ap.rearrange("p (g a f) -> p g a f", a=2, f=16)
                        t4 = t2[:, :W].rearrange("p (g a f) -> p g a f", a=2, f=16)
                        n4 = sn[:, :W].rearrange("p (g a f) -> p g a f", a=2, f=16)
                        P.dve(lambda e: e.tensor_tensor(out=t4[:, :, 0, :], in0=s4[:, :, 1, :], in1=n4[:, :, 0, :], op=ALU.mult),
                              src_keys + [sk], ['t2a'])
                        P.dve(lambda e: e.tensor_tensor(out=t4[:, :, 1, :], in0=s4[:, :, 0, :], in1=n4[:, :, 1, :], op=ALU.mult),
                              src_keys + [sk], ['t2b'])
                        P.pool(lambda e: e.tensor_tensor(out=dst_ap, in0=t1[:, :W], in1=t2[:, :W], op=ALU.add),
                               ['t1', 't2a', 't2b'], [dst_key])

                    ld = {}

                    def issue_loads(i):
                        xt, xk = xring.next()
                        P.dma('sp', xt[:], xsrc(i), [xs_key(i)], [xk], xk)
                        if i >= NT_C:
                            cs, ck = csring.next()
                            sn, sk = snring.next()
                            t0 = (i - NT_C) * 128
                            P.dma('sp', cs[:], cos_in[t0:t0 + 128, :], [], [ck], ck)
                            P.dma('sp', sn[:], sin_in[t0:t0 + 128, :], [], [sk], sk)
                            ld[i] = (xt, xk, cs, ck, sn, sk)
                        else:
                            ld[i] = (xt, xk, None, None, None, None)
                    issue_loads(0)
                    p1_base = len(P.ops)
                    p1_marks = []
                    for i in range(NT):
                        isctx = i < NT_C
                        s = 1 if isctx else 0
                        m_a = len(P.ops)
                        if i + 1 < NT:
                            issue_loads(i + 1)
                        xt, xk, cs, ck, sn, sk = ld.pop(i)
                        h, hk = hring.next()
                        P.dve(lambda e, h=h, xt=xt, s=s: e.tensor_tensor(out=h[:], in0=xt[:], in1=sc1[s][:], op=ALU.mult),
                              [xk, 'sc1_%d' % s], [hk])
                        P.dve(lambda e, h=h, s=s: e.tensor_tensor(out=h[:], in0=h[:], in1=sh1[s][:], op=ALU.add),
                              [hk, 'sh1_%d' % s], [hk])
                        for k in range(8):
                            P.pe(lambda e, h=h, k=k: e.transpose(psum[:, 5 + k // 4, (k % 4) * 128:(k % 4 + 1) * 128],
                                                                 h[:, k * 128:(k + 1) * 128], ident[:]),
                                 [hk, 'ident'], ['pT%d' % k])
                        hT, hTk = hTring.next()
                        for hh in range(2):
                            P.act(lambda e, hT=hT, hh=hh: e.activation(out=hT[:, hh * 4:(hh + 1) * 4, :].rearrange("p k n -> p (k n)"),
                                                                       in_=psum[:, 5 + hh, :], func=AF.Copy),
                                  ['pT%d' % k for k in range(hh * 4, hh * 4 + 4)], [hTk + '_%d' % hh])
                        m_b = len(P.ops)
                        for b in range(5):
                            for k in range(8):
                                P.pe(lambda e, hT=hT, b=b, k=k: e.matmul(bank(b), hT[:, k, :], win[:, k, b * 512:(b + 1) * 512],
                                                                         start=(k == 0), stop=(k == 7)),
                                     [hTk + '_%d' % (k // 4)] + winkeys, ['z%d' % b])
                        pc, pck = pcb.next()
                        P.act(lambda e: e.activation(out=usb[:], in_=psum[:, 0, 0:256], func=AF.Copy), ['z0'], ['usb'])
                        P.dve(lambda e, pc=pc: e.tensor_tensor(out=pc[:, 0:256], in0=psum[:, 0, 256:512], in1=usb[:], op=ALU.mult),
                              ['z0', 'usb'], [pck + 'a'])
                        P.act(lambda e, pc=pc: e.activation(out=pc[:, 256:512], in_=psum[:, 1, 0:256], func=AF.Copy), ['z1'], [pck + 'b'])
                        r0 = prow(i)
                        P.dma('sp', PB[r0:r0 + 128, :], pc[:], [pck + 'a', pck + 'b'], [('PB', i)], pck)
                        rt, rtk = rtok.next()
                        P.act(lambda e, rt=rt: e.activation(out=rt[:, 512:768], in_=psum[:, 1, 256:512], func=AF.Copy), ['z1'], [rtk + 'v'])
                        if isctx:
                            P.act(lambda e: e.activation(out=rq[:, 0, :], in_=psum[:, 2, 0:256], func=AF.Copy), ['z2'], ['rq0'])
                            P.act(lambda e: e.activation(out=rq[:, 3, :], in_=psum[:, 2, 256:512], func=AF.Copy), ['z2'], ['rq3'])
                        else:
                            rope(psum[:, 2, 0:256], 256, rq[:, 0, :], ['z2'], 'rq0', cs, ck, sn, sk)
                            rope(psum[:, 2, 256:512], 256, rq[:, 3, :], ['z2'], 'rq3', cs, ck, sn, sk)
                        TABf = TAB[:].rearrange("p t h d -> p t (h d)")
                        P.dve(lambda e: e.tensor_tensor(out=rq[:, 1, :], in0=rq[:, 0, :], in1=TABf[:, 2, :], op=ALU.mult), ['rq0', 'TAB'], ['rq1'])
                        P.pool(lambda e: e.tensor_tensor(out=rq[:, 2, :], in0=rq[:, 0, :], in1=TABf[:, 3, :], op=ALU.mult), ['rq0', 'TAB'], ['rq2'])
                        P.dve(lambda e, rt=rt: e.tensor_tensor(out=rt[:, 0:256], in0=rq[:, 3, :], in1=TABf[:, 0, :], op=ALU.mult), ['rq3', 'TAB'], [rtk + 'f'])
                        P.pool(lambda e, rt=rt: e.tensor_tensor(out=rt[:, 256:512], in0=rq[:, 3, :], in1=TABf[:, 1, :], op=ALU.mult), ['rq3', 'TAB'], [rtk + 'b'])
                        P.dma('sp', RTOK[i * 128:(i + 1) * 128, :], rt[:], [rtk + 'v', rtk + 'f', rtk + 'b'], [('RTOK', i)], rtk)
                        rg_, rgk = rgt.next()
                        P.act(lambda e, rg_=rg_: e.activation(out=rg_[:], in_=psum[:, 4, 0:256], func=AF.Silu), ['z4'], [rgk])
                        P.dma('act', RG[i * 128:(i + 1) * 128, :], rg_[:], [rgk], [('RG', i)], rgk)
                        for t in range(4):
                            for c2 in range(2):
                                idx = t * 2 + c2
                                P.pe(lambda e, t=t, c2=c2, idx=idx: e.transpose(psum[:, 5 + idx // 4, (idx % 4) * 128:(idx % 4 + 1) * 128],
                                                                                rq[:, t, c2 * 128:(c2 + 1) * 128], ident[:]),
                                     ['rq%d' % t, 'ident'], ['pT%d' % idx])
                        rTt, rTk = rT.next()
                        for hh in range(2):
                            if hh == 0:
                                P.act(lambda e, rTt=rTt: e.activation(out=rTt[:, 0:4, :].rearrange("p k n -> p (k n)"), in_=psum[:, 5, :], func=AF.Copy),
                                      ['pT0', 'pT1', 'pT2', 'pT3'], [rTk + 'a'])
                            else:
                                P.act(lambda e, rTt=rTt: e.activation(out=rTt[:, 4:6, :].rearrange("p k n -> p (k n)"), in_=psum[:, 6, 0:256], func=AF.Copy),
                                      ['pT4', 'pT5'], [rTk + 'b'])
                                P.act(lambda e, rTt=rTt: e.activation(out=rTt[:, 6:8, :].rearrange("p k n -> p (k n)"), in_=psum[:, 6, 256:512], func=AF.Copy, scale=0.125),
                                      ['pT6', 'pT7'], [rTk + 'c'])
                        for t in range(3):
                            P.dma('act', RQT[t, :, :, i * 128:(i + 1) * 128].rearrange("(c q) d n -> (q d) c n", q=2),
                                  rTt[:, 2 * t:2 * t + 2, :], [rTk + 'a', rTk + 'b'], [('RQT', i, t)], rTk + 'q%d' % t)
                        P.dma('act', RKT[:, :, i * 128:(i + 1) * 128].rearrange("(c q) d n -> (q d) c n", q=2),
                              rTt[:, 6:8, :], [rTk + 'c'], [('RKT', i)], rTk + 'k')
                        P.act(lambda e: e.activation(out=sq[:], in_=psum[:, 3, :], func=AF.Square), ['z3'], ['sq'])
                        P.dve(lambda e: e.tensor_reduce(out=ss[:], in_=sq[:].rearrange("p (h d) -> p h d", d=64), axis=AX.X, op=ALU.add), ['sq'], ['ss'])
                        P.dve(lambda e: e.tensor_scalar(out=ss[:], in0=ss[:], scalar1=1.0 / 64, scalar2=EPS, op0=ALU.mult, op1=ALU.add), ['ss'], ['ss'])
                        P.act(lambda e: e.activation(out=ss[:], in_=ss[:], func=AF.Sqrt), ['ss'], ['ss'])
                        P.dve(lambda e: e.reciprocal(out=ss[:], in_=ss[:]), ['ss'], ['ss'])
                        P.dve(lambda e: e.tensor_tensor(out=aqn[:].rearrange("p (h d) -> p h d", d=64), in0=psum[:, 3, :].rearrange("p (h d) -> p h d", d=64),
                                                        in1=ss[:].unsqueeze(2).to_broadcast([128, 8, 64]), op=ALU.mult), ['z3', 'ss'], ['aqn'])
                        P.pool(lambda e: e.tensor_tensor(out=aqn[:], in0=aqn[:], in1=gq8[:].rearrange("p h d -> p (h d)"), op=ALU.mult), ['aqn', 'gq8'], ['aqn'])
                        if isctx:
                            aq_src, aq_key = aqn, 'aqn'
                        else:
                            rope(aqn[:], 512, aq[:], ['aqn'], 'aq', cs, ck, sn, sk)
                            aq_src, aq_key = aq, 'aq'
                        av_, avk = avt.next()
                        P.act(lambda e, av_=av_: e.activation(out=av_[:], in_=psum[:, 4, 384:512], func=AF.Copy), ['z4'], [avk])
                        P.dma('act', AV[i * 128:(i + 1) * 128, :], av_[:], [avk], [('AV', i)], avk)
                        P.act(lambda e: e.activation(out=sq[:, 0:128], in_=psum[:, 4, 256:384], func=AF.Square), ['z4'], ['sqk'])
                        P.dve(lambda e: e.tensor_reduce(out=ss[:, 0:2], in_=sq[:, 0:128].rearrange("p (h d) -> p h d", d=64), axis=AX.X, op=ALU.add), ['sqk'], ['ssk'])
                        P.dve(lambda e: e.tensor_scalar(out=ss[:, 0:2], in0=ss[:, 0:2], scalar1=1.0 / 64, scalar2=EPS, op0=ALU.mult, op1=ALU.add), ['ssk'], ['ssk'])
                        P.act(lambda e: e.activation(out=ss[:, 0:2], in_=ss[:, 0:2], func=AF.Sqrt), ['ssk'], ['ssk'])
                        P.dve(lambda e: e.reciprocal(out=ss[:, 0:2], in_=ss[:, 0:2]), ['ssk'], ['ssk'])
                        P.dve(lambda e: e.tensor_tensor(out=akn[:].rearrange("p (h d) -> p h d", d=64), in0=psum[:, 4, 256:384].rearrange("p (h d) -> p h d", d=64),
                                                        in1=ss[:, 0:2].unsqueeze(2).to_broadcast([128, 2, 64]), op=ALU.mult), ['z4', 'ssk'], ['akn'])
                        P.pool(lambda e: e.tensor_tensor(out=akn[:], in0=akn[:], in1=gk2[:].rearrange("p h d -> p (h d)"), op=ALU.mult), ['akn', 'gk2'], ['akn'])
                        if isctx:
                            ak_src, ak_key = akn, 'akn'
                        else:
                            rope(akn[:], 128, ak[:], ['akn'], 'ak', cs, ck, sn, sk)
                            ak_src, ak_key = ak, 'ak'
                        for c4 in range(4):
                            P.pe(lambda e, c4=c4, aq_src=aq_src: e.transpose(psum[:, 7, c4 * 128:(c4 + 1) * 128], aq_src[:, c4 * 128:(c4 + 1) * 128], ident[:]),
                                 [aq_key, 'ident'], ['pA%d' % c4])
                        P.pe(lambda e, ak_src=ak_src: e.transpose(psum[:, 5, 0:128], ak_src[:, 0:128], ident[:]), [ak_key, 'ident'], ['pT0'])
                        aTt, aTk = aT.next()
                        P.act(lambda e, aTt=aTt: e.activation(out=aTt[:, 0:4, :].rearrange("p k n -> p (k n)"), in_=psum[:, 7, :], func=AF.Copy),
                              ['pA0', 'pA1', 'pA2', 'pA3'], [aTk + 'q'])
                        P.act(lambda e, aTt=aTt: e.activation(out=aTt[:, 4, :], in_=psum[:, 5, 0:128], func=AF.Copy), ['pT0'], [aTk + 'k'])
                        P.dma('sp', AQT[:, :, i * 128:(i + 1) * 128].rearrange("(c q) d n -> (q d) c n", q=2), aTt[:, 0:4, :],
                              [aTk + 'q'], [('AQT', i)], aTk + 'q')
                        P.dma('sp', AKT[:, :, i * 128:(i + 1) * 128].rearrange("q d n -> (q d) n"), aTt[:, 4, :],
                              [aTk + 'k'], [('AKT', i)], aTk + 'k')
                        p1_marks.append((m_a, m_b, len(P.ops)))
                    pipeline_reorder(P, p1_base, p1_marks)
                    P.flush()


            if 'att' in phases:
                with contextlib.ExitStack() as st:
                    def sb(name, shape, dt=F32):
                        return st.enter_context(nc.sbuf_tensor(uname(name), list(shape), dt))
                    KT = sb("KT", [128, 2, N], BF16)
                    P.pool(lambda e: e.memset(KT[64:128, :, :], 0.0), [], ['KTz'])
                    P.dma('sp', KT[0:64, :, :], AKT.rearrange("k d n -> d k n"), [], ['KT'], 'KT')
                    V1 = sb("V1", [128, NT, 2, 65], BF16)
                    P.pool(lambda e: e.memset(V1[:], 1.0), [], ['V1'])
                    for kk_ in range(2):
                        P.dma('act', V1[:, :, kk_, 0:64], AV[:, kk_ * 64:(kk_ + 1) * 64].rearrange("(c p) d -> p c d", p=128), [], ['V1'], 'V1_%d' % kk_)
                    qring = Ring('QT', [sb("QT%d" % i, [128, N], BF16) for i in range(2)])
                    for qi_, qt_ in enumerate(qring.tiles):
                        P.pool(lambda e, qt_=qt_: e.memset(qt_[64:128, :], 0.0), [], ['QTz%d' % qi_])
                    ptring = Ring('PT', [sb("PT%d" % i, [128, 512], BF16) for i in range(4)])
                    aoring = Ring('AO', [sb("AO%d" % i, [128, 4, 64]) for i in range(2)])
                    rcring = Ring('rc', [sb("rc%d" % i, [128, 4]) for i in range(2)])
                    sbank = Ring('S', [0, 1, 2, 3])
                    obank = Ring('O', [4, 5])
                    groups = []
                    if not last:
                        groups.append((0, 2, [0, 1]))
                    for g in range(8):
                        groups.append((LC + g * 512, 4, list(range(NT))))
                    items = []
                    for hq in range(8):
                        for gi, (q0, nq, chunks) in enumerate(groups):
                            for ci, c in enumerate(chunks):
                                items.append((hq, gi, q0, nq, ci, c, len(chunks)))
                    qt_of = {}
                    st_of = {}

                    def get_qt(hq):
                        if hq not in qt_of:
                            QT, qk = qring.next()
                            P.dma('sp', QT[0:64, :], AQT[hq], [], [qk], qk)
                            qt_of[hq] = (QT, qk)
                        return qt_of[hq]

                    def emit_S(t):
                        hq, gi, q0, nq, ci, c, nch = items[t]
                        QT, qk = get_qt(hq)
                        kvh = hq // 4
                        W = nq * 128
                        sbk, sk = sbank.next()
                        P.pe(lambda e, sbk=sbk, c=c, QT=QT, q0=q0, W=W, kvh=kvh: e.matmul(psum[:, sbk, 0:W], KT[:, kvh, c * 128:(c + 1) * 128],
                                                                                         QT[:, q0:q0 + W], start=True, stop=True),
                             ['KT', 'KTz', 'QTz0', 'QTz1', qk], [sk])
                        st_of[t] = (sbk, sk)

                    cur_o = [None]

                    def emit_rest(t):
                        hq, gi, q0, nq, ci, c, nch = items[t]
                        kvh = hq // 4
                        W = nq * 128
                        sbk, sk = st_of.pop(t)
                        if ci == 0:
                            cur_o[0] = obank.next()
                        ob, ok = cur_o[0]
                        Ov = psum[:, ob, 0:260].rearrange("p (j e) -> p j e", e=65)
                        PT, pk = ptring.next()
                        P.act(lambda e, PT=PT, sbk=sbk, W=W: e.activation(out=PT[:, 0:W], in_=psum[:, sbk, 0:W], func=AF.Exp, scale=0.125),
                              [sk], [pk])
                        if t + 2 < len(items):
                            emit_S(t + 2)
                        for j in range(nq):
                            P.pe(lambda e, PT=PT, j=j, c=c, kvh=kvh, ci=ci, Ov=Ov, nch=nch: e.matmul(
                                Ov[:, j, :], PT[:, j * 128:(j + 1) * 128], V1[:, c, kvh, :],
                                start=(ci == 0 and j == 0), stop=(ci == nch - 1), skip_group_check=True),
                                [pk, 'V1'], [ok])
                        if ci == nch - 1:
                            rc, rk_ = rcring.next()
                            AO, ak_ = aoring.next()
                            P.dve(lambda e, rc=rc, Ov=Ov, nq=nq: e.reciprocal(out=rc[:, 0:nq], in_=Ov[:, 0:nq, 64]), [ok], [rk_])
                            P.dve(lambda e, rc=rc, Ov=Ov, nq=nq, AO=AO: e.tensor_tensor(out=AO[:, 0:nq, :], in0=Ov[:, 0:nq, 0:64],
                                                                                      in1=rc[:, 0:nq].unsqueeze(2).to_broadcast([128, nq, 64]), op=ALU.mult),
                                  [ok, rk_], [ak_])
                            P.dma('sp', MIX[q0:q0 + W, 512 + hq * 64:512 + (hq + 1) * 64].rearrange("(j p) d -> p j d", p=128), AO[:, 0:nq, :],
                                  [ak_], [('MIXa', q0, hq)], ak_)
                    emit_S(0)
                    emit_S(1)
                    for t in range(len(items)):
                        emit_rest(t)
                    P.flush()

            if 'ret' in phases:
                with contextlib.ExitStack() as st:
                    def sb(name, shape, dt=F32):
                        return st.enter_context(nc.sbuf_tensor(uname(name), list(shape), dt))
                    RT = sb("RT", [128, NT, 768], BF16)
                    P.dma('sp', RT[:], RTOK.rearrange("(c p) w -> p c w", p=128), [], ['RT'], 'RT')
                    RGt = sb("RGt", [128, NT, 256])
                    P.dma('act', RGt[:], RG.rearrange("(c p) w -> p c w", p=128), [], ['RGt'], 'RGt')
                    gng = sb("gng", [128, 256])
                    bload('act', gng[:], gn_g[l], 'gng')
                    lg = sb("lg", [128, 8])
                    bload('act', lg[:], rde[l].rearrange("a h -> (a h)"), 'lg')
                    P.act(lambda e: e.activation(out=lg[:], in_=lg[:], func=AF.Exp, scale=-math.log(2.0)), ['lg'], ['lg'])
                    P.act(lambda e: e.activation(out=lg[:], in_=lg[:], func=AF.Ln, scale=-1.0, bias=1.0), ['lg'], ['lg'])
                    dec = sb("dec", [128, 8])
                    P.act(lambda e: e.activation(out=dec[:], in_=lg[:], func=AF.Exp, scale=128.0), ['lg'], ['dec'])
                    dpos = sb("dpos", [128, 128]); dneg = sb("dneg", [128, 128]); mge = sb("mge", [128, 128])
                    P.dma('sp', dpos[:], dpos_in, [], ['dpos'], 'dpos')
                    P.dma('sp', dneg[:], dneg_in, [], ['dneg'], 'dneg')
                    P.dma('sp', mge[:], mge_in, [], ['mge'], 'mge')
                    DcT = sb("DcT", [128, 4, 128])
                    e1 = sb("e1", [128, 128])
                    for hh in range(4):
                        P.act(lambda e, hh=hh: e.activation(out=e1[:], in_=dpos[:], func=AF.Exp, scale=lg[:, hh:hh + 1]), ['dpos', 'lg'], ['e1'])
                        P.act(lambda e, hh=hh: e.activation(out=DcT[:, hh, :], in_=dneg[:], func=AF.Exp, scale=lg[:, 4 + hh:5 + hh]), ['dneg', 'lg'], ['DcT%d' % hh])
                        P.dve(lambda e, hh=hh: e.tensor_tensor(out=e1[:], in0=e1[:], in1=DcT[:, hh, :], op=ALU.subtract), ['e1', 'DcT%d' % hh], ['e1'])
                        P.dve(lambda e, hh=hh: e.tensor_tensor(out=e1[:], in0=e1[:], in1=mge[:], op=ALU.mult), ['e1', 'mge'], ['e1'])
                        P.dve(lambda e, hh=hh: e.tensor_tensor(out=DcT[:, hh, :], in0=DcT[:, hh, :], in1=e1[:], op=ALU.add), ['e1', 'DcT%d' % hh], ['DcT%d' % hh])
                    q3ring = Ring('Q3', [sb("Q3_%d" % i, [64, 3, N], BF16) for i in range(2)])
                    ktring = Ring('KTh', [sb("KTh%d" % i, [64, N], BF16) for i in range(2)])
                    Sf = sb("Sf", [64, NT + 1, 64], BF16)
                    Sb = sb("Sb", [64, NT + 1, 64], BF16)
                    srun = Ring('srun', [sb("srun%d" % i, [64, 64]) for i in range(2)])
                    ptr = Ring('PTr', [sb("PTr%d" % i, [128, 128], BF16) for i in range(2)])
                    st6 = sb("st6", [128, 6]); mv = sb("mv", [128, 2]); rs = sb("rs", [128, 1])
                    yo = Ring('yo', [sb("yo%d" % i, [128, 64]) for i in range(2)])
                    kvb = Ring('kv', [0, 1]); scb = Ring('sc', [2, 3]); yb = Ring('y', [4, 5])
                    ytiles = list(range(NT)) if not last else list(range(NT_C, NT))
                    for hh in range(4):
                        Q3, q3k = q3ring.next()
                        KTh, ktk = ktring.next()
                        P.dma('sp', Q3[:], RQT[:, hh].rearrange("t d n -> d t n"), [], [q3k], q3k)
                        P.dma('act', KTh[:], RKT[hh], [], [ktk], ktk)
                        vcol = 512 + hh * 64

                        def scan(order_tiles, S, skey, kcol, dcol, run_init_zero):
                            return None
                        run, runk = srun.next()
                        P.pool(lambda e, run=run: e.memset(run[:], 0.0), [], [runk])
                        for i in range(NT):
                            P.pool(lambda e, run=run, i=i: e.tensor_copy(out=Sf[:, i, :], in_=run[:]), [runk], [('Sf', i)])
                            kb, kk = kvb.next()
                            P.pe(lambda e, kb=kb, i=i, hh=hh, vcol=vcol: e.matmul(psum[0:64, kb, 0:64], RT[:, i, hh * 64:(hh + 1) * 64],
                                                                                 RT[:, i, vcol:vcol + 64], start=True, stop=True), ['RT'], [kk])
                            nrun, nrunk = srun.next()
                            P.dve(lambda e, run=run, nrun=nrun, kb=kb, hh=hh: e.scalar_tensor_tensor(out=nrun[:], in0=run[:], scalar=dec[0:64, hh:hh + 1],
                                                                                                     in1=psum[0:64, kb, 0:64], op0=ALU.mult, op1=ALU.add),
                                  [runk, kk, 'dec'], [nrunk])
                            run, runk = nrun, nrunk
                        run, runk = srun.next()
                        P.pool(lambda e, run=run: e.memset(run[:], 0.0), [], [runk])
                        for i in [1, 0] + list(range(NT - 1, NT_C - 1, -1)):
                            P.pool(lambda e, run=run, i=i: e.tensor_copy(out=Sb[:, i, :], in_=run[:]), [runk], [('Sb', i)])
                            kb, kk = kvb.next()
                            P.pe(lambda e, kb=kb, i=i, hh=hh, vcol=vcol: e.matmul(psum[0:64, kb, 0:64], RT[:, i, 256 + hh * 64:256 + (hh + 1) * 64],
                                                                                 RT[:, i, vcol:vcol + 64], start=True, stop=True), ['RT'], [kk])
                            nrun, nrunk = srun.next()
                            P.dve(lambda e, run=run, nrun=nrun, kb=kb, hh=hh: e.scalar_tensor_tensor(out=nrun[:], in0=run[:], scalar=dec[0:64, 4 + hh:5 + hh],
                                                                                                     in1=psum[0:64, kb, 0:64], op0=ALU.mult, op1=ALU.add),
                                  [runk, kk, 'dec'], [nrunk])
                            run, runk = nrun, nrunk
                        r_base = len(P.ops)
                        r_marks = []
                        for i in ytiles:
                            m_a = len(P.ops)
                            sbk, sk = scb.next()
                            P.pe(lambda e, sbk=sbk, i=i, KTh=KTh, Q3=Q3: e.matmul(psum[:, sbk, 0:128], KTh[:, i * 128:(i + 1) * 128], Q3[:, 0, i * 128:(i + 1) * 128],
                                                                                start=True, stop=True), [ktk, q3k], [sk])
                            PTr, pk = ptr.next()
                            P.dve(lambda e, PTr=PTr, sbk=sbk, hh=hh: e.tensor_tensor(out=PTr[:], in0=psum[:, sbk, 0:128], in1=DcT[:, hh, :], op=ALU.mult),
                                  [sk, 'DcT%d' % hh], [pk])
                            ybk, yk = yb.next()
                            P.pe(lambda e, ybk=ybk, PTr=PTr, i=i, vcol=vcol: e.matmul(psum[:, ybk, 0:64], PTr[:], RT[:, i, vcol:vcol + 64], start=True, stop=False),
                                 [pk, 'RT'], [yk])
                            P.pe(lambda e, ybk=ybk, Q3=Q3, i=i: e.matmul(psum[:, ybk, 0:64], Q3[:, 1, i * 128:(i + 1) * 128], Sf[:, i, :], start=False, stop=False),
                                 [q3k, ('Sf', i)], [yk])
                            P.pe(lambda e, ybk=ybk, Q3=Q3, i=i: e.matmul(psum[:, ybk, 0:64], Q3[:, 2, i * 128:(i + 1) * 128], Sb[:, i, :], start=False, stop=True),
                                 [q3k, ('Sb', i)], [yk])
                            m_b = len(P.ops)
                            P.dve(lambda e, ybk=ybk: e.bn_stats(out=st6[:], in_=psum[:, ybk, 0:64]), [yk], ['st6'])
                            P.dve(lambda e: e.bn_aggr(out=mv[:], in_=st6[:]), ['st6'], ['mv'])
                            P.dve(lambda e: e.tensor_scalar_add(out=rs[:], in0=mv[:, 1:2], scalar1=EPS), ['mv'], ['rs'])
                            P.act(lambda e: e.activation(out=rs[:], in_=rs[:], func=AF.Sqrt), ['rs'], ['rs'])
                            P.dve(lambda e: e.reciprocal(out=rs[:], in_=rs[:]), ['rs'], ['rs'])
                            y_, yok = yo.next()
                            P.dve(lambda e, y_=y_, ybk=ybk: e.tensor_scalar(out=y_[:], in0=psum[:, ybk, 0:64], scalar1=mv[:, 0:1], scalar2=rs[:, 0:1],
                                                                           op0=ALU.subtract, op1=ALU.mult), [yk, 'mv', 'rs'], [yok])
                            P.pool(lambda e, y_=y_, hh=hh: e.tensor_tensor(out=y_[:], in0=y_[:], in1=gng[:, hh * 64:(hh + 1) * 64], op=ALU.mult), [yok, 'gng'], [yok])
                            P.pool(lambda e, y_=y_, hh=hh, i=i: e.tensor_tensor(out=y_[:], in0=y_[:], in1=RGt[:, i, hh * 64:(hh + 1) * 64], op=ALU.mult), [yok, 'RGt'], [yok])
                            P.dma('sp', MIX[i * 128:(i + 1) * 128, 256 + hh * 64:256 + (hh + 1) * 64], y_[:], [yok], [('MIXr', i, hh)], yok)
                            r_marks.append((m_a, m_b, len(P.ops)))
                        pipeline_reorder(P, r_base, r_marks)
                    P.flush()

            if 'p3' in phases:
                with contextlib.ExitStack() as st:
                    def sb(name, shape, dt=F32):
                        return st.enter_context(nc.sbuf_tensor(uname(name), list(shape), dt))
                    wout = sb("wout", [128, 8, D], BF16)
                    for hf in range(2):
                        P.dma('pool', wout[:, :, hf * 512:(hf + 1) * 512], w_out[l, :, hf * 512:(hf + 1) * 512].rearrange("(k p) n -> p k n", p=128),
                              [], ['wout%d' % hf], 'wout%d' % hf)
                    CWr = sb("CWr", [128, 256, 3])
                    P.dma('act', CWr[:].rearrange("p c k -> p (c k)"), conv_w[l].rearrange("c k -> (c k)").partition_broadcast(128), [], ['CWr'], 'CWr')
                    CW = sb("CW", [128, 3, 256])
                    for k in range(3):
                        P.dve(lambda e, k=k: e.tensor_copy(out=CW[:, k, :], in_=CWr[:, :, k]), ['CWr'], ['CW%d' % k])
                    g1 = [sb("g1_%d" % s, [128, D]) for s in range(2)]
                    sc2 = [sb("sc2_%d" % s, [128, D]) for s in range(2)]
                    sh2 = [sb("sh2_%d" % s, [128, D]) for s in range(2)]
                    for s in range(2):
                        if last and s == 1:
                            continue
                        bload('sp', g1[s][:], modv[l, s, 2 * D:3 * D], 'g1_%d' % s)
                        bload('sp', sh2[s][:], modv[l, s, 3 * D:4 * D], 'sh2_%d' % s)
                        bload('sp', sc2[s][:], modv[l, s, 4 * D:5 * D], 'sc2_%d' % s)
                    lng = sb("lng", [128, D]); lnb = sb("lnb", [128, D])
                    bload('act', lng[:], ln_g[l, 0], 'lng')
                    bload('act', lnb[:], ln_b[l, 0], 'lnb')
                    wr = sb("wr", [128, 8, NE])
                    P.dma('act', wr[:], w_router[l].rearrange("(k p) e -> p k e", p=128), [], ['wr'], 'wr')
                    brt = sb("brt", [128, NE])
                    bload('act', brt[:], b_router[l], 'brt')
                    pmr = Ring('pm', [sb("pm%d" % i, [128, 3, 256]) for i in range(3)])
                    btr = Ring('bt', [sb("bt%d" % i, [128, 256]) for i in range(3)])
                    mixr = Ring('mix', [sb("mix%d" % i, [128, D]) for i in range(3)])
                    xr = Ring('x3', [sb("x3_%d" % i, [128, D]) for i in range(3)])
                    ca = sb("ca", [128, 256]); cb = sb("cb", [128, 256])
                    mTr = Ring('mT', [sb("mT%d" % i, [128, 8, 128], BF16) for i in range(2)])
                    rr = sb("rr", [128, D])
                    x1r = Ring('x1', [sb("x1_%d" % i, [128, D]) for i in range(2)])
                    h2 = sb("h2", [128, D])
                    h2br = Ring('h2b', [sb("h2b%d" % i, [128, D], BF16) for i in range(2)])
                    ix8 = sb("ix8", [128, 8], U32)
                    h2f = sb("h2f", [128, 8, 128])
                    st6 = sb("st6", [128, 2, 6]); mv = sb("mv", [128, 2]); rs = sb("rs", [128, 1])
                    lgt = sb("lgt", [128, NE]); mx8 = sb("mx8", [128, 8]); msk = sb("msk", [128, NE]); nmx = sb("nmx", [128, 1])
                    ex = sb("ex", [128, NE]); sm = sb("sm", [128, 1])
                    mwr = Ring('mw', [sb("mw%d" % i, [128, NE]) for i in range(2)])
                    ld3 = {}

                    def issue_loads3(i):
                        r0 = prow(i)
                        pm, pmk = pmr.next()
                        for k in range(3):
                            P.dma('sp', pm[:, k, :], PB[r0 - 1 + k:r0 + 127 + k, 0:256], [], [pmk + str(k)], pmk + str(k))
                        bt, btk = btr.next()
                        P.dma('sp', bt[:], PB[r0:r0 + 128, 256:512], [], [btk], btk)
                        mix, mixk = mixr.next()
                        P.dma('sp', mix[:, 256:D], MIX[i * 128:(i + 1) * 128, 256:D], [], [mixk + 'l'], mixk)
                        xt, xk = xr.next()
                        P.dma('sp', xt[:], xsrc(i), [], [xk], xk)
                        ld3[i] = (pm, pmk, bt, btk, mix, mixk, xt, xk)
                    issue_loads3(out_tiles[0])
                    p3_base = len(P.ops)
                    p3_marks = []
                    for oi, i in enumerate(out_tiles):
                        s = 1 if i < NT_C else 0
                        m_a = len(P.ops)
                        if oi + 1 < len(out_tiles):
                            issue_loads3(out_tiles[oi + 1])
                        pm, pmk, bt, btk, mix, mixk, xt, xk = ld3.pop(i)
                        P.dve(lambda e, pm=pm: e.tensor_tensor(out=ca[:], in0=pm[:, 0, :], in1=CW[:, 0, :], op=ALU.mult), [pmk + '0', 'CW0'], ['ca'])
                        P.pool(lambda e, pm=pm: e.tensor_tensor(out=cb[:], in0=pm[:, 1, :], in1=CW[:, 1, :], op=ALU.mult), [pmk + '1', 'CW1'], ['cb'])
                        P.dve(lambda e: e.tensor_tensor(out=ca[:], in0=ca[:], in1=cb[:], op=ALU.add), ['ca', 'cb'], ['ca'])
                        P.pool(lambda e, pm=pm: e.tensor_tensor(out=cb[:], in0=pm[:, 2, :], in1=CW[:, 2, :], op=ALU.mult), [pmk + '2', 'CW2', 'ca'], ['cb'])
                        P.dve(lambda e: e.tensor_tensor(out=ca[:], in0=ca[:], in1=cb[:], op=ALU.add), ['ca', 'cb'], ['ca'])
                        P.dve(lambda e, mix=mix, bt=bt: e.tensor_tensor(out=mix[:, 0:256], in0=ca[:], in1=bt[:], op=ALU.mult), ['ca', btk], [mixk + 'c'])
                        for k in range(8):
                            P.pe(lambda e, mix=mix, k=k: e.transpose(psum[:, k // 4, (k % 4) * 128:(k % 4 + 1) * 128], mix[:, k * 128:(k + 1) * 128], ident[:]),
                                 [mixk + 'l', mixk + 'c', 'ident'], ['pT%d' % k])
                        mT, mTk = mTr.next()
                        for hf in range(2):
                            P.act(lambda e, mT=mT, hf=hf: e.activation(out=mT[:, hf * 4:(hf + 1) * 4, :].rearrange("p k n -> p (k n)"), in_=psum[:, hf, :], func=AF.Copy),
                                  ['pT%d' % k for k in range(hf * 4, hf * 4 + 4)], [mTk + str(hf)])
                        pyb = 2 + 2 * (oi % 2)
                        for nh in range(2):
                            for k in range(8):
                                P.pe(lambda e, mT=mT, nh=nh, k=k, pyb=pyb: e.matmul(psum[:, pyb + nh, :], mT[:, k, :], wout[:, k, nh * 512:(nh + 1) * 512], start=(k == 0), stop=(k == 7)),
                                     [mTk + str(k // 4), 'wout%d' % nh], ['py%d' % (pyb + nh)])
                        m_b = len(P.ops)
                        for nh in range(2):
                            P.dve(lambda e, nh=nh, s=s, pyb=pyb: e.tensor_tensor(out=rr[:, nh * 512:(nh + 1) * 512], in0=psum[:, pyb + nh, :], in1=g1[s][:, nh * 512:(nh + 1) * 512], op=ALU.mult),
                                  ['py%d' % (pyb + nh), 'g1_%d' % s], ['rr%d' % nh])
                        P.dve(lambda e, xt=xt: e.scalar_tensor_tensor(out=rr[:], in0=xt[:], scalar=ALPHA, in1=rr[:], op0=ALU.mult, op1=ALU.add), [xk, 'rr0', 'rr1'], ['rr'])
                        for nh in range(2):
                            P.dve(lambda e, nh=nh: e.bn_stats(out=st6[:, nh, :], in_=rr[:, nh * 512:(nh + 1) * 512]), ['rr'], ['st6_%d' % nh])
                        P.dve(lambda e: e.bn_aggr(out=mv[:], in_=st6[:]), ['st6_0', 'st6_1'], ['mv'])
                        P.dve(lambda e: e.tensor_scalar_add(out=rs[:], in0=mv[:, 1:2], scalar1=EPS), ['mv'], ['rs'])
                        P.act(lambda e: e.activation(out=rs[:], in_=rs[:], func=AF.Sqrt), ['rs'], ['rs'])
                        P.dve(lambda e: e.reciprocal(out=rs[:], in_=rs[:]), ['rs'], ['rs'])
                        x1, x1k = x1r.next()
                        P.dve(lambda e, x1=x1: e.tensor_scalar(out=x1[:], in0=rr[:], scalar1=mv[:, 0:1], scalar2=rs[:, 0:1], op0=ALU.subtract, op1=ALU.mult), ['rr', 'mv', 'rs'], [x1k])
                        P.dve(lambda e, x1=x1: e.tensor_tensor(out=x1[:], in0=x1[:], in1=lng[:], op=ALU.mult), [x1k, 'lng'], [x1k])
                        P.dve(lambda e, x1=x1: e.tensor_tensor(out=x1[:], in0=x1[:], in1=lnb[:], op=ALU.add), [x1k, 'lnb'], [x1k])
                        P.dma('sp', XM[i * 128:(i + 1) * 128, :], x1[:], [x1k], [('XM', i)], x1k)
                        P.dve(lambda e, x1=x1, s=s: e.tensor_tensor(out=h2[:], in0=x1[:], in1=sc2[s][:], op=ALU.mult), [x1k, 'sc2_%d' % s], ['h2'])
                        P.dve(lambda e, s=s: e.tensor_tensor(out=h2[:], in0=h2[:], in1=sh2[s][:], op=ALU.add), ['h2', 'sh2_%d' % s], ['h2'])
                        for k in range(8):
                            P.pe(lambda e, k=k: e.transpose(psum[:, k // 4, (k % 4) * 128:(k % 4 + 1) * 128], h2[:, k * 128:(k + 1) * 128], ident[:]),
                                 ['h2', 'ident'], ['pT%d' % k])
                        for hf in range(2):
                            P.dve(lambda e, hf=hf: e.tensor_copy(out=h2f[:, hf * 4:(hf + 1) * 4, :].rearrange("p k n -> p (k n)"), in_=psum[:, hf, :]),
                                  ['pT%d' % k for k in range(hf * 4, hf * 4 + 4)], ['h2f%d' % hf])
                        h2b, h2bk = h2br.next()
                        P.act(lambda e, h2b=h2b: e.activation(out=h2b[:], in_=h2[:], func=AF.Copy), ['h2'], [h2bk])
                        P.dma('sp', H2R[i * 128:(i + 1) * 128, :], h2b[:], [h2bk], [('H2R', i)], h2bk)
                        for k in range(8):
                            P.pe(lambda e, k=k: e.matmul(psum[:, 6, 0:NE], h2f[:, k, :], wr[:, k, :], start=(k == 0), stop=(k == 7)), ['h2f%d' % (k // 4), 'wr'], ['pl'])
                        P.dve(lambda e: e.tensor_tensor(out=lgt[:], in0=psum[:, 6, 0:NE], in1=brt[:], op=ALU.add), ['pl', 'brt'], ['lgt'])
                        P.dve(lambda e: e.max(out=mx8[:], in_=lgt[:]), ['lgt'], ['mx8'])
                        P.dve(lambda e: e.max_index(out=ix8[:], in_max=mx8[:], in_values=lgt[:]), ['lgt', 'mx8'], ['ix8'])
                        P.dve(lambda e: e.tensor_scalar_mul(out=nmx[:], in0=mx8[:, 0:1], scalar1=-1.0), ['mx8'], ['nmx'])
                        P.act(lambda e: e.activation(out=ex[:, 0:4], in_=mx8[:, 0:4], func=AF.Exp, bias=nmx[:, 0:1], scale=1.0), ['mx8', 'nmx'], ['ex'])
                        P.dve(lambda e: e.reduce_sum(out=sm[:], in_=ex[:, 0:4], axis=AX.X), ['ex'], ['sm'])
                        P.dve(lambda e: e.reciprocal(out=sm[:], in_=sm[:]), ['sm'], ['sm'])
                        mw_, mwk = mwr.next()
                        P.dve(lambda e, mw_=mw_: e.tensor_scalar_mul(out=mw_[:, 0:4], in0=ex[:, 0:4], scalar1=sm[:, 0:1]), ['ex', 'sm'], [mwk + 'w'])
                        P.dve(lambda e, mw_=mw_: e.tensor_copy(out=mw_[:, 4:8], in_=ix8[:, 0:4]), ['ix8'], [mwk + 'e'])
                        P.dma('sp', RW[i * 128:(i + 1) * 128, :], mw_[:, 0:4], [mwk + 'w'], [('RW', i)], mwk + 'w')
                        P.dma('sp', RE[i * 128:(i + 1) * 128, :], mw_[:, 4:8], [mwk + 'e'], [('RE', i)], mwk + 'e')
                        p3_marks.append((m_a, m_b, len(P.ops)))
                    pipeline_reorder(P, p3_base, p3_marks)
                    P.flush()

            if 'moe' in phases:
                tiles = out_tiles
                nt = len(tiles)
                T = nt * 128
                t0 = tiles[0] * 128
                NB = (4 * T + NE * (BS - 1) + BS - 1) // BS
                assert NB <= NBMAX
                w_gu_rows = w_gu.rearrange("l e r c -> (l e r) c")
                w_dn_rows = w_dn.rearrange("l e r c -> (l e r) c")
                with contextlib.ExitStack() as st:
                    def sb(name, shape, dt=F32):
                        return st.enter_context(nc.sbuf_tensor(uname(name), list(shape), dt))
                    DKi = sb("DKi", [128, nt, 4], I32)
                    IDXG = sb("IDXG", [128, NB, 8], I32)
                    EB = sb("EB", [128, NB])
                    IDXB = sb("IDXB", [128, NB], I32)
                    mwd = sb("mwd", [128, nt, NE])
                    iotap = sb("iotap", [128, 1])
                    P.dma('sp', iotap[:], iotap_in, [], ['iotap'], 'iotap')
                    with contextlib.ExitStack() as st2:
                        def sb2(name, shape, dt=F32):
                            return st2.enter_context(nc.sbuf_tensor(uname(name), list(shape), dt))
                        EF = sb2("EF", [128, nt, 4])
                        W4 = sb2("W4", [128, nt, 4])
                        P.dma('sp', EF[:], RE[t0:t0 + T, :].rearrange("(c p) k -> p c k", p=128), [], ['EF'], 'EF')
                        P.dma('sp', W4[:], RW[t0:t0 + T, :].rearrange("(c p) k -> p c k", p=128), [], ['W4'], 'W4')
                        iota32 = sb2("iota32", [128, NE])
                        P.dma('act', iota32[:], iota32_in, [], ['iota32'], 'iota32')
                        lts = sb2("lts", [128, 128])
                        P.dma('act', lts[:], lts_in, [], ['lts'], 'lts')
                        ones = sb2("ones", [128, 128])
                        P.pool(lambda e: e.memset(ones[:], 1.0), [], ['ones'])
                        bst = sb2("bst", [128, NBMAX])
                        P.dma('act', bst[:], bstart_in, [], ['bst'], 'bst')
                        kp = sb2("kp", [128, 8])
                        P.dma('act', kp[:], kp_in, [], ['kp'], 'kp')
                        OH = sb2("OH", [128, 4, nt, NE])
                        for k in range(4):
                            P.dve(lambda e, k=k: e.tensor_tensor(out=OH[:, k, :, :], in0=iota32[:].unsqueeze(1).to_broadcast([128, nt, NE]),
                                                                 in1=EF[:, :, k].unsqueeze(2).to_broadcast([128, nt, NE]), op=ALU.is_equal),
                                  ['iota32', 'EF'], ['OH%d' % k])
                        mask = sb2("mask", [128, nt, NE])
                        P.dve(lambda e: e.tensor_tensor(out=mask[:], in0=OH[:, 0, :, :], in1=OH[:, 1, :, :], op=ALU.add), ['OH0', 'OH1'], ['mask'])
                        P.dve(lambda e: e.tensor_tensor(out=mask[:], in0=mask[:], in1=OH[:, 2, :, :], op=ALU.add), ['mask', 'OH2'], ['mask'])
                        P.dve(lambda e: e.tensor_tensor(out=mask[:], in0=mask[:], in1=OH[:, 3, :, :], op=ALU.add), ['mask', 'OH3'], ['mask'])
                        for j in range(nt):
                            bk = j // 16
                            col = (j % 16) * NE
                            P.pe(lambda e, j=j, bk=bk, col=col: e.matmul(psum[:, bk, col:col + NE], lts[:], mask[:, j, :], start=True, stop=True, skip_group_check=True),
                                 ['lts', 'mask'], ['rk%d' % bk])
                            P.pe(lambda e, j=j, bk=bk, col=col: e.matmul(psum[:, 4 + bk, col:col + NE], ones[:], mask[:, j, :], start=True, stop=True, skip_group_check=True),
                                 ['ones', 'mask'], ['tt%d' % bk])
                        for m in range(nt):
                            P.pe(lambda e, m=m: e.matmul(psum[:, 3, 0:NE], ones[:], mask[:, m, :], start=(m == 0), stop=(m == nt - 1)), ['ones', 'mask'], ['cnt'])
                        TOT = sb2("TOT", [128, nt, NE])
                        PRE = sb2("PRE", [128, nt, NE])
                        for bk in range((nt + 15) // 16):
                            j0 = bk * 16
                            nj = min(16, nt - j0)
                            P.act(lambda e, bk=bk, j0=j0, nj=nj: e.activation(out=TOT[:, j0:j0 + nj, :].rearrange("p j e -> p (j e)"), in_=psum[:, 4 + bk, 0:nj * NE], func=AF.Copy),
                                  ['tt%d' % bk], ['TOT%d' % bk])
                        P.pool(lambda e: e.memset(PRE[:, 0, :], 0.0), [], ['PRE'])
                        for j in range(1, nt):
                            P.dve(lambda e, j=j: e.tensor_tensor(out=PRE[:, j, :], in0=PRE[:, j - 1, :], in1=TOT[:, j - 1, :], op=ALU.add),
                                  ['PRE'] + ['TOT%d' % bk for bk in range((nt + 15) // 16)], ['PRE'])
                        c0 = sb2("c0", [128, NE]); c1 = sb2("c1", [128, NE]); padded = sb2("padded", [128, NE])
                        P.dve(lambda e: e.tensor_scalar_add(out=c0[:], in0=psum[:, 3, 0:NE], scalar1=float(BS - 1)), ['cnt'], ['c0'])
                        ci32 = sb2("ci32", [128, NE], I32)
                        P.dve(lambda e: e.tensor_scalar(out=c1[:], in0=c0[:], scalar1=1.0 / BS, scalar2=-0.5 + 0.5 / BS, op0=ALU.mult, op1=ALU.add), ['c0'], ['c1'])
                        P.dve(lambda e: e.tensor_copy(out=ci32[:], in_=c1[:]), ['c1'], ['ci32'])
                        P.dve(lambda e: e.tensor_copy(out=c1[:], in_=ci32[:]), ['ci32'], ['c1'])
                        P.dve(lambda e: e.tensor_scalar_mul(out=padded[:], in0=c1[:], scalar1=float(BS)), ['c1'], ['padded'])
                        P.dve(lambda e: e.tensor_copy(out=c0[:], in_=padded[:]), ['padded'], ['c0'])
                        cur, nxt_, ck, nk = c0, c1, 'c0', 'c1'
                        for sft in (1, 2, 4, 8, 16):
                            P.dve(lambda e, cur=cur, nxt_=nxt_, sft=sft: e.tensor_copy(out=nxt_[:, 0:sft], in_=cur[:, 0:sft]), [ck], [nk + 'a'])
                            P.dve(lambda e, cur=cur, nxt_=nxt_, sft=sft: e.tensor_tensor(out=nxt_[:, sft:NE], in0=cur[:, sft:NE], in1=cur[:, 0:NE - sft], op=ALU.add), [ck], [nk + 'b'])
                            P.dve(lambda e: e.engine_nop(), [nk + 'a', nk + 'b'], [nk])
                            cur, nxt_, ck, nk = nxt_, cur, nk, ck
                        pend, pendk = cur, ck
                        pstart = sb2("pstart", [128, NE])
                        P.dve(lambda e: e.tensor_tensor(out=pstart[:], in0=pend[:], in1=padded[:], op=ALU.subtract), [pendk, 'padded'], ['pstart'])
                        dfull = sb2("dfull", [128, nt, NE])
                        for bk in range((nt + 15) // 16):
                            j0 = bk * 16
                            nj = min(16, nt - j0)
                            P.dve(lambda e, bk=bk, j0=j0, nj=nj: e.tensor_tensor(out=dfull[:, j0:j0 + nj, :], in0=psum[:, bk, 0:nj * NE].rearrange("p (j e) -> p j e", e=NE),
                                                                                 in1=pstart[:].unsqueeze(1).to_broadcast([128, nj, NE]), op=ALU.add),
                                  ['rk%d' % bk, 'pstart'], ['dfull%d' % bk])
                            P.dve(lambda e, j0=j0, nj=nj: e.tensor_tensor(out=dfull[:, j0:j0 + nj, :], in0=dfull[:, j0:j0 + nj, :], in1=PRE[:, j0:j0 + nj, :], op=ALU.add),
                                  ['dfull%d' % bk, 'PRE'], ['dfull%d' % bk])
                        dkeys = ['dfull%d' % bk for bk in range((nt + 15) // 16)]
                        tmpd = sb2("tmpd", [128, nt, NE])
                        DKf = sb2("DKf", [128, nt, 4])
                        for k in range(4):
                            P.dve(lambda e, k=k: e.tensor_tensor(out=tmpd[:], in0=dfull[:], in1=OH[:, k, :, :], op=ALU.mult), dkeys + ['OH%d' % k], ['tmpd'])
                            P.dve(lambda e, k=k: e.tensor_reduce(out=DKf[:, :, k], in_=tmpd[:], axis=AX.X, op=ALU.add), ['tmpd'], ['DKf%d' % k])
                        P.dve(lambda e: e.tensor_copy(out=DKi[:], in_=DKf[:]), ['DKf%d' % k for k in range(4)], ['DKi'])
                        cmp_ = sb2("cmp", [128, NB, NE])
                        P.dve(lambda e: e.tensor_tensor(out=cmp_[:], in0=pend[:].unsqueeze(1).to_broadcast([128, NB, NE]),
                                                        in1=bst[:, 0:NB].unsqueeze(2).to_broadcast([128, NB, NE]), op=ALU.is_le), [pendk, 'bst'], ['cmp'])
                        P.dve(lambda e: e.tensor_reduce(out=EB[:], in_=cmp_[:], axis=AX.X, op=ALU.add), ['cmp'], ['EB'])
                        P.dve(lambda e: e.tensor_scalar_min(out=EB[:], in0=EB[:], scalar1=float(NE - 1)), ['EB'], ['EB'])
                        idxf = sb2("idxf", [128, NB, 8])
                        P.dve(lambda e: e.scalar_tensor_tensor(out=idxf[:], in0=EB[:].unsqueeze(2).to_broadcast([128, NB, 8]), scalar=float(D),
                                                               in1=kp[:].unsqueeze(1).to_broadcast([128, NB, 8]), op0=ALU.mult, op1=ALU.add), ['EB', 'kp'], ['idxf'])
                        P.dve(lambda e: e.tensor_scalar_add(out=idxf[:], in0=idxf[:], scalar1=float(l * NE * D)), ['idxf'], ['idxf'])
                        P.dve(lambda e: e.tensor_copy(out=IDXG[:], in_=idxf[:]), ['idxf'], ['IDXG'])
                        idxbf = sb2("idxbf", [128, NB])
                        P.dve(lambda e: e.tensor_scalar(out=idxbf[:], in0=EB[:], scalar1=128.0, scalar2=float(l * NE * 128), op0=ALU.mult, op1=ALU.add), ['EB'], ['idxbf'])
                        P.dve(lambda e: e.tensor_scalar(out=idxbf[:], in0=idxbf[:], scalar1=iotap[:, 0:1], scalar2=None, op0=ALU.add), ['idxbf', 'iotap'], ['idxbf'])
                        P.dve(lambda e: e.tensor_copy(out=IDXB[:], in_=idxbf[:]), ['idxbf'], ['IDXB'])
                        for k in range(4):
                            dst_ = mwd if k == 0 else tmpd
                            P.dve(lambda e, k=k, dst_=dst_: e.tensor_tensor(out=dst_[:], in0=OH[:, k, :, :], in1=W4[:, :, k].unsqueeze(2).to_broadcast([128, nt, NE]), op=ALU.mult),
                                  ['OH%d' % k, 'W4', 'DKf0', 'DKf1', 'DKf2', 'DKf3'], ['mwd' if k == 0 else 'tmpd'])
                            if k > 0:
                                P.dve(lambda e: e.tensor_tensor(out=mwd[:], in0=mwd[:], in1=tmpd[:], op=ALU.add), ['mwd', 'tmpd'], ['mwd'])
                        hr = Ring('hr', [sb2("hr%d" % i, [128, D], BF16) for i in range(3)])
                        for j, i in enumerate(tiles):
                            h_, hk_ = hr.next()
                            P.dma('sp', h_[:], H2R[i * 128:(i + 1) * 128, :], [], [hk_], hk_)
                            for k in range(4):
                                P.add('pool', lambda e, h_=h_, j=j, k=k: e.indirect_dma_start(out=XS[:, :], out_offset=bass.IndirectOffsetOnAxis(ap=DKi[:, j, k:k + 1], axis=0),
                                                                                          in_=h_[:, :], in_offset=None), [hk_, 'DKi'], [('XS', j, k)], 'xsc%d' % ((j * 4 + k) % 4))
                                P.add('pool', lambda e, j=j, k=k: e.indirect_dma_start(out=SW[:, :], out_offset=bass.IndirectOffsetOnAxis(ap=DKi[:, j, k:k + 1], axis=0),
                                                                                    in_=W4[:, j, k:k + 1], in_offset=None), ['W4', 'DKi'], [('SW', j, k)], 'swc%d' % ((j * 4 + k) % 4))
                        P.flush()
                    with contextlib.ExitStack() as st2:
                        def sb2(name, shape, dt=F32):
                            return st2.enter_context(nc.sbuf_tensor(uname(name), list(shape), dt))
                        identb = sb2("identb", [128, 128], BF16)
                        P.act(lambda e: e.activation(out=identb[:], in_=ident[:], func=AF.Copy), ['ident'], ['identb'])
                        bgr = sb2("bgr", [NE, 2 * D])
                        P.dma('act', bgr[:], b_gu[l], [], ['bgr'], 'bgr')
                        for c in range(16):
                            P.pe(lambda e, c=c: e.transpose(psum[:, 6, c * NE:(c + 1) * NE], bgr[:, c * 128:(c + 1) * 128], ident[0:NE, 0:NE]), ['bgr', 'ident'], ['pX0'])
                        X2 = sb2("X2", [128, NE, 16])
                        P.dve(lambda e: e.tensor_copy(out=X2[:], in_=psum[:, 6, :].rearrange("p (c e) -> p e c", e=NE)), ['pX0'], ['X2'])
                        P.dve(lambda e: e.tensor_scalar_add(out=X2[:, :, 8:16], in0=X2[:, :, 8:16], scalar1=1.0), ['X2'], ['X2'])
                        P.dma('sp', BGT[l * NE * 128:(l + 1) * NE * 128, :].rearrange("(e p) c -> p e c", p=128), X2[:], ['X2'], ['BGT'], 'BGT')
                        bbr = Ring('bb', [sb2("bb%d" % i, [128, 16]) for i in range(2)])
                        wgr = Ring('WG', [sb2("WG%d" % i, [128, 8, 2 * D], BF16) for i in range(2)])
                        wdr = Ring('WD', [sb2("WD%d" % i, [128, 8, D], BF16) for i in range(2)])
                        xbr = Ring('XB', [sb2("XB%d" % i, [128, 4, D], BF16) for i in range(2)])
                        XTr = [sb2("XT%d" % i, [128, 8, BS], BF16) for i in range(2)]
                        actr = Ring('act', [sb2("act%d" % i, [128, 8, BS], BF16) for i in range(2)])
                        Ar = Ring('A', [sb2("A%d" % i, [128, BS]) for i in range(2)])
                        Sr = Ring('Sg', [sb2("Sg%d" % i, [128, BS]) for i in range(2)])
                        Ur = Ring('U', [sb2("U%d" % i, [128, BS]) for i in range(2)])
                        swr = Ring('swb', [sb2("swb%d" % i, [128, 4]) for i in range(2)])
                        Yr = Ring('Y', [sb2("Y%d" % i, [128, 4, D]) for i in range(1)])
                        gbr = Ring('pg', [0, 1]); ubr = Ring('pu', [2, 3]); ybr = Ring('py', [4, 5])
                        psb = [psum[:, 6, :].bitcast(BF16), psum[:, 7, :].bitcast(BF16)]

                        def load_blk(b):
                            WG, wgk = wgr.next()
                            WD, wdk = wdr.next()
                            for k in range(8):
                                P.add('pool', lambda e, WG=WG, b=b, k=k: e.indirect_dma_start(out=WG[:, k, :], out_offset=None, in_=w_gu_rows[:, :],
                                                                                             in_offset=bass.IndirectOffsetOnAxis(ap=IDXG[:, b, k:k + 1], axis=0)),
                                      ['IDXG'], [wgk + str(k)], wgk + str(k))
                            for k in range(8):
                                P.add('pool', lambda e, WD=WD, b=b, k=k: e.indirect_dma_start(out=WD[:, k, :], out_offset=None, in_=w_dn_rows[:, :],
                                                                                             in_offset=bass.IndirectOffsetOnAxis(ap=IDXG[:, b, k:k + 1], axis=0)),
                                      ['IDXG'], [wdk + str(k)], wdk + str(k))
                            XB, xbk = xbr.next()
                            P.dma('sp', XB[:], XS[b * BS:(b + 1) * BS, :].rearrange("(s p) d -> p s d", p=128), [], [xbk], xbk)
                            swb, swk = swr.next()
                            P.dma('act', swb[:], SW[b * BS:(b + 1) * BS, :].rearrange("(s p) o -> p (s o)", p=128), [], [swk], swk, allow_slow_non_contiguous=True)
                            bb, bbk = bbr.next()
                            P.add('pool', lambda e, bb=bb, b=b: e.indirect_dma_start(out=bb[:, :], out_offset=None, in_=BGT[:, :],
                                                                                   in_offset=bass.IndirectOffsetOnAxis(ap=IDXB[:, b:b + 1], axis=0)),
                                  ['IDXB', 'BGT'], [bbk], bbk)
                            return WG, wgk, WD, wdk, XB, xbk, swb, swk, bb, bbk
                        def transposes(b, XB, xbk):
                            XT = XTr[b % 2]
                            for half in range(2):
                                for s2 in range(2):
                                    sidx = half * 2 + s2
                                    for k in range(8):
                                        P.pe(lambda e, XB=XB, sidx=sidx, k=k, s2=s2: e.transpose(psb[s2][:, k * 128:(k + 1) * 128], XB[:, sidx, k * 128:(k + 1) * 128], identb[:]),
                                             [xbk, 'identb'], ['pX%d' % s2])
                                    P.act(lambda e, sidx=sidx, s2=s2, XT=XT: e.activation(out=XT[:, :, sidx * 128:(sidx + 1) * 128], in_=psb[s2].rearrange("p (k n) -> p k n", n=128), func=AF.Copy),
                                          ['pX%d' % s2], ['XT%d_%d' % (b % 2, sidx)])
                        nxt = load_blk(0)
                        transposes(0, nxt[4], nxt[5])
                        for b in range(NB):
                            WG, wgk, WD, wdk, XB, xbk, swb, swk, bb, bbk = nxt
                            if b + 1 < NB:
                                nxt = load_blk(b + 1)
                            wgkeys = [wgk + str(k) for k in range(8)]
                            wdkeys = [wdk + str(k) for k in range(8)]
                            XT = XTr[b % 2]
                            xtkeys = ['XT%d_%d' % (b % 2, q) for q in range(4)]
                            at, atk = actr.next()
                            for c in range(8):
                                gb, gbk = gbr.next()
                                ub, ubk = ubr.next()
                                for k in range(8):
                                    P.pe(lambda e, gb=gb, WG=WG, k=k, c=c, XT=XT: e.matmul(psum[:, gb, :], WG[:, k, c * 128:(c + 1) * 128], XT[:, k, :], start=(k == 0), stop=(k == 7)),
                                         wgkeys + xtkeys, [gbk])
                                for k in range(8):
                                    P.pe(lambda e, ub=ub, WG=WG, k=k, c=c, XT=XT: e.matmul(psum[:, ub, :], WG[:, k, D + c * 128:D + (c + 1) * 128], XT[:, k, :], start=(k == 0), stop=(k == 7)),
                                         wgkeys + xtkeys, [ubk])
                                A, Ak = Ar.next(); S_, Sk = Sr.next(); U, Uk = Ur.next()
                                P.dve(lambda e, A=A, gb=gb, bb=bb, c=c: e.tensor_scalar(out=A[:], in0=psum[:, gb, :], scalar1=bb[:, c:c + 1], scalar2=7.0, op0=ALU.add, op1=ALU.min), [gbk, bbk], [Ak])
                                P.act(lambda e, A=A, S_=S_: e.activation(out=S_[:], in_=A[:], func=AF.Sigmoid, scale=1.702), [Ak], [Sk])
                                P.dve(lambda e, U=U, ub=ub, bb=bb, c=c: e.tensor_scalar(out=U[:], in0=psum[:, ub, :], scalar1=bb[:, 8 + c:9 + c], scalar2=8.0, op0=ALU.add, op1=ALU.min), [ubk, bbk], [Uk])
                                P.pool(lambda e, A=A, S_=S_: e.tensor_tensor(out=A[:], in0=A[:], in1=S_[:], op=ALU.mult), [Ak, Sk], [Ak])
                                P.dve(lambda e, at=at, c=c, U=U, A=A: e.scalar_tensor_tensor(out=at[:, c, :], in0=U[:], scalar=-6.0, in1=A[:], op0=ALU.max, op1=ALU.mult), [Uk, Ak], [atk + str(c)])
                            atkeys = [atk + str(c) for c in range(8)]
                            if b + 1 < NB:
                                transposes(b + 1, nxt[4], nxt[5])
                            Y, Yk = Yr.next()
                            for s4 in range(4):
                                for nh in range(2):
                                    yb_, ybk = ybr.next()
                                    for c in range(8):
                                        P.pe(lambda e, yb_=yb_, at=at, c=c, s4=s4, WD=WD, nh=nh: e.matmul(psum[:, yb_, :], at[:, c, s4 * 128:(s4 + 1) * 128], WD[:, c, nh * 512:(nh + 1) * 512],
                                                                                                          start=(c == 0), stop=(c == 7)), atkeys + wdkeys, [ybk])
                                    if yb_ == 4:
                                        P.dve(lambda e, Y=Y, s4=s4, nh=nh, swb=swb: e.tensor_scalar_mul(out=Y[:, s4, nh * 512:(nh + 1) * 512], in0=psum[:, 4, :], scalar1=swb[:, s4:s4 + 1]),
                                              [ybk, swk], [Yk + '%d%d' % (s4, nh)])
                                    else:
                                        P.act(lambda e, Y=Y, s4=s4, nh=nh, swb=swb: e.activation(out=Y[:, s4, nh * 512:(nh + 1) * 512], in_=psum[:, 5, :], func=AF.Copy, scale=swb[:, s4:s4 + 1]),
                                              [ybk, swk], [Yk + '%d%d' % (s4, nh)])
                            P.dma('sp', YS[b * BS:(b + 1) * BS, :].rearrange("(s p) d -> p s d", p=128), Y[:], [Yk + '%d%d' % (q, r_) for q in range(4) for r_ in range(2)], [('YS', b)], Yk)
                        P.flush()
                    with contextlib.ExitStack() as st3:
                        def sb3(name, shape, dt=F32):
                            return st3.enter_context(nc.sbuf_tensor(uname(name), list(shape), dt))
                        g2 = [sb3("g2_%d" % s_, [128, D]) for s_ in range(2)]
                        for s_ in range(2):
                            bload('sp', g2[s_][:], modv[l, s_, 5 * D:6 * D], 'g2_%d' % s_)
                        lng = sb3("lng2", [128, D]); lnb = sb3("lnb2", [128, D])
                        bload('act', lng[:], ln_g[l, 1], 'lng')
                        bload('act', lnb[:], ln_b[l, 1], 'lnb')
                        x1r = Ring('xm', [sb3("xm%d" % i, [128, D]) for i in range(4)])
                        Gr = Ring('G', [sb3("G%d" % i, [128, 4, D]) for i in range(4)])
                        rr = sb3("rr2", [128, D])
                        xor_ = Ring('xo', [sb3("xo%d" % i, [128, D]) for i in range(2)])
                        st6 = sb3("st6b", [128, 2, 6]); mv = sb3("mvb", [128, 2]); rs = sb3("rsb", [128, 1])
                        bdr = sb3("bdr", [NE, D])
                        P.dma('act', bdr[:], b_dn[l], [], ['bdr'], 'bdr')
                        mwTr = Ring('mwT', [sb3("mwT%d" % i, [NE, 128]) for i in range(2)])
                        tbr = Ring('tb', [0, 1]); bbk2 = Ring('bd', [(2, 3), (4, 5)])
                        for j, i in enumerate(tiles):
                            s_ = 1 if i < NT_C else 0
                            tb_, tbk = tbr.next()
                            P.pe(lambda e, j=j, tb_=tb_: e.transpose(psum[0:NE, tb_, 0:128], mwd[:, j, :], ident[:]), ['ident'], [tbk])
                            mwT, mwTk = mwTr.next()
                            P.act(lambda e, mwT=mwT, tb_=tb_: e.activation(out=mwT[:], in_=psum[0:NE, tb_, 0:128], func=AF.Copy), [tbk], [mwTk])
                            (bd0, bd1), bdk = bbk2.next()
                            for nh, bdb_ in enumerate((bd0, bd1)):
                                P.pe(lambda e, mwT=mwT, nh=nh, bdb_=bdb_: e.matmul(psum[:, bdb_, :], mwT[:], bdr[:, nh * 512:(nh + 1) * 512], start=True, stop=True), [mwTk, 'bdr'], [bdk + str(nh)])
                            x1, x1k = x1r.next()
                            P.dma('sp', x1[:], XM[i * 128:(i + 1) * 128, :], [], [x1k], x1k)
                            G, Gk = Gr.next()
                            for k in range(4):
                                P.add('pool', lambda e, G=G, j=j, k=k: e.indirect_dma_start(out=G[:, k, :], out_offset=None, in_=YS[:, :],
                                                                                         in_offset=bass.IndirectOffsetOnAxis(ap=DKi[:, j, k:k + 1], axis=0)), ['DKi'], [Gk + str(k)], Gk + str(k))
                            P.dve(lambda e, G=G: e.tensor_tensor(out=G[:, 0, :], in0=G[:, 0, :], in1=G[:, 1, :], op=ALU.add), [Gk + '0', Gk + '1'], [Gk + '0'])
                            P.dve(lambda e, G=G: e.tensor_tensor(out=G[:, 2, :], in0=G[:, 2, :], in1=G[:, 3, :], op=ALU.add), [Gk + '2', Gk + '3'], [Gk + '2'])
                            P.dve(lambda e, G=G: e.tensor_tensor(out=G[:, 0, :], in0=G[:, 0, :], in1=G[:, 2, :], op=ALU.add), [Gk + '0', Gk + '2'], [Gk + '0'])
                            for nh, bdb_ in enumerate((bd0, bd1)):
                                P.dve(lambda e, G=G, nh=nh, bdb_=bdb_: e.tensor_tensor(out=G[:, 0, nh * 512:(nh + 1) * 512], in0=G[:, 0, nh * 512:(nh + 1) * 512], in1=psum[:, bdb_, :], op=ALU.add),
                                      [Gk + '0', bdk + str(nh)], [Gk + '0'])
                            P.dve(lambda e, G=G, s_=s_: e.tensor_tensor(out=rr[:], in0=G[:, 0, :], in1=g2[s_][:], op=ALU.mult), [Gk + '0', 'g2_%d' % s_], ['rr'])
                            P.dve(lambda e, x1=x1: e.scalar_tensor_tensor(out=rr[:], in0=x1[:], scalar=ALPHA, in1=rr[:], op0=ALU.mult, op1=ALU.add), [x1k, 'rr'], ['rr'])
                            for nh in range(2):
                                P.dve(lambda e, nh=nh: e.bn_stats(out=st6[:, nh, :], in_=rr[:, nh * 512:(nh + 1) * 512]), ['rr'], ['st6_%d' % nh])
                            P.dve(lambda e: e.bn_aggr(out=mv[:], in_=st6[:]), ['st6_0', 'st6_1'], ['mv'])
                            P.dve(lambda e: e.tensor_scalar_add(out=rs[:], in0=mv[:, 1:2], scalar1=EPS), ['mv'], ['rs'])
                            P.act(lambda e: e.activation(out=rs[:], in_=rs[:], func=AF.Sqrt), ['rs'], ['rs'])
                            P.dve(lambda e: e.reciprocal(out=rs[:], in_=rs[:]), ['rs'], ['rs'])
                            xo, xok = xor_.next()
                            P.dve(lambda e, xo=xo: e.tensor_scalar(out=xo[:], in0=rr[:], scalar1=mv[:, 0:1], scalar2=rs[:, 0:1], op0=ALU.subtract, op1=ALU.mult), ['rr', 'mv', 'rs'], [xok])
                            P.dve(lambda e, xo=xo: e.tensor_tensor(out=xo[:], in0=xo[:], in1=lng[:], op=ALU.mult), [xok, 'lng'], [xok])
                            P.dve(lambda e, xo=xo: e.tensor_tensor(out=xo[:], in0=xo[:], in1=lnb[:], op=ALU.add), [xok, 'lnb'], [xok])
                            dst = XS0[i * 128:(i + 1) * 128, :] if not last else out[(i - NT_C) * 128:(i - NT_C + 1) * 128, :]
                            P.dma('sp', dst, xo[:], [xok], [('xout', i)], xok)
                        P.flush()
        P.flush()
    return nc


def make_consts():
    inv = (10000.0 ** (-np.arange(0, 32, 2, dtype=np.float32) / 32.0)).astype(np.float32)
    t = np.arange(L)
    row = (t // 64).astype(np.float32)
    col = (t % 64).astype(np.float32)
    ang = np.stack([row[:, None] * inv, col[:, None] * inv], axis=1)
    cos = np.cos(ang).astype(np.float32)
    sin = np.sin(ang).astype(np.float32)
    c64 = np.stack([cos, cos], axis=2).reshape(L, 64)
    s64 = np.stack([-sin, sin], axis=2).reshape(L, 64)
    p = np.arange(128, dtype=np.float32)
    pos = np.stack([127 - p, p, p + 1, 128 - p], axis=1)
    dif = p[None, :] - p[:, None]
    return dict(
        c_ident=np.eye(128, dtype=np.float32),
        c_cos=np.ascontiguousarray(np.tile(c64, (1, 8))),
        c_sin=np.ascontiguousarray(np.tile(s64, (1, 8))),
        c_pos=np.ascontiguousarray(pos.astype(np.float32)),
        c_dpos=np.maximum(dif, 0).astype(np.float32),
        c_dneg=np.maximum(-dif, 0).astype(np.float32),
        c_mge=(dif >= 0).astype(np.float32),
        c_zeros=np.zeros((2, 256), np.float32),
        c_iota32=np.tile(np.arange(NE, dtype=np.float32)[None, :], (128, 1)),
        c_lts=(p[:, None] < p[None, :]).astype(np.float32),
        c_bstart=np.tile((np.arange(NBMAX, dtype=np.float32) * BS)[None, :], (128, 1)),
        c_kp=(np.arange(8, dtype=np.float32)[None, :] * 128 + p[:, None]).astype(np.float32),
        c_iotap=p[:, None].astype(np.float32).copy(),
    )


WKEYS = ['w_mod', 'b_mod', 'w_in', 'conv_w', 'ret_decay_exp', 'ret_gn_g', 'q_norm_g', 'k_norm_g', 'w_out',
         'ln_g', 'ln_b', 'w_router', 'b_router', 'w_gate_up', 'b_gate_up', 'w_down', 'b_down']


def make_in_maps(inputs, cores, skip=()):
    consts = make_consts()
    shared = {k: np.ascontiguousarray(np.asarray(inputs[k], np.float32)) for k in WKEYS if k not in skip}
    shared['c_ctx'] = np.ascontiguousarray(np.asarray(inputs['c_ctx'], np.float32))
    shared.update(consts)
    maps = []
    for b in cores:
        m = dict(shared)
        m['x'] = np.ascontiguousarray(np.asarray(inputs['x'][b], np.float32))
        m['c'] = np.ascontiguousarray(np.asarray(inputs['c'][b], np.float32))
        m['ctx'] = np.ascontiguousarray(np.asarray(inputs['ctx'][b], np.float32))
        maps.append(m)
    return maps


def kernel(**inputs):
    nc = build()
    maps = make_in_maps(inputs, list(range(8)))
    res = run_bass_kernel_spmd(nc, maps, core_ids=list(range(8)))
    return np.stack([np.asarray(r["out"], np.float32) for r in res.results], axis=0)
```

```python
import contextlib
import math
import numpy as np
import concourse.bass as bass
import concourse.mybir as mybir
from concourse.bass_utils import run_bass_kernel_spmd

F32 = mybir.dt.float32
BF16 = mybir.dt.bfloat16
ALU = mybir.AluOpType
AF = mybir.ActivationFunctionType
AX = mybir.AxisListType

D = 1024
L = 4096
LC = 256
NT_C = 2
NT = 34
DEPTH = 2
NE = 32
BS = 512
NBMAX = 66
I32 = mybir.dt.int32
U32 = mybir.dt.uint32
ALPHA = (2.0 * DEPTH) ** 0.25
EPS = 1e-6


class Prog:
    ENGS = ('pe', 'act', 'dve', 'pool', 'sp')

    def __init__(self, nc, stack):
        self.nc = nc
        self.ops = []
        self.sems = {}
        self.stack = stack
        for eng in ('pe', 'act', 'dve', 'pool'):
            self.sems[('e', eng)] = stack.enter_context(nc.semaphore('sem_' + eng))
        self.cnt = {}
        self.streams = {}
        self.pool_cnt = []
        self.waited = {e: {} for e in self.ENGS}
        self.total_ops = 0

    def add(self, eng, fn, reads=(), writes=(), stream=None):
        self.ops.append((eng, fn, tuple(reads), tuple(writes), stream))

    def pe(self, fn, reads=(), writes=()):
        self.add('pe', fn, reads, writes)

    def act(self, fn, reads=(), writes=()):
        self.add('act', fn, reads, writes)

    def dve(self, fn, reads=(), writes=()):
        self.add('dve', fn, reads, writes)

    def pool(self, fn, reads=(), writes=()):
        self.add('pool', fn, reads, writes)

    def dma(self, q, out, in_, reads, writes, stream, **kw):
        self.add(q, lambda e: e.dma_start(out=out, in_=in_, **kw), reads, writes, stream)

    def flush(self):
        nc = self.nc
        ops = self.ops
        self.ops = []
        n = len(ops)
        if n == 0:
            return
        self.total_ops += n
        last_writer = {}
        readers = {}
        deps = [None] * n
        for i, (eng, fn, rd, wr, st) in enumerate(ops):
            d = set()
            for r in rd:
                j = last_writer.get(r)
                if j is not None:
                    d.add((j, 0))
            for w in wr:
                j = last_writer.get(w)
                if j is not None:
                    d.add((j, 1))
                for k in readers.get(w, ()):
                    if k != i:
                        d.add((k, 2))
            deps[i] = d
            for r in rd:
                readers.setdefault(r, []).append(i)
            for w in wr:
                last_writer[w] = i
                readers[w] = []
        sig = [False] * n
        need = [None] * n
        last_compute = {}
        for i in range(n):
            eng = ops[i][0]
            lst = set()
            for (j, kind) in deps[i]:
                jeng, _, _, _, jst = ops[j]
                if jst is None and jeng == eng:
                    if eng == 'pe' or eng == 'sp':
                        continue
                    if kind == 2:
                        continue
                if jst is None:
                    sig[j] = True
                lst.add(j)
            need[i] = lst
            if ops[i][4] is None and ops[i][1] is not None:
                last_compute[eng] = i
        for eng, i in last_compute.items():
            if eng != 'sp':
                sig[i] = True
        sval = [None] * n
        phase_map = {}
        for i, (eng, fn, rd, wr, st) in enumerate(ops):
            if st is not None:
                if st not in phase_map:
                    k = len(phase_map)
                    phase_map[st] = k
                    if k >= len(self.pool_cnt):
                        self.pool_cnt.append(0)
                        self.sems[('s', k)] = self.stack.enter_context(nc.semaphore('sd_%d' % k))
                k = phase_map[st]
                self.pool_cnt[k] += 1
                sval[i] = (('s', k), 16 * self.pool_cnt[k])
            elif sig[i]:
                self.cnt[eng] = self.cnt.get(eng, 0) + 1
                sval[i] = (('e', eng), self.cnt[eng])
        per_eng = {e: [] for e in self.ENGS}
        for i, op in enumerate(ops):
            per_eng[op[0]].append(i)
        sems = self.sems
        final = {}
        for eng in ('pe', 'act', 'dve', 'pool'):
            if self.cnt.get(eng, 0) > 0:
                final[('e', eng)] = self.cnt[eng]
        for k, c in enumerate(self.pool_cnt):
            final[('s', k)] = 16 * c

        def run(engname, e):
            waited = self.waited[engname]
            for i in per_eng[engname]:
                _, fn, rd, wr, st = ops[i]
                w = {}
                for j in need[i]:
                    key, val = sval[j]
                    if w.get(key, 0) < val:
                        w[key] = val
                for key, val in w.items():
                    if waited.get(key, 0) >= val:
                        continue
                    waited[key] = val
                    e.wait_ge(sems[key], val)
                if fn is None:
                    continue
                ins = fn(e)
                if sval[i] is not None:
                    key, val = sval[i]
                    ins.then_inc(sems[key], 16 if key[0] == 's' else 1)
            for key, val in final.items():
                if key == ('e', engname):
                    continue
                if waited.get(key, 0) >= val:
                    continue
                waited[key] = val
                e.wait_ge(sems[key], val)

        with nc.Block() as block:
            @block.tensor
            def _(e):
                run('pe', e)

            @block.scalar
            def _(e):
                run('act', e)

            @block.vector
            def _(e):
                run('dve', e)

            @block.gpsimd
            def _(e):
                run('pool', e)

            @block.sync
            def _(e):
                run('sp', e)


def pipeline_reorder(P, base, marks):
    ops = P.ops
    H = [ops[a:b] for (a, b, c) in marks]
    T = [ops[b:c] for (a, b, c) in marks]
    new = list(ops[:base]) + H[0]
    for i in range(len(marks)):
        if i + 1 < len(marks):
            new += H[i + 1]
        new += T[i]
    new += list(ops[marks[-1][2]:])
    assert len(new) == len(ops)
    P.ops = new


class Ring:
    def __init__(self, name, tiles):
        self.name = name
        self.tiles = tiles
        self.i = 0

    def next(self):
        k = self.i % len(self.tiles)
        self.i += 1
        return self.tiles[k], '%s%d' % (self.name, k)


SEC = dict(u=(0, 256), B=(256, 256), C=(512, 256), rq=(768, 256), rk=(1024, 256), rv=(1280, 256),
           rg=(1536, 256), aq=(1792, 512), ak=(2304, 128), av=(2432, 128))
MYCOL = dict(u=0, C=256, B=512, rv=768, rq=1024, rk=1280, aq=1536, rg=2048, ak=2304, av=2432)


def build(phases=('p0', 'p1', 'att', 'ret', 'p3', 'moe'), layers=(0, 1), debug=False, moe_experts=NE, cut=99):
    nc = bass.Bass("TRN2", target_bir_lowering=False)

    _uc = [0]

    def uname(name):
        _uc[0] += 1
        return '%s_u%d' % (name, _uc[0])

    def din(name, shape, dt=F32):
        return nc.dram_tensor(name, list(shape), dt, kind="ExternalInput").ap()

    def dscr(name, shape, dt=F32):
        return nc.dram_tensor(name, list(shape), dt, kind=("ExternalOutput" if debug else "Internal")).ap()

    x_in = din("x", [L, D])
    c_in = din("c", [D])
    ctx_in = din("ctx", [LC, D])
    cctx_in = din("c_ctx", [D])
    w_mod = din("w_mod", [DEPTH, D, 6 * D])
    b_mod = din("b_mod", [DEPTH, 6 * D])
    w_in = din("w_in", [DEPTH, D, 2560])
    conv_w = din("conv_w", [DEPTH, 256, 3])
    rde = din("ret_decay_exp", [DEPTH, 2, 4])
    gn_g = din("ret_gn_g", [DEPTH, 256])
    qn_g = din("q_norm_g", [DEPTH, 64])
    kn_g = din("k_norm_g", [DEPTH, 64])
    w_out = din("w_out", [DEPTH, D, D])
    ln_g = din("ln_g", [DEPTH, 2, D])
    ln_b = din("ln_b", [DEPTH, 2, D])
    w_router = din("w_router", [DEPTH, D, NE])
    b_router = din("b_router", [DEPTH, NE])
    if 'moe' in phases:
        w_gu = din("w_gate_up", [DEPTH, NE, D, 2 * D])
        b_gu = din("b_gate_up", [DEPTH, NE, 2 * D])
        w_dn = din("w_down", [DEPTH, NE, D, D])
        b_dn = din("b_down", [DEPTH, NE, D])
    ident_in = din("c_ident", [128, 128])
    cos_in = din("c_cos", [L, 512])
    sin_in = din("c_sin", [L, 512])
    pos_in = din("c_pos", [128, 4])
    dpos_in = din("c_dpos", [128, 128])
    dneg_in = din("c_dneg", [128, 128])
    mge_in = din("c_mge", [128, 128])
    zeros_in = din("c_zeros", [2, 256])
    iota32_in = din("c_iota32", [128, NE])
    lts_in = din("c_lts", [128, 128])
    bstart_in = din("c_bstart", [128, NBMAX])
    kp_in = din("c_kp", [128, 8])
    iotap_in = din("c_iotap", [128, 1])

    out = nc.dram_tensor("out", [L, D], F32, kind="ExternalOutput").ap()

    N = NT * 128
    modv = dscr("modv", [DEPTH, 2, 6 * D])
    PB = dscr("PB", [N + 4, 512])
    RQT = dscr("RQT", [3, 4, 64, N], BF16)
    RKT = dscr("RKT", [4, 64, N], BF16)
    RTOK = dscr("RTOK", [N, 768], BF16)
    RG = dscr("RG", [N, 256])
    AQT = dscr("AQT", [8, 64, N], BF16)
    AKT = dscr("AKT", [2, 64, N], BF16)
    AV = dscr("AV", [N, 128], BF16)
    MIX = dscr("MIX", [N, 1024])
    XS0 = dscr("XS0", [N, D])
    XM = dscr("XM", [N, D])
    H2R = dscr("H2R", [N, D], BF16)
    RW = dscr("RW", [N, 4])
    RE = dscr("RE", [N, 4])
    XS = dscr("XS", [NBMAX * BS, D], BF16)
    SW = dscr("SW", [NBMAX * BS, 1])
    YS = dscr("YS", [NBMAX * BS, D])
    BGT = dscr("BGT", [DEPTH * NE * 128, 16])

    def prow(i):
        return 1 + i * 128 if i < NT_C else 259 + (i - NT_C) * 128

    with contextlib.ExitStack() as gst:
        P = Prog(nc, gst)
        psum = gst.enter_context(nc.psum_tensor("psum", [128, 8, 512], F32))
        ident = gst.enter_context(nc.sbuf_tensor("ident", [128, 128], F32))
        P.dma('sp', ident[:], ident_in, [], ['ident'], 'ident')

        def bank(b):
            return psum[:, b, :]

        if 'p0' in phases:
            with contextlib.ExitStack() as st:
                def sb(name, shape, dt=F32):
                    return st.enter_context(nc.sbuf_tensor(uname(name), list(shape), dt))
                cnd = sb("cnd", [128, 2, 8])
                P.dma('sp', cnd[:, 0, :], c_in.rearrange("(k p) -> p k", p=128), [], ['cnd'], 'cnd0',
                      allow_slow_non_contiguous=True)
                P.dma('sp', cnd[:, 1, :], cctx_in.rearrange("(k p) -> p k", p=128), [], ['cnd'], 'cnd1',
                      allow_slow_non_contiguous=True)
                P.act(lambda e: e.activation(out=cnd[:], in_=cnd[:], func=AF.Silu), ['cnd'], ['cnd'])
                wring = Ring('wm', [sb("wm%d" % i, [128, 8, 512]) for i in range(6)])
                bring = Ring('bm', [sb("bm%d" % i, [2, 512]) for i in range(2)])
                oring = Ring('om', [sb("om%d" % i, [2, 512]) for i in range(2)])
                for l in layers:
                    for n in range(12):
                        wt, wk = wring.next()
                        bt, bk = bring.next()
                        ot, ok = oring.next()
                        P.dma('sp' if n % 2 == 0 else 'act', wt[:], w_mod[l, :, n * 512:(n + 1) * 512].rearrange("(k p) n -> p k n", p=128),
                              [], [wk], wk)
                        P.dma('sp', bt[:], b_mod[l, n * 512:(n + 1) * 512].partition_broadcast(2), [], [bk], bk)
                        pbank = n % 2
                        pb_ = 'ps0_%d' % pbank
                        for k in range(8):
                            P.pe(lambda e, k=k, wt=wt, pbank=pbank: e.matmul(psum[0:2, pbank, :], cnd[:, :, k], wt[:, k, :],
                                                                            start=(k == 0), stop=(k == 7)),
                                 ['cnd', wk], [pb_])
                        P.dve(lambda e, ot=ot, bt=bt, pbank=pbank: e.tensor_tensor(out=ot[:], in0=psum[0:2, pbank, :], in1=bt[:], op=ALU.add),
                              [pb_, bk], [ok])
                        if n in (2, 3, 8, 9):
                            P.dve(lambda e, ot=ot: e.tensor_scalar_add(out=ot[:], in0=ot[:], scalar1=1.0), [ok], [ok])
                        P.dma('sp', modv[l, :, n * 512:(n + 1) * 512], ot[:], [ok], [('modv', l)], ok)
                P.flush()

        def bload(q, tile_ap, vec_ap, key, reads=()):
            P.dma(q, tile_ap, vec_ap.partition_broadcast(128), list(reads), [key], key)

        for l in layers:
            last = (l == DEPTH - 1)
            xsrc = (lambda i: (ctx_in[i * 128:(i + 1) * 128, :] if i < NT_C else x_in[(i - NT_C) * 128:(i - NT_C + 1) * 128, :])) \
                if l == 0 else (lambda i: XS0[i * 128:(i + 1) * 128, :])
            xs_key = (lambda i: ('xin', i)) if l == 0 else (lambda i: ('XS0', i))
            out_tiles = list(range(NT)) if not last else list(range(NT_C, NT))

            if 'p1' in phases:
                with contextlib.ExitStack() as st:
                    def sb(name, shape, dt=F32):
                        return st.enter_context(nc.sbuf_tensor(uname(name), list(shape), dt))
                    win = sb("win", [128, 8, 2560], BF16)
                    for name, (c0, w) in SEC.items():
                        m0 = MYCOL[name]
                        P.dma('pool', win[:, :, m0:m0 + w], w_in[l, :, c0:c0 + w].rearrange("(k p) n -> p k n", p=128),
                              [], ['win_' + name], 'win_' + name)
                    winkeys = ['win_' + k for k in SEC]
                    sc1 = [sb("sc1_%d" % s, [128, D]) for s in range(2)]
                    sh1 = [sb("sh1_%d" % s, [128, D]) for s in range(2)]
                    for s in range(2):
                        bload('sp', sc1[s][:], modv[l, s, D:2 * D], 'sc1_%d' % s, [('modv', l)])
                        bload('sp', sh1[s][:], modv[l, s, 0:D], 'sh1_%d' % s, [('modv', l)])
                    gq = sb("gq", [128, 64])
                    gk = sb("gk", [128, 64])
                    bload('act', gq[:], qn_g[l], 'gq')
                    bload('act', gk[:], kn_g[l], 'gk')
                    zpad = sb("zpad", [2, 256])
                    P.dma('act', zpad[:], zeros_in, [], ['zpad'], 'zpad')
                    for r0 in (0, 257):
                        P.dma('act', PB[r0:r0 + 2, 0:256] if r0 else PB[0:1, 0:256], zpad[0:2, :] if r0 else zpad[0:1, :],
                              ['zpad'], [('PBpad', r0)], 'zp%d' % r0)
                    P.dma('act', PB[N + 3:N + 4, 0:256], zpad[0:1, :], ['zpad'], [('PBpad', 3)], 'zp3')
                    posc = sb("posc", [128, 4])
                    P.dma('act', posc[:], pos_in, [], ['posc'], 'posc')
                    lg = sb("lg", [128, 8])
                    bload('act', lg[:], rde[l].rearrange("a h -> (a h)"), 'lg')
                    P.act(lambda e: e.activation(out=lg[:], in_=lg[:], func=AF.Exp, scale=-math.log(2.0)), ['lg'], ['lg'])
                    P.act(lambda e: e.activation(out=lg[:], in_=lg[:], func=AF.Ln, scale=-1.0, bias=1.0), ['lg'], ['lg'])
                    tab4 = sb("tab4", [128, 4, 4])
                    for ti, (di, pc) in enumerate(((0, 0), (1, 1), (0, 2), (1, 3))):
                        P.act(lambda e, ti=ti, di=di, pc=pc: e.activation(out=tab4[:, ti, :], in_=lg[:, di * 4:(di + 1) * 4],
                                                                          func=AF.Exp, scale=posc[:, pc:pc + 1]),
                              ['lg', 'posc'], ['tab4'])
                    P.dve(lambda e: e.tensor_scalar_mul(out=tab4[:, 0:2, :], in0=tab4[:, 0:2, :], scalar1=0.125), ['tab4'], ['tab4'])
                    TAB = sb("TAB", [128, 4, 4, 64])
                    P.dve(lambda e: e.tensor_copy(out=TAB[:].rearrange("p t h d -> p (t h) d"),
                                                  in_=tab4[:].rearrange("p t h -> p (t h)").unsqueeze(2).to_broadcast([128, 16, 64])),
                          ['tab4'], ['TAB'])
                    gq8 = sb("gq8", [128, 8, 64])
                    P.dve(lambda e: e.tensor_copy(out=gq8[:], in_=gq[:].unsqueeze(1).to_broadcast([128, 8, 64])), ['gq'], ['gq8'])
                    gk2 = sb("gk2", [128, 2, 64])
                    P.dve(lambda e: e.tensor_copy(out=gk2[:], in_=gk[:].unsqueeze(1).to_broadcast([128, 2, 64])), ['gk'], ['gk2'])

                    xring = Ring('xt', [sb("xt%d" % i, [128, D]) for i in range(3)])
                    csring = Ring('cs', [sb("cs%d" % i, [128, 512]) for i in range(3)])
                    snring = Ring('sn', [sb("sn%d" % i, [128, 512]) for i in range(3)])
                    hring = Ring('h', [sb("h%d" % i, [128, D]) for i in range(2)])
                    hTring = Ring('hT', [sb("hT%d" % i, [128, 8, 128], BF16) for i in range(2)])
                    usb = sb("usb", [128, 256])
                    pcb = Ring('pcb', [sb("pcb%d" % i, [128, 512]) for i in range(2)])
                    t1 = sb("t1", [128, 512])
                    t2 = sb("t2", [128, 512])
                    rq = sb("rq", [128, 4, 256])
                    rtok = Ring('rtok', [sb("rtok%d" % i, [128, 768], BF16) for i in range(2)])
                    rgt = Ring('rgt', [sb("rgt%d" % i, [128, 256]) for i in range(2)])
                    rT = Ring('rT', [sb("rT%d" % i, [128, 8, 128], BF16) for i in range(2)])
                    sq = sb("sq", [128, 512])
                    ss = sb("ss", [128, 8])
                    aq = sb("aq", [128, 512])
                    aqn = sb("aqn", [128, 512])
                    akn = sb("akn", [128, 128])
                    ak = sb("ak", [128, 128])
                    aT = Ring('aT', [sb("aT%d" % i, [128, 5, 128], BF16) for i in range(2)])
                    avt = Ring('avt', [sb("avt%d" % i, [128, 128], BF16) for i in range(2)])

                    def rope(src_ap, W, dst_ap, src_keys, dst_key, cs, ck, sn, sk):
                        g = W // 32
                        P.dve(lambda e: e.tensor_tensor(out=t1[:, :W], in0=src_ap, in1=cs[:, :W], op=ALU.mult),
                              src_keys + [ck], ['t1'])
                        s4 = src_ap.rearrange("p (g a f) -> p g a f", a=2, f=16)
                        t4 = t2[:, :W].rearrange("p (g a f) -> p g a f", a=2, f=16)
                        n4 = sn[:, :W].rearrange("p (g a f) -> p g a f", a=2, f=16)
                        P.dve(lambda e: e.tensor_tensor(out=t4[:, :, 0, :], in0=s4[:, :, 1, :], in1=n4[:, :, 0, :], op=ALU.mult),
                              src_keys + [sk], ['t2a'])
                        P.dve(lambda e: e.tensor_tensor(out=t4[:, :, 1, :], in0=s4[:, :, 0, :], in1=n4[:, :, 1, :], op=ALU.mult),
                              src_keys + [sk], ['t2b'])
                        P.dve(lambda e: e.tensor_tensor(out=dst_ap, in0=t1[:, :W], in1=t2[:, :W], op=ALU.add),
                              ['t1', 't2a', 't2b'], [dst_key])

                    ld = {}

                    def issue_loads(i):
                        xt, xk = xring.next()
                        P.dma('sp', xt[:], xsrc(i), [xs_key(i)], [xk], xk)
                        if i >= NT_C:
                            cs, ck = csring.next()
                            sn, sk = snring.next()
                            t0 = (i - NT_C) * 128
                            P.dma('sp', cs[:], cos_in[t0:t0 + 128, :], [], [ck], ck)
                            P.dma('sp', sn[:], sin_in[t0:t0 + 128, :], [], [sk], sk)
                            ld[i] = (xt, xk, cs, ck, sn, sk)
                        else:
                            ld[i] = (xt, xk, None, None, None, None)
                    issue_loads(0)
                    p1_base = len(P.ops)
                    p1_marks = []
                    for i in range(NT):
                        isctx = i < NT_C
                        s = 1 if isctx else 0
                        m_a = len(P.ops)
                        if i + 1 < NT:
                            issue_loads(i + 1)
                        xt, xk, cs, ck, sn, sk = ld.pop(i)
                        h, hk = hring.next()
                        P.dve(lambda e, h=h, xt=xt, s=s: e.tensor_tensor(out=h[:], in0=xt[:], in1=sc1[s][:], op=ALU.mult),
                              [xk, 'sc1_%d' % s], [hk])
                        P.dve(lambda e, h=h, s=s: e.tensor_tensor(out=h[:], in0=h[:], in1=sh1[s][:], op=ALU.add),
                              [hk, 'sh1_%d' % s], [hk])
                        for k in range(8):
                            P.pe(lambda e, h=h, k=k: e.transpose(psum[:, 5 + k // 4, (k % 4) * 128:(k % 4 + 1) * 128],
                                                                 h[:, k * 128:(k + 1) * 128], ident[:]),
                                 [hk, 'ident'], ['pT%d' % k])
                        hT, hTk = hTring.next()
                        for hh in range(2):
                            P.act(lambda e, hT=hT, hh=hh: e.activation(out=hT[:, hh * 4:(hh + 1) * 4, :].rearrange("p k n -> p (k n)"),
                                                                       in_=psum[:, 5 + hh, :], func=AF.Copy),
                                  ['pT%d' % k for k in range(hh * 4, hh * 4 + 4)], [hTk + '_%d' % hh])
                        m_b = len(P.ops)
                        for b in range(5):
                            for k in range(8):
                                P.pe(lambda e, hT=hT, b=b, k=k: e.matmul(bank(b), hT[:, k, :], win[:, k, b * 512:(b + 1) * 512],
                                                                         start=(k == 0), stop=(k == 7)),
                                     [hTk + '_%d' % (k // 4)] + winkeys, ['z%d' % b])
                        pc, pck = pcb.next()
                        P.act(lambda e: e.activation(out=usb[:], in_=psum[:, 0, 0:256], func=AF.Copy), ['z0'], ['usb'])
                        P.dve(lambda e, pc=pc: e.tensor_tensor(out=pc[:, 0:256], in0=psum[:, 0, 256:512], in1=usb[:], op=ALU.mult),
                              ['z0', 'usb'], [pck + 'a'])
                        P.act(lambda e, pc=pc: e.activation(out=pc[:, 256:512], in_=psum[:, 1, 0:256], func=AF.Copy), ['z1'], [pck + 'b'])
                        r0 = prow(i)
                        P.dma('sp', PB[r0:r0 + 128, :], pc[:], [pck + 'a', pck + 'b'], [('PB', i)], pck)
                        rt, rtk = rtok.next()
                        P.act(lambda e, rt=rt: e.activation(out=rt[:, 512:768], in_=psum[:, 1, 256:512], func=AF.Copy), ['z1'], [rtk + 'v'])
                        if isctx:
                            P.act(lambda e: e.activation(out=rq[:, 0, :], in_=psum[:, 2, 0:256], func=AF.Copy), ['z2'], ['rq0'])
                            P.act(lambda e: e.activation(out=rq[:, 3, :], in_=psum[:, 2, 256:512], func=AF.Copy), ['z2'], ['rq3'])
                        else:
                            rope(psum[:, 2, 0:256], 256, rq[:, 0, :], ['z2'], 'rq0', cs, ck, sn, sk)
                            rope(psum[:, 2, 256:512], 256, rq[:, 3, :], ['z2'], 'rq3', cs, ck, sn, sk)
                        TABf = TAB[:].rearrange("p t h d -> p t (h d)")
                        P.dve(lambda e: e.tensor_tensor(out=rq[:, 1, :], in0=rq[:, 0, :], in1=TABf[:, 2, :], op=ALU.mult), ['rq0', 'TAB'], ['rq1'])
                        P.pool(lambda e: e.tensor_tensor(out=rq[:, 2, :], in0=rq[:, 0, :], in1=TABf[:, 3, :], op=ALU.mult), ['rq0', 'TAB'], ['rq2'])
                        P.dve(lambda e, rt=rt: e.tensor_tensor(out=rt[:, 0:256], in0=rq[:, 3, :], in1=TABf[:, 0, :], op=ALU.mult), ['rq3', 'TAB'], [rtk + 'f'])
                        P.pool(lambda e, rt=rt: e.tensor_tensor(out=rt[:, 256:512], in0=rq[:, 3, :], in1=TABf[:, 1, :], op=ALU.mult), ['rq3', 'TAB'], [rtk + 'b'])
                        P.dma('sp', RTOK[i * 128:(i + 1) * 128, :], rt[:], [rtk + 'v', rtk + 'f', rtk + 'b'], [('RTOK', i)], rtk)
                        rg_, rgk = rgt.next()
                        P.act(lambda e, rg_=rg_: e.activation(out=rg_[:], in_=psum[:, 4, 0:256], func=AF.Silu), ['z4'], [rgk])
                        P.dma('act', RG[i * 128:(i + 1) * 128, :], rg_[:], [rgk], [('RG', i)], rgk)
                        for t in range(4):
                            for c2 in range(2):
                                idx = t * 2 + c2
                                P.pe(lambda e, t=t, c2=c2, idx=idx: e.transpose(psum[:, 5 + idx // 4, (idx % 4) * 128:(idx % 4 + 1) * 128],
                                                                                rq[:, t, c2 * 128:(c2 + 1) * 128], ident[:]),
                                     ['rq%d' % t, 'ident'], ['pT%d' % idx])
                        rTt, rTk = rT.next()
                        for hh in range(2):
                            if hh == 0:
                                P.act(lambda e, rTt=rTt: e.activation(out=rTt[:, 0:4, :].rearrange("p k n -> p (k n)"), in_=psum[:, 5, :], func=AF.Copy),
                                      ['pT0', 'pT1', 'pT2', 'pT3'], [rTk + 'a'])
                            else:
                                P.act(lambda e, rTt=rTt: e.activation(out=rTt[:, 4:6, :].rearrange("p k n -> p (k n)"), in_=psum[:, 6, 0:256], func=AF.Copy),
                                      ['pT4', 'pT5'], [rTk + 'b'])
                                P.act(lambda e, rTt=rTt: e.activation(out=rTt[:, 6:8, :].rearrange("p k n -> p (k n)"), in_=psum[:, 6, 256:512], func=AF.Copy, scale=0.125),
                                      ['pT6', 'pT7'], [rTk + 'c'])
                        for t in range(3):
                            P.dma('act', RQT[t, :, :, i * 128:(i + 1) * 128].rearrange("(c q) d n -> (q d) c n", q=2),
                                  rTt[:, 2 * t:2 * t + 2, :], [rTk + 'a', rTk + 'b'], [('RQT', i, t)], rTk + 'q%d' % t)
                        P.dma('act', RKT[:, :, i * 128:(i + 1) * 128].rearrange("(c q) d n -> (q d) c n", q=2),
                              rTt[:, 6:8, :], [rTk + 'c'], [('RKT', i)], rTk + 'k')
                        P.act(lambda e: e.activation(out=sq[:], in_=psum[:, 3, :], func=AF.Square), ['z3'], ['sq'])
                        P.dve(lambda e: e.tensor_reduce(out=ss[:], in_=sq[:].rearrange("p (h d) -> p h d", d=64), axis=AX.X, op=ALU.add), ['sq'], ['ss'])
                        P.dve(lambda e: e.tensor_scalar(out=ss[:], in0=ss[:], scalar1=1.0 / 64, scalar2=EPS, op0=ALU.mult, op1=ALU.add), ['ss'], ['ss'])
                        P.act(lambda e: e.activation(out=ss[:], in_=ss[:], func=AF.Sqrt), ['ss'], ['ss'])
                        P.dve(lambda e: e.reciprocal(out=ss[:], in_=ss[:]), ['ss'], ['ss'])
                        P.dve(lambda e: e.tensor_tensor(out=aqn[:].rearrange("p (h d) -> p h d", d=64), in0=psum[:, 3, :].rearrange("p (h d) -> p h d", d=64),
                                                        in1=ss[:].unsqueeze(2).to_broadcast([128, 8, 64]), op=ALU.mult), ['z3', 'ss'], ['aqn'])
                        P.dve(lambda e: e.tensor_tensor(out=aqn[:], in0=aqn[:], in1=gq8[:].rearrange("p h d -> p (h d)"), op=ALU.mult), ['aqn', 'gq8'], ['aqn'])
                        if isctx:
                            aq_src, aq_key = aqn, 'aqn'
                        else:
                            rope(aqn[:], 512, aq[:], ['aqn'], 'aq', cs, ck, sn, sk)
                            aq_src, aq_key = aq, 'aq'
                        av_, avk = avt.next()
                        P.act(lambda e, av_=av_: e.activation(out=av_[:], in_=psum[:, 4, 384:512], func=AF.Copy), ['z4'], [avk])
                        P.dma('act', AV[i * 128:(i + 1) * 128, :], av_[:], [avk], [('AV', i)], avk)
                        P.act(lambda e: e.activation(out=sq[:, 0:128], in_=psum[:, 4, 256:384], func=AF.Square), ['z4'], ['sqk'])
                        P.dve(lambda e: e.tensor_reduce(out=ss[:, 0:2], in_=sq[:, 0:128].rearrange("p (h d) -> p h d", d=64), axis=AX.X, op=ALU.add), ['sqk'], ['ssk'])
                        P.dve(lambda e: e.tensor_scalar(out=ss[:, 0:2], in0=ss[:, 0:2], scalar1=1.0 / 64, scalar2=EPS, op0=ALU.mult, op1=ALU.add), ['ssk'], ['ssk'])
                        P.act(lambda e: e.activation(out=ss[:, 0:2], in_=ss[:, 0:2], func=AF.Sqrt), ['ssk'], ['ssk'])
                        P.dve(lambda e: e.reciprocal(out=ss[:, 0:2], in_=ss[:, 0:2]), ['ssk'], ['ssk'])
                        P.dve(lambda e: e.tensor_tensor(out=akn[:].rearrange("p (h d) -> p h d", d=64), in0=psum[:, 4, 256:384].rearrange("p (h d) -> p h d", d=64),
                                                        in1=ss[:, 0:2].unsqueeze(2).to_broadcast([128, 2, 64]), op=ALU.mult), ['z4', 'ssk'], ['akn'])
                        P.dve(lambda e: e.tensor_tensor(out=akn[:], in0=akn[:], in1=gk2[:].rearrange("p h d -> p (h d)"), op=ALU.mult), ['akn', 'gk2'], ['akn'])
                        if isctx:
                            ak_src, ak_key = akn, 'akn'
                        else:
                            rope(akn[:], 128, ak[:], ['akn'], 'ak', cs, ck, sn, sk)
                            ak_src, ak_key = ak, 'ak'
                        for c4 in range(4):
                            P.pe(lambda e, c4=c4, aq_src=aq_src: e.transpose(psum[:, 7, c4 * 128:(c4 + 1) * 128], aq_src[:, c4 * 128:(c4 + 1) * 128], ident[:]),
                                 [aq_key, 'ident'], ['pA%d' % c4])
                        P.pe(lambda e, ak_src=ak_src: e.transpose(psum[:, 5, 0:128], ak_src[:, 0:128], ident[:]), [ak_key, 'ident'], ['pT0'])
                        aTt, aTk = aT.next()
                        P.act(lambda e, aTt=aTt: e.activation(out=aTt[:, 0:4, :].rearrange("p k n -> p (k n)"), in_=psum[:, 7, :], func=AF.Copy),
                              ['pA0', 'pA1', 'pA2', 'pA3'], [aTk + 'q'])
                        P.act(lambda e, aTt=aTt: e.activation(out=aTt[:, 4, :], in_=psum[:, 5, 0:128], func=AF.Copy), ['pT0'], [aTk + 'k'])
                        P.dma('sp', AQT[:, :, i * 128:(i + 1) * 128].rearrange("(c q) d n -> (q d) c n", q=2), aTt[:, 0:4, :],
                              [aTk + 'q'], [('AQT', i)], aTk + 'q')
                        P.dma('sp', AKT[:, :, i * 128:(i + 1) * 128].rearrange("q d n -> (q d) n"), aTt[:, 4, :],
                              [aTk + 'k'], [('AKT', i)], aTk + 'k')
                        p1_marks.append((m_a, m_b, len(P.ops)))
                    pipeline_reorder(P, p1_base, p1_marks)
                    P.flush()


            if 'att' in phases:
                with contextlib.ExitStack() as st:
                    def sb(name, shape, dt=F32):
                        return st.enter_context(nc.sbuf_tensor(uname(name), list(shape), dt))
                    KT = sb("KT", [128, 2, N], BF16)
                    P.pool(lambda e: e.memset(KT[64:128, :, :], 0.0), [], ['KTz'])
                    P.dma('sp', KT[0:64, :, :], AKT.rearrange("k d n -> d k n"), [], ['KT'], 'KT')
                    V1 = sb("V1", [128, NT, 2, 65], BF16)
                    P.pool(lambda e: e.memset(V1[:], 1.0), [], ['V1'])
                    for kk_ in range(2):
                        P.dma('act', V1[:, :, kk_, 0:64], AV[:, kk_ * 64:(kk_ + 1) * 64].rearrange("(c p) d -> p c d", p=128), [], ['V1'], 'V1_%d' % kk_)
                    qring = Ring('QT', [sb("QT%d" % i, [128, N], BF16) for i in range(2)])
                    for qi_, qt_ in enumerate(qring.tiles):
                        P.pool(lambda e, qt_=qt_: e.memset(qt_[64:128, :], 0.0), [], ['QTz%d' % qi_])
                    ptring = Ring('PT', [sb("PT%d" % i, [128, 512], BF16) for i in range(4)])
                    aoring = Ring('AO', [sb("AO%d" % i, [128, 4, 64]) for i in range(2)])
                    rcring = Ring('rc', [sb("rc%d" % i, [128, 4]) for i in range(2)])
                    sbank = Ring('S', [0, 1, 2, 3])
                    obank = Ring('O', [4, 5])
                    groups = []
                    if not last:
                        groups.append((0, 2, [0, 1]))
                    for g in range(8):
                        groups.append((LC + g * 512, 4, list(range(NT))))
                    items = []
                    for hq in range(8):
                        for gi, (q0, nq, chunks) in enumerate(groups):
                            for ci, c in enumerate(chunks):
                                items.append((hq, gi, q0, nq, ci, c, len(chunks)))
                    qt_of = {}
                    st_of = {}

                    def get_qt(hq):
                        if hq not in qt_of:
                            QT, qk = qring.next()
                            P.dma('sp', QT[0:64, :], AQT[hq], [], [qk], qk)
                            qt_of[hq] = (QT, qk)
                        return qt_of[hq]

                    def emit_S(t):
                        hq, gi, q0, nq, ci, c, nch = items[t]
                        QT, qk = get_qt(hq)
                        kvh = hq // 4
                        W = nq * 128
                        sbk, sk = sbank.next()
                        P.pe(lambda e, sbk=sbk, c=c, QT=QT, q0=q0, W=W, kvh=kvh: e.matmul(psum[:, sbk, 0:W], KT[:, kvh, c * 128:(c + 1) * 128],
                                                                                         QT[:, q0:q0 + W], start=True, stop=True),
                             ['KT', 'KTz', 'QTz0', 'QTz1', qk], [sk])
                        st_of[t] = (sbk, sk)

                    cur_o = [None]

                    def emit_rest(t):
                        hq, gi, q0, nq, ci, c, nch = items[t]
                        kvh = hq // 4
                        W = nq * 128
                        sbk, sk = st_of.pop(t)
                        if ci == 0:
                            cur_o[0] = obank.next()
                        ob, ok = cur_o[0]
                        Ov = psum[:, ob, 0:260].rearrange("p (j e) -> p j e", e=65)
                        PT, pk = ptring.next()
                        P.act(lambda e, PT=PT, sbk=sbk, W=W: e.activation(out=PT[:, 0:W], in_=psum[:, sbk, 0:W], func=AF.Exp, scale=0.125),
                              [sk], [pk])
                        if t + 2 < len(items):
                            emit_S(t + 2)
                        for j in range(nq):
                            P.pe(lambda e, PT=PT, j=j, c=c, kvh=kvh, ci=ci, Ov=Ov, nch=nch: e.matmul(
                                Ov[:, j, :], PT[:, j * 128:(j + 1) * 128], V1[:, c, kvh, :],
                                start=(ci == 0 and j == 0), stop=(ci == nch - 1), skip_group_check=True),
                                [pk, 'V1'], [ok])
                        if ci == nch - 1:
                            rc, rk_ = rcring.next()
                            AO, ak_ = aoring.next()
                            P.dve(lambda e, rc=rc, Ov=Ov, nq=nq: e.reciprocal(out=rc[:, 0:nq], in_=Ov[:, 0:nq, 64]), [ok], [rk_])
                            P.dve(lambda e, rc=rc, Ov=Ov, nq=nq, AO=AO: e.tensor_tensor(out=AO[:, 0:nq, :], in0=Ov[:, 0:nq, 0:64],
                                                                                      in1=rc[:, 0:nq].unsqueeze(2).to_broadcast([128, nq, 64]), op=ALU.mult),
                                  [ok, rk_], [ak_])
                            P.dma('sp', MIX[q0:q0 + W, 512 + hq * 64:512 + (hq + 1) * 64].rearrange("(j p) d -> p j d", p=128), AO[:, 0:nq, :],
                                  [ak_], [('MIXa', q0, hq)], ak_)
                    emit_S(0)
                    emit_S(1)
                    for t in range(len(items)):
                        emit_rest(t)
                    P.flush()

            if 'ret' in phases:
                with contextlib.ExitStack() as st:
                    def sb(name, shape, dt=F32):
                        return st.enter_context(nc.sbuf_tensor(uname(name), list(shape), dt))
                    RT = sb("RT", [128, NT, 768], BF16)
                    P.dma('sp', RT[:], RTOK.rearrange("(c p) w -> p c w", p=128), [], ['RT'], 'RT')
                    RGt = sb("RGt", [128, NT, 256])
                    P.dma('act', RGt[:], RG.rearrange("(c p) w -> p c w", p=128), [], ['RGt'], 'RGt')
                    gng = sb("gng", [128, 256])
                    bload('act', gng[:], gn_g[l], 'gng')
                    lg = sb("lg", [128, 8])
                    bload('act', lg[:], rde[l].rearrange("a h -> (a h)"), 'lg')
                    P.act(lambda e: e.activation(out=lg[:], in_=lg[:], func=AF.Exp, scale=-math.log(2.0)), ['lg'], ['lg'])
                    P.act(lambda e: e.activation(out=lg[:], in_=lg[:], func=AF.Ln, scale=-1.0, bias=1.0), ['lg'], ['lg'])
                    dec = sb("dec", [128, 8])
                    P.act(lambda e: e.activation(out=dec[:], in_=lg[:], func=AF.Exp, scale=128.0), ['lg'], ['dec'])
                    dpos = sb("dpos", [128, 128]); dneg = sb("dneg", [128, 128]); mge = sb("mge", [128, 128])
                    P.dma('sp', dpos[:], dpos_in, [], ['dpos'], 'dpos')
                    P.dma('sp', dneg[:], dneg_in, [], ['dneg'], 'dneg')
                    P.dma('sp', mge[:], mge_in, [], ['mge'], 'mge')
                    DcT = sb("DcT", [128, 4, 128])
                    e1 = sb("e1", [128, 128])
                    for hh in range(4):
                        P.act(lambda e, hh=hh: e.activation(out=e1[:], in_=dpos[:], func=AF.Exp, scale=lg[:, hh:hh + 1]), ['dpos', 'lg'], ['e1'])
                        P.act(lambda e, hh=hh: e.activation(out=DcT[:, hh, :], in_=dneg[:], func=AF.Exp, scale=lg[:, 4 + hh:5 + hh]), ['dneg', 'lg'], ['DcT%d' % hh])
                        P.dve(lambda e, hh=hh: e.tensor_tensor(out=e1[:], in0=e1[:], in1=DcT[:, hh, :], op=ALU.subtract), ['e1', 'DcT%d' % hh], ['e1'])
                        P.dve(lambda e, hh=hh: e.tensor_tensor(out=e1[:], in0=e1[:], in1=mge[:], op=ALU.mult), ['e1', 'mge'], ['e1'])
                        P.dve(lambda e, hh=hh: e.tensor_tensor(out=DcT[:, hh, :], in0=DcT[:, hh, :], in1=e1[:], op=ALU.add), ['e1', 'DcT%d' % hh], ['DcT%d' % hh])
                    q3ring = Ring('Q3', [sb("Q3_%d" % i, [64, 3, N], BF16) for i in range(2)])
                    ktring = Ring('KTh', [sb("KTh%d" % i, [64, N], BF16) for i in range(2)])
                    Sf = sb("Sf", [64, NT + 1, 64], BF16)
                    Sb = sb("Sb", [64, NT + 1, 64], BF16)
                    srun = Ring('srun', [sb("srun%d" % i, [64, 64]) for i in range(2)])
                    ptr = Ring('PTr', [sb("PTr%d" % i, [128, 128], BF16) for i in range(2)])
                    st6 = sb("st6", [128, 6]); mv = sb("mv", [128, 2]); rs = sb("rs", [128, 1])
                    yo = Ring('yo', [sb("yo%d" % i, [128, 64]) for i in range(2)])
                    kvb = Ring('kv', [0, 1]); scb = Ring('sc', [2, 3]); yb = Ring('y', [4, 5])
                    ytiles = list(range(NT)) if not last else list(range(NT_C, NT))
                    for hh in range(4):
                        Q3, q3k = q3ring.next()
                        KTh, ktk = ktring.next()
                        P.dma('sp', Q3[:], RQT[:, hh].rearrange("t d n -> d t n"), [], [q3k], q3k)
                        P.dma('act', KTh[:], RKT[hh], [], [ktk], ktk)
                        vcol = 512 + hh * 64

                        def scan(order_tiles, S, skey, kcol, dcol, run_init_zero):
                            return None
                        run, runk = srun.next()
                        P.pool(lambda e, run=run: e.memset(run[:], 0.0), [], [runk])
                        for i in range(NT):
                            P.pool(lambda e, run=run, i=i: e.tensor_copy(out=Sf[:, i, :], in_=run[:]), [runk], [('Sf', i)])
                            kb, kk = kvb.next()
                            P.pe(lambda e, kb=kb, i=i, hh=hh, vcol=vcol: e.matmul(psum[0:64, kb, 0:64], RT[:, i, hh * 64:(hh + 1) * 64],
                                                                                 RT[:, i, vcol:vcol + 64], start=True, stop=True), ['RT'], [kk])
                            nrun, nrunk = srun.next()
                            P.dve(lambda e, run=run, nrun=nrun, kb=kb, hh=hh: e.scalar_tensor_tensor(out=nrun[:], in0=run[:], scalar=dec[0:64, hh:hh + 1],
                                                                                                     in1=psum[0:64, kb, 0:64], op0=ALU.mult, op1=ALU.add),
                                  [runk, kk, 'dec'], [nrunk])
                            run, runk = nrun, nrunk
                        run, runk = srun.next()
                        P.pool(lambda e, run=run: e.memset(run[:], 0.0), [], [runk])
                        for i in [1, 0] + list(range(NT - 1, NT_C - 1, -1)):
                            P.pool(lambda e, run=run, i=i: e.tensor_copy(out=Sb[:, i, :], in_=run[:]), [runk], [('Sb', i)])
                            kb, kk = kvb.next()
                            P.pe(lambda e, kb=kb, i=i, hh=hh, vcol=vcol: e.matmul(psum[0:64, kb, 0:64], RT[:, i, 256 + hh * 64:256 + (hh + 1) * 64],
                                                                                 RT[:, i, vcol:vcol + 64], start=True, stop=True), ['RT'], [kk])
                            nrun, nrunk = srun.next()
                            P.dve(lambda e, run=run, nrun=nrun, kb=kb, hh=hh: e.scalar_tensor_tensor(out=nrun[:], in0=run[:], scalar=dec[0:64, 4 + hh:5 + hh],
                                                                                                     in1=psum[0:64, kb, 0:64], op0=ALU.mult, op1=ALU.add),
                                  [runk, kk, 'dec'], [nrunk])
                            run, runk = nrun, nrunk
                        r_base = len(P.ops)
                        r_marks = []
                        for i in ytiles:
                            m_a = len(P.ops)
                            sbk, sk = scb.next()
                            P.pe(lambda e, sbk=sbk, i=i, KTh=KTh, Q3=Q3: e.matmul(psum[:, sbk, 0:128], KTh[:, i * 128:(i + 1) * 128], Q3[:, 0, i * 128:(i + 1) * 128],
                                                                                start=True, stop=True), [ktk, q3k], [sk])
                            PTr, pk = ptr.next()
                            P.dve(lambda e, PTr=PTr, sbk=sbk, hh=hh: e.tensor_tensor(out=PTr[:], in0=psum[:, sbk, 0:128], in1=DcT[:, hh, :], op=ALU.mult),
                                  [sk, 'DcT%d' % hh], [pk])
                            ybk, yk = yb.next()
                            P.pe(lambda e, ybk=ybk, PTr=PTr, i=i, vcol=vcol: e.matmul(psum[:, ybk, 0:64], PTr[:], RT[:, i, vcol:vcol + 64], start=True, stop=False),
                                 [pk, 'RT'], [yk])
                            P.pe(lambda e, ybk=ybk, Q3=Q3, i=i: e.matmul(psum[:, ybk, 0:64], Q3[:, 1, i * 128:(i + 1) * 128], Sf[:, i, :], start=False, stop=False),
                                 [q3k, ('Sf', i)], [yk])
                            P.pe(lambda e, ybk=ybk, Q3=Q3, i=i: e.matmul(psum[:, ybk, 0:64], Q3[:, 2, i * 128:(i + 1) * 128], Sb[:, i, :], start=False, stop=True),
                                 [q3k, ('Sb', i)], [yk])
                            m_b = len(P.ops)
                            P.dve(lambda e, ybk=ybk: e.bn_stats(out=st6[:], in_=psum[:, ybk, 0:64]), [yk], ['st6'])
                            P.dve(lambda e: e.bn_aggr(out=mv[:], in_=st6[:]), ['st6'], ['mv'])
                            P.dve(lambda e: e.tensor_scalar_add(out=rs[:], in0=mv[:, 1:2], scalar1=EPS), ['mv'], ['rs'])
                            P.act(lambda e: e.activation(out=rs[:], in_=rs[:], func=AF.Sqrt), ['rs'], ['rs'])
                            P.dve(lambda e: e.reciprocal(out=rs[:], in_=rs[:]), ['rs'], ['rs'])
                            y_, yok = yo.next()
                            P.dve(lambda e, y_=y_, ybk=ybk: e.tensor_scalar(out=y_[:], in0=psum[:, ybk, 0:64], scalar1=mv[:, 0:1], scalar2=rs[:, 0:1],
                                                                           op0=ALU.subtract, op1=ALU.mult), [yk, 'mv', 'rs'], [yok])
                            P.pool(lambda e, y_=y_, hh=hh: e.tensor_tensor(out=y_[:], in0=y_[:], in1=gng[:, hh * 64:(hh + 1) * 64], op=ALU.mult), [yok, 'gng'], [yok])
                            P.pool(lambda e, y_=y_, hh=hh, i=i: e.tensor_tensor(out=y_[:], in0=y_[:], in1=RGt[:, i, hh * 64:(hh + 1) * 64], op=ALU.mult), [yok, 'RGt'], [yok])
                            P.dma('sp', MIX[i * 128:(i + 1) * 128, 256 + hh * 64:256 + (hh + 1) * 64], y_[:], [yok], [('MIXr', i, hh)], yok)
                            r_marks.append((m_a, m_b, len(P.ops)))
                        pipeline_reorder(P, r_base, r_marks)
                    P.flush()

            if 'p3' in phases:
                with contextlib.ExitStack() as st:
                    def sb(name, shape, dt=F32):
                        return st.enter_context(nc.sbuf_tensor(uname(name), list(shape), dt))
                    wout = sb("wout", [128, 8, D], BF16)
                    for hf in range(2):
                        P.dma('pool', wout[:, :, hf * 512:(hf + 1) * 512], w_out[l, :, hf * 512:(hf + 1) * 512].rearrange("(k p) n -> p k n", p=128),
                              [], ['wout%d' % hf], 'wout%d' % hf)
                    CWr = sb("CWr", [128, 256, 3])
                    P.dma('act', CWr[:].rearrange("p c k -> p (c k)"), conv_w[l].rearrange("c k -> (c k)").partition_broadcast(128), [], ['CWr'], 'CWr')
                    CW = sb("CW", [128, 3, 256])
                    for k in range(3):
                        P.dve(lambda e, k=k: e.tensor_copy(out=CW[:, k, :], in_=CWr[:, :, k]), ['CWr'], ['CW%d' % k])
                    g1 = [sb("g1_%d" % s, [128, D]) for s in range(2)]
                    sc2 = [sb("sc2_%d" % s, [128, D]) for s in range(2)]
                    sh2 = [sb("sh2_%d" % s, [128, D]) for s in range(2)]
                    for s in range(2):
                        if last and s == 1:
                            continue
                        bload('sp', g1[s][:], modv[l, s, 2 * D:3 * D], 'g1_%d' % s)
                        bload('sp', sh2[s][:], modv[l, s, 3 * D:4 * D], 'sh2_%d' % s)
                        bload('sp', sc2[s][:], modv[l, s, 4 * D:5 * D], 'sc2_%d' % s)
                    lng = sb("lng", [128, D]); lnb = sb("lnb", [128, D])
                    bload('act', lng[:], ln_g[l, 0], 'lng')
                    bload('act', lnb[:], ln_b[l, 0], 'lnb')
                    wr = sb("wr", [128, 8, NE])
                    P.dma('act', wr[:], w_router[l].rearrange("(k p) e -> p k e", p=128), [], ['wr'], 'wr')
                    brt = sb("brt", [128, NE])
                    bload('act', brt[:], b_router[l], 'brt')
                    pmr = Ring('pm', [sb("pm%d" % i, [128, 3, 256]) for i in range(3)])
                    btr = Ring('bt', [sb("bt%d" % i, [128, 256]) for i in range(3)])
                    mixr = Ring('mix', [sb("mix%d" % i, [128, D]) for i in range(3)])
                    xr = Ring('x3', [sb("x3_%d" % i, [128, D]) for i in range(3)])
                    ca = sb("ca", [128, 256]); cb = sb("cb", [128, 256])
                    mTr = Ring('mT', [sb("mT%d" % i, [128, 8, 128], BF16) for i in range(2)])
                    rr = sb("rr", [128, D])
                    x1r = Ring('x1', [sb("x1_%d" % i, [128, D]) for i in range(2)])
                    h2 = sb("h2", [128, D])
                    h2br = Ring('h2b', [sb("h2b%d" % i, [128, D], BF16) for i in range(2)])
                    ix8 = sb("ix8", [128, 8], U32)
                    h2f = sb("h2f", [128, 8, 128])
                    st6 = sb("st6", [128, 2, 6]); mv = sb("mv", [128, 2]); rs = sb("rs", [128, 1])
                    lgt = sb("lgt", [128, NE]); mx8 = sb("mx8", [128, 8]); msk = sb("msk", [128, NE]); nmx = sb("nmx", [128, 1])
                    ex = sb("ex", [128, NE]); sm = sb("sm", [128, 1])
                    mwr = Ring('mw', [sb("mw%d" % i, [128, NE]) for i in range(2)])
                    ld3 = {}

                    def issue_loads3(i):
                        r0 = prow(i)
                        pm, pmk = pmr.next()
                        for k in range(3):
                            P.dma('sp', pm[:, k, :], PB[r0 - 1 + k:r0 + 127 + k, 0:256], [], [pmk + str(k)], pmk + str(k))
                        bt, btk = btr.next()
                        P.dma('sp', bt[:], PB[r0:r0 + 128, 256:512], [], [btk], btk)
                        mix, mixk = mixr.next()
                        P.dma('sp', mix[:, 256:D], MIX[i * 128:(i + 1) * 128, 256:D], [], [mixk + 'l'], mixk)
                        xt, xk = xr.next()
                        P.dma('sp', xt[:], xsrc(i), [], [xk], xk)
                        ld3[i] = (pm, pmk, bt, btk, mix, mixk, xt, xk)
                    issue_loads3(out_tiles[0])
                    p3_base = len(P.ops)
                    p3_marks = []
                    for oi, i in enumerate(out_tiles):
                        s = 1 if i < NT_C else 0
                        m_a = len(P.ops)
                        if oi + 1 < len(out_tiles):
                            issue_loads3(out_tiles[oi + 1])
                        pm, pmk, bt, btk, mix, mixk, xt, xk = ld3.pop(i)
                        P.dve(lambda e, pm=pm: e.tensor_tensor(out=ca[:], in0=pm[:, 0, :], in1=CW[:, 0, :], op=ALU.mult), [pmk + '0', 'CW0'], ['ca'])
                        P.pool(lambda e, pm=pm: e.tensor_tensor(out=cb[:], in0=pm[:, 1, :], in1=CW[:, 1, :], op=ALU.mult), [pmk + '1', 'CW1'], ['cb'])
                        P.dve(lambda e: e.tensor_tensor(out=ca[:], in0=ca[:], in1=cb[:], op=ALU.add), ['ca', 'cb'], ['ca'])
                        P.pool(lambda e, pm=pm: e.tensor_tensor(out=cb[:], in0=pm[:, 2, :], in1=CW[:, 2, :], op=ALU.mult), [pmk + '2', 'CW2', 'ca'], ['cb'])
                        P.dve(lambda e: e.tensor_tensor(out=ca[:], in0=ca[:], in1=cb[:], op=ALU.add), ['ca', 'cb'], ['ca'])
                        P.dve(lambda e, mix=mix, bt=bt: e.tensor_tensor(out=mix[:, 0:256], in0=ca[:], in1=bt[:], op=ALU.mult), ['ca', btk], [mixk + 'c'])
                        for k in range(8):
                            P.pe(lambda e, mix=mix, k=k: e.transpose(psum[:, k // 4, (k % 4) * 128:(k % 4 + 1) * 128], mix[:, k * 128:(k + 1) * 128], ident[:]),
                                 [mixk + 'l', mixk + 'c', 'ident'], ['pT%d' % k])
                        mT, mTk = mTr.next()
                        for hf in range(2):
                            P.act(lambda e, mT=mT, hf=hf: e.activation(out=mT[:, hf * 4:(hf + 1) * 4, :].rearrange("p k n -> p (k n)"), in_=psum[:, hf, :], func=AF.Copy),
                                  ['pT%d' % k for k in range(hf * 4, hf * 4 + 4)], [mTk + str(hf)])
                        pyb = 2 + 2 * (oi % 2)
                        for nh in range(2):
                            for k in range(8):
                                P.pe(lambda e, mT=mT, nh=nh, k=k, pyb=pyb: e.matmul(psum[:, pyb + nh, :], mT[:, k, :], wout[:, k, nh * 512:(nh + 1) * 512], start=(k == 0), stop=(k == 7)),
                                     [mTk + str(k // 4), 'wout%d' % nh], ['py%d' % (pyb + nh)])
                        m_b = len(P.ops)
                        for nh in range(2):
                            P.dve(lambda e, nh=nh, s=s, pyb=pyb: e.tensor_tensor(out=rr[:, nh * 512:(nh + 1) * 512], in0=psum[:, pyb + nh, :], in1=g1[s][:, nh * 512:(nh + 1) * 512], op=ALU.mult),
                                  ['py%d' % (pyb + nh), 'g1_%d' % s], ['rr%d' % nh])
                        P.dve(lambda e, xt=xt: e.scalar_tensor_tensor(out=rr[:], in0=xt[:], scalar=ALPHA, in1=rr[:], op0=ALU.mult, op1=ALU.add), [xk, 'rr0', 'rr1'], ['rr'])
                        for nh in range(2):
                            P.dve(lambda e, nh=nh: e.bn_stats(out=st6[:, nh, :], in_=rr[:, nh * 512:(nh + 1) * 512]), ['rr'], ['st6_%d' % nh])
                        P.dve(lambda e: e.bn_aggr(out=mv[:], in_=st6[:]), ['st6_0', 'st6_1'], ['mv'])
                        P.dve(lambda e: e.tensor_scalar_add(out=rs[:], in0=mv[:, 1:2], scalar1=EPS), ['mv'], ['rs'])
                        P.act(lambda e: e.activation(out=rs[:], in_=rs[:], func=AF.Sqrt), ['rs'], ['rs'])
                        P.dve(lambda e: e.reciprocal(out=rs[:], in_=rs[:]), ['rs'], ['rs'])
                        x1, x1k = x1r.next()
                        P.dve(lambda e, x1=x1: e.tensor_scalar(out=x1[:], in0=rr[:], scalar1=mv[:, 0:1], scalar2=rs[:, 0:1], op0=ALU.subtract, op1=ALU.mult), ['rr', 'mv', 'rs'], [x1k])
                        P.dve(lambda e, x1=x1: e.tensor_tensor(out=x1[:], in0=x1[:], in1=lng[:], op=ALU.mult), [x1k, 'lng'], [x1k])
                        P.dve(lambda e, x1=x1: e.tensor_tensor(out=x1[:], in0=x1[:], in1=lnb[:], op=ALU.add), [x1k, 'lnb'], [x1k])
                        P.dma('sp', XM[i * 128:(i + 1) * 128, :], x1[:], [x1k], [('XM', i)], x1k)
                        P.dve(lambda e, x1=x1, s=s: e.tensor_tensor(out=h2[:], in0=x1[:], in1=sc2[s][:], op=ALU.mult), [x1k, 'sc2_%d' % s], ['h2'])
                        P.dve(lambda e, s=s: e.tensor_tensor(out=h2[:], in0=h2[:], in1=sh2[s][:], op=ALU.add), ['h2', 'sh2_%d' % s], ['h2'])
                        for k in range(8):
                            P.pe(lambda e, k=k: e.transpose(psum[:, k // 4, (k % 4) * 128:(k % 4 + 1) * 128], h2[:, k * 128:(k + 1) * 128], ident[:]),
                                 ['h2', 'ident'], ['pT%d' % k])
                        for hf in range(2):
                            P.dve(lambda e, hf=hf: e.tensor_copy(out=h2f[:, hf * 4:(hf + 1) * 4, :].rearrange("p k n -> p (k n)"), in_=psum[:, hf, :]),
                                  ['pT%d' % k for k in range(hf * 4, hf * 4 + 4)], ['h2f%d' % hf])
                        h2b, h2bk = h2br.next()
                        P.act(lambda e, h2b=h2b: e.activation(out=h2b[:], in_=h2[:], func=AF.Copy), ['h2'], [h2bk])
                        P.dma('sp', H2R[i * 128:(i + 1) * 128, :], h2b[:], [h2bk], [('H2R', i)], h2bk)
                        for k in range(8):
                            P.pe(lambda e, k=k: e.matmul(psum[:, 6, 0:NE], h2f[:, k, :], wr[:, k, :], start=(k == 0), stop=(k == 7)), ['h2f%d' % (k // 4), 'wr'], ['pl'])
                        P.dve(lambda e: e.tensor_tensor(out=lgt[:], in0=psum[:, 6, 0:NE], in1=brt[:], op=ALU.add), ['pl', 'brt'], ['lgt'])
                        P.dve(lambda e: e.max(out=mx8[:], in_=lgt[:]), ['lgt'], ['mx8'])
                        P.dve(lambda e: e.max_index(out=ix8[:], in_max=mx8[:], in_values=lgt[:]), ['lgt', 'mx8'], ['ix8'])
                        P.dve(lambda e: e.tensor_scalar_mul(out=nmx[:], in0=mx8[:, 0:1], scalar1=-1.0), ['mx8'], ['nmx'])
                        P.act(lambda e: e.activation(out=ex[:, 0:4], in_=mx8[:, 0:4], func=AF.Exp, bias=nmx[:, 0:1], scale=1.0), ['mx8', 'nmx'], ['ex'])
                        P.dve(lambda e: e.reduce_sum(out=sm[:], in_=ex[:, 0:4], axis=AX.X), ['ex'], ['sm'])
                        P.dve(lambda e: e.reciprocal(out=sm[:], in_=sm[:]), ['sm'], ['sm'])
                        mw_, mwk = mwr.next()
                        P.dve(lambda e, mw_=mw_: e.tensor_scalar_mul(out=mw_[:, 0:4], in0=ex[:, 0:4], scalar1=sm[:, 0:1]), ['ex', 'sm'], [mwk + 'w'])
                        P.dve(lambda e, mw_=mw_: e.tensor_copy(out=mw_[:, 4:8], in_=ix8[:, 0:4]), ['ix8'], [mwk + 'e'])
                        P.dma('sp', RW[i * 128:(i + 1) * 128, :], mw_[:, 0:4], [mwk + 'w'], [('RW', i)], mwk + 'w')
                        P.dma('sp', RE[i * 128:(i + 1) * 128, :], mw_[:, 4:8], [mwk + 'e'], [('RE', i)], mwk + 'e')
                        p3_marks.append((m_a, m_b, len(P.ops)))
                    pipeline_reorder(P, p3_base, p3_marks)
                    P.flush()

            if 'moe' in phases:
                tiles = out_tiles
                nt = len(tiles)
                T = nt * 128
                t0 = tiles[0] * 128
                NB = (4 * T + NE * (BS - 1) + BS - 1) // BS
                assert NB <= NBMAX
                w_gu_rows = w_gu.rearrange("l e r c -> (l e r) c")
                w_dn_rows = w_dn.rearrange("l e r c -> (l e r) c")
                with contextlib.ExitStack() as st:
                    def sb(name, shape, dt=F32):
                        return st.enter_context(nc.sbuf_tensor(uname(name), list(shape), dt))
                    DKi = sb("DKi", [128, nt, 4], I32)
                    IDXG = sb("IDXG", [128, NB, 8], I32)
                    IDXD = sb("IDXD", [128, NB, 8], I32)
                    EB = sb("EB", [128, NB])
                    IDXB = sb("IDXB", [128, NB], I32)
                    mwd = sb("mwd", [128, nt, NE])
                    iotap = sb("iotap", [128, 1])
                    P.dma('sp', iotap[:], iotap_in, [], ['iotap'], 'iotap')
                    with contextlib.ExitStack() as st2:
                        def sb2(name, shape, dt=F32):
                            return st2.enter_context(nc.sbuf_tensor(uname(name), list(shape), dt))
                        EF = sb2("EF", [128, nt, 4])
                        W4 = sb2("W4", [128, nt, 4])
                        P.dma('sp', EF[:], RE[t0:t0 + T, :].rearrange("(c p) k -> p c k", p=128), [], ['EF'], 'EF')
                        P.dma('sp', W4[:], RW[t0:t0 + T, :].rearrange("(c p) k -> p c k", p=128), [], ['W4'], 'W4')
                        iota32 = sb2("iota32", [128, NE])
                        P.dma('act', iota32[:], iota32_in, [], ['iota32'], 'iota32')
                        lts = sb2("lts", [128, 128])
                        P.dma('act', lts[:], lts_in, [], ['lts'], 'lts')
                        ones = sb2("ones", [128, 128])
                        P.pool(lambda e: e.memset(ones[:], 1.0), [], ['ones'])
                        bst = sb2("bst", [128, NBMAX])
                        P.dma('act', bst[:], bstart_in, [], ['bst'], 'bst')
                        kp = sb2("kp", [128, 8])
                        P.dma('act', kp[:], kp_in, [], ['kp'], 'kp')
                        OH = sb2("OH", [128, 4, nt, NE])
                        for k in range(4):
                            P.dve(lambda e, k=k: e.tensor_tensor(out=OH[:, k, :, :], in0=iota32[:].unsqueeze(1).to_broadcast([128, nt, NE]),
                                                                 in1=EF[:, :, k].unsqueeze(2).to_broadcast([128, nt, NE]), op=ALU.is_equal),
                                  ['iota32', 'EF'], ['OH%d' % k])
                        mask = sb2("mask", [128, nt, NE])
                        P.dve(lambda e: e.tensor_tensor(out=mask[:], in0=OH[:, 0, :, :], in1=OH[:, 1, :, :], op=ALU.add), ['OH0', 'OH1'], ['mask'])
                        P.dve(lambda e: e.tensor_tensor(out=mask[:], in0=mask[:], in1=OH[:, 2, :, :], op=ALU.add), ['mask', 'OH2'], ['mask'])
                        P.dve(lambda e: e.tensor_tensor(out=mask[:], in0=mask[:], in1=OH[:, 3, :, :], op=ALU.add), ['mask', 'OH3'], ['mask'])
                        for j in range(nt):
                            bk = j // 16
                            col = (j % 16) * NE
                            P.pe(lambda e, j=j, bk=bk, col=col: e.matmul(psum[:, bk, col:col + NE], lts[:], mask[:, j, :], start=True, stop=True, skip_group_check=True),
                                 ['lts', 'mask'], ['rk%d' % bk])
                            P.pe(lambda e, j=j, bk=bk, col=col: e.matmul(psum[:, 4 + bk, col:col + NE], ones[:], mask[:, j, :], start=True, stop=True, skip_group_check=True),
                                 ['ones', 'mask'], ['tt%d' % bk])
                        for m in range(nt):
                            P.pe(lambda e, m=m: e.matmul(psum[:, 3, 0:NE], ones[:], mask[:, m, :], start=(m == 0), stop=(m == nt - 1)), ['ones', 'mask'], ['cnt'])
                        TOT = sb2("TOT", [128, nt, NE])
                        PRE = sb2("PRE", [128, nt, NE])
                        for bk in range((nt + 15) // 16):
                            j0 = bk * 16
                            nj = min(16, nt - j0)
                            P.act(lambda e, bk=bk, j0=j0, nj=nj: e.activation(out=TOT[:, j0:j0 + nj, :].rearrange("p j e -> p (j e)"), in_=psum[:, 4 + bk, 0:nj * NE], func=AF.Copy),
                                  ['tt%d' % bk], ['TOT%d' % bk])
                        P.pool(lambda e: e.memset(PRE[:, 0, :], 0.0), [], ['PRE'])
                        for j in range(1, nt):
                            P.dve(lambda e, j=j: e.tensor_tensor(out=PRE[:, j, :], in0=PRE[:, j - 1, :], in1=TOT[:, j - 1, :], op=ALU.add),
                                  ['PRE'] + ['TOT%d' % bk for bk in range((nt + 15) // 16)], ['PRE'])
                        c0 = sb2("c0", [128, NE]); c1 = sb2("c1", [128, NE]); padded = sb2("padded", [128, NE])
                        P.dve(lambda e: e.tensor_scalar_add(out=c0[:], in0=psum[:, 3, 0:NE], scalar1=float(BS - 1)), ['cnt'], ['c0'])
                        ci32 = sb2("ci32", [128, NE], I32)
                        P.dve(lambda e: e.tensor_scalar(out=c1[:], in0=c0[:], scalar1=1.0 / BS, scalar2=-0.5 + 0.5 / BS, op0=ALU.mult, op1=ALU.add), ['c0'], ['c1'])
                        P.dve(lambda e: e.tensor_copy(out=ci32[:], in_=c1[:]), ['c1'], ['ci32'])
                        P.dve(lambda e: e.tensor_copy(out=c1[:], in_=ci32[:]), ['ci32'], ['c1'])
                        P.dve(lambda e: e.tensor_scalar_mul(out=padded[:], in0=c1[:], scalar1=float(BS)), ['c1'], ['padded'])
                        P.dve(lambda e: e.tensor_copy(out=c0[:], in_=padded[:]), ['padded'], ['c0'])
                        cur, nxt_, ck, nk = c0, c1, 'c0', 'c1'
                        for sft in (1, 2, 4, 8, 16):
                            P.dve(lambda e, cur=cur, nxt_=nxt_, sft=sft: e.tensor_copy(out=nxt_[:, 0:sft], in_=cur[:, 0:sft]), [ck], [nk + 'a'])
                            P.dve(lambda e, cur=cur, nxt_=nxt_, sft=sft: e.tensor_tensor(out=nxt_[:, sft:NE], in0=cur[:, sft:NE], in1=cur[:, 0:NE - sft], op=ALU.add), [ck], [nk + 'b'])
                            P.dve(lambda e: e.engine_nop(), [nk + 'a', nk + 'b'], [nk])
                            cur, nxt_, ck, nk = nxt_, cur, nk, ck
                        pend, pendk = cur, ck
                        pstart = sb2("pstart", [128, NE])
                        P.dve(lambda e: e.tensor_tensor(out=pstart[:], in0=pend[:], in1=padded[:], op=ALU.subtract), [pendk, 'padded'], ['pstart'])
                        dfull = sb2("dfull", [128, nt, NE])
                        for bk in range((nt + 15) // 16):
                            j0 = bk * 16
                            nj = min(16, nt - j0)
                            P.dve(lambda e, bk=bk, j0=j0, nj=nj: e.tensor_tensor(out=dfull[:, j0:j0 + nj, :], in0=psum[:, bk, 0:nj * NE].rearrange("p (j e) -> p j e", e=NE),
                                                                                 in1=pstart[:].unsqueeze(1).to_broadcast([128, nj, NE]), op=ALU.add),
                                  ['rk%d' % bk, 'pstart'], ['dfull%d' % bk])
                            P.dve(lambda e, j0=j0, nj=nj: e.tensor_tensor(out=dfull[:, j0:j0 + nj, :], in0=dfull[:, j0:j0 + nj, :], in1=PRE[:, j0:j0 + nj, :], op=ALU.add),
                                  ['dfull%d' % bk, 'PRE'], ['dfull%d' % bk])
                        dkeys = ['dfull%d' % bk for bk in range((nt + 15) // 16)]
                        tmpd = sb2("tmpd", [128, nt, NE])
                        DKf = sb2("DKf", [128, nt, 4])
                        for k in range(4):
                            P.dve(lambda e, k=k: e.tensor_tensor(out=tmpd[:], in0=dfull[:], in1=OH[:, k, :, :], op=ALU.mult), dkeys + ['OH%d' % k], ['tmpd'])
                            P.dve(lambda e, k=k: e.tensor_reduce(out=DKf[:, :, k], in_=tmpd[:], axis=AX.X, op=ALU.add), ['tmpd'], ['DKf%d' % k])
                        P.dve(lambda e: e.tensor_copy(out=DKi[:], in_=DKf[:]), ['DKf%d' % k for k in range(4)], ['DKi'])
                        cmp_ = sb2("cmp", [128, NB, NE])
                        P.dve(lambda e: e.tensor_tensor(out=cmp_[:], in0=pend[:].unsqueeze(1).to_broadcast([128, NB, NE]),
                                                        in1=bst[:, 0:NB].unsqueeze(2).to_broadcast([128, NB, NE]), op=ALU.is_le), [pendk, 'bst'], ['cmp'])
                        P.dve(lambda e: e.tensor_reduce(out=EB[:], in_=cmp_[:], axis=AX.X, op=ALU.add), ['cmp'], ['EB'])
                        P.dve(lambda e: e.tensor_scalar_min(out=EB[:], in0=EB[:], scalar1=float(NE - 1)), ['EB'], ['EB'])
                        idxf = sb2("idxf", [128, NB, 8])
                        P.dve(lambda e: e.scalar_tensor_tensor(out=idxf[:], in0=EB[:].unsqueeze(2).to_broadcast([128, NB, 8]), scalar=float(D),
                                                               in1=kp[:].unsqueeze(1).to_broadcast([128, NB, 8]), op0=ALU.mult, op1=ALU.add), ['EB', 'kp'], ['idxf'])
                        P.dve(lambda e: e.tensor_scalar_add(out=idxf[:], in0=idxf[:], scalar1=float(l * NE * D)), ['idxf'], ['idxf'])
                        P.dve(lambda e: e.tensor_copy(out=IDXD[:], in_=idxf[:]), ['idxf'], ['IDXD'])
                        unused = sb2("unused", [128, NB])
                        P.dve(lambda e: e.tensor_scalar(out=unused[:], in0=bst[:, 0:NB], scalar1=pend[:, NE - 1:NE], scalar2=1.0e6, op0=ALU.is_ge, op1=ALU.mult), ['bst', pendk], ['unused'])
                        P.dve(lambda e: e.tensor_tensor(out=idxf[:], in0=idxf[:], in1=unused[:].unsqueeze(2).to_broadcast([128, NB, 8]), op=ALU.add), ['idxf', 'unused'], ['idxf'])
                        P.dve(lambda e: e.tensor_copy(out=IDXG[:], in_=idxf[:]), ['idxf'], ['IDXG'])
                        idxbf = sb2("idxbf", [128, NB])
                        P.dve(lambda e: e.tensor_scalar(out=idxbf[:], in0=EB[:], scalar1=128.0, scalar2=float(l * NE * 128), op0=ALU.mult, op1=ALU.add), ['EB'], ['idxbf'])
                        P.dve(lambda e: e.tensor_scalar(out=idxbf[:], in0=idxbf[:], scalar1=iotap[:, 0:1], scalar2=None, op0=ALU.add), ['idxbf', 'iotap'], ['idxbf'])
                        P.dve(lambda e: e.tensor_copy(out=IDXB[:], in_=idxbf[:]), ['idxbf'], ['IDXB'])
                        for k in range(4):
                            dst_ = mwd if k == 0 else tmpd
                            P.dve(lambda e, k=k, dst_=dst_: e.tensor_tensor(out=dst_[:], in0=OH[:, k, :, :], in1=W4[:, :, k].unsqueeze(2).to_broadcast([128, nt, NE]), op=ALU.mult),
                                  ['OH%d' % k, 'W4', 'DKf0', 'DKf1', 'DKf2', 'DKf3'], ['mwd' if k == 0 else 'tmpd'])
                            if k > 0:
                                P.dve(lambda e: e.tensor_tensor(out=mwd[:], in0=mwd[:], in1=tmpd[:], op=ALU.add), ['mwd', 'tmpd'], ['mwd'])
                        hr = Ring('hr', [sb2("hr%d" % i, [128, D], BF16) for i in range(3)])
                        for j, i in enumerate(tiles):
                            h_, hk_ = hr.next()
                            P.dma('sp', h_[:], H2R[i * 128:(i + 1) * 128, :], [], [hk_], hk_)
                            for k in range(4):
                                P.add('pool', lambda e, h_=h_, j=j, k=k: e.indirect_dma_start(out=XS[:, :], out_offset=bass.IndirectOffsetOnAxis(ap=DKi[:, j, k:k + 1], axis=0),
                                                                                          in_=h_[:, :], in_offset=None), [hk_, 'DKi'], [('XS', j, k)], 'xsc%d' % ((j * 4 + k) % 4))
                                P.add('pool', lambda e, j=j, k=k: e.indirect_dma_start(out=SW[:, :], out_offset=bass.IndirectOffsetOnAxis(ap=DKi[:, j, k:k + 1], axis=0),
                                                                                    in_=W4[:, j, k:k + 1], in_offset=None), ['W4', 'DKi'], [('SW', j, k)], 'swc%d' % ((j * 4 + k) % 4))
                        P.flush()
                    with contextlib.ExitStack() as st2:
                        def sb2(name, shape, dt=F32):
                            return st2.enter_context(nc.sbuf_tensor(uname(name), list(shape), dt))
                        identb = sb2("identb", [128, 128], BF16)
                        P.act(lambda e: e.activation(out=identb[:], in_=ident[:], func=AF.Copy), ['ident'], ['identb'])
                        bgr = sb2("bgr", [NE, 2 * D])
                        P.dma('act', bgr[:], b_gu[l], [], ['bgr'], 'bgr')
                        for c in range(16):
                            P.pe(lambda e, c=c: e.transpose(psum[:, 6, c * NE:(c + 1) * NE], bgr[:, c * 128:(c + 1) * 128], ident[0:NE, 0:NE]), ['bgr', 'ident'], ['pX0'])
                        X2 = sb2("X2", [128, NE, 16])
                        P.dve(lambda e: e.tensor_copy(out=X2[:], in_=psum[:, 6, :].rearrange("p (c e) -> p e c", e=NE)), ['pX0'], ['X2'])
                        P.dve(lambda e: e.tensor_scalar_add(out=X2[:, :, 8:16], in0=X2[:, :, 8:16], scalar1=1.0), ['X2'], ['X2'])
                        P.dma('sp', BGT[l * NE * 128:(l + 1) * NE * 128, :].rearrange("(e p) c -> p e c", p=128), X2[:], ['X2'], ['BGT'], 'BGT')
                        bbr = Ring('bb', [sb2("bb%d" % i, [128, 16]) for i in range(2)])
                        wgr = Ring('WG', [sb2("WG%d" % i, [128, 8, 2 * D], BF16) for i in range(2)])
                        wdr = Ring('WD', [sb2("WD%d" % i, [128, 8, D], BF16) for i in range(2)])
                        xbr = Ring('XB', [sb2("XB%d" % i, [128, 4, D], BF16) for i in range(2)])
                        XTr = [sb2("XT%d" % i, [128, 8, BS], BF16) for i in range(2)]
                        actr = Ring('act', [sb2("act%d" % i, [128, 8, BS], BF16) for i in range(2)])
                        Ar = Ring('A', [sb2("A%d" % i, [128, BS]) for i in range(2)])
                        Sr = Ring('Sg', [sb2("Sg%d" % i, [128, BS]) for i in range(2)])
                        Ur = Ring('U', [sb2("U%d" % i, [128, BS]) for i in range(2)])
                        swr = Ring('swb', [sb2("swb%d" % i, [128, 4]) for i in range(2)])
                        Yr = Ring('Y', [sb2("Y%d" % i, [128, 4, D]) for i in range(1)])
                        gbr = Ring('pg', [0, 1]); ubr = Ring('pu', [2, 3]); ybr = Ring('py', [4, 5])
                        psb = [psum[:, 6, :].bitcast(BF16), psum[:, 7, :].bitcast(BF16)]

                        def load_blk(b):
                            WG, wgk = wgr.next()
                            WD, wdk = wdr.next()
                            for k in range(8):
                                P.add('pool', lambda e, WG=WG, b=b, k=k: e.indirect_dma_start(out=WG[:, k, :], out_offset=None, in_=w_gu_rows[:, :],
                                                                                             in_offset=bass.IndirectOffsetOnAxis(ap=IDXD[:, b, k:k + 1], axis=0)),
                                      ['IDXG'], [wgk + str(k)], wgk + str(k))
                            for k in range(8):
                                P.add('pool', lambda e, WD=WD, b=b, k=k: e.indirect_dma_start(out=WD[:, k, :], out_offset=None, in_=w_dn_rows[:, :],
                                                                                             in_offset=bass.IndirectOffsetOnAxis(ap=IDXD[:, b, k:k + 1], axis=0)),
                                      ['IDXD'], [wdk + str(k)], wdk + str(k))
                            XB, xbk = xbr.next()
                            P.dma('sp', XB[:], XS[b * BS:(b + 1) * BS, :].rearrange("(s p) d -> p s d", p=128), [], [xbk], xbk)
                            swb, swk = swr.next()
                            P.dma('act', swb[:], SW[b * BS:(b + 1) * BS, :].rearrange("(s p) o -> p (s o)", p=128), [], [swk], swk, allow_slow_non_contiguous=True)
                            bb, bbk = bbr.next()
                            P.add('pool', lambda e, bb=bb, b=b: e.indirect_dma_start(out=bb[:, :], out_offset=None, in_=BGT[:, :],
                                                                                   in_offset=bass.IndirectOffsetOnAxis(ap=IDXB[:, b:b + 1], axis=0)),
                                  ['IDXB', 'BGT'], [bbk], bbk)
                            return WG, wgk, WD, wdk, XB, xbk, swb, swk, bb, bbk
                        def transposes(b, XB, xbk):
                            XT = XTr[b % 2]
                            for half in range(2):
                                for s2 in range(2):
                                    sidx = half * 2 + s2
                                    for k in range(8):
                                        P.pe(lambda e, XB=XB, sidx=sidx, k=k, s2=s2: e.transpose(psb[s2][:, k * 128:(k + 1) * 128], XB[:, sidx, k * 128:(k + 1) * 128], identb[:]),
                                             [xbk, 'identb'], ['pX%d' % s2])
                                    P.act(lambda e, sidx=sidx, s2=s2, XT=XT: e.activation(out=XT[:, :, sidx * 128:(sidx + 1) * 128], in_=psb[s2].rearrange("p (k n) -> p k n", n=128), func=AF.Copy),
                                          ['pX%d' % s2], ['XT%d_%d' % (b % 2, sidx)])
                        nxt = load_blk(0)
                        transposes(0, nxt[4], nxt[5])
                        for b in range(NB):
                            WG, wgk, WD, wdk, XB, xbk, swb, swk, bb, bbk = nxt
                            if b + 1 < NB:
                                nxt = load_blk(b + 1)
                            wgkeys = [wgk + str(k) for k in range(8)]
                            wdkeys = [wdk + str(k) for k in range(8)]
                            XT = XTr[b % 2]
                            xtkeys = ['XT%d_%d' % (b % 2, q) for q in range(4)]
                            at, atk = actr.next()
                            for c in range(8):
                                gb, gbk = gbr.next()
                                ub, ubk = ubr.next()
                                for k in range(8):
                                    P.pe(lambda e, gb=gb, WG=WG, k=k, c=c, XT=XT: e.matmul(psum[:, gb, :], WG[:, k, c * 128:(c + 1) * 128], XT[:, k, :], start=(k == 0), stop=(k == 7)),
                                         wgkeys + xtkeys, [gbk])
                                for k in range(8):
                                    P.pe(lambda e, ub=ub, WG=WG, k=k, c=c, XT=XT: e.matmul(psum[:, ub, :], WG[:, k, D + c * 128:D + (c + 1) * 128], XT[:, k, :], start=(k == 0), stop=(k == 7)),
                                         wgkeys + xtkeys, [ubk])
                                A, Ak = Ar.next(); S_, Sk = Sr.next(); U, Uk = Ur.next()
                                P.dve(lambda e, A=A, gb=gb, bb=bb, c=c: e.tensor_scalar(out=A[:], in0=psum[:, gb, :], scalar1=bb[:, c:c + 1], scalar2=7.0, op0=ALU.add, op1=ALU.min), [gbk, bbk], [Ak])
                                P.act(lambda e, A=A, S_=S_: e.activation(out=S_[:], in_=A[:], func=AF.Sigmoid, scale=1.702), [Ak], [Sk])
                                P.dve(lambda e, U=U, ub=ub, bb=bb, c=c: e.tensor_scalar(out=U[:], in0=psum[:, ub, :], scalar1=bb[:, 8 + c:9 + c], scalar2=8.0, op0=ALU.add, op1=ALU.min), [ubk, bbk], [Uk])
                                P.pool(lambda e, A=A, S_=S_: e.tensor_tensor(out=A[:], in0=A[:], in1=S_[:], op=ALU.mult), [Ak, Sk], [Ak])
                                P.dve(lambda e, at=at, c=c, U=U, A=A: e.scalar_tensor_tensor(out=at[:, c, :], in0=U[:], scalar=-6.0, in1=A[:], op0=ALU.max, op1=ALU.mult), [Uk, Ak], [atk + str(c)])
                            atkeys = [atk + str(c) for c in range(8)]
                            if b + 1 < NB:
                                transposes(b + 1, nxt[4], nxt[5])
                            Y, Yk = Yr.next()
                            for s4 in range(4):
                                for nh in range(2):
                                    yb_, ybk = ybr.next()
                                    for c in range(8):
                                        P.pe(lambda e, yb_=yb_, at=at, c=c, s4=s4, WD=WD, nh=nh: e.matmul(psum[:, yb_, :], at[:, c, s4 * 128:(s4 + 1) * 128], WD[:, c, nh * 512:(nh + 1) * 512],
                                                                                                          start=(c == 0), stop=(c == 7)), atkeys + wdkeys, [ybk])
                                    if yb_ == 4:
                                        P.dve(lambda e, Y=Y, s4=s4, nh=nh, swb=swb: e.tensor_scalar_mul(out=Y[:, s4, nh * 512:(nh + 1) * 512], in0=psum[:, 4, :], scalar1=swb[:, s4:s4 + 1]),
                                              [ybk, swk], [Yk + '%d%d' % (s4, nh)])
                                    else:
                                        P.act(lambda e, Y=Y, s4=s4, nh=nh, swb=swb: e.activation(out=Y[:, s4, nh * 512:(nh + 1) * 512], in_=psum[:, 5, :], func=AF.Copy, scale=swb[:, s4:s4 + 1]),
                                              [ybk, swk], [Yk + '%d%d' % (s4, nh)])
                            P.dma('sp', YS[b * BS:(b + 1) * BS, :].rearrange("(s p) d -> p s d", p=128), Y[:], [Yk + '%d%d' % (q, r_) for q in range(4) for r_ in range(2)], [('YS', b)], Yk)
                        P.flush()
                    with contextlib.ExitStack() as st3:
                        def sb3(name, shape, dt=F32):
                            return st3.enter_context(nc.sbuf_tensor(uname(name), list(shape), dt))
                        g2 = [sb3("g2_%d" % s_, [128, D]) for s_ in range(2)]
                        for s_ in range(2):
                            bload('sp', g2[s_][:], modv[l, s_, 5 * D:6 * D], 'g2_%d' % s_)
                        lng = sb3("lng2", [128, D]); lnb = sb3("lnb2", [128, D])
                        bload('act', lng[:], ln_g[l, 1], 'lng')
                        bload('act', lnb[:], ln_b[l, 1], 'lnb')
                        x1r = Ring('xm', [sb3("xm%d" % i, [128, D]) for i in range(4)])
                        Gr = Ring('G', [sb3("G%d" % i, [128, 4, D]) for i in range(4)])
                        rr = sb3("rr2", [128, D])
                        xor_ = Ring('xo', [sb3("xo%d" % i, [128, D]) for i in range(2)])
                        st6 = sb3("st6b", [128, 2, 6]); mv = sb3("mvb", [128, 2]); rs = sb3("rsb", [128, 1])
                        bdr = sb3("bdr", [NE, D])
                        P.dma('act', bdr[:], b_dn[l], [], ['bdr'], 'bdr')
                        mwTr = Ring('mwT', [sb3("mwT%d" % i, [NE, 128]) for i in range(2)])
                        tbr = Ring('tb', [0, 1]); bbk2 = Ring('bd', [(2, 3), (4, 5)])
                        for j, i in enumerate(tiles):
                            s_ = 1 if i < NT_C else 0
                            tb_, tbk = tbr.next()
                            P.pe(lambda e, j=j, tb_=tb_: e.transpose(psum[0:NE, tb_, 0:128], mwd[:, j, :], ident[:]), ['ident'], [tbk])
                            mwT, mwTk = mwTr.next()
                            P.act(lambda e, mwT=mwT, tb_=tb_: e.activation(out=mwT[:], in_=psum[0:NE, tb_, 0:128], func=AF.Copy), [tbk], [mwTk])
                            (bd0, bd1), bdk = bbk2.next()
                            for nh, bdb_ in enumerate((bd0, bd1)):
                                P.pe(lambda e, mwT=mwT, nh=nh, bdb_=bdb_: e.matmul(psum[:, bdb_, :], mwT[:], bdr[:, nh * 512:(nh + 1) * 512], start=True, stop=True), [mwTk, 'bdr'], [bdk + str(nh)])
                            x1, x1k = x1r.next()
                            P.dma('sp', x1[:], XM[i * 128:(i + 1) * 128, :], [], [x1k], x1k)
                            G, Gk = Gr.next()
                            for k in range(4):
                                P.add('pool', lambda e, G=G, j=j, k=k: e.indirect_dma_start(out=G[:, k, :], out_offset=None, in_=YS[:, :],
                                                                                         in_offset=bass.IndirectOffsetOnAxis(ap=DKi[:, j, k:k + 1], axis=0)), ['DKi'], [Gk + str(k)], Gk + str(k))
                            P.dve(lambda e, G=G: e.tensor_tensor(out=G[:, 0, :], in0=G[:, 0, :], in1=G[:, 1, :], op=ALU.add), [Gk + '0', Gk + '1'], [Gk + '0'])
                            P.dve(lambda e, G=G: e.tensor_tensor(out=G[:, 2, :], in0=G[:, 2, :], in1=G[:, 3, :], op=ALU.add), [Gk + '2', Gk + '3'], [Gk + '2'])
                            P.dve(lambda e, G=G: e.tensor_tensor(out=G[:, 0, :], in0=G[:, 0, :], in1=G[:, 2, :], op=ALU.add), [Gk + '0', Gk + '2'], [Gk + '0'])
                            for nh, bdb_ in enumerate((bd0, bd1)):
                                P.dve(lambda e, G=G, nh=nh, bdb_=bdb_: e.tensor_tensor(out=G[:, 0, nh * 512:(nh + 1) * 512], in0=G[:, 0, nh * 512:(nh + 1) * 512], in1=psum[:, bdb_, :], op=ALU.add),
                                      [Gk + '0', bdk + str(nh)], [Gk + '0'])
                            P.dve(lambda e, G=G, s_=s_: e.tensor_tensor(out=rr[:], in0=G[:, 0, :], in1=g2[s_][:], op=ALU.mult), [Gk + '0', 'g2_%d' % s_], ['rr'])
                            P.dve(lambda e, x1=x1: e.scalar_tensor_tensor(out=rr[:], in0=x1[:], scalar=ALPHA, in1=rr[:], op0=ALU.mult, op1=ALU.add), [x1k, 'rr'], ['rr'])
                            for nh in range(2):
                                P.dve(lambda e, nh=nh: e.bn_stats(out=st6[:, nh, :], in_=rr[:, nh * 512:(nh + 1) * 512]), ['rr'], ['st6_%d' % nh])
                            P.dve(lambda e: e.bn_aggr(out=mv[:], in_=st6[:]), ['st6_0', 'st6_1'], ['mv'])
                            P.dve(lambda e: e.tensor_scalar_add(out=rs[:], in0=mv[:, 1:2], scalar1=EPS), ['mv'], ['rs'])
                            P.act(lambda e: e.activation(out=rs[:], in_=rs[:], func=AF.Sqrt), ['rs'], ['rs'])
                            P.dve(lambda e: e.reciprocal(out=rs[:], in_=rs[:]), ['rs'], ['rs'])
                            xo, xok = xor_.next()
                            P.dve(lambda e, xo=xo: e.tensor_scalar(out=xo[:], in0=rr[:], scalar1=mv[:, 0:1], scalar2=rs[:, 0:1], op0=ALU.subtract, op1=ALU.mult), ['rr', 'mv', 'rs'], [xok])
                            P.dve(lambda e, xo=xo: e.tensor_tensor(out=xo[:], in0=xo[:], in1=lng[:], op=ALU.mult), [xok, 'lng'], [xok])
                            P.dve(lambda e, xo=xo: e.tensor_tensor(out=xo[:], in0=xo[:], in1=lnb[:], op=ALU.add), [xok, 'lnb'], [xok])
                            dst = XS0[i * 128:(i + 1) * 128, :] if not last else out[(i - NT_C) * 128:(i - NT_C + 1) * 128, :]
                            P.dma('sp', dst, xo[:], [xok], [('xout', i)], xok)
                        P.flush()
        P.flush()
    return nc


def make_consts():
    inv = (10000.0 ** (-np.arange(0, 32, 2, dtype=np.float32) / 32.0)).astype(np.float32)
    t = np.arange(L)
    row = (t // 64).astype(np.float32)
    col = (t % 64).astype(np.float32)
    ang = np.stack([row[:, None] * inv, col[:, None] * inv], axis=1)
    cos = np.cos(ang).astype(np.float32)
    sin = np.sin(ang).astype(np.float32)
    c64 = np.stack([cos, cos], axis=2).reshape(L, 64)
    s64 = np.stack([-sin, sin], axis=2).reshape(L, 64)
    p = np.arange(128, dtype=np.float32)
    pos = np.stack([127 - p, p, p + 1, 128 - p], axis=1)
    dif = p[None, :] - p[:, None]
    return dict(
        c_ident=np.eye(128, dtype=np.float32),
        c_cos=np.ascontiguousarray(np.tile(c64, (1, 8))),
        c_sin=np.ascontiguousarray(np.tile(s64, (1, 8))),
        c_pos=np.ascontiguousarray(pos.astype(np.float32)),
        c_dpos=np.maximum(dif, 0).astype(np.float32),
        c_dneg=np.maximum(-dif, 0).astype(np.float32),
        c_mge=(dif >= 0).astype(np.float32),
        c_zeros=np.zeros((2, 256), np.float32),
        c_iota32=np.tile(np.arange(NE, dtype=np.float32)[None, :], (128, 1)),
        c_lts=(p[:, None] < p[None, :]).astype(np.float32),
        c_bstart=np.tile((np.arange(NBMAX, dtype=np.float32) * BS)[None, :], (128, 1)),
        c_kp=(np.arange(8, dtype=np.float32)[None, :] * 128 + p[:, None]).astype(np.float32),
        c_iotap=p[:, None].astype(np.float32).copy(),
    )


WKEYS = ['w_mod', 'b_mod', 'w_in', 'conv_w', 'ret_decay_exp', 'ret_gn_g', 'q_norm_g', 'k_norm_g', 'w_out',
         'ln_g', 'ln_b', 'w_router', 'b_router', 'w_gate_up', 'b_gate_up', 'w_down', 'b_down']


def make_in_maps(inputs, cores, skip=()):
    consts = make_consts()
    shared = {k: np.ascontiguousarray(np.asarray(inputs[k], np.float32)) for k in WKEYS if k not in skip}
    shared['c_ctx'] = np.ascontiguousarray(np.asarray(inputs['c_ctx'], np.float32))
    shared.update(consts)
    maps = []
    for b in cores:
        m = dict(shared)
        m['x'] = np.ascontiguousarray(np.asarray(inputs['x'][b], np.float32))
        m['c'] = np.ascontiguousarray(np.asarray(inputs['c'][b], np.float32))
        m['ctx'] = np.ascontiguousarray(np.asarray(inputs['ctx'][b], np.float32))
        maps.append(m)
    return maps


def kernel(**inputs):
    nc = build()
    maps = make_in_maps(inputs, list(range(8)))
    res = run_bass_kernel_spmd(nc, maps, core_ids=list(range(8)))
    return np.stack([np.asarray(r["out"], np.float32) for r in res.results], axis=0)
```

```python
import contextlib
import math
import numpy as np
import concourse.bass as bass
import concourse.mybir as mybir
from concourse.bass_utils import run_bass_kernel_spmd

F32 = mybir.dt.float32
BF16 = mybir.dt.bfloat16
ALU = mybir.AluOpType
AF = mybir.ActivationFunctionType
AX = mybir.AxisListType

D = 1024
L = 4096
LC = 256
NT_C = 2
NT = 34
DEPTH = 2
NE = 32
BS = 512
NBMAX = 66
I32 = mybir.dt.int32
U32 = mybir.dt.uint32
ALPHA = (2.0 * DEPTH) ** 0.25
EPS = 1e-6


class Prog:
    ENGS = ('pe', 'act', 'dve', 'pool', 'sp')

    def __init__(self, nc, stack):
        self.nc = nc
        self.ops = []
        self.sems = {}
        self.stack = stack
        for eng in ('pe', 'act', 'dve', 'pool'):
            self.sems[('e', eng)] = stack.enter_context(nc.semaphore('sem_' + eng))
        self.cnt = {}
        self.streams = {}
        self.pool_cnt = []
        self.pool_idx = {}
        self.waited = {e: {} for e in self.ENGS}
        self.total_ops = 0

    def add(self, eng, fn, reads=(), writes=(), stream=None):
        self.ops.append((eng, fn, tuple(reads), tuple(writes), stream))

    def pe(self, fn, reads=(), writes=()):
        self.add('pe', fn, reads, writes)

    def act(self, fn, reads=(), writes=()):
        self.add('act', fn, reads, writes)

    def dve(self, fn, reads=(), writes=()):
        self.add('dve', fn, reads, writes)

    def pool(self, fn, reads=(), writes=()):
        self.add('pool', fn, reads, writes)

    def dma(self, q, out, in_, reads, writes, stream, **kw):
        self.add(q, lambda e: e.dma_start(out=out, in_=in_, **kw), reads, writes, stream)

    def flush(self):
        nc = self.nc
        ops = self.ops
        self.ops = []
        n = len(ops)
        if n == 0:
            return
        self.total_ops += n
        last_writer = {}
        readers = {}
        deps = [None] * n
        for i, (eng, fn, rd, wr, st) in enumerate(ops):
            d = set()
            for r in rd:
                j = last_writer.get(r)
                if j is not None:
                    d.add((j, 0))
            for w in wr:
                j = last_writer.get(w)
                if j is not None:
                    d.add((j, 1))
                for k in readers.get(w, ()):
                    if k != i:
                        d.add((k, 2))
            deps[i] = d
            for r in rd:
                readers.setdefault(r, []).append(i)
            for w in wr:
                last_writer[w] = i
                readers[w] = []
        sig = [False] * n
        need = [None] * n
        last_compute = {}
        for i in range(n):
            eng = ops[i][0]
            lst = set()
            for (j, kind) in deps[i]:
                jeng, _, _, _, jst = ops[j]
                if jst is None and jeng == eng:
                    if eng == 'pe' or eng == 'sp':
                        continue
                    if kind == 2:
                        continue
                if jst is None:
                    sig[j] = True
                lst.add(j)
            need[i] = lst
            if ops[i][4] is None and ops[i][1] is not None:
                last_compute[eng] = i
        for eng, i in last_compute.items():
            if eng != 'sp':
                sig[i] = True
        sval = [None] * n
        phase_map = {}
        for i, (eng, fn, rd, wr, st) in enumerate(ops):
            if st is not None:
                kind = 'w' if eng == 'pool' else 'h'
                if (kind, st) not in phase_map:
                    nk = sum(1 for kk in phase_map if kk[0] == kind)
                    k = kind + str(nk)
                    phase_map[(kind, st)] = k
                    if k not in self.pool_idx:
                        self.pool_idx[k] = len(self.pool_cnt)
                        self.pool_cnt.append(0)
                        self.sems[('s', self.pool_idx[k])] = self.stack.enter_context(nc.semaphore('sd_%s' % k))
                k = self.pool_idx[phase_map[(kind, st)]]
                self.pool_cnt[k] += 1
                sval[i] = (('s', k), 16 * self.pool_cnt[k])
            elif sig[i]:
                self.cnt[eng] = self.cnt.get(eng, 0) + 1
                sval[i] = (('e', eng), self.cnt[eng])
        per_eng = {e: [] for e in self.ENGS}
        for i, op in enumerate(ops):
            per_eng[op[0]].append(i)
        sems = self.sems
        final = {}
        for eng in ('pe', 'act', 'dve', 'pool'):
            if self.cnt.get(eng, 0) > 0:
                final[('e', eng)] = self.cnt[eng]
        for k, c in enumerate(self.pool_cnt):
            final[('s', k)] = 16 * c

        def run(engname, e):
            waited = self.waited[engname]
            for i in per_eng[engname]:
                _, fn, rd, wr, st = ops[i]
                w = {}
                for j in need[i]:
                    key, val = sval[j]
                    if w.get(key, 0) < val:
                        w[key] = val
                for key, val in w.items():
                    if waited.get(key, 0) >= val:
                        continue
                    waited[key] = val
                    e.wait_ge(sems[key], val)
                if fn is None:
                    continue
                ins = fn(e)
                if sval[i] is not None:
                    key, val = sval[i]
                    ins.then_inc(sems[key], 16 if key[0] == 's' else 1)
            for key, val in final.items():
                if key == ('e', engname):
                    continue
                if waited.get(key, 0) >= val:
                    continue
                waited[key] = val
                e.wait_ge(sems[key], val)

        with nc.Block() as block:
            @block.tensor
            def _(e):
                run('pe', e)

            @block.scalar
            def _(e):
                run('act', e)

            @block.vector
            def _(e):
                run('dve', e)

            @block.gpsimd
            def _(e):
                run('pool', e)

            @block.sync
            def _(e):
                run('sp', e)


def pipeline_reorder(P, base, marks):
    ops = P.ops
    H = [ops[a:b] for (a, b, c) in marks]
    T = [ops[b:c] for (a, b, c) in marks]
    new = list(ops[:base]) + H[0]
    for i in range(len(marks)):
        if i + 1 < len(marks):
            new += H[i + 1]
        new += T[i]
    new += list(ops[marks[-1][2]:])
    assert len(new) == len(ops)
    P.ops = new


class Ring:
    def __init__(self, name, tiles):
        self.name = name
        self.tiles = tiles
        self.i = 0

    def next(self):
        k = self.i % len(self.tiles)
        self.i += 1
        return self.tiles[k], '%s%d' % (self.name, k)


SEC = dict(u=(0, 256), B=(256, 256), C=(512, 256), rq=(768, 256), rk=(1024, 256), rv=(1280, 256),
           rg=(1536, 256), aq=(1792, 512), ak=(2304, 128), av=(2432, 128))
MYCOL = dict(u=0, C=256, B=512, rv=768, rq=1024, rk=1280, aq=1536, rg=2048, ak=2304, av=2432)


def build(phases=('p0', 'p1', 'att', 'ret', 'p3', 'moe'), layers=(0, 1), debug=False, moe_experts=NE, cut=99):
    nc = bass.Bass("TRN2", target_bir_lowering=False)

    _uc = [0]

    def uname(name):
        _uc[0] += 1
        return '%s_u%d' % (name, _uc[0])

    def din(name, shape, dt=F32):
        return nc.dram_tensor(name, list(shape), dt, kind="ExternalInput").ap()

    def dscr(name, shape, dt=F32):
        return nc.dram_tensor(name, list(shape), dt, kind=("ExternalOutput" if debug else "Internal")).ap()

    x_in = din("x", [L, D])
    c_in = din("c", [D])
    ctx_in = din("ctx", [LC, D])
    cctx_in = din("c_ctx", [D])
    w_mod = din("w_mod", [DEPTH, D, 6 * D])
    b_mod = din("b_mod", [DEPTH, 6 * D])
    w_in = din("w_in", [DEPTH, D, 2560])
    conv_w = din("conv_w", [DEPTH, 256, 3])
    rde = din("ret_decay_exp", [DEPTH, 2, 4])
    gn_g = din("ret_gn_g", [DEPTH, 256])
    qn_g = din("q_norm_g", [DEPTH, 64])
    kn_g = din("k_norm_g", [DEPTH, 64])
    w_out = din("w_out", [DEPTH, D, D])
    ln_g = din("ln_g", [DEPTH, 2, D])
    ln_b = din("ln_b", [DEPTH, 2, D])
    w_router = din("w_router", [DEPTH, D, NE])
    b_router = din("b_router", [DEPTH, NE])
    if 'moe' in phases:
        w_gu = din("w_gate_up", [DEPTH, NE, D, 2 * D])
        b_gu = din("b_gate_up", [DEPTH, NE, 2 * D])
        w_dn = din("w_down", [DEPTH, NE, D, D])
        b_dn = din("b_down", [DEPTH, NE, D])
    ident_in = din("c_ident", [128, 128])
    cos_in = din("c_cos", [L, 512])
    sin_in = din("c_sin", [L, 512])
    pos_in = din("c_pos", [128, 4])
    dpos_in = din("c_dpos", [128, 128])
    dneg_in = din("c_dneg", [128, 128])
    mge_in = din("c_mge", [128, 128])
    zeros_in = din("c_zeros", [2, 256])
    iota32_in = din("c_iota32", [128, NE])
    lts_in = din("c_lts", [128, 128])
    bstart_in = din("c_bstart", [128, NBMAX])
    kp_in = din("c_kp", [128, 8])
    iotap_in = din("c_iotap", [128, 1])

    out = nc.dram_tensor("out", [L, D], F32, kind="ExternalOutput").ap()

    N = NT * 128
    modv = dscr("modv", [DEPTH, 2, 6 * D])
    PB = dscr("PB", [N + 4, 512])
    RQT = dscr("RQT", [3, 4, 64, N], BF16)
    RKT = dscr("RKT", [4, 64, N], BF16)
    RTOK = dscr("RTOK", [N, 768], BF16)
    RG = dscr("RG", [N, 256])
    AQT = dscr("AQT", [8, 64, N], BF16)
    AKT = dscr("AKT", [2, 64, N], BF16)
    AV = dscr("AV", [N, 128], BF16)
    MIX = dscr("MIX", [N, 1024])
    XS0 = dscr("XS0", [N, D])
    XM = dscr("XM", [N, D])
    H2R = dscr("H2R", [N, D], BF16)
    RW = dscr("RW", [N, 4])
    RE = dscr("RE", [N, 4])
    XS = dscr("XS", [NBMAX * BS, D], BF16)
    SW = dscr("SW", [NBMAX * BS, 1])
    YS = dscr("YS", [NBMAX * BS, D])
    BGT = dscr("BGT", [DEPTH * NE * 128, 16])

    def prow(i):
        return 1 + i * 128 if i < NT_C else 259 + (i - NT_C) * 128

    with contextlib.ExitStack() as gst:
        P = Prog(nc, gst)
        psum = gst.enter_context(nc.psum_tensor("psum", [128, 8, 512], F32))
        ident = gst.enter_context(nc.sbuf_tensor("ident", [128, 128], F32))
        P.dma('sp', ident[:], ident_in, [], ['ident'], 'ident')

        def bank(b):
            return psum[:, b, :]

        if 'p0' in phases:
            with contextlib.ExitStack() as st:
                def sb(name, shape, dt=F32):
                    return st.enter_context(nc.sbuf_tensor(uname(name), list(shape), dt))
                cnd = sb("cnd", [128, 2, 8])
                P.dma('sp', cnd[:, 0, :], c_in.rearrange("(k p) -> p k", p=128), [], ['cnd'], 'cnd0',
                      allow_slow_non_contiguous=True)
                P.dma('sp', cnd[:, 1, :], cctx_in.rearrange("(k p) -> p k", p=128), [], ['cnd'], 'cnd1',
                      allow_slow_non_contiguous=True)
                P.act(lambda e: e.activation(out=cnd[:], in_=cnd[:], func=AF.Silu), ['cnd'], ['cnd'])
                wring = Ring('wm', [sb("wm%d" % i, [128, 8, 512]) for i in range(6)])
                bring = Ring('bm', [sb("bm%d" % i, [2, 512]) for i in range(2)])
                oring = Ring('om', [sb("om%d" % i, [2, 512]) for i in range(2)])
                for l in layers:
                    for n in range(12):
                        wt, wk = wring.next()
                        bt, bk = bring.next()
                        ot, ok = oring.next()
                        P.dma('sp' if n % 2 == 0 else 'act', wt[:], w_mod[l, :, n * 512:(n + 1) * 512].rearrange("(k p) n -> p k n", p=128),
                              [], [wk], wk)
                        P.dma('sp', bt[:], b_mod[l, n * 512:(n + 1) * 512].partition_broadcast(2), [], [bk], bk)
                        pbank = n % 2
                        pb_ = 'ps0_%d' % pbank
                        for k in range(8):
                            P.pe(lambda e, k=k, wt=wt, pbank=pbank: e.matmul(psum[0:2, pbank, :], cnd[:, :, k], wt[:, k, :],
                                                                            start=(k == 0), stop=(k == 7)),
                                 ['cnd', wk], [pb_])
                        P.dve(lambda e, ot=ot, bt=bt, pbank=pbank: e.tensor_tensor(out=ot[:], in0=psum[0:2, pbank, :], in1=bt[:], op=ALU.add),
                              [pb_, bk], [ok])
                        if n in (2, 3, 8, 9):
                            P.dve(lambda e, ot=ot: e.tensor_scalar_add(out=ot[:], in0=ot[:], scalar1=1.0), [ok], [ok])
                        P.dma('sp', modv[l, :, n * 512:(n + 1) * 512], ot[:], [ok], [('modv', l)], ok)
                P.flush()

        def bload(q, tile_ap, vec_ap, key, reads=()):
            P.dma(q, tile_ap, vec_ap.partition_broadcast(128), list(reads), [key], key)

        for l in layers:
            last = (l == DEPTH - 1)
            xsrc = (lambda i: (ctx_in[i * 128:(i + 1) * 128, :] if i < NT_C else x_in[(i - NT_C) * 128:(i - NT_C + 1) * 128, :])) \
                if l == 0 else (lambda i: XS0[i * 128:(i + 1) * 128, :])
            xs_key = (lambda i: ('xin', i)) if l == 0 else (lambda i: ('XS0', i))
            out_tiles = list(range(NT)) if not last else list(range(NT_C, NT))

            if 'p1' in phases:
                with contextlib.ExitStack() as st:
                    def sb(name, shape, dt=F32):
                        return st.enter_context(nc.sbuf_tensor(uname(name), list(shape), dt))
                    win = sb("win", [128, 8, 2560], BF16)
                    for name, (c0, w) in SEC.items():
                        m0 = MYCOL[name]
                        P.dma('pool', win[:, :, m0:m0 + w], w_in[l, :, c0:c0 + w].rearrange("(k p) n -> p k n", p=128),
                              [], ['win_' + name], 'win_' + name)
                    winkeys = ['win_' + k for k in SEC]
                    sc1 = [sb("sc1_%d" % s, [128, D]) for s in range(2)]
                    sh1 = [sb("sh1_%d" % s, [128, D]) for s in range(2)]
                    for s in range(2):
                        bload('sp', sc1[s][:], modv[l, s, D:2 * D], 'sc1_%d' % s, [('modv', l)])
                        bload('sp', sh1[s][:], modv[l, s, 0:D], 'sh1_%d' % s, [('modv', l)])
                    gq = sb("gq", [128, 64])
                    gk = sb("gk", [128, 64])
                    bload('act', gq[:], qn_g[l], 'gq')
                    bload('act', gk[:], kn_g[l], 'gk')
                    zpad = sb("zpad", [2, 256])
                    P.dma('act', zpad[:], zeros_in, [], ['zpad'], 'zpad')
                    for r0 in (0, 257):
                        P.dma('act', PB[r0:r0 + 2, 0:256] if r0 else PB[0:1, 0:256], zpad[0:2, :] if r0 else zpad[0:1, :],
                              ['zpad'], [('PBpad', r0)], 'zp%d' % r0)
                    P.dma('act', PB[N + 3:N + 4, 0:256], zpad[0:1, :], ['zpad'], [('PBpad', 3)], 'zp3')
                    posc = sb("posc", [128, 4])
                    P.dma('act', posc[:], pos_in, [], ['posc'], 'posc')
                    lg = sb("lg", [128, 8])
                    bload('act', lg[:], rde[l].rearrange("a h -> (a h)"), 'lg')
                    P.act(lambda e: e.activation(out=lg[:], in_=lg[:], func=AF.Exp, scale=-math.log(2.0)), ['lg'], ['lg'])
                    P.act(lambda e: e.activation(out=lg[:], in_=lg[:], func=AF.Ln, scale=-1.0, bias=1.0), ['lg'], ['lg'])
                    tab4 = sb("tab4", [128, 4, 4])
                    for ti, (di, pc) in enumerate(((0, 0), (1, 1), (0, 2), (1, 3))):
                        P.act(lambda e, ti=ti, di=di, pc=pc: e.activation(out=tab4[:, ti, :], in_=lg[:, di * 4:(di + 1) * 4],
                                                                          func=AF.Exp, scale=posc[:, pc:pc + 1]),
                              ['lg', 'posc'], ['tab4'])
                    P.dve(lambda e: e.tensor_scalar_mul(out=tab4[:, 0:2, :], in0=tab4[:, 0:2, :], scalar1=0.125), ['tab4'], ['tab4'])
                    TAB = sb("TAB", [128, 4, 4, 64])
                    P.dve(lambda e: e.tensor_copy(out=TAB[:].rearrange("p t h d -> p (t h) d"),
                                                  in_=tab4[:].rearrange("p t h -> p (t h)").unsqueeze(2).to_broadcast([128, 16, 64])),
                          ['tab4'], ['TAB'])
                    gq8 = sb("gq8", [128, 8, 64])
                    P.dve(lambda e: e.tensor_copy(out=gq8[:], in_=gq[:].unsqueeze(1).to_broadcast([128, 8, 64])), ['gq'], ['gq8'])
                    gk2 = sb("gk2", [128, 2, 64])
                    P.dve(lambda e: e.tensor_copy(out=gk2[:], in_=gk[:].unsqueeze(1).to_broadcast([128, 2, 64])), ['gk'], ['gk2'])

                    xring = Ring('xt', [sb("xt%d" % i, [128, D]) for i in range(3)])
                    csring = Ring('cs', [sb("cs%d" % i, [128, 512]) for i in range(3)])
                    snring = Ring('sn', [sb("sn%d" % i, [128, 512]) for i in range(3)])
                    hring = Ring('h', [sb("h%d" % i, [128, D]) for i in range(2)])
                    hTring = Ring('hT', [sb("hT%d" % i, [128, 8, 128], BF16) for i in range(2)])
                    usb = sb("usb", [128, 256])
                    pcb = Ring('pcb', [sb("pcb%d" % i, [128, 512]) for i in range(2)])
                    t1 = sb("t1", [128, 512])
                    t2 = sb("t2", [128, 512])
                    rq = sb("rq", [128, 4, 256])
                    rtok = Ring('rtok', [sb("rtok%d" % i, [128, 768], BF16) for i in range(2)])
                    rgt = Ring('rgt', [sb("rgt%d" % i, [128, 256]) for i in range(2)])
                    rT = Ring('rT', [sb("rT%d" % i, [128, 8, 128], BF16) for i in range(2)])
                    sq = sb("sq", [128, 512])
                    ss = sb("ss", [128, 8])
                    aq = sb("aq", [128, 512])
                    aqn = sb("aqn", [128, 512])
                    akn = sb("akn", [128, 128])
                    ak = sb("ak", [128, 128])
                    aT = Ring('aT', [sb("aT%d" % i, [128, 5, 128], BF16) for i in range(2)])
                    avt = Ring('avt', [sb("avt%d" % i, [128, 128], BF16) for i in range(2)])

                    def rope(src_ap, W, dst_ap, src_keys, dst_key, cs, ck, sn, sk):
                        g = W // 32
                        P.dve(lambda e: e.tensor_tensor(out=t1[:, :W], in0=src_ap, in1=cs[:, :W], op=ALU.mult),
                              src_keys + [ck], ['t1'])
                        s4 = src_ap.rearrange("p (g a f) -> p g a f", a=2, f=16)
                        t4 = t2[:, :W].rearrange("p (g a f) -> p g a f", a=2, f=16)
                        n4 = sn[:, :W].rearrange("p (g a f) -> p g a f", a=2, f=16)
                        P.dve(lambda e: e.tensor_tensor(out=t4[:, :, 0, :], in0=s4[:, :, 1, :], in1=n4[:, :, 0, :], op=ALU.mult),
                              src_keys + [sk], ['t2a'])
                        P.dve(lambda e: e.tensor_tensor(out=t4[:, :, 1, :], in0=s4[:, :, 0, :], in1=n4[:, :, 1, :], op=ALU.mult),
                              src_keys + [sk], ['t2b'])
                        P.dve(lambda e: e.tensor_tensor(out=dst_ap, in0=t1[:, :W], in1=t2[:, :W], op=ALU.add),
                              ['t1', 't2a', 't2b'], [dst_key])

                    ld = {}

                    def issue_loads(i):
                        xt, xk = xring.next()
                        P.dma('sp', xt[:], xsrc(i), [xs_key(i)], [xk], xk)
                        if i >= NT_C:
                            cs, ck = csring.next()
                            sn, sk = snring.next()
                            t0 = (i - NT_C) * 128
                            P.dma('sp', cs[:], cos_in[t0:t0 + 128, :], [], [ck], ck)
                            P.dma('sp', sn[:], sin_in[t0:t0 + 128, :], [], [sk], sk)
                            ld[i] = (xt, xk, cs, ck, sn, sk)
                        else:
                            ld[i] = (xt, xk, None, None, None, None)
                    issue_loads(0)
                    p1_base = len(P.ops)
                    p1_marks = []
                    for i in range(NT):
                        isctx = i < NT_C
                        s = 1 if isctx else 0
                        m_a = len(P.ops)
                        if i + 1 < NT:
                            issue_loads(i + 1)
                        xt, xk, cs, ck, sn, sk = ld.pop(i)
                        h, hk = hring.next()
                        P.dve(lambda e, h=h, xt=xt, s=s: e.tensor_tensor(out=h[:], in0=xt[:], in1=sc1[s][:], op=ALU.mult),
                              [xk, 'sc1_%d' % s], [hk])
                        P.dve(lambda e, h=h, s=s: e.tensor_tensor(out=h[:], in0=h[:], in1=sh1[s][:], op=ALU.add),
                              [hk, 'sh1_%d' % s], [hk])
                        for k in range(8):
                            P.pe(lambda e, h=h, k=k: e.transpose(psum[:, 5 + k // 4, (k % 4) * 128:(k % 4 + 1) * 128],
                                                                 h[:, k * 128:(k + 1) * 128], ident[:]),
                                 [hk, 'ident'], ['pT%d' % k])
                        hT, hTk = hTring.next()
                        for hh in range(2):
                            P.act(lambda e, hT=hT, hh=hh: e.activation(out=hT[:, hh * 4:(hh + 1) * 4, :].rearrange("p k n -> p (k n)"),
                                                                       in_=psum[:, 5 + hh, :], func=AF.Copy),
                                  ['pT%d' % k for k in range(hh * 4, hh * 4 + 4)], [hTk + '_%d' % hh])
                        m_b = len(P.ops)
                        for b in range(5):
                            for k in range(8):
                                P.pe(lambda e, hT=hT, b=b, k=k: e.matmul(bank(b), hT[:, k, :], win[:, k, b * 512:(b + 1) * 512],
                                                                         start=(k == 0), stop=(k == 7)),
                                     [hTk + '_%d' % (k // 4)] + winkeys, ['z%d' % b])
                        pc, pck = pcb.next()
                        P.act(lambda e: e.activation(out=usb[:], in_=psum[:, 0, 0:256], func=AF.Copy), ['z0'], ['usb'])
                        P.dve(lambda e, pc=pc: e.tensor_tensor(out=pc[:, 0:256], in0=psum[:, 0, 256:512], in1=usb[:], op=ALU.mult),
                              ['z0', 'usb'], [pck + 'a'])
                        P.act(lambda e, pc=pc: e.activation(out=pc[:, 256:512], in_=psum[:, 1, 0:256], func=AF.Copy), ['z1'], [pck + 'b'])
                        r0 = prow(i)
                        P.dma('sp', PB[r0:r0 + 128, :], pc[:], [pck + 'a', pck + 'b'], [('PB', i)], pck)
                        rt, rtk = rtok.next()
                        P.act(lambda e, rt=rt: e.activation(out=rt[:, 512:768], in_=psum[:, 1, 256:512], func=AF.Copy), ['z1'], [rtk + 'v'])
                        if isctx:
                            P.act(lambda e: e.activation(out=rq[:, 0, :], in_=psum[:, 2, 0:256], func=AF.Copy), ['z2'], ['rq0'])
                            P.act(lambda e: e.activation(out=rq[:, 3, :], in_=psum[:, 2, 256:512], func=AF.Copy), ['z2'], ['rq3'])
                        else:
                            rope(psum[:, 2, 0:256], 256, rq[:, 0, :], ['z2'], 'rq0', cs, ck, sn, sk)
                            rope(psum[:, 2, 256:512], 256, rq[:, 3, :], ['z2'], 'rq3', cs, ck, sn, sk)
                        TABf = TAB[:].rearrange("p t h d -> p t (h d)")
                        P.dve(lambda e: e.tensor_tensor(out=rq[:, 1, :], in0=rq[:, 0, :], in1=TABf[:, 2, :], op=ALU.mult), ['rq0', 'TAB'], ['rq1'])
                        P.pool(lambda e: e.tensor_tensor(out=rq[:, 2, :], in0=rq[:, 0, :], in1=TABf[:, 3, :], op=ALU.mult), ['rq0', 'TAB'], ['rq2'])
                        P.dve(lambda e, rt=rt: e.tensor_tensor(out=rt[:, 0:256], in0=rq[:, 3, :], in1=TABf[:, 0, :], op=ALU.mult), ['rq3', 'TAB'], [rtk + 'f'])
                        P.pool(lambda e, rt=rt: e.tensor_tensor(out=rt[:, 256:512], in0=rq[:, 3, :], in1=TABf[:, 1, :], op=ALU.mult), ['rq3', 'TAB'], [rtk + 'b'])
                        P.dma('sp', RTOK[i * 128:(i + 1) * 128, :], rt[:], [rtk + 'v', rtk + 'f', rtk + 'b'], [('RTOK', i)], rtk)
                        rg_, rgk = rgt.next()
                        P.act(lambda e, rg_=rg_: e.activation(out=rg_[:], in_=psum[:, 4, 0:256], func=AF.Silu), ['z4'], [rgk])
                        P.dma('act', RG[i * 128:(i + 1) * 128, :], rg_[:], [rgk], [('RG', i)], rgk)
                        for t in range(4):
                            for c2 in range(2):
                                idx = t * 2 + c2
                                P.pe(lambda e, t=t, c2=c2, idx=idx: e.transpose(psum[:, 5 + idx // 4, (idx % 4) * 128:(idx % 4 + 1) * 128],
                                                                                rq[:, t, c2 * 128:(c2 + 1) * 128], ident[:]),
                                     ['rq%d' % t, 'ident'], ['pT%d' % idx])
                        rTt, rTk = rT.next()
                        for hh in range(2):
                            if hh == 0:
                                P.act(lambda e, rTt=rTt: e.activation(out=rTt[:, 0:4, :].rearrange("p k n -> p (k n)"), in_=psum[:, 5, :], func=AF.Copy),
                                      ['pT0', 'pT1', 'pT2', 'pT3'], [rTk + 'a'])
                            else:
                                P.act(lambda e, rTt=rTt: e.activation(out=rTt[:, 4:6, :].rearrange("p k n -> p (k n)"), in_=psum[:, 6, 0:256], func=AF.Copy),
                                      ['pT4', 'pT5'], [rTk + 'b'])
                                P.act(lambda e, rTt=rTt: e.activation(out=rTt[:, 6:8, :].rearrange("p k n -> p (k n)"), in_=psum[:, 6, 256:512], func=AF.Copy, scale=0.125),
                                      ['pT6', 'pT7'], [rTk + 'c'])
                        for t in range(3):
                            P.dma('act', RQT[t, :, :, i * 128:(i + 1) * 128].rearrange("(c q) d n -> (q d) c n", q=2),
                                  rTt[:, 2 * t:2 * t + 2, :], [rTk + 'a', rTk + 'b'], [('RQT', i, t)], rTk + 'q%d' % t)
                        P.dma('act', RKT[:, :, i * 128:(i + 1) * 128].rearrange("(c q) d n -> (q d) c n", q=2),
                              rTt[:, 6:8, :], [rTk + 'c'], [('RKT', i)], rTk + 'k')
                        P.act(lambda e: e.activation(out=sq[:], in_=psum[:, 3, :], func=AF.Square), ['z3'], ['sq'])
                        P.dve(lambda e: e.tensor_reduce(out=ss[:], in_=sq[:].rearrange("p (h d) -> p h d", d=64), axis=AX.X, op=ALU.add), ['sq'], ['ss'])
                        P.dve(lambda e: e.tensor_scalar(out=ss[:], in0=ss[:], scalar1=1.0 / 64, scalar2=EPS, op0=ALU.mult, op1=ALU.add), ['ss'], ['ss'])
                        P.act(lambda e: e.activation(out=ss[:], in_=ss[:], func=AF.Sqrt), ['ss'], ['ss'])
                        P.dve(lambda e: e.reciprocal(out=ss[:], in_=ss[:]), ['ss'], ['ss'])
                        P.dve(lambda e: e.tensor_tensor(out=aqn[:].rearrange("p (h d) -> p h d", d=64), in0=psum[:, 3, :].rearrange("p (h d) -> p h d", d=64),
                                                        in1=ss[:].unsqueeze(2).to_broadcast([128, 8, 64]), op=ALU.mult), ['z3', 'ss'], ['aqn'])
                        P.dve(lambda e: e.tensor_tensor(out=aqn[:], in0=aqn[:], in1=gq8[:].rearrange("p h d -> p (h d)"), op=ALU.mult), ['aqn', 'gq8'], ['aqn'])
                        if isctx:
                            aq_src, aq_key = aqn, 'aqn'
                        else:
                            rope(aqn[:], 512, aq[:], ['aqn'], 'aq', cs, ck, sn, sk)
                            aq_src, aq_key = aq, 'aq'
                        av_, avk = avt.next()
                        P.act(lambda e, av_=av_: e.activation(out=av_[:], in_=psum[:, 4, 384:512], func=AF.Copy), ['z4'], [avk])
                        P.dma('act', AV[i * 128:(i + 1) * 128, :], av_[:], [avk], [('AV', i)], avk)
                        P.act(lambda e: e.activation(out=sq[:, 0:128], in_=psum[:, 4, 256:384], func=AF.Square), ['z4'], ['sqk'])
                        P.dve(lambda e: e.tensor_reduce(out=ss[:, 0:2], in_=sq[:, 0:128].rearrange("p (h d) -> p h d", d=64), axis=AX.X, op=ALU.add), ['sqk'], ['ssk'])
                        P.dve(lambda e: e.tensor_scalar(out=ss[:, 0:2], in0=ss[:, 0:2], scalar1=1.0 / 64, scalar2=EPS, op0=ALU.mult, op1=ALU.add), ['ssk'], ['ssk'])
                        P.act(lambda e: e.activation(out=ss[:, 0:2], in_=ss[:, 0:2], func=AF.Sqrt), ['ssk'], ['ssk'])
                        P.dve(lambda e: e.reciprocal(out=ss[:, 0:2], in_=ss[:, 0:2]), ['ssk'], ['ssk'])
                        P.dve(lambda e: e.tensor_tensor(out=akn[:].rearrange("p (h d) -> p h d", d=64), in0=psum[:, 4, 256:384].rearrange("p (h d) -> p h d", d=64),
                                                        in1=ss[:, 0:2].unsqueeze(2).to_broadcast([128, 2, 64]), op=ALU.mult), ['z4', 'ssk'], ['akn'])
                        P.dve(lambda e: e.tensor_tensor(out=akn[:], in0=akn[:], in1=gk2[:].rearrange("p h d -> p (h d)"), op=ALU.mult), ['akn', 'gk2'], ['akn'])
                        if isctx:
                            ak_src, ak_key = akn, 'akn'
                        else:
                            rope(akn[:], 128, ak[:], ['akn'], 'ak', cs, ck, sn, sk)
                            ak_src, ak_key = ak, 'ak'
                        for c4 in range(4):
                            P.pe(lambda e, c4=c4, aq_src=aq_src: e.transpose(psum[:, 7, c4 * 128:(c4 + 1) * 128], aq_src[:, c4 * 128:(c4 + 1) * 128], ident[:]),
                                 [aq_key, 'ident'], ['pA%d' % c4])
                        P.pe(lambda e, ak_src=ak_src: e.transpose(psum[:, 5, 0:128], ak_src[:, 0:128], ident[:]), [ak_key, 'ident'], ['pT0'])
                        aTt, aTk = aT.next()
                        P.act(lambda e, aTt=aTt: e.activation(out=aTt[:, 0:4, :].rearrange("p k n -> p (k n)"), in_=psum[:, 7, :], func=AF.Copy),
                              ['pA0', 'pA1', 'pA2', 'pA3'], [aTk + 'q'])
                        P.act(lambda e, aTt=aTt: e.activation(out=aTt[:, 4, :], in_=psum[:, 5, 0:128], func=AF.Copy), ['pT0'], [aTk + 'k'])
                        P.dma('sp', AQT[:, :, i * 128:(i + 1) * 128].rearrange("(c q) d n -> (q d) c n", q=2), aTt[:, 0:4, :],
                              [aTk + 'q'], [('AQT', i)], aTk + 'q')
                        P.dma('sp', AKT[:, :, i * 128:(i + 1) * 128].rearrange("q d n -> (q d) n"), aTt[:, 4, :],
                              [aTk + 'k'], [('AKT', i)], aTk + 'k')
                        p1_marks.append((m_a, m_b, len(P.ops)))
                    pipeline_reorder(P, p1_base, p1_marks)
                    P.flush()


            if 'att' in phases:
                with contextlib.ExitStack() as st:
                    def sb(name, shape, dt=F32):
                        return st.enter_context(nc.sbuf_tensor(uname(name), list(shape), dt))
                    KT = sb("KT", [128, 2, N], BF16)
                    P.pool(lambda e: e.memset(KT[64:128, :, :], 0.0), [], ['KTz'])
                    P.dma('sp', KT[0:64, :, :], AKT.rearrange("k d n -> d k n"), [], ['KT'], 'KT')
                    V1 = sb("V1", [128, NT, 2, 65], BF16)
                    P.pool(lambda e: e.memset(V1[:], 1.0), [], ['V1'])
                    for kk_ in range(2):
                        P.dma('act', V1[:, :, kk_, 0:64], AV[:, kk_ * 64:(kk_ + 1) * 64].rearrange("(c p) d -> p c d", p=128), [], ['V1'], 'V1_%d' % kk_)
                    qring = Ring('QT', [sb("QT%d" % i, [128, N], BF16) for i in range(2)])
                    for qi_, qt_ in enumerate(qring.tiles):
                        P.pool(lambda e, qt_=qt_: e.memset(qt_[64:128, :], 0.0), [], ['QTz%d' % qi_])
                    ptring = Ring('PT', [sb("PT%d" % i, [128, 512], BF16) for i in range(4)])
                    aoring = Ring('AO', [sb("AO%d" % i, [128, 4, 64]) for i in range(2)])
                    rcring = Ring('rc', [sb("rc%d" % i, [128, 4]) for i in range(2)])
                    sbank = Ring('S', [0, 1, 2, 3])
                    obank = Ring('O', [4, 5])
                    groups = []
                    if not last:
                        groups.append((0, 2, [0, 1]))
                    for g in range(8):
                        groups.append((LC + g * 512, 4, list(range(NT))))
                    items = []
                    for hq in range(8):
                        for gi, (q0, nq, chunks) in enumerate(groups):
                            for ci, c in enumerate(chunks):
                                items.append((hq, gi, q0, nq, ci, c, len(chunks)))
                    qt_of = {}
                    st_of = {}

                    def get_qt(hq):
                        if hq not in qt_of:
                            QT, qk = qring.next()
                            P.dma('sp', QT[0:64, :], AQT[hq], [], [qk], qk)
                            qt_of[hq] = (QT, qk)
                        return qt_of[hq]

                    def emit_S(t):
                        hq, gi, q0, nq, ci, c, nch = items[t]
                        QT, qk = get_qt(hq)
                        kvh = hq // 4
                        W = nq * 128
                        sbk, sk = sbank.next()
                        P.pe(lambda e, sbk=sbk, c=c, QT=QT, q0=q0, W=W, kvh=kvh: e.matmul(psum[:, sbk, 0:W], KT[:, kvh, c * 128:(c + 1) * 128],
                                                                                         QT[:, q0:q0 + W], start=True, stop=True),
                             ['KT', 'KTz', 'QTz0', 'QTz1', qk], [sk])
                        st_of[t] = (sbk, sk)

                    cur_o = [None]

                    def emit_rest(t):
                        hq, gi, q0, nq, ci, c, nch = items[t]
                        kvh = hq // 4
                        W = nq * 128
                        sbk, sk = st_of.pop(t)
                        if ci == 0:
                            cur_o[0] = obank.next()
                        ob, ok = cur_o[0]
                        Ov = psum[:, ob, 0:260].rearrange("p (j e) -> p j e", e=65)
                        PT, pk = ptring.next()
                        P.act(lambda e, PT=PT, sbk=sbk, W=W: e.activation(out=PT[:, 0:W], in_=psum[:, sbk, 0:W], func=AF.Exp, scale=0.125),
                              [sk], [pk])
                        if t + 2 < len(items):
                            emit_S(t + 2)
                        for j in range(nq):
                            P.pe(lambda e, PT=PT, j=j, c=c, kvh=kvh, ci=ci, Ov=Ov, nch=nch: e.matmul(
                                Ov[:, j, :], PT[:, j * 128:(j + 1) * 128], V1[:, c, kvh, :],
                                start=(ci == 0 and j == 0), stop=(ci == nch - 1), skip_group_check=True),
                                [pk, 'V1'], [ok])
                        if ci == nch - 1:
                            rc, rk_ = rcring.next()
                            AO, ak_ = aoring.next()
                            P.dve(lambda e, rc=rc, Ov=Ov, nq=nq: e.reciprocal(out=rc[:, 0:nq], in_=Ov[:, 0:nq, 64]), [ok], [rk_])
                            P.dve(lambda e, rc=rc, Ov=Ov, nq=nq, AO=AO: e.tensor_tensor(out=AO[:, 0:nq, :], in0=Ov[:, 0:nq, 0:64],
                                                                                      in1=rc[:, 0:nq].unsqueeze(2).to_broadcast([128, nq, 64]), op=ALU.mult),
                                  [ok, rk_], [ak_])
                            P.dma('sp', MIX[q0:q0 + W, 512 + hq * 64:512 + (hq + 1) * 64].rearrange("(j p) d -> p j d", p=128), AO[:, 0:nq, :],
                                  [ak_], [('MIXa', q0, hq)], ak_)
                    emit_S(0)
                    emit_S(1)
                    for t in range(len(items)):
                        emit_rest(t)
                    P.flush()

            if 'ret' in phases:
                with contextlib.ExitStack() as st:
                    def sb(name, shape, dt=F32):
                        return st.enter_context(nc.sbuf_tensor(uname(name), list(shape), dt))
                    RT = sb("RT", [128, NT, 768], BF16)
                    P.dma('sp', RT[:], RTOK.rearrange("(c p) w -> p c w", p=128), [], ['RT'], 'RT')
                    RGt = sb("RGt", [128, NT, 256])
                    P.dma('act', RGt[:], RG.rearrange("(c p) w -> p c w", p=128), [], ['RGt'], 'RGt')
                    gng = sb("gng", [128, 256])
                    bload('act', gng[:], gn_g[l], 'gng')
                    lg = sb("lg", [128, 8])
                    bload('act', lg[:], rde[l].rearrange("a h -> (a h)"), 'lg')
                    P.act(lambda e: e.activation(out=lg[:], in_=lg[:], func=AF.Exp, scale=-math.log(2.0)), ['lg'], ['lg'])
                    P.act(lambda e: e.activation(out=lg[:], in_=lg[:], func=AF.Ln, scale=-1.0, bias=1.0), ['lg'], ['lg'])
                    dec = sb("dec", [128, 8])
                    P.act(lambda e: e.activation(out=dec[:], in_=lg[:], func=AF.Exp, scale=128.0), ['lg'], ['dec'])
                    dpos = sb("dpos", [128, 128]); dneg = sb("dneg", [128, 128]); mge = sb("mge", [128, 128])
                    P.dma('sp', dpos[:], dpos_in, [], ['dpos'], 'dpos')
                    P.dma('sp', dneg[:], dneg_in, [], ['dneg'], 'dneg')
                    P.dma('sp', mge[:], mge_in, [], ['mge'], 'mge')
                    DcT = sb("DcT", [128, 4, 128])
                    e1 = sb("e1", [128, 128])
                    for hh in range(4):
                        P.act(lambda e, hh=hh: e.activation(out=e1[:], in_=dpos[:], func=AF.Exp, scale=lg[:, hh:hh + 1]), ['dpos', 'lg'], ['e1'])
                        P.act(lambda e, hh=hh: e.activation(out=DcT[:, hh, :], in_=dneg[:], func=AF.Exp, scale=lg[:, 4 + hh:5 + hh]), ['dneg', 'lg'], ['DcT%d' % hh])
                        P.dve(lambda e, hh=hh: e.tensor_tensor(out=e1[:], in0=e1[:], in1=DcT[:, hh, :], op=ALU.subtract), ['e1', 'DcT%d' % hh], ['e1'])
                        P.dve(lambda e, hh=hh: e.tensor_tensor(out=e1[:], in0=e1[:], in1=mge[:], op=ALU.mult), ['e1', 'mge'], ['e1'])
                        P.dve(lambda e, hh=hh: e.tensor_tensor(out=DcT[:, hh, :], in0=DcT[:, hh, :], in1=e1[:], op=ALU.add), ['e1', 'DcT%d' % hh], ['DcT%d' % hh])
                    q3ring = Ring('Q3', [sb("Q3_%d" % i, [64, 3, N], BF16) for i in range(2)])
                    ktring = Ring('KTh', [sb("KTh%d" % i, [64, N], BF16) for i in range(2)])
                    Sf = sb("Sf", [64, NT + 1, 64], BF16)
                    Sb = sb("Sb", [64, NT + 1, 64], BF16)
                    srun = Ring('srun', [sb("srun%d" % i, [64, 64]) for i in range(2)])
                    ptr = Ring('PTr', [sb("PTr%d" % i, [128, 128], BF16) for i in range(2)])
                    st6 = sb("st6", [128, 6]); mv = sb("mv", [128, 2]); rs = sb("rs", [128, 1])
                    yo = Ring('yo', [sb("yo%d" % i, [128, 64]) for i in range(2)])
                    kvb = Ring('kv', [0, 1]); scb = Ring('sc', [2, 3]); yb = Ring('y', [4, 5])
                    ytiles = list(range(NT)) if not last else list(range(NT_C, NT))
                    for hh in range(4):
                        Q3, q3k = q3ring.next()
                        KTh, ktk = ktring.next()
                        P.dma('sp', Q3[:], RQT[:, hh].rearrange("t d n -> d t n"), [], [q3k], q3k)
                        P.dma('act', KTh[:], RKT[hh], [], [ktk], ktk)
                        vcol = 512 + hh * 64

                        def scan(order_tiles, S, skey, kcol, dcol, run_init_zero):
                            return None
                        run, runk = srun.next()
                        P.pool(lambda e, run=run: e.memset(run[:], 0.0), [], [runk])
                        for i in range(NT):
                            P.pool(lambda e, run=run, i=i: e.tensor_copy(out=Sf[:, i, :], in_=run[:]), [runk], [('Sf', i)])
                            kb, kk = kvb.next()
                            P.pe(lambda e, kb=kb, i=i, hh=hh, vcol=vcol: e.matmul(psum[0:64, kb, 0:64], RT[:, i, hh * 64:(hh + 1) * 64],
                                                                                 RT[:, i, vcol:vcol + 64], start=True, stop=True), ['RT'], [kk])
                            nrun, nrunk = srun.next()
                            P.dve(lambda e, run=run, nrun=nrun, kb=kb, hh=hh: e.scalar_tensor_tensor(out=nrun[:], in0=run[:], scalar=dec[0:64, hh:hh + 1],
                                                                                                     in1=psum[0:64, kb, 0:64], op0=ALU.mult, op1=ALU.add),
                                  [runk, kk, 'dec'], [nrunk])
                            run, runk = nrun, nrunk
                        run, runk = srun.next()
                        P.pool(lambda e, run=run: e.memset(run[:], 0.0), [], [runk])
                        for i in [1, 0] + list(range(NT - 1, NT_C - 1, -1)):
                            P.pool(lambda e, run=run, i=i: e.tensor_copy(out=Sb[:, i, :], in_=run[:]), [runk], [('Sb', i)])
                            kb, kk = kvb.next()
                            P.pe(lambda e, kb=kb, i=i, hh=hh, vcol=vcol: e.matmul(psum[0:64, kb, 0:64], RT[:, i, 256 + hh * 64:256 + (hh + 1) * 64],
                                                                                 RT[:, i, vcol:vcol + 64], start=True, stop=True), ['RT'], [kk])
                            nrun, nrunk = srun.next()
                            P.dve(lambda e, run=run, nrun=nrun, kb=kb, hh=hh: e.scalar_tensor_tensor(out=nrun[:], in0=run[:], scalar=dec[0:64, 4 + hh:5 + hh],
                                                                                                     in1=psum[0:64, kb, 0:64], op0=ALU.mult, op1=ALU.add),
                                  [runk, kk, 'dec'], [nrunk])
                            run, runk = nrun, nrunk
                        r_base = len(P.ops)
                        r_marks = []
                        for i in ytiles:
                            m_a = len(P.ops)
                            sbk, sk = scb.next()
                            P.pe(lambda e, sbk=sbk, i=i, KTh=KTh, Q3=Q3: e.matmul(psum[:, sbk, 0:128], KTh[:, i * 128:(i + 1) * 128], Q3[:, 0, i * 128:(i + 1) * 128],
                                                                                start=True, stop=True), [ktk, q3k], [sk])
                            PTr, pk = ptr.next()
                            P.dve(lambda e, PTr=PTr, sbk=sbk, hh=hh: e.tensor_tensor(out=PTr[:], in0=psum[:, sbk, 0:128], in1=DcT[:, hh, :], op=ALU.mult),
                                  [sk, 'DcT%d' % hh], [pk])
                            ybk, yk = yb.next()
                            P.pe(lambda e, ybk=ybk, PTr=PTr, i=i, vcol=vcol: e.matmul(psum[:, ybk, 0:64], PTr[:], RT[:, i, vcol:vcol + 64], start=True, stop=False),
                                 [pk, 'RT'], [yk])
                            P.pe(lambda e, ybk=ybk, Q3=Q3, i=i: e.matmul(psum[:, ybk, 0:64], Q3[:, 1, i * 128:(i + 1) * 128], Sf[:, i, :], start=False, stop=False),
                                 [q3k, ('Sf', i)], [yk])
                            P.pe(lambda e, ybk=ybk, Q3=Q3, i=i: e.matmul(psum[:, ybk, 0:64], Q3[:, 2, i * 128:(i + 1) * 128], Sb[:, i, :], start=False, stop=True),
                                 [q3k, ('Sb', i)], [yk])
                            m_b = len(P.ops)
                            P.dve(lambda e, ybk=ybk: e.bn_stats(out=st6[:], in_=psum[:, ybk, 0:64]), [yk], ['st6'])
                            P.dve(lambda e: e.bn_aggr(out=mv[:], in_=st6[:]), ['st6'], ['mv'])
                            P.dve(lambda e: e.tensor_scalar_add(out=rs[:], in0=mv[:, 1:2], scalar1=EPS), ['mv'], ['rs'])
                            P.act(lambda e: e.activation(out=rs[:], in_=rs[:], func=AF.Sqrt), ['rs'], ['rs'])
                            P.dve(lambda e: e.reciprocal(out=rs[:], in_=rs[:]), ['rs'], ['rs'])
                            y_, yok = yo.next()
                            P.dve(lambda e, y_=y_, ybk=ybk: e.tensor_scalar(out=y_[:], in0=psum[:, ybk, 0:64], scalar1=mv[:, 0:1], scalar2=rs[:, 0:1],
                                                                           op0=ALU.subtract, op1=ALU.mult), [yk, 'mv', 'rs'], [yok])
                            P.pool(lambda e, y_=y_, hh=hh: e.tensor_tensor(out=y_[:], in0=y_[:], in1=gng[:, hh * 64:(hh + 1) * 64], op=ALU.mult), [yok, 'gng'], [yok])
                            P.pool(lambda e, y_=y_, hh=hh, i=i: e.tensor_tensor(out=y_[:], in0=y_[:], in1=RGt[:, i, hh * 64:(hh + 1) * 64], op=ALU.mult), [yok, 'RGt'], [yok])
                            P.dma('sp', MIX[i * 128:(i + 1) * 128, 256 + hh * 64:256 + (hh + 1) * 64], y_[:], [yok], [('MIXr', i, hh)], yok)
                            r_marks.append((m_a, m_b, len(P.ops)))
                        pipeline_reorder(P, r_base, r_marks)
                    P.flush()

            if 'p3' in phases:
                with contextlib.ExitStack() as st:
                    def sb(name, shape, dt=F32):
                        return st.enter_context(nc.sbuf_tensor(uname(name), list(shape), dt))
                    wout = sb("wout", [128, 8, D], BF16)
                    for hf in range(2):
                        P.dma('pool', wout[:, :, hf * 512:(hf + 1) * 512], w_out[l, :, hf * 512:(hf + 1) * 512].rearrange("(k p) n -> p k n", p=128),
                              [], ['wout%d' % hf], 'wout%d' % hf)
                    CWr = sb("CWr", [128, 256, 3])
                    P.dma('act', CWr[:].rearrange("p c k -> p (c k)"), conv_w[l].rearrange("c k -> (c k)").partition_broadcast(128), [], ['CWr'], 'CWr')
                    CW = sb("CW", [128, 3, 256])
                    for k in range(3):
                        P.dve(lambda e, k=k: e.tensor_copy(out=CW[:, k, :], in_=CWr[:, :, k]), ['CWr'], ['CW%d' % k])
                    g1 = [sb("g1_%d" % s, [128, D]) for s in range(2)]
                    sc2 = [sb("sc2_%d" % s, [128, D]) for s in range(2)]
                    sh2 = [sb("sh2_%d" % s, [128, D]) for s in range(2)]
                    for s in range(2):
                        if last and s == 1:
                            continue
                        bload('sp', g1[s][:], modv[l, s, 2 * D:3 * D], 'g1_%d' % s)
                        bload('sp', sh2[s][:], modv[l, s, 3 * D:4 * D], 'sh2_%d' % s)
                        bload('sp', sc2[s][:], modv[l, s, 4 * D:5 * D], 'sc2_%d' % s)
                    lng = sb("lng", [128, D]); lnb = sb("lnb", [128, D])
                    bload('act', lng[:], ln_g[l, 0], 'lng')
                    bload('act', lnb[:], ln_b[l, 0], 'lnb')
                    wr = sb("wr", [128, 8, NE])
                    P.dma('act', wr[:], w_router[l].rearrange("(k p) e -> p k e", p=128), [], ['wr'], 'wr')
                    brt = sb("brt", [128, NE])
                    bload('act', brt[:], b_router[l], 'brt')
                    pmr = Ring('pm', [sb("pm%d" % i, [128, 3, 256]) for i in range(3)])
                    btr = Ring('bt', [sb("bt%d" % i, [128, 256]) for i in range(3)])
                    mixr = Ring('mix', [sb("mix%d" % i, [128, D]) for i in range(3)])
                    xr = Ring('x3', [sb("x3_%d" % i, [128, D]) for i in range(3)])
                    ca = sb("ca", [128, 256]); cb = sb("cb", [128, 256])
                    mTr = Ring('mT', [sb("mT%d" % i, [128, 8, 128], BF16) for i in range(2)])
                    rr = sb("rr", [128, D])
                    x1r = Ring('x1', [sb("x1_%d" % i, [128, D]) for i in range(2)])
                    h2 = sb("h2", [128, D])
                    h2br = Ring('h2b', [sb("h2b%d" % i, [128, D], BF16) for i in range(2)])
                    ix8 = sb("ix8", [128, 8], U32)
                    h2f = sb("h2f", [128, 8, 128])
                    st6 = sb("st6", [128, 2, 6]); mv = sb("mv", [128, 2]); rs = sb("rs", [128, 1])
                    lgt = sb("lgt", [128, NE]); mx8 = sb("mx8", [128, 8]); msk = sb("msk", [128, NE]); nmx = sb("nmx", [128, 1])
                    ex = sb("ex", [128, NE]); sm = sb("sm", [128, 1])
                    mwr = Ring('mw', [sb("mw%d" % i, [128, NE]) for i in range(2)])
                    ld3 = {}

                    def issue_loads3(i):
                        r0 = prow(i)
                        pm, pmk = pmr.next()
                        for k in range(3):
                            P.dma('sp', pm[:, k, :], PB[r0 - 1 + k:r0 + 127 + k, 0:256], [], [pmk + str(k)], pmk + str(k))
                        bt, btk = btr.next()
                        P.dma('sp', bt[:], PB[r0:r0 + 128, 256:512], [], [btk], btk)
                        mix, mixk = mixr.next()
                        P.dma('sp', mix[:, 256:D], MIX[i * 128:(i + 1) * 128, 256:D], [], [mixk + 'l'], mixk)
                        xt, xk = xr.next()
                        P.dma('sp', xt[:], xsrc(i), [], [xk], xk)
                        ld3[i] = (pm, pmk, bt, btk, mix, mixk, xt, xk)
                    issue_loads3(out_tiles[0])
                    p3_base = len(P.ops)
                    p3_marks = []
                    for oi, i in enumerate(out_tiles):
                        s = 1 if i < NT_C else 0
                        m_a = len(P.ops)
                        if oi + 1 < len(out_tiles):
                            issue_loads3(out_tiles[oi + 1])
                        pm, pmk, bt, btk, mix, mixk, xt, xk = ld3.pop(i)
                        P.dve(lambda e, pm=pm: e.tensor_tensor(out=ca[:], in0=pm[:, 0, :], in1=CW[:, 0, :], op=ALU.mult), [pmk + '0', 'CW0'], ['ca'])
                        P.pool(lambda e, pm=pm: e.tensor_tensor(out=cb[:], in0=pm[:, 1, :], in1=CW[:, 1, :], op=ALU.mult), [pmk + '1', 'CW1'], ['cb'])
                        P.dve(lambda e: e.tensor_tensor(out=ca[:], in0=ca[:], in1=cb[:], op=ALU.add), ['ca', 'cb'], ['ca'])
                        P.pool(lambda e, pm=pm: e.tensor_tensor(out=cb[:], in0=pm[:, 2, :], in1=CW[:, 2, :], op=ALU.mult), [pmk + '2', 'CW2', 'ca'], ['cb'])
                        P.dve(lambda e: e.tensor_tensor(out=ca[:], in0=ca[:], in1=cb[:], op=ALU.add), ['ca', 'cb'], ['ca'])
                        P.dve(lambda e, mix=mix, bt=bt: e.tensor_tensor(out=mix[:, 0:256], in0=ca[:], in1=bt[:], op=ALU.mult), ['ca', btk], [mixk + 'c'])
                        for k in range(8):
                            P.pe(lambda e, mix=mix, k=k: e.transpose(psum[:, k // 4, (k % 4) * 128:(k % 4 + 1) * 128], mix[:, k * 128:(k + 1) * 128], ident[:]),
                                 [mixk + 'l', mixk + 'c', 'ident'], ['pT%d' % k])
                        mT, mTk = mTr.next()
                        for hf in range(2):
                            P.act(lambda e, mT=mT, hf=hf: e.activation(out=mT[:, hf * 4:(hf + 1) * 4, :].rearrange("p k n -> p (k n)"), in_=psum[:, hf, :], func=AF.Copy),
                                  ['pT%d' % k for k in range(hf * 4, hf * 4 + 4)], [mTk + str(hf)])
                        pyb = 2 + 2 * (oi % 2)
                        for nh in range(2):
                            for k in range(8):
                                P.pe(lambda e, mT=mT, nh=nh, k=k, pyb=pyb: e.matmul(psum[:, pyb + nh, :], mT[:, k, :], wout[:, k, nh * 512:(nh + 1) * 512], start=(k == 0), stop=(k == 7)),
                                     [mTk + str(k // 4), 'wout%d' % nh], ['py%d' % (pyb + nh)])
                        m_b = len(P.ops)
                        for nh in range(2):
                            P.dve(lambda e, nh=nh, s=s, pyb=pyb: e.tensor_tensor(out=rr[:, nh * 512:(nh + 1) * 512], in0=psum[:, pyb + nh, :], in1=g1[s][:, nh * 512:(nh + 1) * 512], op=ALU.mult),
                                  ['py%d' % (pyb + nh), 'g1_%d' % s], ['rr%d' % nh])
                        P.dve(lambda e, xt=xt: e.scalar_tensor_tensor(out=rr[:], in0=xt[:], scalar=ALPHA, in1=rr[:], op0=ALU.mult, op1=ALU.add), [xk, 'rr0', 'rr1'], ['rr'])
                        for nh in range(2):
                            P.dve(lambda e, nh=nh: e.bn_stats(out=st6[:, nh, :], in_=rr[:, nh * 512:(nh + 1) * 512]), ['rr'], ['st6_%d' % nh])
                        P.dve(lambda e: e.bn_aggr(out=mv[:], in_=st6[:]), ['st6_0', 'st6_1'], ['mv'])
                        P.dve(lambda e: e.tensor_scalar_add(out=rs[:], in0=mv[:, 1:2], scalar1=EPS), ['mv'], ['rs'])
                        P.act(lambda e: e.activation(out=rs[:], in_=rs[:], func=AF.Sqrt), ['rs'], ['rs'])
                        P.dve(lambda e: e.reciprocal(out=rs[:], in_=rs[:]), ['rs'], ['rs'])
                        x1, x1k = x1r.next()
                        P.dve(lambda e, x1=x1: e.tensor_scalar(out=x1[:], in0=rr[:], scalar1=mv[:, 0:1], scalar2=rs[:, 0:1], op0=ALU.subtract, op1=ALU.mult), ['rr', 'mv', 'rs'], [x1k])
                        P.dve(lambda e, x1=x1: e.tensor_tensor(out=x1[:], in0=x1[:], in1=lng[:], op=ALU.mult), [x1k, 'lng'], [x1k])
                        P.dve(lambda e, x1=x1: e.tensor_tensor(out=x1[:], in0=x1[:], in1=lnb[:], op=ALU.add), [x1k, 'lnb'], [x1k])
                        P.dma('sp', XM[i * 128:(i + 1) * 128, :], x1[:], [x1k], [('XM', i)], x1k)
                        P.dve(lambda e, x1=x1, s=s: e.tensor_tensor(out=h2[:], in0=x1[:], in1=sc2[s][:], op=ALU.mult), [x1k, 'sc2_%d' % s], ['h2'])
                        P.dve(lambda e, s=s: e.tensor_tensor(out=h2[:], in0=h2[:], in1=sh2[s][:], op=ALU.add), ['h2', 'sh2_%d' % s], ['h2'])
                        for k in range(8):
                            P.pe(lambda e, k=k: e.transpose(psum[:, k // 4, (k % 4) * 128:(k % 4 + 1) * 128], h2[:, k * 128:(k + 1) * 128], ident[:]),
                                 ['h2', 'ident'], ['pT%d' % k])
                        for hf in range(2):
                            P.dve(lambda e, hf=hf: e.tensor_copy(out=h2f[:, hf * 4:(hf + 1) * 4, :].rearrange("p k n -> p (k n)"), in_=psum[:, hf, :]),
                                  ['pT%d' % k for k in range(hf * 4, hf * 4 + 4)], ['h2f%d' % hf])
                        h2b, h2bk = h2br.next()
                        P.act(lambda e, h2b=h2b: e.activation(out=h2b[:], in_=h2[:], func=AF.Copy), ['h2'], [h2bk])
                        P.dma('sp', H2R[i * 128:(i + 1) * 128, :], h2b[:], [h2bk], [('H2R', i)], h2bk)
                        for k in range(8):
                            P.pe(lambda e, k=k: e.matmul(psum[:, 6, 0:NE], h2f[:, k, :], wr[:, k, :], start=(k == 0), stop=(k == 7)), ['h2f%d' % (k // 4), 'wr'], ['pl'])
                        P.dve(lambda e: e.tensor_tensor(out=lgt[:], in0=psum[:, 6, 0:NE], in1=brt[:], op=ALU.add), ['pl', 'brt'], ['lgt'])
                        P.dve(lambda e: e.max(out=mx8[:], in_=lgt[:]), ['lgt'], ['mx8'])
                        P.dve(lambda e: e.max_index(out=ix8[:], in_max=mx8[:], in_values=lgt[:]), ['lgt', 'mx8'], ['ix8'])
                        P.dve(lambda e: e.tensor_scalar_mul(out=nmx[:], in0=mx8[:, 0:1], scalar1=-1.0), ['mx8'], ['nmx'])
                        P.act(lambda e: e.activation(out=ex[:, 0:4], in_=mx8[:, 0:4], func=AF.Exp, bias=nmx[:, 0:1], scale=1.0), ['mx8', 'nmx'], ['ex'])
                        P.dve(lambda e: e.reduce_sum(out=sm[:], in_=ex[:, 0:4], axis=AX.X), ['ex'], ['sm'])
                        P.dve(lambda e: e.reciprocal(out=sm[:], in_=sm[:]), ['sm'], ['sm'])
                        mw_, mwk = mwr.next()
                        P.dve(lambda e, mw_=mw_: e.tensor_scalar_mul(out=mw_[:, 0:4], in0=ex[:, 0:4], scalar1=sm[:, 0:1]), ['ex', 'sm'], [mwk + 'w'])
                        P.dve(lambda e, mw_=mw_: e.tensor_copy(out=mw_[:, 4:8], in_=ix8[:, 0:4]), ['ix8'], [mwk + 'e'])
                        P.dma('sp', RW[i * 128:(i + 1) * 128, :], mw_[:, 0:4], [mwk + 'w'], [('RW', i)], mwk + 'w')
                        P.dma('sp', RE[i * 128:(i + 1) * 128, :], mw_[:, 4:8], [mwk + 'e'], [('RE', i)], mwk + 'e')
                        p3_marks.append((m_a, m_b, len(P.ops)))
                    pipeline_reorder(P, p3_base, p3_marks)
                    P.flush()

            if 'moe' in phases:
                tiles = out_tiles
                nt = len(tiles)
                T = nt * 128
                t0 = tiles[0] * 128
                NB = (4 * T + NE * (BS - 1) + BS - 1) // BS
                assert NB <= NBMAX
                w_gu_rows = w_gu.rearrange("l e r c -> (l e r) c")
                w_dn_rows = w_dn.rearrange("l e r c -> (l e r) c")
                with contextlib.ExitStack() as st:
                    def sb(name, shape, dt=F32):
                        return st.enter_context(nc.sbuf_tensor(uname(name), list(shape), dt))
                    DKi = sb("DKi", [128, nt, 4], I32)
                    IDXG = sb("IDXG", [128, NB, 8], I32)
                    IDXD = sb("IDXD", [128, NB, 8], I32)
                    EB = sb("EB", [128, NB])
                    IDXB = sb("IDXB", [128, NB], I32)
                    mwd = sb("mwd", [128, nt, NE])
                    iotap = sb("iotap", [128, 1])
                    P.dma('sp', iotap[:], iotap_in, [], ['iotap'], 'iotap')
                    with contextlib.ExitStack() as st2:
                        def sb2(name, shape, dt=F32):
                            return st2.enter_context(nc.sbuf_tensor(uname(name), list(shape), dt))
                        EF = sb2("EF", [128, nt, 4])
                        W4 = sb2("W4", [128, nt, 4])
                        P.dma('sp', EF[:], RE[t0:t0 + T, :].rearrange("(c p) k -> p c k", p=128), [], ['EF'], 'EF')
                        P.dma('sp', W4[:], RW[t0:t0 + T, :].rearrange("(c p) k -> p c k", p=128), [], ['W4'], 'W4')
                        iota32 = sb2("iota32", [128, NE])
                        P.dma('act', iota32[:], iota32_in, [], ['iota32'], 'iota32')
                        lts = sb2("lts", [128, 128])
                        P.dma('act', lts[:], lts_in, [], ['lts'], 'lts')
                        ones = sb2("ones", [128, 128])
                        P.pool(lambda e: e.memset(ones[:], 1.0), [], ['ones'])
                        bst = sb2("bst", [128, NBMAX])
                        P.dma('act', bst[:], bstart_in, [], ['bst'], 'bst')
                        kp = sb2("kp", [128, 8])
                        P.dma('act', kp[:], kp_in, [], ['kp'], 'kp')
                        OH = sb2("OH", [128, 4, nt, NE])
                        for k in range(4):
                            P.dve(lambda e, k=k: e.tensor_tensor(out=OH[:, k, :, :], in0=iota32[:].unsqueeze(1).to_broadcast([128, nt, NE]),
                                                                 in1=EF[:, :, k].unsqueeze(2).to_broadcast([128, nt, NE]), op=ALU.is_equal),
                                  ['iota32', 'EF'], ['OH%d' % k])
                        mask = sb2("mask", [128, nt, NE])
                        P.dve(lambda e: e.tensor_tensor(out=mask[:], in0=OH[:, 0, :, :], in1=OH[:, 1, :, :], op=ALU.add), ['OH0', 'OH1'], ['mask'])
                        P.dve(lambda e: e.tensor_tensor(out=mask[:], in0=mask[:], in1=OH[:, 2, :, :], op=ALU.add), ['mask', 'OH2'], ['mask'])
                        P.dve(lambda e: e.tensor_tensor(out=mask[:], in0=mask[:], in1=OH[:, 3, :, :], op=ALU.add), ['mask', 'OH3'], ['mask'])
                        for j in range(nt):
                            bk = j // 16
                            col = (j % 16) * NE
                            P.pe(lambda e, j=j, bk=bk, col=col: e.matmul(psum[:, bk, col:col + NE], lts[:], mask[:, j, :], start=True, stop=True, skip_group_check=True),
                                 ['lts', 'mask'], ['rk%d' % bk])
                            P.pe(lambda e, j=j, bk=bk, col=col: e.matmul(psum[:, 4 + bk, col:col + NE], ones[:], mask[:, j, :], start=True, stop=True, skip_group_check=True),
                                 ['ones', 'mask'], ['tt%d' % bk])
                        for m in range(nt):
                            P.pe(lambda e, m=m: e.matmul(psum[:, 3, 0:NE], ones[:], mask[:, m, :], start=(m == 0), stop=(m == nt - 1)), ['ones', 'mask'], ['cnt'])
                        TOT = sb2("TOT", [128, nt, NE])
                        PRE = sb2("PRE", [128, nt, NE])
                        for bk in range((nt + 15) // 16):
                            j0 = bk * 16
                            nj = min(16, nt - j0)
                            P.act(lambda e, bk=bk, j0=j0, nj=nj: e.activation(out=TOT[:, j0:j0 + nj, :].rearrange("p j e -> p (j e)"), in_=psum[:, 4 + bk, 0:nj * NE], func=AF.Copy),
                                  ['tt%d' % bk], ['TOT%d' % bk])
                        P.pool(lambda e: e.memset(PRE[:, 0, :], 0.0), [], ['PRE'])
                        for j in range(1, nt):
                            P.dve(lambda e, j=j: e.tensor_tensor(out=PRE[:, j, :], in0=PRE[:, j - 1, :], in1=TOT[:, j - 1, :], op=ALU.add),
                                  ['PRE'] + ['TOT%d' % bk for bk in range((nt + 15) // 16)], ['PRE'])
                        c0 = sb2("c0", [128, NE]); c1 = sb2("c1", [128, NE]); padded = sb2("padded", [128, NE])
                        P.dve(lambda e: e.tensor_scalar_add(out=c0[:], in0=psum[:, 3, 0:NE], scalar1=float(BS - 1)), ['cnt'], ['c0'])
                        ci32 = sb2("ci32", [128, NE], I32)
                        P.dve(lambda e: e.tensor_scalar(out=c1[:], in0=c0[:], scalar1=1.0 / BS, scalar2=-0.5 + 0.5 / BS, op0=ALU.mult, op1=ALU.add), ['c0'], ['c1'])
                        P.dve(lambda e: e.tensor_copy(out=ci32[:], in_=c1[:]), ['c1'], ['ci32'])
                        P.dve(lambda e: e.tensor_copy(out=c1[:], in_=ci32[:]), ['ci32'], ['c1'])
                        P.dve(lambda e: e.tensor_scalar_mul(out=padded[:], in0=c1[:], scalar1=float(BS)), ['c1'], ['padded'])
                        P.dve(lambda e: e.tensor_copy(out=c0[:], in_=padded[:]), ['padded'], ['c0'])
                        cur, nxt_, ck, nk = c0, c1, 'c0', 'c1'
                        for sft in (1, 2, 4, 8, 16):
                            P.dve(lambda e, cur=cur, nxt_=nxt_, sft=sft: e.tensor_copy(out=nxt_[:, 0:sft], in_=cur[:, 0:sft]), [ck], [nk + 'a'])
                            P.dve(lambda e, cur=cur, nxt_=nxt_, sft=sft: e.tensor_tensor(out=nxt_[:, sft:NE], in0=cur[:, sft:NE], in1=cur[:, 0:NE - sft], op=ALU.add), [ck], [nk + 'b'])
                            P.dve(lambda e: e.engine_nop(), [nk + 'a', nk + 'b'], [nk])
                            cur, nxt_, ck, nk = nxt_, cur, nk, ck
                        pend, pendk = cur, ck
                        pstart = sb2("pstart", [128, NE])
                        P.dve(lambda e: e.tensor_tensor(out=pstart[:], in0=pend[:], in1=padded[:], op=ALU.subtract), [pendk, 'padded'], ['pstart'])
                        dfull = sb2("dfull", [128, nt, NE])
                        for bk in range((nt + 15) // 16):
                            j0 = bk * 16
                            nj = min(16, nt - j0)
                            P.dve(lambda e, bk=bk, j0=j0, nj=nj: e.tensor_tensor(out=dfull[:, j0:j0 + nj, :], in0=psum[:, bk, 0:nj * NE].rearrange("p (j e) -> p j e", e=NE),
                                                                                 in1=pstart[:].unsqueeze(1).to_broadcast([128, nj, NE]), op=ALU.add),
                                  ['rk%d' % bk, 'pstart'], ['dfull%d' % bk])
                            P.dve(lambda e, j0=j0, nj=nj: e.tensor_tensor(out=dfull[:, j0:j0 + nj, :], in0=dfull[:, j0:j0 + nj, :], in1=PRE[:, j0:j0 + nj, :], op=ALU.add),
                                  ['dfull%d' % bk, 'PRE'], ['dfull%d' % bk])
                        dkeys = ['dfull%d' % bk for bk in range((nt + 15) // 16)]
                        tmpd = sb2("tmpd", [128, nt, NE])
                        DKf = sb2("DKf", [128, nt, 4])
                        for k in range(4):
                            P.dve(lambda e, k=k: e.tensor_tensor(out=tmpd[:], in0=dfull[:], in1=OH[:, k, :, :], op=ALU.mult), dkeys + ['OH%d' % k], ['tmpd'])
                            P.dve(lambda e, k=k: e.tensor_reduce(out=DKf[:, :, k], in_=tmpd[:], axis=AX.X, op=ALU.add), ['tmpd'], ['DKf%d' % k])
                        P.dve(lambda e: e.tensor_copy(out=DKi[:], in_=DKf[:]), ['DKf%d' % k for k in range(4)], ['DKi'])
                        cmp_ = sb2("cmp", [128, NB, NE])
                        P.dve(lambda e: e.tensor_tensor(out=cmp_[:], in0=pend[:].unsqueeze(1).to_broadcast([128, NB, NE]),
                                                        in1=bst[:, 0:NB].unsqueeze(2).to_broadcast([128, NB, NE]), op=ALU.is_le), [pendk, 'bst'], ['cmp'])
                        P.dve(lambda e: e.tensor_reduce(out=EB[:], in_=cmp_[:], axis=AX.X, op=ALU.add), ['cmp'], ['EB'])
                        P.dve(lambda e: e.tensor_scalar_min(out=EB[:], in0=EB[:], scalar1=float(NE - 1)), ['EB'], ['EB'])
                        idxf = sb2("idxf", [128, NB, 8])
                        P.dve(lambda e: e.scalar_tensor_tensor(out=idxf[:], in0=EB[:].unsqueeze(2).to_broadcast([128, NB, 8]), scalar=float(D),
                                                               in1=kp[:].unsqueeze(1).to_broadcast([128, NB, 8]), op0=ALU.mult, op1=ALU.add), ['EB', 'kp'], ['idxf'])
                        P.dve(lambda e: e.tensor_scalar_add(out=idxf[:], in0=idxf[:], scalar1=float(l * NE * D)), ['idxf'], ['idxf'])
                        P.dve(lambda e: e.tensor_copy(out=IDXD[:], in_=idxf[:]), ['idxf'], ['IDXD'])
                        unused = sb2("unused", [128, NB])
                        P.dve(lambda e: e.tensor_scalar(out=unused[:], in0=bst[:, 0:NB], scalar1=pend[:, NE - 1:NE], scalar2=1.0e6, op0=ALU.is_ge, op1=ALU.mult), ['bst', pendk], ['unused'])
                        P.dve(lambda e: e.tensor_tensor(out=idxf[:], in0=idxf[:], in1=unused[:].unsqueeze(2).to_broadcast([128, NB, 8]), op=ALU.add), ['idxf', 'unused'], ['idxf'])
                        P.dve(lambda e: e.tensor_copy(out=IDXG[:], in_=idxf[:]), ['idxf'], ['IDXG'])
                        idxbf = sb2("idxbf", [128, NB])
                        P.dve(lambda e: e.tensor_scalar(out=idxbf[:], in0=EB[:], scalar1=128.0, scalar2=float(l * NE * 128), op0=ALU.mult, op1=ALU.add), ['EB'], ['idxbf'])
                        P.dve(lambda e: e.tensor_scalar(out=idxbf[:], in0=idxbf[:], scalar1=iotap[:, 0:1], scalar2=None, op0=ALU.add), ['idxbf', 'iotap'], ['idxbf'])
                        P.dve(lambda e: e.tensor_copy(out=IDXB[:], in_=idxbf[:]), ['idxbf'], ['IDXB'])
                        for k in range(4):
                            dst_ = mwd if k == 0 else tmpd
                            P.dve(lambda e, k=k, dst_=dst_: e.tensor_tensor(out=dst_[:], in0=OH[:, k, :, :], in1=W4[:, :, k].unsqueeze(2).to_broadcast([128, nt, NE]), op=ALU.mult),
                                  ['OH%d' % k, 'W4', 'DKf0', 'DKf1', 'DKf2', 'DKf3'], ['mwd' if k == 0 else 'tmpd'])
                            if k > 0:
                                P.dve(lambda e: e.tensor_tensor(out=mwd[:], in0=mwd[:], in1=tmpd[:], op=ALU.add), ['mwd', 'tmpd'], ['mwd'])
                        hr = Ring('hr', [sb2("hr%d" % i, [128, D], BF16) for i in range(3)])
                        for j, i in enumerate(tiles):
                            h_, hk_ = hr.next()
                            P.dma('sp', h_[:], H2R[i * 128:(i + 1) * 128, :], [], [hk_], hk_)
                            for k in range(4):
                                P.add('pool', lambda e, h_=h_, j=j, k=k: e.indirect_dma_start(out=XS[:, :], out_offset=bass.IndirectOffsetOnAxis(ap=DKi[:, j, k:k + 1], axis=0),
                                                                                          in_=h_[:, :], in_offset=None), [hk_, 'DKi'], [('XS', j, k)], 'xsc%d' % ((j * 4 + k) % 4))
                                P.add('pool', lambda e, j=j, k=k: e.indirect_dma_start(out=SW[:, :], out_offset=bass.IndirectOffsetOnAxis(ap=DKi[:, j, k:k + 1], axis=0),
                                                                                    in_=W4[:, j, k:k + 1], in_offset=None), ['W4', 'DKi'], [('SW', j, k)], 'swc%d' % ((j * 4 + k) % 4))
                        P.flush()
                    with contextlib.ExitStack() as st2:
                        def sb2(name, shape, dt=F32):
                            return st2.enter_context(nc.sbuf_tensor(uname(name), list(shape), dt))
                        identb = sb2("identb", [128, 128], BF16)
                        P.act(lambda e: e.activation(out=identb[:], in_=ident[:], func=AF.Copy), ['ident'], ['identb'])
                        bgr = sb2("bgr", [NE, 2 * D])
                        P.dma('act', bgr[:], b_gu[l], [], ['bgr'], 'bgr')
                        for c in range(16):
                            P.pe(lambda e, c=c: e.transpose(psum[:, 6, c * NE:(c + 1) * NE], bgr[:, c * 128:(c + 1) * 128], ident[0:NE, 0:NE]), ['bgr', 'ident'], ['pX0'])
                        X2 = sb2("X2", [128, NE, 16])
                        P.dve(lambda e: e.tensor_copy(out=X2[:], in_=psum[:, 6, :].rearrange("p (c e) -> p e c", e=NE)), ['pX0'], ['X2'])
                        P.dve(lambda e: e.tensor_scalar_add(out=X2[:, :, 8:16], in0=X2[:, :, 8:16], scalar1=1.0), ['X2'], ['X2'])
                        P.dma('sp', BGT[l * NE * 128:(l + 1) * NE * 128, :].rearrange("(e p) c -> p e c", p=128), X2[:], ['X2'], ['BGT'], 'BGT')
                        bbr = Ring('bb', [sb2("bb%d" % i, [128, 16]) for i in range(2)])
                        wgr = Ring('WG', [sb2("WG%d" % i, [128, 8, 2 * D], BF16) for i in range(2)])
                        wdr = Ring('WD', [sb2("WD%d" % i, [128, 8, D], BF16) for i in range(2)])
                        xbr = Ring('XB', [sb2("XB%d" % i, [128, 4, D], BF16) for i in range(2)])
                        XTr = [sb2("XT%d" % i, [128, 8, BS], BF16) for i in range(2)]
                        actr = Ring('act', [sb2("act%d" % i, [128, 8, BS], BF16) for i in range(2)])
                        Ar = Ring('A', [sb2("A%d" % i, [128, BS]) for i in range(2)])
                        Sr = Ring('Sg', [sb2("Sg%d" % i, [128, BS]) for i in range(2)])
                        Ur = Ring('U', [sb2("U%d" % i, [128, BS]) for i in range(2)])
                        swr = Ring('swb', [sb2("swb%d" % i, [128, 4]) for i in range(2)])
                        Yr = Ring('Y', [sb2("Y%d" % i, [128, 4, D]) for i in range(1)])
                        gbr = Ring('pg', [0, 1]); ubr = Ring('pu', [2, 3]); ybr = Ring('py', [4, 5])
                        psb = [psum[:, 6, :].bitcast(BF16), psum[:, 7, :].bitcast(BF16)]

                        def load_blk(b):
                            WG, wgk = wgr.next()
                            WD, wdk = wdr.next()
                            for k in range(8):
                                P.add('pool', lambda e, WG=WG, b=b, k=k: e.indirect_dma_start(out=WG[:, k, :], out_offset=None, in_=w_gu_rows[:, :],
                                                                                             in_offset=bass.IndirectOffsetOnAxis(ap=IDXD[:, b, k:k + 1], axis=0)),
                                      ['IDXG'], [wgk + str(k)], wgk + str(k))
                            for k in range(8):
                                P.add('pool', lambda e, WD=WD, b=b, k=k: e.indirect_dma_start(out=WD[:, k, :], out_offset=None, in_=w_dn_rows[:, :],
                                                                                             in_offset=bass.IndirectOffsetOnAxis(ap=IDXD[:, b, k:k + 1], axis=0)),
                                      ['IDXD'], [wdk + str(k)], wdk + str(k))
                            XB, xbk = xbr.next()
                            P.dma('sp', XB[:], XS[b * BS:(b + 1) * BS, :].rearrange("(s p) d -> p s d", p=128), [], [xbk], xbk)
                            swb, swk = swr.next()
                            P.dma('act', swb[:], SW[b * BS:(b + 1) * BS, :].rearrange("(s p) o -> p (s o)", p=128), [], [swk], swk, allow_slow_non_contiguous=True)
                            bb, bbk = bbr.next()
                            P.add('pool', lambda e, bb=bb, b=b: e.indirect_dma_start(out=bb[:, :], out_offset=None, in_=BGT[:, :],
                                                                                   in_offset=bass.IndirectOffsetOnAxis(ap=IDXB[:, b:b + 1], axis=0)),
                                  ['IDXB', 'BGT'], [bbk], bbk)
                            return WG, wgk, WD, wdk, XB, xbk, swb, swk, bb, bbk
                        def transposes(b, XB, xbk):
                            XT = XTr[b % 2]
                            for half in range(2):
                                for s2 in range(2):
                                    sidx = half * 2 + s2
                                    for k in range(8):
                                        P.pe(lambda e, XB=XB, sidx=sidx, k=k, s2=s2: e.transpose(psb[s2][:, k * 128:(k + 1) * 128], XB[:, sidx, k * 128:(k + 1) * 128], identb[:]),
                                             [xbk, 'identb'], ['pX%d' % s2])
                                    P.act(lambda e, sidx=sidx, s2=s2, XT=XT: e.activation(out=XT[:, :, sidx * 128:(sidx + 1) * 128], in_=psb[s2].rearrange("p (k n) -> p k n", n=128), func=AF.Copy),
                                          ['pX%d' % s2], ['XT%d_%d' % (b % 2, sidx)])
                        nxt = load_blk(0)
                        transposes(0, nxt[4], nxt[5])
                        for b in range(NB):
                            WG, wgk, WD, wdk, XB, xbk, swb, swk, bb, bbk = nxt
                            if b + 1 < NB:
                                nxt = load_blk(b + 1)
                            wgkeys = [wgk + str(k) for k in range(8)]
                            wdkeys = [wdk + str(k) for k in range(8)]
                            XT = XTr[b % 2]
                            xtkeys = ['XT%d_%d' % (b % 2, q) for q in range(4)]
                            at, atk = actr.next()
                            for c in range(8):
                                gb, gbk = gbr.next()
                                ub, ubk = ubr.next()
                                for k in range(8):
                                    P.pe(lambda e, gb=gb, WG=WG, k=k, c=c, XT=XT: e.matmul(psum[:, gb, :], WG[:, k, c * 128:(c + 1) * 128], XT[:, k, :], start=(k == 0), stop=(k == 7)),
                                         wgkeys + xtkeys, [gbk])
                                for k in range(8):
                                    P.pe(lambda e, ub=ub, WG=WG, k=k, c=c, XT=XT: e.matmul(psum[:, ub, :], WG[:, k, D + c * 128:D + (c + 1) * 128], XT[:, k, :], start=(k == 0), stop=(k == 7)),
                                         wgkeys + xtkeys, [ubk])
                                A, Ak = Ar.next(); S_, Sk = Sr.next(); U, Uk = Ur.next()
                                P.dve(lambda e, A=A, gb=gb, bb=bb, c=c: e.tensor_scalar(out=A[:], in0=psum[:, gb, :], scalar1=bb[:, c:c + 1], scalar2=7.0, op0=ALU.add, op1=ALU.min), [gbk, bbk], [Ak])
                                P.act(lambda e, A=A, S_=S_: e.activation(out=S_[:], in_=A[:], func=AF.Sigmoid, scale=1.702), [Ak], [Sk])
                                P.dve(lambda e, U=U, ub=ub, bb=bb, c=c: e.tensor_scalar(out=U[:], in0=psum[:, ub, :], scalar1=bb[:, 8 + c:9 + c], scalar2=8.0, op0=ALU.add, op1=ALU.min), [ubk, bbk], [Uk])
                                P.pool(lambda e, A=A, S_=S_: e.tensor_tensor(out=A[:], in0=A[:], in1=S_[:], op=ALU.mult), [Ak, Sk], [Ak])
                                P.dve(lambda e, at=at, c=c, U=U, A=A: e.scalar_tensor_tensor(out=at[:, c, :], in0=U[:], scalar=-6.0, in1=A[:], op0=ALU.max, op1=ALU.mult), [Uk, Ak], [atk + str(c)])
                            atkeys = [atk + str(c) for c in range(8)]
                            if b + 1 < NB:
                                transposes(b + 1, nxt[4], nxt[5])
                            Y, Yk = Yr.next()
                            for s4 in range(4):
                                for nh in range(2):
                                    yb_, ybk = ybr.next()
                                    for c in range(8):
                                        P.pe(lambda e, yb_=yb_, at=at, c=c, s4=s4, WD=WD, nh=nh: e.matmul(psum[:, yb_, :], at[:, c, s4 * 128:(s4 + 1) * 128], WD[:, c, nh * 512:(nh + 1) * 512],
                                                                                                          start=(c == 0), stop=(c == 7)), atkeys + wdkeys, [ybk])
                                    if yb_ == 4:
                                        P.dve(lambda e, Y=Y, s4=s4, nh=nh, swb=swb: e.tensor_scalar_mul(out=Y[:, s4, nh * 512:(nh + 1) * 512], in0=psum[:, 4, :], scalar1=swb[:, s4:s4 + 1]),
                                              [ybk, swk], [Yk + '%d%d' % (s4, nh)])
                                    else:
                                        P.act(lambda e, Y=Y, s4=s4, nh=nh, swb=swb: e.activation(out=Y[:, s4, nh * 512:(nh + 1) * 512], in_=psum[:, 5, :], func=AF.Copy, scale=swb[:, s4:s4 + 1]),
                                              [ybk, swk], [Yk + '%d%d' % (s4, nh)])
                            P.dma('sp', YS[b * BS:(b + 1) * BS, :].rearrange("(s p) d -> p s d", p=128), Y[:], [Yk + '%d%d' % (q, r_) for q in range(4) for r_ in range(2)], [('YS', b)], Yk)
                        P.flush()
                    with contextlib.ExitStack() as st3:
                        def sb3(name, shape, dt=F32):
                            return st3.enter_context(nc.sbuf_tensor(uname(name), list(shape), dt))
                        g2 = [sb3("g2_%d" % s_, [128, D]) for s_ in range(2)]
                        for s_ in range(2):
                            bload('sp', g2[s_][:], modv[l, s_, 5 * D:6 * D], 'g2_%d' % s_)
                        lng = sb3("lng2", [128, D]); lnb = sb3("lnb2", [128, D])
                        bload('act', lng[:], ln_g[l, 1], 'lng')
                        bload('act', lnb[:], ln_b[l, 1], 'lnb')
                        x1r = Ring('xm', [sb3("xm%d" % i, [128, D]) for i in range(4)])
                        Gr = Ring('G', [sb3("G%d" % i, [128, 4, D]) for i in range(4)])
                        rr = sb3("rr2", [128, D])
                        xor_ = Ring('xo', [sb3("xo%d" % i, [128, D]) for i in range(2)])
                        st6 = sb3("st6b", [128, 2, 6]); mv = sb3("mvb", [128, 2]); rs = sb3("rsb", [128, 1])
                        bdr = sb3("bdr", [NE, D])
                        P.dma('act', bdr[:], b_dn[l], [], ['bdr'], 'bdr')
                        mwTr = Ring('mwT', [sb3("mwT%d" % i, [NE, 128]) for i in range(2)])
                        tbr = Ring('tb', [0, 1]); bbk2 = Ring('bd', [(2, 3), (4, 5)])
                        for j, i in enumerate(tiles):
                            s_ = 1 if i < NT_C else 0
                            tb_, tbk = tbr.next()
                            P.pe(lambda e, j=j, tb_=tb_: e.transpose(psum[0:NE, tb_, 0:128], mwd[:, j, :], ident[:]), ['ident'], [tbk])
                            mwT, mwTk = mwTr.next()
                            P.act(lambda e, mwT=mwT, tb_=tb_: e.activation(out=mwT[:], in_=psum[0:NE, tb_, 0:128], func=AF.Copy), [tbk], [mwTk])
                            (bd0, bd1), bdk = bbk2.next()
                            for nh, bdb_ in enumerate((bd0, bd1)):
                                P.pe(lambda e, mwT=mwT, nh=nh, bdb_=bdb_: e.matmul(psum[:, bdb_, :], mwT[:], bdr[:, nh * 512:(nh + 1) * 512], start=True, stop=True), [mwTk, 'bdr'], [bdk + str(nh)])
                            x1, x1k = x1r.next()
                            P.dma('sp', x1[:], XM[i * 128:(i + 1) * 128, :], [], [x1k], x1k)
                            G, Gk = Gr.next()
                            for k in range(4):
                                P.add('pool', lambda e, G=G, j=j, k=k: e.indirect_dma_start(out=G[:, k, :], out_offset=None, in_=YS[:, :],
                                                                                         in_offset=bass.IndirectOffsetOnAxis(ap=DKi[:, j, k:k + 1], axis=0)), ['DKi'], [Gk + str(k)], Gk + str(k))
                            P.dve(lambda e, G=G: e.tensor_tensor(out=G[:, 0, :], in0=G[:, 0, :], in1=G[:, 1, :], op=ALU.add), [Gk + '0', Gk + '1'], [Gk + '0'])
                            P.dve(lambda e, G=G: e.tensor_tensor(out=G[:, 2, :], in0=G[:, 2, :], in1=G[:, 3, :], op=ALU.add), [Gk + '2', Gk + '3'], [Gk + '2'])
                            P.dve(lambda e, G=G: e.tensor_tensor(out=G[:, 0, :], in0=G[:, 0, :], in1=G[:, 2, :], op=ALU.add), [Gk + '0', Gk + '2'], [Gk + '0'])
                            for nh, bdb_ in enumerate((bd0, bd1)):
                                P.dve(lambda e, G=G, nh=nh, bdb_=bdb_: e.tensor_tensor(out=G[:, 0, nh * 512:(nh + 1) * 512], in0=G[:, 0, nh * 512:(nh + 1) * 512], in1=psum[:, bdb_, :], op=ALU.add),
                                      [Gk + '0', bdk + str(nh)], [Gk + '0'])
                            P.dve(lambda e, G=G, s_=s_: e.tensor_tensor(out=rr[:], in0=G[:, 0, :], in1=g2[s_][:], op=ALU.mult), [Gk + '0', 'g2_%d' % s_], ['rr'])
                            P.dve(lambda e, x1=x1: e.scalar_tensor_tensor(out=rr[:], in0=x1[:], scalar=ALPHA, in1=rr[:], op0=ALU.mult, op1=ALU.add), [x1k, 'rr'], ['rr'])
                            for nh in range(2):
                                P.dve(lambda e, nh=nh: e.bn_stats(out=st6[:, nh, :], in_=rr[:, nh * 512:(nh + 1) * 512]), ['rr'], ['st6_%d' % nh])
                            P.dve(lambda e: e.bn_aggr(out=mv[:], in_=st6[:]), ['st6_0', 'st6_1'], ['mv'])
                            P.dve(lambda e: e.tensor_scalar_add(out=rs[:], in0=mv[:, 1:2], scalar1=EPS), ['mv'], ['rs'])
                            P.act(lambda e: e.activation(out=rs[:], in_=rs[:], func=AF.Sqrt), ['rs'], ['rs'])
                            P.dve(lambda e: e.reciprocal(out=rs[:], in_=rs[:]), ['rs'], ['rs'])
                            xo, xok = xor_.next()
                            P.dve(lambda e, xo=xo: e.tensor_scalar(out=xo[:], in0=rr[:], scalar1=mv[:, 0:1], scalar2=rs[:, 0:1], op0=ALU.subtract, op1=ALU.mult), ['rr', 'mv', 'rs'], [xok])
                            P.dve(lambda e, xo=xo: e.tensor_tensor(out=xo[:], in0=xo[:], in1=lng[:], op=ALU.mult), [xok, 'lng'], [xok])
                            P.dve(lambda e, xo=xo: e.tensor_tensor(out=xo[:], in0=xo[:], in1=lnb[:], op=ALU.add), [xok, 'lnb'], [xok])
                            dst = XS0[i * 128:(i + 1) * 128, :] if not last else out[(i - NT_C) * 128:(i - NT_C + 1) * 128, :]
                            P.dma('sp', dst, xo[:], [xok], [('xout', i)], xok)
                        P.flush()
        P.flush()
    return nc


def make_consts():
    inv = (10000.0 ** (-np.arange(0, 32, 2, dtype=np.float32) / 32.0)).astype(np.float32)
    t = np.arange(L)
    row = (t // 64).astype(np.float32)
    col = (t % 64).astype(np.float32)
    ang = np.stack([row[:, None] * inv, col[:, None] * inv], axis=1)
    cos = np.cos(ang).astype(np.float32)
    sin = np.sin(ang).astype(np.float32)
    c64 = np.stack([cos, cos], axis=2).reshape(L, 64)
    s64 = np.stack([-sin, sin], axis=2).reshape(L, 64)
    p = np.arange(128, dtype=np.float32)
    pos = np.stack([127 - p, p, p + 1, 128 - p], axis=1)
    dif = p[None, :] - p[:, None]
    return dict(
        c_ident=np.eye(128, dtype=np.float32),
        c_cos=np.ascontiguousarray(np.tile(c64, (1, 8))),
        c_sin=np.ascontiguousarray(np.tile(s64, (1, 8))),
        c_pos=np.ascontiguousarray(pos.astype(np.float32)),
        c_dpos=np.maximum(dif, 0).astype(np.float32),
        c_dneg=np.maximum(-dif, 0).astype(np.float32),
        c_mge=(dif >= 0).astype(np.float32),
        c_zeros=np.zeros((2, 256), np.float32),
        c_iota32=np.tile(np.arange(NE, dtype=np.float32)[None, :], (128, 1)),
        c_lts=(p[:, None] < p[None, :]).astype(np.float32),
        c_bstart=np.tile((np.arange(NBMAX, dtype=np.float32) * BS)[None, :], (128, 1)),
        c_kp=(np.arange(8, dtype=np.float32)[None, :] * 128 + p[:, None]).astype(np.float32),
        c_iotap=p[:, None].astype(np.float32).copy(),
    )


WKEYS = ['w_mod', 'b_mod', 'w_in', 'conv_w', 'ret_decay_exp', 'ret_gn_g', 'q_norm_g', 'k_norm_g', 'w_out',
         'ln_g', 'ln_b', 'w_router', 'b_router', 'w_gate_up', 'b_gate_up', 'w_down', 'b_down']


def make_in_maps(inputs, cores, skip=()):
    consts = make_consts()
    shared = {k: np.ascontiguousarray(np.asarray(inputs[k], np.float32)) for k in WKEYS if k not in skip}
    shared['c_ctx'] = np.ascontiguousarray(np.asarray(inputs['c_ctx'], np.float32))
    shared.update(consts)
    maps = []
    for b in cores:
        m = dict(shared)
        m['x'] = np.ascontiguousarray(np.asarray(inputs['x'][b], np.float32))
        m['c'] = np.ascontiguousarray(np.asarray(inputs['c'][b], np.float32))
        m['ctx'] = np.ascontiguousarray(np.asarray(inputs['ctx'][b], np.float32))
        maps.append(m)
    return maps


def kernel(**inputs):
    nc = build()
    maps = make_in_maps(inputs, list(range(8)))
    res = run_bass_kernel_spmd(nc, maps, core_ids=list(range(8)))
    return np.stack([np.asarray(r["out"], np.float32) for r in res.results], axis=0)
```

```python
import contextlib
import math
import numpy as np
import concourse.bass as bass
import concourse.mybir as mybir
from concourse.bass_utils import run_bass_kernel_spmd

F32 = mybir.dt.float32
BF16 = mybir.dt.bfloat16
ALU = mybir.AluOpType
AF = mybir.ActivationFunctionType
AX = mybir.AxisListType

D = 1024
L = 4096
LC = 256
NT_C = 2
NT = 34
DEPTH = 2
NE = 32
BS = 512
NBMAX = 66
I32 = mybir.dt.int32
U32 = mybir.dt.uint32
ALPHA = (2.0 * DEPTH) ** 0.25
EPS = 1e-6


class Prog:
    ENGS = ('pe', 'act', 'dve', 'pool', 'sp')

    def __init__(self, nc, stack):
        self.nc = nc
        self.ops = []
        self.sems = {}
        self.stack = stack
        for eng in ('pe', 'act', 'dve', 'pool'):
            self.sems[('e', eng)] = stack.enter_context(nc.semaphore('sem_' + eng))
        self.cnt = {}
        self.streams = {}
        self.pool_cnt = []
        self.pool_idx = {}
        self.waited = {e: {} for e in self.ENGS}
        self.total_ops = 0

    def add(self, eng, fn, reads=(), writes=(), stream=None):
        self.ops.append((eng, fn, tuple(reads), tuple(writes), stream))

    def pe(self, fn, reads=(), writes=()):
        self.add('pe', fn, reads, writes)

    def act(self, fn, reads=(), writes=()):
        self.add('act', fn, reads, writes)

    def dve(self, fn, reads=(), writes=()):
        self.add('dve', fn, reads, writes)

    def pool(self, fn, reads=(), writes=()):
        self.add('pool', fn, reads, writes)

    def dma(self, q, out, in_, reads, writes, stream, **kw):
        self.add(q, lambda e: e.dma_start(out=out, in_=in_, **kw), reads, writes, stream)

    def flush(self):
        nc = self.nc
        ops = self.ops
        self.ops = []
        n = len(ops)
        if n == 0:
            return
        self.total_ops += n
        last_writer = {}
        readers = {}
        deps = [None] * n
        for i, (eng, fn, rd, wr, st) in enumerate(ops):
            d = set()
            for r in rd:
                j = last_writer.get(r)
                if j is not None:
                    d.add((j, 0))
            for w in wr:
                j = last_writer.get(w)
                if j is not None:
                    d.add((j, 1))
                for k in readers.get(w, ()):
                    if k != i:
                        d.add((k, 2))
            deps[i] = d
            for r in rd:
                readers.setdefault(r, []).append(i)
            for w in wr:
                last_writer[w] = i
                readers[w] = []
        sig = [False] * n
        need = [None] * n
        last_compute = {}
        for i in range(n):
            eng = ops[i][0]
            lst = set()
            for (j, kind) in deps[i]:
                jeng, _, _, _, jst = ops[j]
                if jst is None and jeng == eng:
                    if eng == 'pe' or eng == 'sp':
                        continue
                    if kind == 2:
                        continue
                if jst is None:
                    sig[j] = True
                lst.add(j)
            need[i] = lst
            if ops[i][4] is None and ops[i][1] is not None:
                last_compute[eng] = i
        for eng, i in last_compute.items():
            if eng != 'sp':
                sig[i] = True
        sval = [None] * n
        phase_map = {}
        for i, (eng, fn, rd, wr, st) in enumerate(ops):
            if st is not None:
                kind = 'w' if eng == 'pool' else 'h'
                if (kind, st) not in phase_map:
                    nk = sum(1 for kk in phase_map if kk[0] == kind)
                    k = kind + str(nk)
                    phase_map[(kind, st)] = k
                    if k not in self.pool_idx:
                        self.pool_idx[k] = len(self.pool_cnt)
                        self.pool_cnt.append(0)
                        self.sems[('s', self.pool_idx[k])] = self.stack.enter_context(nc.semaphore('sd_%s' % k))
                k = self.pool_idx[phase_map[(kind, st)]]
                self.pool_cnt[k] += 1
                sval[i] = (('s', k), 16 * self.pool_cnt[k])
            elif sig[i]:
                self.cnt[eng] = self.cnt.get(eng, 0) + 1
                sval[i] = (('e', eng), self.cnt[eng])
        per_eng = {e: [] for e in self.ENGS}
        for i, op in enumerate(ops):
            per_eng[op[0]].append(i)
        sems = self.sems
        final = {}
        for eng in ('pe', 'act', 'dve', 'pool'):
            if self.cnt.get(eng, 0) > 0:
                final[('e', eng)] = self.cnt[eng]
        for k, c in enumerate(self.pool_cnt):
            final[('s', k)] = 16 * c

        def run(engname, e):
            waited = self.waited[engname]
            for i in per_eng[engname]:
                _, fn, rd, wr, st = ops[i]
                w = {}
                for j in need[i]:
                    key, val = sval[j]
                    if w.get(key, 0) < val:
                        w[key] = val
                for key, val in w.items():
                    if waited.get(key, 0) >= val:
                        continue
                    waited[key] = val
                    e.wait_ge(sems[key], val)
                if fn is None:
                    continue
                ins = fn(e)
                if sval[i] is not None:
                    key, val = sval[i]
                    ins.then_inc(sems[key], 16 if key[0] == 's' else 1)
            for key, val in final.items():
                if key == ('e', engname):
                    continue
                if waited.get(key, 0) >= val:
                    continue
                waited[key] = val
                e.wait_ge(sems[key], val)

        with nc.Block() as block:
            @block.tensor
            def _(e):
                run('pe', e)

            @block.scalar
            def _(e):
                run('act', e)

            @block.vector
            def _(e):
                run('dve', e)

            @block.gpsimd
            def _(e):
                run('pool', e)

            @block.sync
            def _(e):
                run('sp', e)


def pipeline_reorder(P, base, marks):
    ops = P.ops
    H = [ops[a:b] for (a, b, c) in marks]
    T = [ops[b:c] for (a, b, c) in marks]
    new = list(ops[:base]) + H[0]
    for i in range(len(marks)):
        if i + 1 < len(marks):
            new += H[i + 1]
        new += T[i]
    new += list(ops[marks[-1][2]:])
    assert len(new) == len(ops)
    P.ops = new


class Ring:
    def __init__(self, name, tiles):
        self.name = name
        self.tiles = tiles
        self.i = 0

    def next(self):
        k = self.i % len(self.tiles)
        self.i += 1
        return self.tiles[k], '%s%d' % (self.name, k)


SEC = dict(u=(0, 256), B=(256, 256), C=(512, 256), rq=(768, 256), rk=(1024, 256), rv=(1280, 256),
           rg=(1536, 256), aq=(1792, 512), ak=(2304, 128), av=(2432, 128))
MYCOL = dict(u=0, C=256, B=512, rv=768, rq=1024, rk=1280, aq=1536, rg=2048, ak=2304, av=2432)


def build(phases=('p0', 'p1', 'att', 'ret', 'p3', 'moe'), layers=(0, 1), debug=False, moe_experts=NE, cut=99):
    nc = bass.Bass("TRN2", target_bir_lowering=False)

    _uc = [0]

    def uname(name):
        _uc[0] += 1
        return '%s_u%d' % (name, _uc[0])

    def din(name, shape, dt=F32):
        return nc.dram_tensor(name, list(shape), dt, kind="ExternalInput").ap()

    def dscr(name, shape, dt=F32):
        return nc.dram_tensor(name, list(shape), dt, kind=("ExternalOutput" if debug else "Internal")).ap()

    x_in = din("x", [L, D])
    c_in = din("c", [D])
    ctx_in = din("ctx", [LC, D])
    cctx_in = din("c_ctx", [D])
    w_mod = din("w_mod", [DEPTH, D, 6 * D])
    b_mod = din("b_mod", [DEPTH, 6 * D])
    w_in = din("w_in", [DEPTH, D, 2560])
    conv_w = din("conv_w", [DEPTH, 256, 3])
    rde = din("ret_decay_exp", [DEPTH, 2, 4])
    gn_g = din("ret_gn_g", [DEPTH, 256])
    qn_g = din("q_norm_g", [DEPTH, 64])
    kn_g = din("k_norm_g", [DEPTH, 64])
    w_out = din("w_out", [DEPTH, D, D])
    ln_g = din("ln_g", [DEPTH, 2, D])
    ln_b = din("ln_b", [DEPTH, 2, D])
    w_router = din("w_router", [DEPTH, D, NE])
    b_router = din("b_router", [DEPTH, NE])
    if 'moe' in phases:
        w_gu = din("w_gate_up", [DEPTH, NE, D, 2 * D])
        b_gu = din("b_gate_up", [DEPTH, NE, 2 * D])
        w_dn = din("w_down", [DEPTH, NE, D, D])
        b_dn = din("b_down", [DEPTH, NE, D])
    ident_in = din("c_ident", [128, 128])
    cos_in = din("c_cos", [L, 512])
    sin_in = din("c_sin", [L, 512])
    pos_in = din("c_pos", [128, 4])
    dpos_in = din("c_dpos", [128, 128])
    dneg_in = din("c_dneg", [128, 128])
    mge_in = din("c_mge", [128, 128])
    zeros_in = din("c_zeros", [2, 256])
    iota32_in = din("c_iota32", [128, NE])
    lts_in = din("c_lts", [128, 128])
    bstart_in = din("c_bstart", [128, NBMAX])
    kp_in = din("c_kp", [128, 8])
    iotap_in = din("c_iotap", [128, 1])

    out = nc.dram_tensor("out", [L, D], F32, kind="ExternalOutput").ap()

    N = NT * 128
    modv = dscr("modv", [DEPTH, 2, 6 * D])
    PB = dscr("PB", [N + 4, 512])
    RQT = dscr("RQT", [3, 4, 64, N], BF16)
    RKT = dscr("RKT", [4, 64, N], BF16)
    RTOK = dscr("RTOK", [N, 768], BF16)
    RG = dscr("RG", [N, 256])
    AQT = dscr("AQT", [8, 64, N], BF16)
    AKT = dscr("AKT", [2, 64, N], BF16)
    AV = dscr("AV", [N, 128], BF16)
    MIX = dscr("MIX", [N, 1024])
    XS0 = dscr("XS0", [N, D])
    XM = dscr("XM", [N, D])
    H2R = dscr("H2R", [N, D], BF16)
    RW = dscr("RW", [N, 4])
    RE = dscr("RE", [N, 4])
    XS = dscr("XS", [NBMAX * BS, D], BF16)
    SW = dscr("SW", [NBMAX * BS, 1])
    YS = dscr("YS", [NBMAX * BS, D])
    BGT = dscr("BGT", [DEPTH * NE * 128, 16])

    def prow(i):
        return 1 + i * 128 if i < NT_C else 259 + (i - NT_C) * 128

    with contextlib.ExitStack() as gst:
        P = Prog(nc, gst)
        psum = gst.enter_context(nc.psum_tensor("psum", [128, 8, 512], F32))
        ident = gst.enter_context(nc.sbuf_tensor("ident", [128, 128], F32))
        P.dma('sp', ident[:], ident_in, [], ['ident'], 'ident')

        def bank(b):
            return psum[:, b, :]

        if 'p0' in phases:
            with contextlib.ExitStack() as st:
                def sb(name, shape, dt=F32):
                    return st.enter_context(nc.sbuf_tensor(uname(name), list(shape), dt))
                cnd = sb("cnd", [128, 2, 8])
                P.dma('sp', cnd[:, 0, :], c_in.rearrange("(k p) -> p k", p=128), [], ['cnd'], 'cnd0',
                      allow_slow_non_contiguous=True)
                P.dma('sp', cnd[:, 1, :], cctx_in.rearrange("(k p) -> p k", p=128), [], ['cnd'], 'cnd1',
                      allow_slow_non_contiguous=True)
                P.act(lambda e: e.activation(out=cnd[:], in_=cnd[:], func=AF.Silu), ['cnd'], ['cnd'])
                wring = Ring('wm', [sb("wm%d" % i, [128, 8, 512]) for i in range(6)])
                bring = Ring('bm', [sb("bm%d" % i, [2, 512]) for i in range(2)])
                oring = Ring('om', [sb("om%d" % i, [2, 512]) for i in range(2)])
                for l in layers:
                    for n in range(12):
                        wt, wk = wring.next()
                        bt, bk = bring.next()
                        ot, ok = oring.next()
                        P.dma('sp' if n % 2 == 0 else 'act', wt[:], w_mod[l, :, n * 512:(n + 1) * 512].rearrange("(k p) n -> p k n", p=128),
                              [], [wk], wk)
                        P.dma('sp', bt[:], b_mod[l, n * 512:(n + 1) * 512].partition_broadcast(2), [], [bk], bk)
                        pbank = n % 2
                        pb_ = 'ps0_%d' % pbank
                        for k in range(8):
                            P.pe(lambda e, k=k, wt=wt, pbank=pbank: e.matmul(psum[0:2, pbank, :], cnd[:, :, k], wt[:, k, :],
                                                                            start=(k == 0), stop=(k == 7)),
                                 ['cnd', wk], [pb_])
                        P.dve(lambda e, ot=ot, bt=bt, pbank=pbank: e.tensor_tensor(out=ot[:], in0=psum[0:2, pbank, :], in1=bt[:], op=ALU.add),
                              [pb_, bk], [ok])
                        if n in (2, 3, 8, 9):
                            P.dve(lambda e, ot=ot: e.tensor_scalar_add(out=ot[:], in0=ot[:], scalar1=1.0), [ok], [ok])
                        P.dma('sp', modv[l, :, n * 512:(n + 1) * 512], ot[:], [ok], [('modv', l)], ok)
                P.flush()

        def bload(q, tile_ap, vec_ap, key, reads=()):
            P.dma(q, tile_ap, vec_ap.partition_broadcast(128), list(reads), [key], key)

        for l in layers:
            last = (l == DEPTH - 1)
            xsrc = (lambda i: (ctx_in[i * 128:(i + 1) * 128, :] if i < NT_C else x_in[(i - NT_C) * 128:(i - NT_C + 1) * 128, :])) \
                if l == 0 else (lambda i: XS0[i * 128:(i + 1) * 128, :])
            xs_key = (lambda i: ('xin', i)) if l == 0 else (lambda i: ('XS0', i))
            out_tiles = list(range(NT)) if not last else list(range(NT_C, NT))

            if 'p1' in phases:
                with contextlib.ExitStack() as st:
                    def sb(name, shape, dt=F32):
                        return st.enter_context(nc.sbuf_tensor(uname(name), list(shape), dt))
                    win = sb("win", [128, 8, 2560], BF16)
                    for name, (c0, w) in SEC.items():
                        m0 = MYCOL[name]
                        P.dma('pool', win[:, :, m0:m0 + w], w_in[l, :, c0:c0 + w].rearrange("(k p) n -> p k n", p=128),
                              [], ['win_' + name], 'win_' + name)
                    winkeys = ['win_' + k for k in SEC]
                    sc1 = [sb("sc1_%d" % s, [128, D]) for s in range(2)]
                    sh1 = [sb("sh1_%d" % s, [128, D]) for s in range(2)]
                    for s in range(2):
                        bload('sp', sc1[s][:], modv[l, s, D:2 * D], 'sc1_%d' % s, [('modv', l)])
                        bload('sp', sh1[s][:], modv[l, s, 0:D], 'sh1_%d' % s, [('modv', l)])
                    gq = sb("gq", [128, 64])
                    gk = sb("gk", [128, 64])
                    bload('act', gq[:], qn_g[l], 'gq')
                    bload('act', gk[:], kn_g[l], 'gk')
                    zpad = sb("zpad", [2, 256])
                    P.dma('act', zpad[:], zeros_in, [], ['zpad'], 'zpad')
                    for r0 in (0, 257):
                        P.dma('act', PB[r0:r0 + 2, 0:256] if r0 else PB[0:1, 0:256], zpad[0:2, :] if r0 else zpad[0:1, :],
                              ['zpad'], [('PBpad', r0)], 'zp%d' % r0)
                    P.dma('act', PB[N + 3:N + 4, 0:256], zpad[0:1, :], ['zpad'], [('PBpad', 3)], 'zp3')
                    posc = sb("posc", [128, 4])
                    P.dma('act', posc[:], pos_in, [], ['posc'], 'posc')
                    lg = sb("lg", [128, 8])
                    bload('act', lg[:], rde[l].rearrange("a h -> (a h)"), 'lg')
                    P.act(lambda e: e.activation(out=lg[:], in_=lg[:], func=AF.Exp, scale=-math.log(2.0)), ['lg'], ['lg'])
                    P.act(lambda e: e.activation(out=lg[:], in_=lg[:], func=AF.Ln, scale=-1.0, bias=1.0), ['lg'], ['lg'])
                    tab4 = sb("tab4", [128, 4, 4])
                    for ti, (di, pc) in enumerate(((0, 0), (1, 1), (0, 2), (1, 3))):
                        P.act(lambda e, ti=ti, di=di, pc=pc: e.activation(out=tab4[:, ti, :], in_=lg[:, di * 4:(di + 1) * 4],
                                                                          func=AF.Exp, scale=posc[:, pc:pc + 1]),
                              ['lg', 'posc'], ['tab4'])
                    P.dve(lambda e: e.tensor_scalar_mul(out=tab4[:, 0:2, :], in0=tab4[:, 0:2, :], scalar1=0.125), ['tab4'], ['tab4'])
                    TAB = sb("TAB", [128, 4, 4, 64])
                    P.dve(lambda e: e.tensor_copy(out=TAB[:].rearrange("p t h d -> p (t h) d"),
                                                  in_=tab4[:].rearrange("p t h -> p (t h)").unsqueeze(2).to_broadcast([128, 16, 64])),
                          ['tab4'], ['TAB'])
                    gq8 = sb("gq8", [128, 8, 64])
                    P.dve(lambda e: e.tensor_copy(out=gq8[:], in_=gq[:].unsqueeze(1).to_broadcast([128, 8, 64])), ['gq'], ['gq8'])
                    gk2 = sb("gk2", [128, 2, 64])
                    P.dve(lambda e: e.tensor_copy(out=gk2[:], in_=gk[:].unsqueeze(1).to_broadcast([128, 2, 64])), ['gk'], ['gk2'])

                    xring = Ring('xt', [sb("xt%d" % i, [128, D]) for i in range(3)])
                    csring = Ring('cs', [sb("cs%d" % i, [128, 512]) for i in range(3)])
                    snring = Ring('sn', [sb("sn%d" % i, [128, 512]) for i in range(3)])
                    hring = Ring('h', [sb("h%d" % i, [128, D]) for i in range(2)])
                    hTring = Ring('hT', [sb("hT%d" % i, [128, 8, 128], BF16) for i in range(2)])
                    usb = sb("usb", [128, 256])
                    pcb = Ring('pcb', [sb("pcb%d" % i, [128, 512]) for i in range(2)])
                    t1 = sb("t1", [128, 512])
                    t2 = sb("t2", [128, 512])
                    rq = sb("rq", [128, 4, 256])
                    rtok = Ring('rtok', [sb("rtok%d" % i, [128, 768], BF16) for i in range(2)])
                    rgt = Ring('rgt', [sb("rgt%d" % i, [128, 256]) for i in range(2)])
                    rT = Ring('rT', [sb("rT%d" % i, [128, 8, 128], BF16) for i in range(2)])
                    sq = sb("sq", [128, 512])
                    ss = sb("ss", [128, 8])
                    aq = sb("aq", [128, 512])
                    aqn = sb("aqn", [128, 512])
                    akn = sb("akn", [128, 128])
                    ak = sb("ak", [128, 128])
                    aT = Ring('aT', [sb("aT%d" % i, [128, 5, 128], BF16) for i in range(2)])
                    avt = Ring('avt', [sb("avt%d" % i, [128, 128], BF16) for i in range(2)])

                    def rope(src_ap, W, dst_ap, src_keys, dst_key, cs, ck, sn, sk):
                        g = W // 32
                        P.dve(lambda e: e.tensor_tensor(out=t1[:, :W], in0=src_ap, in1=cs[:, :W], op=ALU.mult),
                              src_keys + [ck], ['t1'])
                        s4 = src_ap.rearrange("p (g a f) -> p g a f", a=2, f=16)
                        t4 = t2[:, :W].rearrange("p (g a f) -> p g a f", a=2, f=16)
                        n4 = sn[:, :W].rearrange("p (g a f) -> p g a f", a=2, f=16)
                        P.dve(lambda e: e.tensor_tensor(out=t4[:, :, 0, :], in0=s4[:, :, 1, :], in1=n4[:, :, 0, :], op=ALU.mult),
                              src_keys + [sk], ['t2a'])
                        P.dve(lambda e: e.tensor_tensor(out=t4[:, :, 1, :], in0=s4[:, :, 0, :], in1=n4[:, :, 1, :], op=ALU.mult),
                              src_keys + [sk], ['t2b'])
                        P.dve(lambda e: e.tensor_tensor(out=dst_ap, in0=t1[:, :W], in1=t2[:, :W], op=ALU.add),
                              ['t1', 't2a', 't2b'], [dst_key])

                    ld = {}

                    def issue_loads(i):
                        xt, xk = xring.next()
                        P.dma('sp', xt[:], xsrc(i), [xs_key(i)], [xk], xk)
                        if i >= NT_C:
                            cs, ck = csring.next()
                            sn, sk = snring.next()
                            t0 = (i - NT_C) * 128
                            P.dma('sp', cs[:], cos_in[t0:t0 + 128, :], [], [ck], ck)
                            P.dma('sp', sn[:], sin_in[t0:t0 + 128, :], [], [sk], sk)
                            ld[i] = (xt, xk, cs, ck, sn, sk)
                        else:
                            ld[i] = (xt, xk, None, None, None, None)
                    issue_loads(0)
                    p1_base = len(P.ops)
                    p1_marks = []
                    for i in range(NT):
                        isctx = i < NT_C
                        s = 1 if isctx else 0
                        m_a = len(P.ops)
                        if i + 1 < NT:
                            issue_loads(i + 1)
                        xt, xk, cs, ck, sn, sk = ld.pop(i)
                        h, hk = hring.next()
                        P.dve(lambda e, h=h, xt=xt, s=s: e.tensor_tensor(out=h[:], in0=xt[:], in1=sc1[s][:], op=ALU.mult),
                              [xk, 'sc1_%d' % s], [hk])
                        P.dve(lambda e, h=h, s=s: e.tensor_tensor(out=h[:], in0=h[:], in1=sh1[s][:], op=ALU.add),
                              [hk, 'sh1_%d' % s], [hk])
                        for k in range(8):
                            P.pe(lambda e, h=h, k=k: e.transpose(psum[:, 5 + k // 4, (k % 4) * 128:(k % 4 + 1) * 128],
                                                                 h[:, k * 128:(k + 1) * 128], ident[:]),
                                 [hk, 'ident'], ['pT%d' % k])
                        hT, hTk = hTring.next()
                        for hh in range(2):
                            P.act(lambda e, hT=hT, hh=hh: e.activation(out=hT[:, hh * 4:(hh + 1) * 4, :].rearrange("p k n -> p (k n)"),
                                                                       in_=psum[:, 5 + hh, :], func=AF.Copy),
                                  ['pT%d' % k for k in range(hh * 4, hh * 4 + 4)], [hTk + '_%d' % hh])
                        m_b = len(P.ops)
                        for b in range(5):
                            for k in range(8):
                                P.pe(lambda e, hT=hT, b=b, k=k: e.matmul(bank(b), hT[:, k, :], win[:, k, b * 512:(b + 1) * 512],
                                                                         start=(k == 0), stop=(k == 7)),
                                     [hTk + '_%d' % (k // 4)] + winkeys, ['z%d' % b])
                        pc, pck = pcb.next()
                        P.act(lambda e: e.activation(out=usb[:], in_=psum[:, 0, 0:256], func=AF.Copy), ['z0'], ['usb'])
                        P.dve(lambda e, pc=pc: e.tensor_tensor(out=pc[:, 0:256], in0=psum[:, 0, 256:512], in1=usb[:], op=ALU.mult),
                              ['z0', 'usb'], [pck + 'a'])
                        P.act(lambda e, pc=pc: e.activation(out=pc[:, 256:512], in_=psum[:, 1, 0:256], func=AF.Copy), ['z1'], [pck + 'b'])
                        r0 = prow(i)
                        P.dma('sp', PB[r0:r0 + 128, :], pc[:], [pck + 'a', pck + 'b'], [('PB', i)], pck)
                        rt, rtk = rtok.next()
                        P.act(lambda e, rt=rt: e.activation(out=rt[:, 512:768], in_=psum[:, 1, 256:512], func=AF.Copy), ['z1'], [rtk + 'v'])
                        if isctx:
                            P.act(lambda e: e.activation(out=rq[:, 0, :], in_=psum[:, 2, 0:256], func=AF.Copy), ['z2'], ['rq0'])
                            P.act(lambda e: e.activation(out=rq[:, 3, :], in_=psum[:, 2, 256:512], func=AF.Copy), ['z2'], ['rq3'])
                        else:
                            rope(psum[:, 2, 0:256], 256, rq[:, 0, :], ['z2'], 'rq0', cs, ck, sn, sk)
                            rope(psum[:, 2, 256:512], 256, rq[:, 3, :], ['z2'], 'rq3', cs, ck, sn, sk)
                        TABf = TAB[:].rearrange("p t h d -> p t (h d)")
                        P.dve(lambda e: e.tensor_tensor(out=rq[:, 1, :], in0=rq[:, 0, :], in1=TABf[:, 2, :], op=ALU.mult), ['rq0', 'TAB'], ['rq1'])
                        P.pool(lambda e: e.tensor_tensor(out=rq[:, 2, :], in0=rq[:, 0, :], in1=TABf[:, 3, :], op=ALU.mult), ['rq0', 'TAB'], ['rq2'])
                        P.dve(lambda e, rt=rt: e.tensor_tensor(out=rt[:, 0:256], in0=rq[:, 3, :], in1=TABf[:, 0, :], op=ALU.mult), ['rq3', 'TAB'], [rtk + 'f'])
                        P.pool(lambda e, rt=rt: e.tensor_tensor(out=rt[:, 256:512], in0=rq[:, 3, :], in1=TABf[:, 1, :], op=ALU.mult), ['rq3', 'TAB'], [rtk + 'b'])
                        P.dma('sp', RTOK[i * 128:(i + 1) * 128, :], rt[:], [rtk + 'v', rtk + 'f', rtk + 'b'], [('RTOK', i)], rtk)
                        rg_, rgk = rgt.next()
                        P.act(lambda e, rg_=rg_: e.activation(out=rg_[:], in_=psum[:, 4, 0:256], func=AF.Silu), ['z4'], [rgk])
                        P.dma('act', RG[i * 128:(i + 1) * 128, :], rg_[:], [rgk], [('RG', i)], rgk)
                        for t in range(4):
                            for c2 in range(2):
                                idx = t * 2 + c2
                                P.pe(lambda e, t=t, c2=c2, idx=idx: e.transpose(psum[:, 5 + idx // 4, (idx % 4) * 128:(idx % 4 + 1) * 128],
                                                                                rq[:, t, c2 * 128:(c2 + 1) * 128], ident[:]),
                                     ['rq%d' % t, 'ident'], ['pT%d' % idx])
                        rTt, rTk = rT.next()
                        for hh in range(2):
                            if hh == 0:
                                P.act(lambda e, rTt=rTt: e.activation(out=rTt[:, 0:4, :].rearrange("p k n -> p (k n)"), in_=psum[:, 5, :], func=AF.Copy),
                                      ['pT0', 'pT1', 'pT2', 'pT3'], [rTk + 'a'])
                            else:
                                P.act(lambda e, rTt=rTt: e.activation(out=rTt[:, 4:6, :].rearrange("p k n -> p (k n)"), in_=psum[:, 6, 0:256], func=AF.Copy),
                                      ['pT4', 'pT5'], [rTk + 'b'])
                                P.act(lambda e, rTt=rTt: e.activation(out=rTt[:, 6:8, :].rearrange("p k n -> p (k n)"), in_=psum[:, 6, 256:512], func=AF.Copy, scale=0.125),
                                      ['pT6', 'pT7'], [rTk + 'c'])
                        for t in range(3):
                            P.dma('act', RQT[t, :, :, i * 128:(i + 1) * 128].rearrange("(c q) d n -> (q d) c n", q=2),
                                  rTt[:, 2 * t:2 * t + 2, :], [rTk + 'a', rTk + 'b'], [('RQT', i, t)], rTk + 'q%d' % t)
                        P.dma('act', RKT[:, :, i * 128:(i + 1) * 128].rearrange("(c q) d n -> (q d) c n", q=2),
                              rTt[:, 6:8, :], [rTk + 'c'], [('RKT', i)], rTk + 'k')
                        P.act(lambda e: e.activation(out=sq[:], in_=psum[:, 3, :], func=AF.Square), ['z3'], ['sq'])
                        P.dve(lambda e: e.tensor_reduce(out=ss[:], in_=sq[:].rearrange("p (h d) -> p h d", d=64), axis=AX.X, op=ALU.add), ['sq'], ['ss'])
                        P.dve(lambda e: e.tensor_scalar(out=ss[:], in0=ss[:], scalar1=1.0 / 64, scalar2=EPS, op0=ALU.mult, op1=ALU.add), ['ss'], ['ss'])
                        P.act(lambda e: e.activation(out=ss[:], in_=ss[:], func=AF.Sqrt), ['ss'], ['ss'])
                        P.dve(lambda e: e.reciprocal(out=ss[:], in_=ss[:]), ['ss'], ['ss'])
                        P.dve(lambda e: e.tensor_tensor(out=aqn[:].rearrange("p (h d) -> p h d", d=64), in0=psum[:, 3, :].rearrange("p (h d) -> p h d", d=64),
                                                        in1=ss[:].unsqueeze(2).to_broadcast([128, 8, 64]), op=ALU.mult), ['z3', 'ss'], ['aqn'])
                        P.dve(lambda e: e.tensor_tensor(out=aqn[:], in0=aqn[:], in1=gq8[:].rearrange("p h d -> p (h d)"), op=ALU.mult), ['aqn', 'gq8'], ['aqn'])
                        if isctx:
                            aq_src, aq_key = aqn, 'aqn'
                        else:
                            rope(aqn[:], 512, aq[:], ['aqn'], 'aq', cs, ck, sn, sk)
                            aq_src, aq_key = aq, 'aq'
                        av_, avk = avt.next()
                        P.act(lambda e, av_=av_: e.activation(out=av_[:], in_=psum[:, 4, 384:512], func=AF.Copy), ['z4'], [avk])
                        P.dma('act', AV[i * 128:(i + 1) * 128, :], av_[:], [avk], [('AV', i)], avk)
                        P.act(lambda e: e.activation(out=sq[:, 0:128], in_=psum[:, 4, 256:384], func=AF.Square), ['z4'], ['sqk'])
                        P.dve(lambda e: e.tensor_reduce(out=ss[:, 0:2], in_=sq[:, 0:128].rearrange("p (h d) -> p h d", d=64), axis=AX.X, op=ALU.add), ['sqk'], ['ssk'])
                        P.dve(lambda e: e.tensor_scalar(out=ss[:, 0:2], in0=ss[:, 0:2], scalar1=1.0 / 64, scalar2=EPS, op0=ALU.mult, op1=ALU.add), ['ssk'], ['ssk'])
                        P.act(lambda e: e.activation(out=ss[:, 0:2], in_=ss[:, 0:2], func=AF.Sqrt), ['ssk'], ['ssk'])
                        P.dve(lambda e: e.reciprocal(out=ss[:, 0:2], in_=ss[:, 0:2]), ['ssk'], ['ssk'])
                        P.dve(lambda e: e.tensor_tensor(out=akn[:].rearrange("p (h d) -> p h d", d=64), in0=psum[:, 4, 256:384].rearrange("p (h d) -> p h d", d=64),
                                                        in1=ss[:, 0:2].unsqueeze(2).to_broadcast([128, 2, 64]), op=ALU.mult), ['z4', 'ssk'], ['akn'])
                        P.dve(lambda e: e.tensor_tensor(out=akn[:], in0=akn[:], in1=gk2[:].rearrange("p h d -> p (h d)"), op=ALU.mult), ['akn', 'gk2'], ['akn'])
                        if isctx:
                            ak_src, ak_key = akn, 'akn'
                        else:
                            rope(akn[:], 128, ak[:], ['akn'], 'ak', cs, ck, sn, sk)
                            ak_src, ak_key = ak, 'ak'
                        for c4 in range(4):
                            P.pe(lambda e, c4=c4, aq_src=aq_src: e.transpose(psum[:, 7, c4 * 128:(c4 + 1) * 128], aq_src[:, c4 * 128:(c4 + 1) * 128], ident[:]),
                                 [aq_key, 'ident'], ['pA%d' % c4])
                        P.pe(lambda e, ak_src=ak_src: e.transpose(psum[:, 5, 0:128], ak_src[:, 0:128], ident[:]), [ak_key, 'ident'], ['pT0'])
                        aTt, aTk = aT.next()
                        P.act(lambda e, aTt=aTt: e.activation(out=aTt[:, 0:4, :].rearrange("p k n -> p (k n)"), in_=psum[:, 7, :], func=AF.Copy),
                              ['pA0', 'pA1', 'pA2', 'pA3'], [aTk + 'q'])
                        P.act(lambda e, aTt=aTt: e.activation(out=aTt[:, 4, :], in_=psum[:, 5, 0:128], func=AF.Copy), ['pT0'], [aTk + 'k'])
                        P.dma('sp', AQT[:, :, i * 128:(i + 1) * 128].rearrange("(c q) d n -> (q d) c n", q=2), aTt[:, 0:4, :],
                              [aTk + 'q'], [('AQT', i)], aTk + 'q')
                        P.dma('sp', AKT[:, :, i * 128:(i + 1) * 128].rearrange("q d n -> (q d) n"), aTt[:, 4, :],
                              [aTk + 'k'], [('AKT', i)], aTk + 'k')
                        p1_marks.append((m_a, m_b, len(P.ops)))
                    pipeline_reorder(P, p1_base, p1_marks)
                    P.flush()


            if 'att' in phases:
                with contextlib.ExitStack() as st:
                    def sb(name, shape, dt=F32):
                        return st.enter_context(nc.sbuf_tensor(uname(name), list(shape), dt))
                    KT = sb("KT", [128, 2, N], BF16)
                    P.pool(lambda e: e.memset(KT[64:128, :, :], 0.0), [], ['KTz'])
                    P.dma('sp', KT[0:64, :, :], AKT.rearrange("k d n -> d k n"), [], ['KT'], 'KT')
                    V1 = sb("V1", [128, NT, 2, 65], BF16)
                    P.pool(lambda e: e.memset(V1[:], 1.0), [], ['V1'])
                    for kk_ in range(2):
                        P.dma('act', V1[:, :, kk_, 0:64], AV[:, kk_ * 64:(kk_ + 1) * 64].rearrange("(c p) d -> p c d", p=128), [], ['V1'], 'V1_%d' % kk_)
                    qring = Ring('QT', [sb("QT%d" % i, [128, N], BF16) for i in range(2)])
                    for qi_, qt_ in enumerate(qring.tiles):
                        P.pool(lambda e, qt_=qt_: e.memset(qt_[64:128, :], 0.0), [], ['QTz%d' % qi_])
                    ptring = Ring('PT', [sb("PT%d" % i, [128, 512], BF16) for i in range(4)])
                    aoring = Ring('AO', [sb("AO%d" % i, [128, 4, 64]) for i in range(2)])
                    rcring = Ring('rc', [sb("rc%d" % i, [128, 4]) for i in range(2)])
                    sbank = Ring('S', [0, 1, 2, 3])
                    obank = Ring('O', [4, 5])
                    groups = []
                    if not last:
                        groups.append((0, 2, [0, 1]))
                    for g in range(8):
                        groups.append((LC + g * 512, 4, list(range(NT))))
                    items = []
                    for hq in range(8):
                        for gi, (q0, nq, chunks) in enumerate(groups):
                            for ci, c in enumerate(chunks):
                                items.append((hq, gi, q0, nq, ci, c, len(chunks)))
                    qt_of = {}
                    st_of = {}

                    def get_qt(hq):
                        if hq not in qt_of:
                            QT, qk = qring.next()
                            P.dma('sp', QT[0:64, :], AQT[hq], [], [qk], qk)
                            qt_of[hq] = (QT, qk)
                        return qt_of[hq]

                    def emit_S(t):
                        hq, gi, q0, nq, ci, c, nch = items[t]
                        QT, qk = get_qt(hq)
                        kvh = hq // 4
                        W = nq * 128
                        sbk, sk = sbank.next()
                        P.pe(lambda e, sbk=sbk, c=c, QT=QT, q0=q0, W=W, kvh=kvh: e.matmul(psum[:, sbk, 0:W], KT[:, kvh, c * 128:(c + 1) * 128],
                                                                                         QT[:, q0:q0 + W], start=True, stop=True),
                             ['KT', 'KTz', 'QTz0', 'QTz1', qk], [sk])
                        st_of[t] = (sbk, sk)

                    cur_o = [None]

                    def emit_rest(t):
                        hq, gi, q0, nq, ci, c, nch = items[t]
                        kvh = hq // 4
                        W = nq * 128
                        sbk, sk = st_of.pop(t)
                        if ci == 0:
                            cur_o[0] = obank.next()
                        ob, ok = cur_o[0]
                        Ov = psum[:, ob, 0:260].rearrange("p (j e) -> p j e", e=65)
                        PT, pk = ptring.next()
                        P.act(lambda e, PT=PT, sbk=sbk, W=W: e.activation(out=PT[:, 0:W], in_=psum[:, sbk, 0:W], func=AF.Exp, scale=0.125),
                              [sk], [pk])
                        if t + 2 < len(items):
                            emit_S(t + 2)
                        for j in range(nq):
                            P.pe(lambda e, PT=PT, j=j, c=c, kvh=kvh, ci=ci, Ov=Ov, nch=nch: e.matmul(
                                Ov[:, j, :], PT[:, j * 128:(j + 1) * 128], V1[:, c, kvh, :],
                                start=(ci == 0 and j == 0), stop=(ci == nch - 1), skip_group_check=True),
                                [pk, 'V1'], [ok])
                        if ci == nch - 1:
                            rc, rk_ = rcring.next()
                            AO, ak_ = aoring.next()
                            P.dve(lambda e, rc=rc, Ov=Ov, nq=nq: e.reciprocal(out=rc[:, 0:nq], in_=Ov[:, 0:nq, 64]), [ok], [rk_])
                            P.dve(lambda e, rc=rc, Ov=Ov, nq=nq, AO=AO: e.tensor_tensor(out=AO[:, 0:nq, :], in0=Ov[:, 0:nq, 0:64],
                                                                                      in1=rc[:, 0:nq].unsqueeze(2).to_broadcast([128, nq, 64]), op=ALU.mult),
                                  [ok, rk_], [ak_])
                            P.dma('sp', MIX[q0:q0 + W, 512 + hq * 64:512 + (hq + 1) * 64].rearrange("(j p) d -> p j d", p=128), AO[:, 0:nq, :],
                                  [ak_], [('MIXa', q0, hq)], ak_)
                    emit_S(0)
                    emit_S(1)
                    for t in range(len(items)):
                        emit_rest(t)
                    P.flush()

            if 'ret' in phases:
                with contextlib.ExitStack() as st:
                    def sb(name, shape, dt=F32):
                        return st.enter_context(nc.sbuf_tensor(uname(name), list(shape), dt))
                    RT = sb("RT", [128, NT, 768], BF16)
                    P.dma('sp', RT[:], RTOK.rearrange("(c p) w -> p c w", p=128), [], ['RT'], 'RT')
                    RGt = sb("RGt", [128, NT, 256])
                    P.dma('act', RGt[:], RG.rearrange("(c p) w -> p c w", p=128), [], ['RGt'], 'RGt')
                    gng = sb("gng", [128, 256])
                    bload('act', gng[:], gn_g[l], 'gng')
                    lg = sb("lg", [128, 8])
                    bload('act', lg[:], rde[l].rearrange("a h -> (a h)"), 'lg')
                    P.act(lambda e: e.activation(out=lg[:], in_=lg[:], func=AF.Exp, scale=-math.log(2.0)), ['lg'], ['lg'])
                    P.act(lambda e: e.activation(out=lg[:], in_=lg[:], func=AF.Ln, scale=-1.0, bias=1.0), ['lg'], ['lg'])
                    dec = sb("dec", [128, 8])
                    P.act(lambda e: e.activation(out=dec[:], in_=lg[:], func=AF.Exp, scale=128.0), ['lg'], ['dec'])
                    dpos = sb("dpos", [128, 128]); dneg = sb("dneg", [128, 128]); mge = sb("mge", [128, 128])
                    P.dma('sp', dpos[:], dpos_in, [], ['dpos'], 'dpos')
                    P.dma('sp', dneg[:], dneg_in, [], ['dneg'], 'dneg')
                    P.dma('sp', mge[:], mge_in, [], ['mge'], 'mge')
                    DcT = sb("DcT", [128, 4, 128])
                    e1 = sb("e1", [128, 128])
                    for hh in range(4):
                        P.act(lambda e, hh=hh: e.activation(out=e1[:], in_=dpos[:], func=AF.Exp, scale=lg[:, hh:hh + 1]), ['dpos', 'lg'], ['e1'])
                        P.act(lambda e, hh=hh: e.activation(out=DcT[:, hh, :], in_=dneg[:], func=AF.Exp, scale=lg[:, 4 + hh:5 + hh]), ['dneg', 'lg'], ['DcT%d' % hh])
                        P.dve(lambda e, hh=hh: e.tensor_tensor(out=e1[:], in0=e1[:], in1=DcT[:, hh, :], op=ALU.subtract), ['e1', 'DcT%d' % hh], ['e1'])
                        P.dve(lambda e, hh=hh: e.tensor_tensor(out=e1[:], in0=e1[:], in1=mge[:], op=ALU.mult), ['e1', 'mge'], ['e1'])
                        P.dve(lambda e, hh=hh: e.tensor_tensor(out=DcT[:, hh, :], in0=DcT[:, hh, :], in1=e1[:], op=ALU.add), ['e1', 'DcT%d' % hh], ['DcT%d' % hh])
                    q3ring = Ring('Q3', [sb("Q3_%d" % i, [64, 3, N], BF16) for i in range(2)])
                    ktring = Ring('KTh', [sb("KTh%d" % i, [64, N], BF16) for i in range(2)])
                    Sf = sb("Sf", [64, NT + 1, 64], BF16)
                    Sb = sb("Sb", [64, NT + 1, 64], BF16)
                    srun = Ring('srun', [sb("srun%d" % i, [64, 64]) for i in range(2)])
                    ptr = Ring('PTr', [sb("PTr%d" % i, [128, 128], BF16) for i in range(2)])
                    st6 = sb("st6", [128, 6]); mv = sb("mv", [128, 2]); rs = sb("rs", [128, 1])
                    yo = Ring('yo', [sb("yo%d" % i, [128, 64]) for i in range(2)])
                    kvb = Ring('kv', [0, 1]); scb = Ring('sc', [2, 3]); yb = Ring('y', [4, 5])
                    ytiles = list(range(NT)) if not last else list(range(NT_C, NT))
                    for hh in range(4):
                        Q3, q3k = q3ring.next()
                        KTh, ktk = ktring.next()
                        P.dma('sp', Q3[:], RQT[:, hh].rearrange("t d n -> d t n"), [], [q3k], q3k)
                        P.dma('act', KTh[:], RKT[hh], [], [ktk], ktk)
                        vcol = 512 + hh * 64

                        def scan(order_tiles, S, skey, kcol, dcol, run_init_zero):
                            return None
                        run, runk = srun.next()
                        P.pool(lambda e, run=run: e.memset(run[:], 0.0), [], [runk])
                        for i in range(NT):
                            P.pool(lambda e, run=run, i=i: e.tensor_copy(out=Sf[:, i, :], in_=run[:]), [runk], [('Sf', i)])
                            kb, kk = kvb.next()
                            P.pe(lambda e, kb=kb, i=i, hh=hh, vcol=vcol: e.matmul(psum[0:64, kb, 0:64], RT[:, i, hh * 64:(hh + 1) * 64],
                                                                                 RT[:, i, vcol:vcol + 64], start=True, stop=True), ['RT'], [kk])
                            nrun, nrunk = srun.next()
                            P.dve(lambda e, run=run, nrun=nrun, kb=kb, hh=hh: e.scalar_tensor_tensor(out=nrun[:], in0=run[:], scalar=dec[0:64, hh:hh + 1],
                                                                                                     in1=psum[0:64, kb, 0:64], op0=ALU.mult, op1=ALU.add),
                                  [runk, kk, 'dec'], [nrunk])
                            run, runk = nrun, nrunk
                        run, runk = srun.next()
                        P.pool(lambda e, run=run: e.memset(run[:], 0.0), [], [runk])
                        for i in [1, 0] + list(range(NT - 1, NT_C - 1, -1)):
                            P.pool(lambda e, run=run, i=i: e.tensor_copy(out=Sb[:, i, :], in_=run[:]), [runk], [('Sb', i)])
                            kb, kk = kvb.next()
                            P.pe(lambda e, kb=kb, i=i, hh=hh, vcol=vcol: e.matmul(psum[0:64, kb, 0:64], RT[:, i, 256 + hh * 64:256 + (hh + 1) * 64],
                                                                                 RT[:, i, vcol:vcol + 64], start=True, stop=True), ['RT'], [kk])
                            nrun, nrunk = srun.next()
                            P.dve(lambda e, run=run, nrun=nrun, kb=kb, hh=hh: e.scalar_tensor_tensor(out=nrun[:], in0=run[:], scalar=dec[0:64, 4 + hh:5 + hh],
                                                                                                     in1=psum[0:64, kb, 0:64], op0=ALU.mult, op1=ALU.add),
                                  [runk, kk, 'dec'], [nrunk])
                            run, runk = nrun, nrunk
                        r_base = len(P.ops)
                        r_marks = []
                        for i in ytiles:
                            m_a = len(P.ops)
                            sbk, sk = scb.next()
                            P.pe(lambda e, sbk=sbk, i=i, KTh=KTh, Q3=Q3: e.matmul(psum[:, sbk, 0:128], KTh[:, i * 128:(i + 1) * 128], Q3[:, 0, i * 128:(i + 1) * 128],
                                                                                start=True, stop=True), [ktk, q3k], [sk])
                            PTr, pk = ptr.next()
                            P.dve(lambda e, PTr=PTr, sbk=sbk, hh=hh: e.tensor_tensor(out=PTr[:], in0=psum[:, sbk, 0:128], in1=DcT[:, hh, :], op=ALU.mult),
                                  [sk, 'DcT%d' % hh], [pk])
                            ybk, yk = yb.next()
                            P.pe(lambda e, ybk=ybk, PTr=PTr, i=i, vcol=vcol: e.matmul(psum[:, ybk, 0:64], PTr[:], RT[:, i, vcol:vcol + 64], start=True, stop=False),
                                 [pk, 'RT'], [yk])
                            P.pe(lambda e, ybk=ybk, Q3=Q3, i=i: e.matmul(psum[:, ybk, 0:64], Q3[:, 1, i * 128:(i + 1) * 128], Sf[:, i, :], start=False, stop=False),
                                 [q3k, ('Sf', i)], [yk])
                            P.pe(lambda e, ybk=ybk, Q3=Q3, i=i: e.matmul(psum[:, ybk, 0:64], Q3[:, 2, i * 128:(i + 1) * 128], Sb[:, i, :], start=False, stop=True),
                                 [q3k, ('Sb', i)], [yk])
                            m_b = len(P.ops)
                            P.dve(lambda e, ybk=ybk: e.bn_stats(out=st6[:], in_=psum[:, ybk, 0:64]), [yk], ['st6'])
                            P.dve(lambda e: e.bn_aggr(out=mv[:], in_=st6[:]), ['st6'], ['mv'])
                            P.dve(lambda e: e.tensor_scalar_add(out=rs[:], in0=mv[:, 1:2], scalar1=EPS), ['mv'], ['rs'])
                            P.act(lambda e: e.activation(out=rs[:], in_=rs[:], func=AF.Sqrt), ['rs'], ['rs'])
                            P.dve(lambda e: e.reciprocal(out=rs[:], in_=rs[:]), ['rs'], ['rs'])
                            y_, yok = yo.next()
                            P.dve(lambda e, y_=y_, ybk=ybk: e.tensor_scalar(out=y_[:], in0=psum[:, ybk, 0:64], scalar1=mv[:, 0:1], scalar2=rs[:, 0:1],
                                                                           op0=ALU.subtract, op1=ALU.mult), [yk, 'mv', 'rs'], [yok])
                            P.pool(lambda e, y_=y_, hh=hh: e.tensor_tensor(out=y_[:], in0=y_[:], in1=gng[:, hh * 64:(hh + 1) * 64], op=ALU.mult), [yok, 'gng'], [yok])
                            P.pool(lambda e, y_=y_, hh=hh, i=i: e.tensor_tensor(out=y_[:], in0=y_[:], in1=RGt[:, i, hh * 64:(hh + 1) * 64], op=ALU.mult), [yok, 'RGt'], [yok])
                            P.dma('sp', MIX[i * 128:(i + 1) * 128, 256 + hh * 64:256 + (hh + 1) * 64], y_[:], [yok], [('MIXr', i, hh)], yok)
                            r_marks.append((m_a, m_b, len(P.ops)))
                        pipeline_reorder(P, r_base, r_marks)
                    P.flush()

            if 'p3' in phases:
                with contextlib.ExitStack() as st:
                    def sb(name, shape, dt=F32):
                        return st.enter_context(nc.sbuf_tensor(uname(name), list(shape), dt))
                    wout = sb("wout", [128, 8, D], BF16)
                    for hf in range(2):
                        P.dma('pool', wout[:, :, hf * 512:(hf + 1) * 512], w_out[l, :, hf * 512:(hf + 1) * 512].rearrange("(k p) n -> p k n", p=128),
                              [], ['wout%d' % hf], 'wout%d' % hf)
                    CWr = sb("CWr", [128, 256, 3])
                    P.dma('act', CWr[:].rearrange("p c k -> p (c k)"), conv_w[l].rearrange("c k -> (c k)").partition_broadcast(128), [], ['CWr'], 'CWr')
                    CW = sb("CW", [128, 3, 256])
                    for k in range(3):
                        P.dve(lambda e, k=k: e.tensor_copy(out=CW[:, k, :], in_=CWr[:, :, k]), ['CWr'], ['CW%d' % k])
                    g1 = [sb("g1_%d" % s, [128, D]) for s in range(2)]
                    sc2 = [sb("sc2_%d" % s, [128, D]) for s in range(2)]
                    sh2 = [sb("sh2_%d" % s, [128, D]) for s in range(2)]
                    for s in range(2):
                        if last and s == 1:
                            continue
                        bload('sp', g1[s][:], modv[l, s, 2 * D:3 * D], 'g1_%d' % s)
                        bload('sp', sh2[s][:], modv[l, s, 3 * D:4 * D], 'sh2_%d' % s)
                        bload('sp', sc2[s][:], modv[l, s, 4 * D:5 * D], 'sc2_%d' % s)
                    lng = sb("lng", [128, D]); lnb = sb("lnb", [128, D])
                    bload('act', lng[:], ln_g[l, 0], 'lng')
                    bload('act', lnb[:], ln_b[l, 0], 'lnb')
                    G2 = [sb("G2_%d" % s, [128, D]) for s in range(2)]
                    B2 = [sb("B2_%d" % s, [128, D]) for s in range(2)]
                    for s in range(2):
                        if last and s == 1:
                            continue
                        P.dve(lambda e, s=s: e.tensor_tensor(out=G2[s][:], in0=lng[:], in1=sc2[s][:], op=ALU.mult), ['lng', 'sc2_%d' % s], ['G2_%d' % s])
                        P.dve(lambda e, s=s: e.tensor_tensor(out=B2[s][:], in0=lnb[:], in1=sc2[s][:], op=ALU.mult), ['lnb', 'sc2_%d' % s], ['B2_%d' % s])
                        P.dve(lambda e, s=s: e.tensor_tensor(out=B2[s][:], in0=B2[s][:], in1=sh2[s][:], op=ALU.add), ['B2_%d' % s, 'sh2_%d' % s], ['B2_%d' % s])
                    x1fr = Ring('x1f', [sb("x1f%d" % i, [128, D]) for i in range(2)])
                    wr = sb("wr", [128, 8, NE])
                    P.dma('act', wr[:], w_router[l].rearrange("(k p) e -> p k e", p=128), [], ['wr'], 'wr')
                    brt = sb("brt", [128, NE])
                    bload('act', brt[:], b_router[l], 'brt')
                    pmr = Ring('pm', [sb("pm%d" % i, [128, 3, 256]) for i in range(3)])
                    btr = Ring('bt', [sb("bt%d" % i, [128, 256]) for i in range(3)])
                    mixr = Ring('mix', [sb("mix%d" % i, [128, D]) for i in range(3)])
                    xr = Ring('x3', [sb("x3_%d" % i, [128, D]) for i in range(3)])
                    ca = sb("ca", [128, 256]); cb = sb("cb", [128, 256])
                    mTr = Ring('mT', [sb("mT%d" % i, [128, 8, 128], BF16) for i in range(2)])
                    rr = sb("rr", [128, D])
                    x1r = Ring('x1', [sb("x1_%d" % i, [128, D]) for i in range(2)])
                    h2 = sb("h2", [128, D])
                    h2br = Ring('h2b', [sb("h2b%d" % i, [128, D], BF16) for i in range(2)])
                    ix8 = sb("ix8", [128, 8], U32)
                    h2f = sb("h2f", [128, 8, 128])
                    st6 = sb("st6", [128, 2, 6]); mv = sb("mv", [128, 2]); rs = sb("rs", [128, 1])
                    lgt = sb("lgt", [128, NE]); mx8 = sb("mx8", [128, 8]); msk = sb("msk", [128, NE]); nmx = sb("nmx", [128, 1])
                    ex = sb("ex", [128, NE]); sm = sb("sm", [128, 1])
                    mwr = Ring('mw', [sb("mw%d" % i, [128, NE]) for i in range(2)])
                    ld3 = {}

                    def issue_loads3(i):
                        r0 = prow(i)
                        pm, pmk = pmr.next()
                        for k in range(3):
                            P.dma('sp', pm[:, k, :], PB[r0 - 1 + k:r0 + 127 + k, 0:256], [], [pmk + str(k)], pmk + str(k))
                        bt, btk = btr.next()
                        P.dma('sp', bt[:], PB[r0:r0 + 128, 256:512], [], [btk], btk)
                        mix, mixk = mixr.next()
                        P.dma('sp', mix[:, 256:D], MIX[i * 128:(i + 1) * 128, 256:D], [], [mixk + 'l'], mixk)
                        xt, xk = xr.next()
                        P.dma('sp', xt[:], xsrc(i), [], [xk], xk)
                        ld3[i] = (pm, pmk, bt, btk, mix, mixk, xt, xk)
                    issue_loads3(out_tiles[0])
                    p3_base = len(P.ops)
                    p3_marks = []
                    for oi, i in enumerate(out_tiles):
                        s = 1 if i < NT_C else 0
                        m_a = len(P.ops)
                        if oi + 1 < len(out_tiles):
                            issue_loads3(out_tiles[oi + 1])
                        pm, pmk, bt, btk, mix, mixk, xt, xk = ld3.pop(i)
                        P.dve(lambda e, pm=pm: e.tensor_tensor(out=ca[:], in0=pm[:, 0, :], in1=CW[:, 0, :], op=ALU.mult), [pmk + '0', 'CW0'], ['ca'])
                        P.pool(lambda e, pm=pm: e.tensor_tensor(out=cb[:], in0=pm[:, 1, :], in1=CW[:, 1, :], op=ALU.mult), [pmk + '1', 'CW1'], ['cb'])
                        P.dve(lambda e: e.tensor_tensor(out=ca[:], in0=ca[:], in1=cb[:], op=ALU.add), ['ca', 'cb'], ['ca'])
                        P.pool(lambda e, pm=pm: e.tensor_tensor(out=cb[:], in0=pm[:, 2, :], in1=CW[:, 2, :], op=ALU.mult), [pmk + '2', 'CW2', 'ca'], ['cb'])
                        P.dve(lambda e: e.tensor_tensor(out=ca[:], in0=ca[:], in1=cb[:], op=ALU.add), ['ca', 'cb'], ['ca'])
                        P.dve(lambda e, mix=mix, bt=bt: e.tensor_tensor(out=mix[:, 0:256], in0=ca[:], in1=bt[:], op=ALU.mult), ['ca', btk], [mixk + 'c'])
                        for k in range(8):
                            P.pe(lambda e, mix=mix, k=k: e.transpose(psum[:, k // 4, (k % 4) * 128:(k % 4 + 1) * 128], mix[:, k * 128:(k + 1) * 128], ident[:]),
                                 [mixk + 'l', mixk + 'c', 'ident'], ['pT%d' % k])
                        mT, mTk = mTr.next()
                        for hf in range(2):
                            P.act(lambda e, mT=mT, hf=hf: e.activation(out=mT[:, hf * 4:(hf + 1) * 4, :].rearrange("p k n -> p (k n)"), in_=psum[:, hf, :], func=AF.Copy),
                                  ['pT%d' % k for k in range(hf * 4, hf * 4 + 4)], [mTk + str(hf)])
                        pyb = 2 + 2 * (oi % 2)
                        for nh in range(2):
                            for k in range(8):
                                P.pe(lambda e, mT=mT, nh=nh, k=k, pyb=pyb: e.matmul(psum[:, pyb + nh, :], mT[:, k, :], wout[:, k, nh * 512:(nh + 1) * 512], start=(k == 0), stop=(k == 7)),
                                     [mTk + str(k // 4), 'wout%d' % nh], ['py%d' % (pyb + nh)])
                        m_b = len(P.ops)
                        for nh in range(2):
                            P.dve(lambda e, nh=nh, s=s, pyb=pyb: e.tensor_tensor(out=rr[:, nh * 512:(nh + 1) * 512], in0=psum[:, pyb + nh, :], in1=g1[s][:, nh * 512:(nh + 1) * 512], op=ALU.mult),
                                  ['py%d' % (pyb + nh), 'g1_%d' % s], ['rr%d' % nh])
                        P.dve(lambda e, xt=xt: e.scalar_tensor_tensor(out=rr[:], in0=xt[:], scalar=ALPHA, in1=rr[:], op0=ALU.mult, op1=ALU.add), [xk, 'rr0', 'rr1'], ['rr'])
                        for nh in range(2):
                            P.dve(lambda e, nh=nh: e.bn_stats(out=st6[:, nh, :], in_=rr[:, nh * 512:(nh + 1) * 512]), ['rr'], ['st6_%d' % nh])
                        P.dve(lambda e: e.bn_aggr(out=mv[:], in_=st6[:]), ['st6_0', 'st6_1'], ['mv'])
                        P.dve(lambda e: e.tensor_scalar_add(out=rs[:], in0=mv[:, 1:2], scalar1=EPS), ['mv'], ['rs'])
                        P.act(lambda e: e.activation(out=rs[:], in_=rs[:], func=AF.Sqrt), ['rs'], ['rs'])
                        P.dve(lambda e: e.reciprocal(out=rs[:], in_=rs[:]), ['rs'], ['rs'])
                        x1, x1k = x1r.next()
                        P.dve(lambda e, x1=x1: e.tensor_scalar(out=x1[:], in0=rr[:], scalar1=mv[:, 0:1], scalar2=rs[:, 0:1], op0=ALU.subtract, op1=ALU.mult), ['rr', 'mv', 'rs'], [x1k])
                        P.dve(lambda e, x1=x1, s=s: e.tensor_tensor(out=h2[:], in0=x1[:], in1=G2[s][:], op=ALU.mult), [x1k, 'G2_%d' % s], ['h2'])
                        P.dve(lambda e, s=s: e.tensor_tensor(out=h2[:], in0=h2[:], in1=B2[s][:], op=ALU.add), ['h2', 'B2_%d' % s], ['h2'])
                        x1f, x1fk = x1fr.next()
                        P.pool(lambda e, x1=x1, x1f=x1f: e.tensor_tensor(out=x1f[:], in0=x1[:], in1=lng[:], op=ALU.mult), [x1k, 'lng'], [x1fk])
                        P.pool(lambda e, x1f=x1f: e.tensor_tensor(out=x1f[:], in0=x1f[:], in1=lnb[:], op=ALU.add), [x1fk, 'lnb'], [x1fk])
                        P.dma('sp', XM[i * 128:(i + 1) * 128, :], x1f[:], [x1fk], [('XM', i)], x1fk)
                        for k in range(8):
                            P.pe(lambda e, k=k: e.transpose(psum[:, k // 4, (k % 4) * 128:(k % 4 + 1) * 128], h2[:, k * 128:(k + 1) * 128], ident[:]),
                                 ['h2', 'ident'], ['pT%d' % k])
                        for hf in range(2):
                            P.dve(lambda e, hf=hf: e.tensor_copy(out=h2f[:, hf * 4:(hf + 1) * 4, :].rearrange("p k n -> p (k n)"), in_=psum[:, hf, :]),
                                  ['pT%d' % k for k in range(hf * 4, hf * 4 + 4)], ['h2f%d' % hf])
                        h2b, h2bk = h2br.next()
                        P.act(lambda e, h2b=h2b: e.activation(out=h2b[:], in_=h2[:], func=AF.Copy), ['h2'], [h2bk])
                        P.dma('sp', H2R[i * 128:(i + 1) * 128, :], h2b[:], [h2bk], [('H2R', i)], h2bk)
                        for k in range(8):
                            P.pe(lambda e, k=k: e.matmul(psum[:, 6, 0:NE], h2f[:, k, :], wr[:, k, :], start=(k == 0), stop=(k == 7)), ['h2f%d' % (k // 4), 'wr'], ['pl'])
                        P.dve(lambda e: e.tensor_tensor(out=lgt[:], in0=psum[:, 6, 0:NE], in1=brt[:], op=ALU.add), ['pl', 'brt'], ['lgt'])
                        P.dve(lambda e: e.max(out=mx8[:], in_=lgt[:]), ['lgt'], ['mx8'])
                        P.dve(lambda e: e.max_index(out=ix8[:], in_max=mx8[:], in_values=lgt[:]), ['lgt', 'mx8'], ['ix8'])
                        P.dve(lambda e: e.tensor_scalar_mul(out=nmx[:], in0=mx8[:, 0:1], scalar1=-1.0), ['mx8'], ['nmx'])
                        P.act(lambda e: e.activation(out=ex[:, 0:4], in_=mx8[:, 0:4], func=AF.Exp, bias=nmx[:, 0:1], scale=1.0), ['mx8', 'nmx'], ['ex'])
                        P.dve(lambda e: e.reduce_sum(out=sm[:], in_=ex[:, 0:4], axis=AX.X), ['ex'], ['sm'])
                        P.dve(lambda e: e.reciprocal(out=sm[:], in_=sm[:]), ['sm'], ['sm'])
                        mw_, mwk = mwr.next()
                        P.dve(lambda e, mw_=mw_: e.tensor_scalar_mul(out=mw_[:, 0:4], in0=ex[:, 0:4], scalar1=sm[:, 0:1]), ['ex', 'sm'], [mwk + 'w'])
                        P.dve(lambda e, mw_=mw_: e.tensor_copy(out=mw_[:, 4:8], in_=ix8[:, 0:4]), ['ix8'], [mwk + 'e'])
                        P.dma('sp', RW[i * 128:(i + 1) * 128, :], mw_[:, 0:4], [mwk + 'w'], [('RW', i)], mwk + 'w')
                        P.dma('sp', RE[i * 128:(i + 1) * 128, :], mw_[:, 4:8], [mwk + 'e'], [('RE', i)], mwk + 'e')
                        p3_marks.append((m_a, m_b, len(P.ops)))
                    pipeline_reorder(P, p3_base, p3_marks)
                    P.flush()

            if 'moe' in phases:
                tiles = out_tiles
                nt = len(tiles)
                T = nt * 128
                t0 = tiles[0] * 128
                NB = (4 * T + NE * (BS - 1) + BS - 1) // BS
                assert NB <= NBMAX
                w_gu_rows = w_gu.rearrange("l e r c -> (l e r) c")
                w_dn_rows = w_dn.rearrange("l e r c -> (l e r) c")
                with contextlib.ExitStack() as st:
                    def sb(name, shape, dt=F32):
                        return st.enter_context(nc.sbuf_tensor(uname(name), list(shape), dt))
                    DKi = sb("DKi", [128, nt, 4], I32)
                    IDXG = sb("IDXG", [128, NB, 8], I32)
                    IDXD = sb("IDXD", [128, NB, 8], I32)
                    EB = sb("EB", [128, NB])
                    IDXB = sb("IDXB", [128, NB], I32)
                    mwd = sb("mwd", [128, nt, NE])
                    W4 = sb("W4", [128, nt, 4])
                    iotap = sb("iotap", [128, 1])
                    P.dma('sp', iotap[:], iotap_in, [], ['iotap'], 'iotap')
                    with contextlib.ExitStack() as st2:
                        def sb2(name, shape, dt=F32):
                            return st2.enter_context(nc.sbuf_tensor(uname(name), list(shape), dt))
                        EF = sb2("EF", [128, nt, 4])
                        P.dma('sp', EF[:], RE[t0:t0 + T, :].rearrange("(c p) k -> p c k", p=128), [], ['EF'], 'EF')
                        P.dma('sp', W4[:], RW[t0:t0 + T, :].rearrange("(c p) k -> p c k", p=128), [], ['W4'], 'W4')
                        iota32 = sb2("iota32", [128, NE])
                        P.dma('act', iota32[:], iota32_in, [], ['iota32'], 'iota32')
                        lts = sb2("lts", [128, 128])
                        P.dma('act', lts[:], lts_in, [], ['lts'], 'lts')
                        ones = sb2("ones", [128, 128])
                        P.pool(lambda e: e.memset(ones[:], 1.0), [], ['ones'])
                        bst = sb2("bst", [128, NBMAX])
                        P.dma('act', bst[:], bstart_in, [], ['bst'], 'bst')
                        kp = sb2("kp", [128, 8])
                        P.dma('act', kp[:], kp_in, [], ['kp'], 'kp')
                        OH = sb2("OH", [128, 4, nt, NE])
                        for k in range(4):
                            P.dve(lambda e, k=k: e.tensor_tensor(out=OH[:, k, :, :], in0=iota32[:].unsqueeze(1).to_broadcast([128, nt, NE]),
                                                                 in1=EF[:, :, k].unsqueeze(2).to_broadcast([128, nt, NE]), op=ALU.is_equal),
                                  ['iota32', 'EF'], ['OH%d' % k])
                        mask = sb2("mask", [128, nt, NE])
                        P.dve(lambda e: e.tensor_tensor(out=mask[:], in0=OH[:, 0, :, :], in1=OH[:, 1, :, :], op=ALU.add), ['OH0', 'OH1'], ['mask'])
                        P.dve(lambda e: e.tensor_tensor(out=mask[:], in0=mask[:], in1=OH[:, 2, :, :], op=ALU.add), ['mask', 'OH2'], ['mask'])
                        P.dve(lambda e: e.tensor_tensor(out=mask[:], in0=mask[:], in1=OH[:, 3, :, :], op=ALU.add), ['mask', 'OH3'], ['mask'])
                        for j in range(nt):
                            bk = j // 16
                            col = (j % 16) * NE
                            P.pe(lambda e, j=j, bk=bk, col=col: e.matmul(psum[:, bk, col:col + NE], lts[:], mask[:, j, :], start=True, stop=True, skip_group_check=True),
                                 ['lts', 'mask'], ['rk%d' % bk])
                            P.pe(lambda e, j=j, bk=bk, col=col: e.matmul(psum[:, 4 + bk, col:col + NE], ones[:], mask[:, j, :], start=True, stop=True, skip_group_check=True),
                                 ['ones', 'mask'], ['tt%d' % bk])
                        for m in range(nt):
                            P.pe(lambda e, m=m: e.matmul(psum[:, 3, 0:NE], ones[:], mask[:, m, :], start=(m == 0), stop=(m == nt - 1)), ['ones', 'mask'], ['cnt'])
                        TOT = sb2("TOT", [128, nt, NE])
                        PRE = sb2("PRE", [128, nt, NE])
                        for bk in range((nt + 15) // 16):
                            j0 = bk * 16
                            nj = min(16, nt - j0)
                            P.act(lambda e, bk=bk, j0=j0, nj=nj: e.activation(out=TOT[:, j0:j0 + nj, :].rearrange("p j e -> p (j e)"), in_=psum[:, 4 + bk, 0:nj * NE], func=AF.Copy),
                                  ['tt%d' % bk], ['TOT%d' % bk])
                        P.pool(lambda e: e.memset(PRE[:, 0, :], 0.0), [], ['PRE'])
                        for j in range(1, nt):
                            P.dve(lambda e, j=j: e.tensor_tensor(out=PRE[:, j, :], in0=PRE[:, j - 1, :], in1=TOT[:, j - 1, :], op=ALU.add),
                                  ['PRE'] + ['TOT%d' % bk for bk in range((nt + 15) // 16)], ['PRE'])
                        c0 = sb2("c0", [128, NE]); c1 = sb2("c1", [128, NE]); padded = sb2("padded", [128, NE])
                        P.dve(lambda e: e.tensor_scalar_add(out=c0[:], in0=psum[:, 3, 0:NE], scalar1=float(BS - 1)), ['cnt'], ['c0'])
                        ci32 = sb2("ci32", [128, NE], I32)
                        P.dve(lambda e: e.tensor_scalar(out=c1[:], in0=c0[:], scalar1=1.0 / BS, scalar2=-0.5 + 0.5 / BS, op0=ALU.mult, op1=ALU.add), ['c0'], ['c1'])
                        P.dve(lambda e: e.tensor_copy(out=ci32[:], in_=c1[:]), ['c1'], ['ci32'])
                        P.dve(lambda e: e.tensor_copy(out=c1[:], in_=ci32[:]), ['ci32'], ['c1'])
                        P.dve(lambda e: e.tensor_scalar_mul(out=padded[:], in0=c1[:], scalar1=float(BS)), ['c1'], ['padded'])
                        P.dve(lambda e: e.tensor_copy(out=c0[:], in_=padded[:]), ['padded'], ['c0'])
                        cur, nxt_, ck, nk = c0, c1, 'c0', 'c1'
                        for sft in (1, 2, 4, 8, 16):
                            P.dve(lambda e, cur=cur, nxt_=nxt_, sft=sft: e.tensor_copy(out=nxt_[:, 0:sft], in_=cur[:, 0:sft]), [ck], [nk + 'a'])
                            P.dve(lambda e, cur=cur, nxt_=nxt_, sft=sft: e.tensor_tensor(out=nxt_[:, sft:NE], in0=cur[:, sft:NE], in1=cur[:, 0:NE - sft], op=ALU.add), [ck], [nk + 'b'])
                            P.dve(lambda e: e.engine_nop(), [nk + 'a', nk + 'b'], [nk])
                            cur, nxt_, ck, nk = nxt_, cur, nk, ck
                        pend, pendk = cur, ck
                        pstart = sb2("pstart", [128, NE])
                        P.dve(lambda e: e.tensor_tensor(out=pstart[:], in0=pend[:], in1=padded[:], op=ALU.subtract), [pendk, 'padded'], ['pstart'])
                        dfull = sb2("dfull", [128, nt, NE])
                        for bk in range((nt + 15) // 16):
                            j0 = bk * 16
                            nj = min(16, nt - j0)
                            P.dve(lambda e, bk=bk, j0=j0, nj=nj: e.tensor_tensor(out=dfull[:, j0:j0 + nj, :], in0=psum[:, bk, 0:nj * NE].rearrange("p (j e) -> p j e", e=NE),
                                                                                 in1=pstart[:].unsqueeze(1).to_broadcast([128, nj, NE]), op=ALU.add),
                                  ['rk%d' % bk, 'pstart'], ['dfull%d' % bk])
                            P.dve(lambda e, j0=j0, nj=nj: e.tensor_tensor(out=dfull[:, j0:j0 + nj, :], in0=dfull[:, j0:j0 + nj, :], in1=PRE[:, j0:j0 + nj, :], op=ALU.add),
                                  ['dfull%d' % bk, 'PRE'], ['dfull%d' % bk])
                        dkeys = ['dfull%d' % bk for bk in range((nt + 15) // 16)]
                        tmpd = sb2("tmpd", [128, nt, NE])
                        DKf = sb2("DKf", [128, nt, 4])
                        for k in range(4):
                            P.dve(lambda e, k=k: e.tensor_tensor(out=tmpd[:], in0=dfull[:], in1=OH[:, k, :, :], op=ALU.mult), dkeys + ['OH%d' % k], ['tmpd'])
                            P.dve(lambda e, k=k: e.tensor_reduce(out=DKf[:, :, k], in_=tmpd[:], axis=AX.X, op=ALU.add), ['tmpd'], ['DKf%d' % k])
                        P.dve(lambda e: e.tensor_copy(out=DKi[:], in_=DKf[:]), ['DKf%d' % k for k in range(4)], ['DKi'])
                        cmp_ = sb2("cmp", [128, NB, NE])
                        P.dve(lambda e: e.tensor_tensor(out=cmp_[:], in0=pend[:].unsqueeze(1).to_broadcast([128, NB, NE]),
                                                        in1=bst[:, 0:NB].unsqueeze(2).to_broadcast([128, NB, NE]), op=ALU.is_le), [pendk, 'bst'], ['cmp'])
                        P.dve(lambda e: e.tensor_reduce(out=EB[:], in_=cmp_[:], axis=AX.X, op=ALU.add), ['cmp'], ['EB'])
                        P.dve(lambda e: e.tensor_scalar_min(out=EB[:], in0=EB[:], scalar1=float(NE - 1)), ['EB'], ['EB'])
                        idxf = sb2("idxf", [128, NB, 8])
                        P.dve(lambda e: e.scalar_tensor_tensor(out=idxf[:], in0=EB[:].unsqueeze(2).to_broadcast([128, NB, 8]), scalar=float(D),
                                                               in1=kp[:].unsqueeze(1).to_broadcast([128, NB, 8]), op0=ALU.mult, op1=ALU.add), ['EB', 'kp'], ['idxf'])
                        P.dve(lambda e: e.tensor_scalar_add(out=idxf[:], in0=idxf[:], scalar1=float(l * NE * D)), ['idxf'], ['idxf'])
                        P.dve(lambda e: e.tensor_copy(out=IDXD[:], in_=idxf[:]), ['idxf'], ['IDXD'])
                        unused = sb2("unused", [128, NB])
                        P.dve(lambda e: e.tensor_scalar(out=unused[:], in0=bst[:, 0:NB], scalar1=pend[:, NE - 1:NE], scalar2=1.0e6, op0=ALU.is_ge, op1=ALU.mult), ['bst', pendk], ['unused'])
                        P.dve(lambda e: e.tensor_tensor(out=idxf[:], in0=idxf[:], in1=unused[:].unsqueeze(2).to_broadcast([128, NB, 8]), op=ALU.add), ['idxf', 'unused'], ['idxf'])
                        P.dve(lambda e: e.tensor_copy(out=IDXG[:], in_=idxf[:]), ['idxf'], ['IDXG'])
                        idxbf = sb2("idxbf", [128, NB])
                        P.dve(lambda e: e.tensor_scalar(out=idxbf[:], in0=EB[:], scalar1=128.0, scalar2=float(l * NE * 128), op0=ALU.mult, op1=ALU.add), ['EB'], ['idxbf'])
                        P.dve(lambda e: e.tensor_scalar(out=idxbf[:], in0=idxbf[:], scalar1=iotap[:, 0:1], scalar2=None, op0=ALU.add), ['idxbf', 'iotap'], ['idxbf'])
                        P.dve(lambda e: e.tensor_copy(out=IDXB[:], in_=idxbf[:]), ['idxbf'], ['IDXB'])
                        for k in range(4):
                            dst_ = mwd if k == 0 else tmpd
                            P.dve(lambda e, k=k, dst_=dst_: e.tensor_tensor(out=dst_[:], in0=OH[:, k, :, :], in1=W4[:, :, k].unsqueeze(2).to_broadcast([128, nt, NE]), op=ALU.mult),
                                  ['OH%d' % k, 'W4', 'DKf0', 'DKf1', 'DKf2', 'DKf3'], ['mwd' if k == 0 else 'tmpd'])
                            if k > 0:
                                P.dve(lambda e: e.tensor_tensor(out=mwd[:], in0=mwd[:], in1=tmpd[:], op=ALU.add), ['mwd', 'tmpd'], ['mwd'])
                        hr = Ring('hr', [sb2("hr%d" % i, [128, D], BF16) for i in range(3)])
                        for j, i in enumerate(tiles):
                            h_, hk_ = hr.next()
                            P.dma('sp', h_[:], H2R[i * 128:(i + 1) * 128, :], [], [hk_], hk_)
                            for k in range(4):
                                P.add('pool', lambda e, h_=h_, j=j, k=k: e.indirect_dma_start(out=XS[:, :], out_offset=bass.IndirectOffsetOnAxis(ap=DKi[:, j, k:k + 1], axis=0),
                                                                                          in_=h_[:, :], in_offset=None), [hk_, 'DKi'], [('XS', j, k)], 'xsc%d' % ((j * 4 + k) % 4))
                        P.flush()
                    with contextlib.ExitStack() as st2:
                        def sb2(name, shape, dt=F32):
                            return st2.enter_context(nc.sbuf_tensor(uname(name), list(shape), dt))
                        identb = sb2("identb", [128, 128], BF16)
                        P.act(lambda e: e.activation(out=identb[:], in_=ident[:], func=AF.Copy), ['ident'], ['identb'])
                        bgr = sb2("bgr", [NE, 2 * D])
                        P.dma('act', bgr[:], b_gu[l], [], ['bgr'], 'bgr')
                        for c in range(16):
                            P.pe(lambda e, c=c: e.transpose(psum[:, 6, c * NE:(c + 1) * NE], bgr[:, c * 128:(c + 1) * 128], ident[0:NE, 0:NE]), ['bgr', 'ident'], ['pX0'])
                        X2 = sb2("X2", [128, NE, 16])
                        P.dve(lambda e: e.tensor_copy(out=X2[:], in_=psum[:, 6, :].rearrange("p (c e) -> p e c", e=NE)), ['pX0'], ['X2'])
                        P.dve(lambda e: e.tensor_scalar_add(out=X2[:, :, 8:16], in0=X2[:, :, 8:16], scalar1=1.0), ['X2'], ['X2'])
                        P.dma('sp', BGT[l * NE * 128:(l + 1) * NE * 128, :].rearrange("(e p) c -> p e c", p=128), X2[:], ['X2'], ['BGT'], 'BGT')
                        bbr = Ring('bb', [sb2("bb%d" % i, [128, 16]) for i in range(2)])
                        wgr = Ring('WG', [sb2("WG%d" % i, [128, 8, 2 * D], BF16) for i in range(2)])
                        wdr = Ring('WD', [sb2("WD%d" % i, [128, 8, D], BF16) for i in range(2)])
                        xbr = Ring('XB', [sb2("XB%d" % i, [128, 4, D], BF16) for i in range(2)])
                        XTr = [sb2("XT%d" % i, [128, 8, BS], BF16) for i in range(2)]
                        actr = Ring('act', [sb2("act%d" % i, [128, 8, BS], BF16) for i in range(2)])
                        Ar = Ring('A', [sb2("A%d" % i, [128, BS]) for i in range(2)])
                        Sr = Ring('Sg', [sb2("Sg%d" % i, [128, BS]) for i in range(2)])
                        Ur = Ring('U', [sb2("U%d" % i, [128, BS]) for i in range(2)])
                        swr = Ring('swb', [sb2("swb%d" % i, [128, 4]) for i in range(2)])
                        Yr = Ring('Y', [sb2("Y%d" % i, [128, 4, D]) for i in range(1)])
                        gbr = Ring('pg', [0, 1]); ubr = Ring('pu', [2, 3]); ybr = Ring('py', [4, 5])
                        psb = [psum[:, 6, :].bitcast(BF16), psum[:, 7, :].bitcast(BF16)]

                        def load_blk(b):
                            WG, wgk = wgr.next()
                            WD, wdk = wdr.next()
                            for k in range(8):
                                P.add('pool', lambda e, WG=WG, b=b, k=k: e.indirect_dma_start(out=WG[:, k, :], out_offset=None, in_=w_gu_rows[:, :],
                                                                                             in_offset=bass.IndirectOffsetOnAxis(ap=IDXD[:, b, k:k + 1], axis=0)),
                                      ['IDXG'], [wgk + str(k)], wgk + str(k))
                            for k in range(8):
                                P.add('pool', lambda e, WD=WD, b=b, k=k: e.indirect_dma_start(out=WD[:, k, :], out_offset=None, in_=w_dn_rows[:, :],
                                                                                             in_offset=bass.IndirectOffsetOnAxis(ap=IDXD[:, b, k:k + 1], axis=0)),
                                      ['IDXD'], [wdk + str(k)], wdk + str(k))
                            XB, xbk = xbr.next()
                            P.dma('sp', XB[:], XS[b * BS:(b + 1) * BS, :].rearrange("(s p) d -> p s d", p=128), [], [xbk], xbk)
                            swb, swk = None, None
                            bb, bbk = bbr.next()
                            P.add('pool', lambda e, bb=bb, b=b: e.indirect_dma_start(out=bb[:, :], out_offset=None, in_=BGT[:, :],
                                                                                   in_offset=bass.IndirectOffsetOnAxis(ap=IDXB[:, b:b + 1], axis=0)),
                                  ['IDXB', 'BGT'], [bbk], bbk)
                            return WG, wgk, WD, wdk, XB, xbk, swb, swk, bb, bbk
                        def transposes(b, XB, xbk):
                            XT = XTr[b % 2]
                            for half in range(2):
                                for s2 in range(2):
                                    sidx = half * 2 + s2
                                    for k in range(8):
                                        P.pe(lambda e, XB=XB, sidx=sidx, k=k, s2=s2: e.transpose(psb[s2][:, k * 128:(k + 1) * 128], XB[:, sidx, k * 128:(k + 1) * 128], identb[:]),
                                             [xbk, 'identb'], ['pX%d' % s2])
                                    P.act(lambda e, sidx=sidx, s2=s2, XT=XT: e.activation(out=XT[:, :, sidx * 128:(sidx + 1) * 128], in_=psb[s2].rearrange("p (k n) -> p k n", n=128), func=AF.Copy),
                                          ['pX%d' % s2], ['XT%d_%d' % (b % 2, sidx)])
                        nxt = load_blk(0)
                        transposes(0, nxt[4], nxt[5])
                        for b in range(NB):
                            WG, wgk, WD, wdk, XB, xbk, swb, swk, bb, bbk = nxt
                            if b + 1 < NB:
                                nxt = load_blk(b + 1)
                            wgkeys = [wgk + str(k) for k in range(8)]
                            wdkeys = [wdk + str(k) for k in range(8)]
                            XT = XTr[b % 2]
                            xtkeys = ['XT%d_%d' % (b % 2, q) for q in range(4)]
                            at, atk = actr.next()
                            for c in range(8):
                                gb, gbk = gbr.next()
                                ub, ubk = ubr.next()
                                for k in range(8):
                                    P.pe(lambda e, gb=gb, WG=WG, k=k, c=c, XT=XT: e.matmul(psum[:, gb, :], WG[:, k, c * 128:(c + 1) * 128], XT[:, k, :], start=(k == 0), stop=(k == 7)),
                                         wgkeys + xtkeys, [gbk])
                                for k in range(8):
                                    P.pe(lambda e, ub=ub, WG=WG, k=k, c=c, XT=XT: e.matmul(psum[:, ub, :], WG[:, k, D + c * 128:D + (c + 1) * 128], XT[:, k, :], start=(k == 0), stop=(k == 7)),
                                         wgkeys + xtkeys, [ubk])
                                A, Ak = Ar.next(); S_, Sk = Sr.next(); U, Uk = Ur.next()
                                P.dve(lambda e, A=A, gb=gb, bb=bb, c=c: e.tensor_scalar(out=A[:], in0=psum[:, gb, :], scalar1=bb[:, c:c + 1], scalar2=7.0, op0=ALU.add, op1=ALU.min), [gbk, bbk], [Ak])
                                P.act(lambda e, A=A, S_=S_: e.activation(out=S_[:], in_=A[:], func=AF.Sigmoid, scale=1.702), [Ak], [Sk])
                                P.dve(lambda e, U=U, ub=ub, bb=bb, c=c: e.tensor_scalar(out=U[:], in0=psum[:, ub, :], scalar1=bb[:, 8 + c:9 + c], scalar2=8.0, op0=ALU.add, op1=ALU.min), [ubk, bbk], [Uk])
                                P.pool(lambda e, A=A, S_=S_: e.tensor_tensor(out=A[:], in0=A[:], in1=S_[:], op=ALU.mult), [Ak, Sk], [Ak])
                                P.dve(lambda e, at=at, c=c, U=U, A=A: e.scalar_tensor_tensor(out=at[:, c, :], in0=U[:], scalar=-6.0, in1=A[:], op0=ALU.max, op1=ALU.mult), [Uk, Ak], [atk + str(c)])
                            atkeys = [atk + str(c) for c in range(8)]
                            if b + 1 < NB:
                                transposes(b + 1, nxt[4], nxt[5])
                            Y, Yk = Yr.next()
                            for s4 in range(4):
                                for nh in range(2):
                                    yb_, ybk = ybr.next()
                                    for c in range(8):
                                        P.pe(lambda e, yb_=yb_, at=at, c=c, s4=s4, WD=WD, nh=nh: e.matmul(psum[:, yb_, :], at[:, c, s4 * 128:(s4 + 1) * 128], WD[:, c, nh * 512:(nh + 1) * 512],
                                                                                                          start=(c == 0), stop=(c == 7)), atkeys + wdkeys, [ybk])
                                    if yb_ == 4:
                                        P.dve(lambda e, Y=Y, s4=s4, nh=nh: e.tensor_copy(out=Y[:, s4, nh * 512:(nh + 1) * 512], in_=psum[:, 4, :]),
                                              [ybk], [Yk + '%d%d' % (s4, nh)])
                                    else:
                                        P.act(lambda e, Y=Y, s4=s4, nh=nh: e.activation(out=Y[:, s4, nh * 512:(nh + 1) * 512], in_=psum[:, 5, :], func=AF.Copy),
                                              [ybk], [Yk + '%d%d' % (s4, nh)])
                            P.dma('sp', YS[b * BS:(b + 1) * BS, :].rearrange("(s p) d -> p s d", p=128), Y[:], [Yk + '%d%d' % (q, r_) for q in range(4) for r_ in range(2)], [('YS', b)], Yk)
                        P.flush()
                    with contextlib.ExitStack() as st3:
                        def sb3(name, shape, dt=F32):
                            return st3.enter_context(nc.sbuf_tensor(uname(name), list(shape), dt))
                        g2 = [sb3("g2_%d" % s_, [128, D]) for s_ in range(2)]
                        for s_ in range(2):
                            bload('sp', g2[s_][:], modv[l, s_, 5 * D:6 * D], 'g2_%d' % s_)
                        lng = sb3("lng2", [128, D]); lnb = sb3("lnb2", [128, D])
                        bload('act', lng[:], ln_g[l, 1], 'lng')
                        bload('act', lnb[:], ln_b[l, 1], 'lnb')
                        x1r = Ring('xm', [sb3("xm%d" % i, [128, D]) for i in range(4)])
                        Gr = Ring('G', [sb3("G%d" % i, [128, 4, D]) for i in range(4)])
                        rr = sb3("rr2", [128, D])
                        xor_ = Ring('xo', [sb3("xo%d" % i, [128, D]) for i in range(2)])
                        st6 = sb3("st6b", [128, 2, 6]); mv = sb3("mvb", [128, 2]); rs = sb3("rsb", [128, 1])
                        bdr = sb3("bdr", [NE, D])
                        P.dma('act', bdr[:], b_dn[l], [], ['bdr'], 'bdr')
                        mwTr = Ring('mwT', [sb3("mwT%d" % i, [NE, 128]) for i in range(2)])
                        tbr = Ring('tb', [0, 1]); bbk2 = Ring('bd', [(2, 3), (4, 5)])
                        for j, i in enumerate(tiles):
                            s_ = 1 if i < NT_C else 0
                            tb_, tbk = tbr.next()
                            P.pe(lambda e, j=j, tb_=tb_: e.transpose(psum[0:NE, tb_, 0:128], mwd[:, j, :], ident[:]), ['ident'], [tbk])
                            mwT, mwTk = mwTr.next()
                            P.act(lambda e, mwT=mwT, tb_=tb_: e.activation(out=mwT[:], in_=psum[0:NE, tb_, 0:128], func=AF.Copy), [tbk], [mwTk])
                            (bd0, bd1), bdk = bbk2.next()
                            for nh, bdb_ in enumerate((bd0, bd1)):
                                P.pe(lambda e, mwT=mwT, nh=nh, bdb_=bdb_: e.matmul(psum[:, bdb_, :], mwT[:], bdr[:, nh * 512:(nh + 1) * 512], start=True, stop=True), [mwTk, 'bdr'], [bdk + str(nh)])
                            x1, x1k = x1r.next()
                            P.dma('sp', x1[:], XM[i * 128:(i + 1) * 128, :], [], [x1k], x1k)
                            G, Gk = Gr.next()
                            for k in range(4):
                                P.add('pool', lambda e, G=G, j=j, k=k: e.indirect_dma_start(out=G[:, k, :], out_offset=None, in_=YS[:, :],
                                                                                         in_offset=bass.IndirectOffsetOnAxis(ap=DKi[:, j, k:k + 1], axis=0)), ['DKi'], [Gk + str(k)], Gk + str(k))
                            P.dve(lambda e, G=G, j=j: e.tensor_scalar_mul(out=G[:, 0, :], in0=G[:, 0, :], scalar1=W4[:, j, 0:1]), [Gk + '0'], [Gk + '0'])
                            for k in range(1, 4):
                                P.dve(lambda e, G=G, j=j, k=k: e.scalar_tensor_tensor(out=G[:, 0, :], in0=G[:, k, :], scalar=W4[:, j, k:k + 1], in1=G[:, 0, :], op0=ALU.mult, op1=ALU.add),
                                      [Gk + '0', Gk + str(k)], [Gk + '0'])
                            for nh, bdb_ in enumerate((bd0, bd1)):
                                P.dve(lambda e, G=G, nh=nh, bdb_=bdb_: e.tensor_tensor(out=G[:, 0, nh * 512:(nh + 1) * 512], in0=G[:, 0, nh * 512:(nh + 1) * 512], in1=psum[:, bdb_, :], op=ALU.add),
                                      [Gk + '0', bdk + str(nh)], [Gk + '0'])
                            P.dve(lambda e, G=G, s_=s_: e.tensor_tensor(out=rr[:], in0=G[:, 0, :], in1=g2[s_][:], op=ALU.mult), [Gk + '0', 'g2_%d' % s_], ['rr'])
                            P.dve(lambda e, x1=x1: e.scalar_tensor_tensor(out=rr[:], in0=x1[:], scalar=ALPHA, in1=rr[:], op0=ALU.mult, op1=ALU.add), [x1k, 'rr'], ['rr'])
                            for nh in range(2):
                                P.dve(lambda e, nh=nh: e.bn_stats(out=st6[:, nh, :], in_=rr[:, nh * 512:(nh + 1) * 512]), ['rr'], ['st6_%d' % nh])
                            P.dve(lambda e: e.bn_aggr(out=mv[:], in_=st6[:]), ['st6_0', 'st6_1'], ['mv'])
                            P.dve(lambda e: e.tensor_scalar_add(out=rs[:], in0=mv[:, 1:2], scalar1=EPS), ['mv'], ['rs'])
                            P.act(lambda e: e.activation(out=rs[:], in_=rs[:], func=AF.Sqrt), ['rs'], ['rs'])
                            P.dve(lambda e: e.reciprocal(out=rs[:], in_=rs[:]), ['rs'], ['rs'])
                            xo, xok = xor_.next()
                            P.dve(lambda e, xo=xo: e.tensor_scalar(out=xo[:], in0=rr[:], scalar1=mv[:, 0:1], scalar2=rs[:, 0:1], op0=ALU.subtract, op1=ALU.mult), ['rr', 'mv', 'rs'], [xok])
                            P.dve(lambda e, xo=xo: e.tensor_tensor(out=xo[:], in0=xo[:], in1=lng[:], op=ALU.mult), [xok, 'lng'], [xok])
                            P.dve(lambda e, xo=xo: e.tensor_tensor(out=xo[:], in0=xo[:], in1=lnb[:], op=ALU.add), [xok, 'lnb'], [xok])
                            dst = XS0[i * 128:(i + 1) * 128, :] if not last else out[(i - NT_C) * 128:(i - NT_C + 1) * 128, :]
                            P.dma('sp', dst, xo[:], [xok], [('xout', i)], xok)
                        P.flush()
        P.flush()
    return nc


def make_consts():
    inv = (10000.0 ** (-np.arange(0, 32, 2, dtype=np.float32) / 32.0)).astype(np.float32)
    t = np.arange(L)
    row = (t // 64).astype(np.float32)
    col = (t % 64).astype(np.float32)
    ang = np.stack([row[:, None] * inv, col[:, None] * inv], axis=1)
    cos = np.cos(ang).astype(np.float32)
    sin = np.sin(ang).astype(np.float32)
    c64 = np.stack([cos, cos], axis=2).reshape(L, 64)
    s64 = np.stack([-sin, sin], axis=2).reshape(L, 64)
    p = np.arange(128, dtype=np.float32)
    pos = np.stack([127 - p, p, p + 1, 128 - p], axis=1)
    dif = p[None, :] - p[:, None]
    return dict(
        c_ident=np.eye(128, dtype=np.float32),
        c_cos=np.ascontiguousarray(np.tile(c64, (1, 8))),
        c_sin=np.ascontiguousarray(np.tile(s64, (1, 8))),
        c_pos=np.ascontiguousarray(pos.astype(np.float32)),
        c_dpos=np.maximum(dif, 0).astype(np.float32),
        c_dneg=np.maximum(-dif, 0).astype(np.float32),
        c_mge=(dif >= 0).astype(np.float32),
        c_zeros=np.zeros((2, 256), np.float32),
        c_iota32=np.tile(np.arange(NE, dtype=np.float32)[None, :], (128, 1)),
        c_lts=(p[:, None] < p[None, :]).astype(np.float32),
        c_bstart=np.tile((np.arange(NBMAX, dtype=np.float32) * BS)[None, :], (128, 1)),
        c_kp=(np.arange(8, dtype=np.float32)[None, :] * 128 + p[:, None]).astype(np.float32),
        c_iotap=p[:, None].astype(np.float32).copy(),
    )


WKEYS = ['w_mod', 'b_mod', 'w_in', 'conv_w', 'ret_decay_exp', 'ret_gn_g', 'q_norm_g', 'k_norm_g', 'w_out',
         'ln_g', 'ln_b', 'w_router', 'b_router', 'w_gate_up', 'b_gate_up', 'w_down', 'b_down']


def make_in_maps(inputs, cores, skip=()):
    consts = make_consts()
    shared = {k: np.ascontiguousarray(np.asarray(inputs[k], np.float32)) for k in WKEYS if k not in skip}
    shared['c_ctx'] = np.ascontiguousarray(np.asarray(inputs['c_ctx'], np.float32))
    shared.update(consts)
    maps = []
    for b in cores:
        m = dict(shared)
        m['x'] = np.ascontiguousarray(np.asarray(inputs['x'][b], np.float32))
        m['c'] = np.ascontiguousarray(np.asarray(inputs['c'][b], np.float32))
        m['ctx'] = np.ascontiguousarray(np.asarray(inputs['ctx'][b], np.float32))
        maps.append(m)
    return maps


def kernel(**inputs):
    nc = build()
    maps = make_in_maps(inputs, list(range(8)))
    res = run_bass_kernel_spmd(nc, maps, core_ids=list(range(8)))
    return np.stack([np.asarray(r["out"], np.float32) for r in res.results], axis=0)
```
